# Optimizing a Trainium2 kernel written in Bass

```python
import math
import jax, jax.numpy as jnp
from jax import lax
import numpy as np

D_MODEL = 1024
BATCH = 8
SEQ = 4096
DEPTH = 2

N_MIXERS = 2
RET_HEADS = 4
RET_QK_DIM = D_MODEL // RET_HEADS
RET_V_DIM = 2 * RET_QK_DIM
RET_QK_WIDTH = RET_HEADS * RET_QK_DIM
RET_V_WIDTH = RET_HEADS * RET_V_DIM
RET_IN_WIDTH = 2 * RET_QK_WIDTH + 2 * RET_V_WIDTH
RET_CHUNK = 128
ROPE_BASE = 10000.0
FNET_GROUPS = 4
FNET_GROUP_DIM = D_MODEL // FNET_GROUPS
D_FF_DENSE = ((8 * D_MODEL // 3 + 255) // 256) * 256
N_EXPERTS = 8
TOP_K = 2
D_FF_EXPERT = 7 * D_MODEL // 2
EPS = 1e-6

kernel_name = "hybrid_retention_fnet_moe_encoder"


def rms_norm(x, g):
    xf = x.astype(jnp.float32)
    y = xf * lax.rsqrt(jnp.mean(xf * xf, axis=-1, keepdims=True) + EPS)
    return (y * g.astype(jnp.float32)).astype(x.dtype)


def rotary(t):
    s, dh = t.shape[1], t.shape[-1]
    half = dh // 2
    inv = ROPE_BASE ** (-jnp.arange(half, dtype=jnp.float32) / half)
    ang = jnp.arange(s, dtype=jnp.float32)[:, None] * inv[None, :]
    cos = jnp.cos(ang)[None, :, None, :]
    sin = jnp.sin(ang)[None, :, None, :]
    t1 = t[..., :half].astype(jnp.float32)
    t2 = t[..., half:].astype(jnp.float32)
    return jnp.concatenate([t1 * cos - t2 * sin, t1 * sin + t2 * cos], axis=-1).astype(t.dtype)


def chunk_retention(q, k, v, log_gamma, inclusive):
    b, h, s, dk = q.shape
    dv = v.shape[-1]
    c = RET_CHUNK
    n = s // c
    dt = q.dtype
    qc = q.reshape(b, h, n, c, dk)
    kc = k.reshape(b, h, n, c, dk)
    vc = v.reshape(b, h, n, c, dv)
    idx = jnp.arange(c, dtype=jnp.float32)
    diff = idx[:, None] - idx[None, :]
    mask = (diff >= 0) if inclusive else (diff > 0)
    lg = log_gamma[:, None, None]
    decay = jnp.where(mask[None], jnp.exp(jnp.where(mask[None], diff[None] * lg, 0.0)), 0.0)
    scores = jnp.einsum('bhnid,bhnjd->bhnij', qc, kc) * decay[None, :, None].astype(dt)
    intra = jnp.einsum('bhnij,bhnje->bhnie', scores, vc)

    xi = jnp.exp((idx[None, :] + 1.0) * log_gamma[:, None]).astype(dt)
    zeta = jnp.exp((c - 1.0 - idx[None, :]) * log_gamma[:, None]).astype(dt)
    g_chunk = jnp.exp(c * log_gamma).astype(dt)

    def step(state, inp):
        q_n, k_n, v_n = inp
        cross = jnp.einsum('bhid,bhde->bhie', q_n * xi[None, :, :, None], state)
        state = state * g_chunk[None, :, None, None] + jnp.einsum(
            'bhjd,bhje->bhde', k_n * zeta[None, :, :, None], v_n)
        return state, cross

    state0 = jnp.zeros((b, h, dk, dv), dt)
    xs = (jnp.moveaxis(qc, 2, 0), jnp.moveaxis(kc, 2, 0), jnp.moveaxis(vc, 2, 0))
    _, cross = lax.scan(step, state0, xs)
    cross = jnp.moveaxis(cross, 0, 2)
    return (intra + cross).reshape(b, h, s, dv)


def retention_mixer(h, w_in, decay_logit, gn_gain, w_out):
    b, s, _ = h.shape
    proj = h @ w_in
    q, k, v, g = jnp.split(proj, [RET_QK_WIDTH, 2 * RET_QK_WIDTH, 2 * RET_QK_WIDTH + RET_V_WIDTH], axis=-1)
    q = rotary(q.reshape(b, s, RET_HEADS, RET_QK_DIM))
    k = rotary(k.reshape(b, s, RET_HEADS, RET_QK_DIM)) * (RET_QK_DIM ** -0.5)
    v = v.reshape(b, s, RET_HEADS, RET_V_DIM)
    q, k, v = (jnp.transpose(t, (0, 2, 1, 3)) for t in (q, k, v))
    log_g = jax.nn.log_sigmoid(decay_logit.astype(jnp.float32))
    fwd = chunk_retention(q, k, v, log_g[0], True)
    bwd = jnp.flip(chunk_retention(jnp.flip(q, 2), jnp.flip(k, 2), jnp.flip(v, 2), log_g[1], False), 2)
    y = (fwd + bwd).astype(jnp.float32)
    mu = jnp.mean(y, axis=-1, keepdims=True)
    var = jnp.mean(jnp.square(y - mu), axis=-1, keepdims=True)
    yn = (y - mu) * lax.rsqrt(var + EPS)
    yn = jnp.transpose(yn, (0, 2, 1, 3)).reshape(b, s, RET_V_WIDTH) * gn_gain.astype(jnp.float32)
    return (jax.nn.silu(g) * yn.astype(h.dtype)) @ w_out


def fourier_mixer(h, w_out):
    b, s, d = h.shape
    hg = h.astype(jnp.float32).reshape(b, s, FNET_GROUPS, FNET_GROUP_DIM)
    y = jnp.fft.fft2(hg, axes=(1, 3), norm='ortho').real
    return y.reshape(b, s, d).astype(h.dtype) @ w_out


def swiglu(t, w_gate, w_up, w_down):
    return (jax.nn.silu(t @ w_gate) * (t @ w_up)) @ w_down


def moe_swiglu(h, w_router, w_gate, w_up, w_down):
    b, s, d = h.shape
    t = h.reshape(-1, d)
    logits = (t @ w_router).astype(jnp.float32)
    top_val, top_idx = lax.top_k(logits, TOP_K)
    top_w = jax.nn.softmax(top_val, axis=-1)
    gates = jnp.sum(jax.nn.one_hot(top_idx, N_EXPERTS, dtype=jnp.float32) * top_w[..., None], axis=1)
    gates = gates.astype(h.dtype)
    out = jnp.zeros_like(t)
    for e in range(N_EXPERTS):
        out = out + gates[:, e:e + 1] * swiglu(t, w_gate[e], w_up[e], w_down[e])
    return out.reshape(b, s, d)


def setup_inputs(seed: int = 0) -> dict:
    key = jax.random.key(seed)
    ks = jax.random.split(key, 20)
    n_even = (DEPTH + 1) // 2
    n_odd = DEPTH // 2
    f32 = jnp.float32

    def nrm(k, shape, fan_in):
        return jax.random.normal(k, shape, f32) * (fan_in ** -0.5)

    x = jax.random.normal(ks[0], (BATCH, SEQ, D_MODEL), f32)
    mix_norm = 1.0 + 0.02 * jax.random.normal(ks[1], (DEPTH, D_MODEL), f32)
    ffn_norm = 1.0 + 0.02 * jax.random.normal(ks[2], (DEPTH, D_MODEL), f32)
    ret_w_in = nrm(ks[3], (n_even, D_MODEL, RET_IN_WIDTH), D_MODEL)
    gam = 1.0 - jnp.exp(jnp.linspace(math.log(1.0 / 32), math.log(1.0 / 512), RET_HEADS)).astype(f32)
    base_logit = jnp.log(gam) - jnp.log1p(-gam)
    ret_decay_logit = base_logit[None, None, :] + 0.05 * jax.random.normal(ks[4], (n_even, 2, RET_HEADS), f32)
    ret_gn_gain = 1.0 + 0.02 * jax.random.normal(ks[5], (n_even, RET_V_WIDTH), f32)
    ret_w_out = nrm(ks[6], (n_even, RET_V_WIDTH, D_MODEL), RET_V_WIDTH)
    dense_w_gate = nrm(ks[7], (n_even, D_MODEL, D_FF_DENSE), D_MODEL)
    dense_w_up = nrm(ks[8], (n_even, D_MODEL, D_FF_DENSE), D_MODEL)
    dense_w_down = nrm(ks[9], (n_even, D_FF_DENSE, D_MODEL), D_FF_DENSE)
    fnet_w_out = nrm(ks[10], (n_odd, D_MODEL, D_MODEL), D_MODEL)
    moe_router = nrm(ks[11], (n_odd, D_MODEL, N_EXPERTS), D_MODEL)
    moe_w_gate = nrm(ks[12], (n_odd, N_EXPERTS, D_MODEL, D_FF_EXPERT), D_MODEL)
    moe_w_up = nrm(ks[13], (n_odd, N_EXPERTS, D_MODEL, D_FF_EXPERT), D_MODEL)
    moe_w_down = nrm(ks[14], (n_odd, N_EXPERTS, D_FF_EXPERT, D_MODEL), D_FF_EXPERT)
    final_norm = 1.0 + 0.02 * jax.random.normal(ks[15], (D_MODEL,), f32)
    return {"x": x, "mix_norm": mix_norm, "ffn_norm": ffn_norm,
            "ret_w_in": ret_w_in, "ret_decay_logit": ret_decay_logit,
            "ret_gn_gain": ret_gn_gain, "ret_w_out": ret_w_out,
            "dense_w_gate": dense_w_gate, "dense_w_up": dense_w_up, "dense_w_down": dense_w_down,
            "fnet_w_out": fnet_w_out, "moe_router": moe_router,
            "moe_w_gate": moe_w_gate, "moe_w_up": moe_w_up, "moe_w_down": moe_w_down,
            "final_norm": final_norm}


def reference(x, mix_norm, ffn_norm, ret_w_in, ret_decay_logit, ret_gn_gain, ret_w_out,
              dense_w_gate, dense_w_up, dense_w_down, fnet_w_out, moe_router,
              moe_w_gate, moe_w_up, moe_w_down, final_norm):
    h = x
    for i in range(DEPTH):
        j = i // 2
        hn = rms_norm(h, mix_norm[i])
        if i % N_MIXERS == 0:
            h = h + retention_mixer(hn, ret_w_in[j], ret_decay_logit[j], ret_gn_gain[j], ret_w_out[j])
        else:
            h = h + fourier_mixer(hn, fnet_w_out[j])
        hn = rms_norm(h, ffn_norm[i])
        if i % 2 == 0:
            h = h + swiglu(hn, dense_w_gate[j], dense_w_up[j], dense_w_down[j])
        else:
            h = h + moe_swiglu(hn, moe_router[j], moe_w_gate[j], moe_w_up[j], moe_w_down[j])
    return rms_norm(h, final_norm)
```

```python
import numpy as np
import ml_dtypes
from contextlib import ExitStack
import concourse.bass as bass
import concourse.mybir as mybir
from concourse.bass_utils import run_bass_kernel_spmd

F32 = mybir.dt.float32
BF16 = mybir.dt.bfloat16
ALU = mybir.AluOpType
AF = mybir.ActivationFunctionType
AX = mybir.AxisListType

D = 1024
S = 4096
NCORES = 8
EPS = 1e-6
KT = D // 128


class Res:
    __slots__ = ("name", "writes", "reads", "accum", "lsem", "ssem")

    def __init__(self, name, accum=False):
        self.name = name
        self.writes = {}
        self.reads = {}
        self.accum = accum
        self.lsem = None
        self.ssem = None


class KB:
    def __init__(self, nc, es):
        self.nc = nc
        self.es = es
        self.engs = {"pe": nc.tensor, "act": nc.scalar, "dve": nc.vector, "pool": nc.gpsimd, "sp": nc.sync}
        self.esem = {}
        self.cnt = {}
        self.waited = {n: {} for n in self.engs}
        self.free_sems = []
        self.phase_sems = []
        self.nsem = 0
        for n in self.engs:
            s = es.enter_context(nc.semaphore("es_" + n))
            self.esem[n] = s
            self.cnt[s] = 0

    def get_sem(self, fresh=False):
        if self.free_sems and not fresh:
            s = self.free_sems.pop()
        else:
            s = self.es.enter_context(self.nc.semaphore("ds%d" % self.nsem))
            self.nsem += 1
            self.cnt[s] = 0
        if not fresh:
            self.phase_sems.append(s)
        return s

    def _wait(self, eng, evs):
        w = self.waited[eng]
        e = self.engs[eng]
        for sem, val in evs.items():
            if w.get(sem, 0) < val:
                e.wait_ge(sem, val)
                w[sem] = val

    @staticmethod
    def _deps(reads, writes):
        evs = {}
        for r in reads:
            for s, v in r.writes.items():
                if evs.get(s, 0) < v:
                    evs[s] = v
        for wr in writes:
            if not wr.accum:
                for s, v in wr.writes.items():
                    if evs.get(s, 0) < v:
                        evs[s] = v
            for s, v in wr.reads.items():
                if evs.get(s, 0) < v:
                    evs[s] = v
        return evs

    @staticmethod
    def _register(sem, v, reads, writes):
        for r in reads:
            r.reads[sem] = v
        for w in writes:
            if w.accum:
                w.writes[sem] = v
            else:
                w.writes = {sem: v}
                w.reads = {}

    def op(self, eng, fn, reads=(), writes=(), signal=True):
        self._wait(eng, self._deps(reads, writes))
        ins = fn(self.engs[eng])
        if signal:
            sem = self.esem[eng]
            self.cnt[sem] += 1
            ins.then_inc(sem, 1)
            self._register(sem, self.cnt[sem], reads, writes)
        return ins

    def dma(self, q, out, in_, reads, writes, own, store=False):
        self._wait(q, self._deps(reads, writes))
        if store:
            if own.ssem is None:
                own.ssem = self.get_sem(fresh=(q == "pool"))
            sem = own.ssem
        else:
            if own.lsem is None:
                own.lsem = self.get_sem(fresh=(q == "pool"))
            sem = own.lsem
        ins = self.engs[q].dma_start(out=out, in_=in_)
        ins.then_inc(sem, 16)
        self.cnt[sem] += 16
        self._register(sem, self.cnt[sem], reads, writes)
        return ins

    def barrier(self, recycle=True):
        allev = {s: c for s, c in self.cnt.items() if c > 0}
        for n in self.engs:
            self._wait(n, allev)
        if recycle:
            self.free_sems.extend(self.phase_sems)
            self.phase_sems = []

    def final_wait(self, eng="sp"):
        allev = {s: c for s, c in self.cnt.items() if c > 0}
        self._wait(eng, allev)


def bcast_row(ap1d, n):
    return ap1d.rearrange("(o n) -> o n", o=1).partition_broadcast(128)


class Front:
    def __init__(self, kb, es, ident, ident_r, gvec_ap, tag, with_T=True):
        nc = kb.nc
        self.kb = kb
        self.ident = ident
        self.ident_r = ident_r
        self.g = es.enter_context(nc.sbuf_tensor(tag + "_g", [128, D], F32))
        self.g_r = Res(tag + "_g")
        kb.dma("sp", self.g[:], bcast_row(gvec_ap, D), [], [self.g_r], self.g_r)
        self.junk = es.enter_context(nc.sbuf_tensor(tag + "_junk", [128, D], BF16))
        self.junk_r = Res(tag + "_junk")
        self.NR = 4
        self.ss_l = [es.enter_context(nc.sbuf_tensor(tag + "_ss%d" % i, [128, 8], F32)) for i in range(self.NR)]
        self.ss_rl = [Res(tag + "_ss%d" % i) for i in range(self.NR)]
        self.rs_l = [es.enter_context(nc.sbuf_tensor(tag + "_rs%d" % i, [128, 8], F32)) for i in range(self.NR)]
        self.rs_rl = [Res(tag + "_rs%d" % i) for i in range(self.NR)]
        self.cur = 0
        self.n = 0
        if not with_T:
            return
        self.hn = [es.enter_context(nc.sbuf_tensor(tag + "_hn%d" % i, [128, D], BF16)) for i in range(2)]
        self.hn_r = [Res(tag + "_hn%d" % i) for i in range(2)]
        self.ps = [es.enter_context(nc.psum_tensor(tag + "_pst%d" % i, [128, D], BF16)) for i in range(2)]
        self.ps_r = [Res(tag + "_pst%d" % i) for i in range(2)]

    @property
    def ss(self):
        return self.ss_l[self.cur]

    @property
    def ss_r(self):
        return self.ss_rl[self.cur]

    @property
    def rs(self):
        return self.rs_l[self.cur]

    @property
    def rs_r(self):
        return self.rs_rl[self.cur]

    def rstd(self, x3, x_r, nj):
        kb = self.kb
        self.cur = (self.cur + 1) % self.NR
        for j in range(nj):
            kb.op("act", lambda e, j=j: e.activation(out=self.junk[:], in_=x3[:, j, :], func=AF.Square,
                                                     accum_out=self.ss[:, j:j + 1]),
                  reads=[x_r], writes=[self.junk_r, self.ss_r])
        kb.op("dve", lambda e: e.tensor_scalar(out=self.rs[:, 0:nj], in0=self.ss[:, 0:nj], scalar1=1.0 / D,
                                                scalar2=EPS, op0=ALU.mult, op1=ALU.add),
              reads=[self.ss_r], writes=[self.rs_r])
        kb.op("act", lambda e: e.activation(out=self.rs[:, 0:nj], in_=self.rs[:, 0:nj], func=AF.Sqrt),
              reads=[self.rs_r], writes=[self.rs_r])
        kb.op("dve", lambda e: e.reciprocal(out=self.rs[:, 0:nj], in_=self.rs[:, 0:nj]),
              reads=[self.rs_r], writes=[self.rs_r])

    def norm_to(self, out_ap, out_r, x2, x_r, j):
        self.kb.op("dve", lambda e: e.scalar_tensor_tensor(out=out_ap, in0=x2, scalar=self.rs[:, j:j + 1],
                                                            in1=self.g[:], op0=ALU.mult, op1=ALU.mult),
                   reads=[x_r, self.rs_r, self.g_r], writes=[out_r])

    def norm_T(self, x3, x_r, nj, xT, xT_r, tok0):
        kb = self.kb
        self.rstd(x3, x_r, nj)
        for j in range(nj):
            b = self.n % 2
            self.n += 1
            hn, hn_r, ps, ps_r = self.hn[b], self.hn_r[b], self.ps[b], self.ps_r[b]
            self.norm_to(hn[:], hn_r, x3[:, j, :], x_r, j)
            for kt in range(KT):
                last = kt == KT - 1
                kb.op("pe", lambda e, kt=kt: e.transpose(out=ps[:, kt * 128:(kt + 1) * 128],
                                                         in_=hn[:, kt * 128:(kt + 1) * 128], identity=self.ident[:]),
                      reads=[hn_r, self.ident_r], writes=[ps_r], signal=last)
            t0 = tok0 + j * 128
            kb.op("act", lambda e, t0=t0: e.activation(out=xT[:, :, t0:t0 + 128],
                                                       in_=ps[:].rearrange("p (k t) -> p k t", k=KT),
                                                       func=AF.Copy),
                  reads=[ps_r], writes=[xT_r])


def ffn_phase(kb, src, dst, norm_g, experts, ident_d, router=None, final_g=None, FC=7, tag="ffn", chunk_sizes=None, nbuf=1):
    nc = kb.nc
    src_ap, src_r = src
    dst_ap, dst_r = dst
    F = experts[0][0].shape[1]
    NFT = F // 128
    if chunk_sizes is None:
        assert NFT % FC == 0
        chunk_sizes = [FC] * (NFT // FC)
    assert sum(chunk_sizes) == NFT
    FC = max(chunk_sizes)
    NCH = len(chunk_sizes)
    choff = [sum(chunk_sizes[:i]) for i in range(NCH)]
    TT = 1024
    NJ = TT // 128
    NE = len(experts)
    with ExitStack() as es:
        ident = es.enter_context(nc.sbuf_tensor(tag + "_ident", [128, 128], BF16))
        ident_r = Res("ident")
        kb.dma("sp", ident[:], ident_d, [], [ident_r], ident_r)
        fr = Front(kb, es, ident, ident_r, norm_g, tag + "f")
        if final_g is not None:
            gf = es.enter_context(nc.sbuf_tensor(tag + "_gf", [128, D], F32))
            gf_r = Res("gf")
            kb.dma("sp", gf[:], bcast_row(final_g, D), [], [gf_r], gf_r)
        assert nbuf == 1 or router is None
        accs = [es.enter_context(nc.sbuf_tensor(tag + "_acc%d" % i, [128, NJ, D], F32)) for i in range(nbuf)]
        accs_r = [Res("acc%d" % i) for i in range(nbuf)]
        xTs = [es.enter_context(nc.sbuf_tensor(tag + "_xT%d" % i, [128, KT, TT], BF16)) for i in range(nbuf)]
        xTs_r = [Res("xT%d" % i) for i in range(nbuf)]
        actT = [es.enter_context(nc.sbuf_tensor(tag + "_actT%d" % i, [128, FC, TT], BF16)) for i in range(2)]
        actT_r = [Res("actT%d" % i) for i in range(2)]
        wg = [es.enter_context(nc.sbuf_tensor(tag + "_wg%d" % i, [128, KT, FC * 128], BF16)) for i in range(2)]
        wu = [es.enter_context(nc.sbuf_tensor(tag + "_wu%d" % i, [128, KT, FC * 128], BF16)) for i in range(2)]
        wd = [es.enter_context(nc.sbuf_tensor(tag + "_wd%d" % i, [128, FC, D], BF16)) for i in range(2)]
        wg_r = [Res("wg%d" % i) for i in range(2)]
        wu_r = [Res("wu%d" % i) for i in range(2)]
        wd_r = [Res("wd%d" % i) for i in range(2)]
        sg = [es.enter_context(nc.sbuf_tensor(tag + "_sg%d" % i, [128, 512], F32)) for i in range(2)]
        sg_r = [Res("sg%d" % i) for i in range(2)]
        psG = [es.enter_context(nc.psum_tensor(tag + "_psG%d" % i, [128, 512], F32)) for i in range(2)]
        psU = [es.enter_context(nc.psum_tensor(tag + "_psU%d" % i, [128, 512], F32)) for i in range(2)]
        psG_r = [Res("psG%d" % i) for i in range(2)]
        psU_r = [Res("psU%d" % i) for i in range(2)]
        psO = [es.enter_context(nc.psum_tensor(tag + "_psO%d" % i, [128, 512], F32)) for i in range(2)]
        psO_r = [Res("psO%d" % i) for i in range(2)]
        if router is not None:
            wr = es.enter_context(nc.sbuf_tensor(tag + "_wr", [128, KT, 8], BF16))
            wr_r = Res("wr")
            kb.dma("pool", wr[:], router.rearrange("(kt p) e -> p kt e", p=128), [], [wr_r], wr_r)
            gates = es.enter_context(nc.sbuf_tensor(tag + "_gates", [128, NJ, 8], F32))
            gates_r = Res("gates")
            lg = es.enter_context(nc.sbuf_tensor(tag + "_lg", [128, 8], F32))
            lg_r = Res("lg")
            mx = es.enter_context(nc.sbuf_tensor(tag + "_mx", [128, 8], F32))
            mx_r = Res("mx")
            msk = es.enter_context(nc.sbuf_tensor(tag + "_msk", [128, 8], F32))
            msk_r = Res("msk")
            ex = es.enter_context(nc.sbuf_tensor(tag + "_ex", [128, 8], F32))
            ex_r = Res("ex")
            sm = es.enter_context(nc.sbuf_tensor(tag + "_sm", [128, 2], F32))
            sm_r = Res("sm")

        wseq = [(T, e, ch) for T in range(S // TT) for e in range(NE) for ch in range(NCH)]
        state = {"loaded": 0}

        def load_w(i):
            T, e, ch = wseq[i]
            b = i % 2
            wg_d, wu_d, wd_d = experts[e]
            f0 = choff[ch] * 128
            cw = chunk_sizes[ch] * 128
            kb.dma("pool", wg[b][:, :, 0:cw], wg_d[:, f0:f0 + cw].rearrange("(kt p) f -> p kt f", p=128),
                   [], [wg_r[b]], wg_r[b])
            kb.dma("pool", wu[b][:, :, 0:cw], wu_d[:, f0:f0 + cw].rearrange("(kt p) f -> p kt f", p=128),
                   [], [wu_r[b]], wu_r[b])
            kb.dma("pool", wd[b][:, 0:chunk_sizes[ch], :], wd_d[f0:f0 + cw, :].rearrange("(ft p) d -> p ft d", p=128),
                   [], [wd_r[b]], wd_r[b])

        load_w(0)
        wi = 0
        gcount = 0
        ocount = 0
        NT = S // TT

        def front(T):
            t0 = T * TT
            acc, acc_r = accs[T % nbuf], accs_r[T % nbuf]
            xT, xT_r = [xTs[T % nbuf]], [xTs_r[T % nbuf]]
            nonlocal ocount
            for hf in range(2):
                kb.dma("sp", acc[:, hf * 4:(hf + 1) * 4, :],
                       src_ap[t0 + hf * 512:t0 + (hf + 1) * 512, :].rearrange("(j p) d -> p j d", p=128),
                       [src_r], [acc_r], acc_r)
            for hf in range(2):
                fr.norm_T(acc[:, hf * 4:(hf + 1) * 4, :], acc_r, 4, xT[0], xT_r[0], hf * 512)
            if router is not None:
                for j in range(NJ):
                    pl = psO[ocount % 2]
                    pl_r = psO_r[ocount % 2]
                    ocount += 1
                    for kt in range(KT):
                        kb.op("pe", lambda e, kt=kt, j=j: e.matmul(out=pl[:, 0:8], lhsT=xT[0][:, kt, j * 128:(j + 1) * 128],
                                                                  rhs=wr[:, kt, :], start=(kt == 0), stop=(kt == KT - 1)),
                              reads=[xT_r[0], wr_r], writes=[pl_r], signal=(kt == KT - 1))
                    kb.op("act", lambda e: e.activation(out=lg[:], in_=pl[:, 0:8], func=AF.Copy),
                          reads=[pl_r], writes=[lg_r])
                    kb.op("dve", lambda e: e.max(out=mx[:], in_=lg[:]), reads=[lg_r], writes=[mx_r])
                    kb.op("dve", lambda e: e.tensor_scalar(out=msk[:], in0=lg[:], scalar1=mx[:, 1:2], scalar2=None,
                                                           op0=ALU.is_ge), reads=[lg_r, mx_r], writes=[msk_r])
                    kb.op("dve", lambda e: e.tensor_scalar(out=ex[:], in0=lg[:], scalar1=mx[:, 0:1], scalar2=None,
                                                           op0=ALU.subtract), reads=[lg_r, mx_r], writes=[ex_r])
                    kb.op("act", lambda e: e.activation(out=ex[:], in_=ex[:], func=AF.Exp), reads=[ex_r], writes=[ex_r])
                    kb.op("dve", lambda e: e.tensor_tensor(out=ex[:], in0=ex[:], in1=msk[:], op=ALU.mult),
                          reads=[ex_r, msk_r], writes=[ex_r])
                    kb.op("dve", lambda e: e.reduce_sum(out=sm[:, 0:1], in_=ex[:], axis=AX.X), reads=[ex_r], writes=[sm_r])
                    kb.op("dve", lambda e: e.reciprocal(out=sm[:, 1:2], in_=sm[:, 0:1]), reads=[sm_r], writes=[sm_r])
                    kb.op("dve", lambda e, j=j: e.tensor_scalar(out=gates[:, j, :], in0=ex[:], scalar1=sm[:, 1:2],
                                                                scalar2=None, op0=ALU.mult),
                          reads=[ex_r, sm_r], writes=[gates_r])
        front(0)
        for T in range(NT):
            t0 = T * TT
            acc, acc_r = accs[T % nbuf], accs_r[T % nbuf]
            xT, xT_r = [xTs[T % nbuf]], [xTs_r[T % nbuf]]
            for e_i in range(NE):
                for ch in range(NCH):
                    if nbuf == 2 and e_i == 0 and ch == min(1, NCH - 1) and T + 1 < NT:
                        front(T + 1)
                    b = wi % 2
                    if wi + 1 < len(wseq):
                        load_w(wi + 1)
                    ab = wi % 2
                    CS = chunk_sizes[ch]
                    for fl in range(CS):
                        for hf in range(2):
                            gb = gcount % 2
                            gcount += 1
                            for (wt, wt_r, pst, pst_r) in ((wg[b], wg_r[b], psG[gb], psG_r[gb]),
                                                           (wu[b], wu_r[b], psU[gb], psU_r[gb])):
                                for kt in range(KT):
                                    kb.op("pe", lambda e, kt=kt, wt=wt, pst=pst, fl=fl, hf=hf: e.matmul(
                                        out=pst[:], lhsT=wt[:, kt, fl * 128:(fl + 1) * 128],
                                        rhs=xT[0][:, kt, hf * 512:(hf + 1) * 512],
                                        start=(kt == 0), stop=(kt == KT - 1)),
                                        reads=[wt_r, xT_r[0]], writes=[pst_r], signal=(kt == KT - 1))
                            kb.op("act", lambda e, gb=gb: e.activation(out=sg[gb][:], in_=psG[gb][:], func=AF.Silu),
                                  reads=[psG_r[gb]], writes=[sg_r[gb]])
                            kb.op("dve", lambda e, gb=gb, fl=fl, hf=hf, ab=ab: e.tensor_tensor(
                                out=actT[ab][:, fl, hf * 512:(hf + 1) * 512], in0=sg[gb][:], in1=psU[gb][:], op=ALU.mult),
                                reads=[sg_r[gb], psU_r[gb]], writes=[actT_r[ab]])
                    for j in range(NJ):
                        for dh in range(2):
                            ob = ocount % 2
                            ocount += 1
                            for fl in range(CS):
                                kb.op("pe", lambda e, fl=fl, j=j, dh=dh, ob=ob, ab=ab, b=b: e.matmul(
                                    out=psO[ob][:], lhsT=actT[ab][:, fl, j * 128:(j + 1) * 128],
                                    rhs=wd[b][:, fl, dh * 512:(dh + 1) * 512],
                                    start=(fl == 0), stop=(fl == CS - 1)),
                                    reads=[actT_r[ab], wd_r[b]], writes=[psO_r[ob]], signal=(fl == CS - 1))
                            if router is not None:
                                kb.op("dve", lambda e, j=j, dh=dh, ob=ob, e_i=e_i: e.scalar_tensor_tensor(
                                    out=acc[:, j, dh * 512:(dh + 1) * 512], in0=psO[ob][:],
                                    scalar=gates[:, j, e_i:e_i + 1], in1=acc[:, j, dh * 512:(dh + 1) * 512],
                                    op0=ALU.mult, op1=ALU.add),
                                    reads=[psO_r[ob], gates_r, acc_r], writes=[acc_r])
                            else:
                                kb.op("dve", lambda e, j=j, dh=dh, ob=ob: e.tensor_tensor(
                                    out=acc[:, j, dh * 512:(dh + 1) * 512], in0=psO[ob][:],
                                    in1=acc[:, j, dh * 512:(dh + 1) * 512], op=ALU.add),
                                    reads=[psO_r[ob], acc_r], writes=[acc_r])
                    wi += 1
            if final_g is not None:
                fr.rstd(acc[:], acc_r, NJ)
                for j in range(NJ):
                    kb.op("dve", lambda e, j=j: e.scalar_tensor_tensor(out=acc[:, j, :], in0=acc[:, j, :],
                                                                       scalar=fr.rs[:, j:j + 1], in1=gf[:],
                                                                       op0=ALU.mult, op1=ALU.mult),
                          reads=[acc_r, fr.rs_r, gf_r], writes=[acc_r])
            for hf in range(2):
                kb.dma("sp", dst_ap[t0 + hf * 512:t0 + (hf + 1) * 512, :].rearrange("(j p) d -> p j d", p=128),
                       acc[:, hf * 4:(hf + 1) * 4, :], [acc_r], [dst_r], acc_r, store=True)
            if nbuf == 1 and T + 1 < NT:
                front(T + 1)
        kb.barrier()


def fourier_phase(kb, src, dst, norm_g, w_fnet, ident_d, dftc, dfts, cc_d, nsc_d, sc_d, rev_d, tag="fn"):
    nc = kb.nc
    src_ap, src_r = src
    dst_ap, dst_r = dst
    NST = S // 128
    with ExitStack() as es:
        ident = es.enter_context(nc.sbuf_tensor(tag + "_ident", [128, 128], BF16))
        ident_r = Res("ident")
        kb.dma("sp", ident[:], ident_d, [], [ident_r], ident_r)
        hnS = es.enter_context(nc.sbuf_tensor(tag + "_hnS", [128, NST, D], BF16))
        hnS_r = Res("hnS")
        wf = es.enter_context(nc.sbuf_tensor(tag + "_wf", [128, KT, D], BF16))
        wf_r = Res("wf")
        kb.dma("pool", wf[:], w_fnet.rearrange("(kt p) d -> p kt d", p=128), [], [wf_r], wf_r)
        cc = es.enter_context(nc.sbuf_tensor(tag + "_cc", [128, 2, 256], BF16))
        cc_r = Res("cc")
        kb.dma("sp", cc[:], cc_d, [], [cc_r], cc_r)
        nsc = es.enter_context(nc.sbuf_tensor(tag + "_nsc", [128, 2, 256], BF16))
        nsc_r = Res("nsc")
        kb.dma("sp", nsc[:], nsc_d, [], [nsc_r], nsc_r)
        with ExitStack() as es0:
            fr = Front(kb, es0, ident, ident_r, norm_g, tag + "f", with_T=False)
            x4 = [es0.enter_context(nc.sbuf_tensor(tag + "_x4%d" % i, [128, 4, D], F32)) for i in range(4)]
            x4_r = [Res("x4%d" % i) for i in range(4)]
            for T in range(S // 512):
                b = T % 4
                kb.dma("sp", x4[b][:], src_ap[T * 512:(T + 1) * 512, :].rearrange("(j p) d -> p j d", p=128),
                       [src_r], [x4_r[b]], x4_r[b])
                fr.rstd(x4[b][:], x4_r[b], 4)
                for j in range(4):
                    fr.norm_to(hnS[:, T * 4 + j, :], hnS_r, x4[b][:, j, :], x4_r[b], j)
            kb.barrier()
        with ExitStack() as es1:
            tc = [es1.enter_context(nc.sbuf_tensor(tag + "_tc%d" % i, [128, NST, 128], BF16)) for i in range(2)]
            ts = [es1.enter_context(nc.sbuf_tensor(tag + "_ts%d" % i, [128, NST, 128], BF16)) for i in range(2)]
            tc_r = [Res("tc%d" % i) for i in range(2)]
            ts_r = [Res("ts%d" % i) for i in range(2)]
            rev = es1.enter_context(nc.sbuf_tensor(tag + "_rev", [128, 128], BF16))
            rev_r = Res("rev")
            kb.dma("sp", rev[:], rev_d, [], [rev_r], rev_r)
            sc = es1.enter_context(nc.sbuf_tensor(tag + "_sc", [128, 2, 256], BF16))
            sc_r = Res("sc")
            kb.dma("sp", sc[:], sc_d, [], [sc_r], sc_r)
            Pb = es1.enter_context(nc.sbuf_tensor(tag + "_Pb", [128, D], BF16))
            Qb = es1.enter_context(nc.sbuf_tensor(tag + "_Qb", [128, D], BF16))
            Pb_r, Qb_r = Res("Pb"), Res("Qb")
            PT = [es1.enter_context(nc.sbuf_tensor(tag + "_PT%d" % i, [128, KT, 128], BF16)) for i in range(2)]
            QT = [es1.enter_context(nc.sbuf_tensor(tag + "_QT%d" % i, [128, KT, 128], BF16)) for i in range(2)]
            PT_r = [Res("PT%d" % i) for i in range(2)]
            QT_r = [Res("QT%d" % i) for i in range(2)]
            YT = [es1.enter_context(nc.sbuf_tensor(tag + "_YT%d" % i, [128, KT, 128], BF16)) for i in range(2)]
            YT_r = [Res("YT%d" % i) for i in range(2)]
            xt = [es1.enter_context(nc.sbuf_tensor(tag + "_xt%d" % i, [128, D], F32)) for i in range(4)]
            xt_r = [Res("xt%d" % i) for i in range(4)]
            psP = [es1.enter_context(nc.psum_tensor(tag + "_psP%d" % i, [128, 512], F32)) for i in range(2)]
            psQ = [es1.enter_context(nc.psum_tensor(tag + "_psQ%d" % i, [128, 512], F32)) for i in range(2)]
            psP_r = [Res("psP%d" % i) for i in range(2)]
            psQ_r = [Res("psQ%d" % i) for i in range(2)]
            psT = [es1.enter_context(nc.psum_tensor(tag + "_psT%d" % i, [128, D], BF16)) for i in range(2)]
            psT_r = [Res("psT%d" % i) for i in range(2)]
            psY = es1.enter_context(nc.psum_tensor(tag + "_psY", [128, D], F32))
            psY_r = Res("psY")
            NSRC = 17

            def load_tab(ai):
                b = ai % 2
                kb.dma("sp", tc[b][:], dftc[ai], [], [tc_r[b]], tc_r[b])
                kb.dma("sp", ts[b][:], dfts[ai], [], [ts_r[b]], ts_r[b])

            def load_x(ai):
                a_ = ai - 1
                xd, xd_r = xt[(ai % 2) * 2], xt_r[(ai % 2) * 2]
                xm, xm_r = xt[(ai % 2) * 2 + 1], xt_r[(ai % 2) * 2 + 1]
                if a_ < 0:
                    kb.op("dve", lambda e: e.memset(xd[:], 0.0), writes=[xd_r])
                    kb.dma("sp", xd[127:128, :], src_ap[0:1, :], [src_r], [xd_r], xd_r)
                else:
                    kb.dma("sp", xd[:], src_ap[128 * a_ + 1:128 * a_ + 129, :], [src_r], [xd_r], xd_r)
                    r0 = 128 * (31 - a_)
                    kb.dma("sp", xm[:], src_ap[r0:r0 + 128, :], [src_r], [xm_r], xm_r)

            load_tab(0)
            load_x(0)
            for ai in range(NSRC):
                a_ = ai - 1
                b = ai % 2
                if ai + 1 < NSRC:
                    load_tab(ai + 1)
                    load_x(ai + 1)
                for (tab, tab_r, psX, psX_r) in ((tc[b], tc_r[b], psP, psP_r), (ts[b], ts_r[b], psQ, psQ_r)):
                    for ch in range(2):
                        for st in range(NST):
                            kb.op("pe", lambda e: e.matmul(
                                out=psX[ch][:], lhsT=tab[:, st, :], rhs=hnS[:, st, ch * 512:(ch + 1) * 512],
                                start=(st == 0), stop=(st == NST - 1)),
                                reads=[tab_r, hnS_r], writes=[psX_r[ch]], signal=(st == NST - 1))
                for (psX, psX_r, Xb, Xb_r) in ((psP, psP_r, Pb, Pb_r), (psQ, psQ_r, Qb, Qb_r)):
                    for ch in range(2):
                        kb.op("act", lambda e: e.activation(
                            out=Xb[:, ch * 512:(ch + 1) * 512], in_=psX[ch][:], func=AF.Copy),
                            reads=[psX_r[ch]], writes=[Xb_r])
                variants = [(0, ident, ident_r, nsc, nsc_r)]
                if a_ >= 0:
                    variants.append((1, rev, rev_r, sc, sc_r))
                for (mi, perm, perm_r, stab, stab_r) in variants:
                    for i, (Xb, Xb_r, XT, XT_r) in enumerate(((Pb, Pb_r, PT[mi], PT_r[mi]), (Qb, Qb_r, QT[mi], QT_r[mi]))):
                        for ct in range(KT):
                            kb.op("pe", lambda e: e.transpose(
                                out=psT[i][:, ct * 128:(ct + 1) * 128], in_=Xb[:, ct * 128:(ct + 1) * 128],
                                identity=perm[:]), reads=[Xb_r, perm_r], writes=[psT_r[i]], signal=(ct == KT - 1))
                        kb.op("dve", lambda e: e.tensor_copy(
                            out=XT[:], in_=psT[i][:].rearrange("p (k t) -> p k t", k=KT)),
                            reads=[psT_r[i]], writes=[XT_r])
                    for g in range(4):
                        for c2 in range(2):
                            o = (g * 2 + c2) * 128
                            n = 0
                            for (tabc, XT) in ((cc, PT[mi]), (stab, QT[mi])):
                                for ct in range(2):
                                    kb.op("pe", lambda e: e.matmul(
                                        out=psY[:, o:o + 128], lhsT=tabc[:, ct, c2 * 128:(c2 + 1) * 128],
                                        rhs=XT[:, g * 2 + ct, :], start=(n == 0), stop=(n == 3)),
                                        reads=[cc_r, stab_r, PT_r[mi], QT_r[mi]], writes=[psY_r],
                                        signal=(n == 3 and g == 3 and c2 == 1))
                                    n += 1
                    kb.op("act", lambda e: e.activation(out=YT[mi][:], in_=psY[:].rearrange("p (k t) -> p k t", k=KT),
                                                        func=AF.Identity, scale=1.0 / 1024.0),
                          reads=[psY_r], writes=[YT_r[mi]])
                    xo, xo_r = xt[(ai % 2) * 2 + mi], xt_r[(ai % 2) * 2 + mi]
                    for dh in range(2):
                        for ft in range(KT):
                            kb.op("pe", lambda e: e.matmul(
                                out=psP[dh][:], lhsT=YT[mi][:, ft, :], rhs=wf[:, ft, dh * 512:(dh + 1) * 512],
                                start=(ft == 0), stop=(ft == KT - 1)),
                                reads=[YT_r[mi], wf_r], writes=[psP_r[dh]], signal=(ft == KT - 1))
                        kb.op("dve", lambda e: e.tensor_tensor(
                            out=xo[:, dh * 512:(dh + 1) * 512], in0=psP[dh][:], in1=xo[:, dh * 512:(dh + 1) * 512],
                            op=ALU.add), reads=[psP_r[dh], xo_r], writes=[xo_r])
                    if mi == 0:
                        if a_ < 0:
                            kb.dma("sp", dst_ap[0:1, :], xo[127:128, :], [xo_r], [dst_r], xo_r, store=True)
                        elif a_ == 15:
                            kb.dma("sp", dst_ap[128 * a_ + 1:128 * a_ + 128, :], xo[0:127, :], [xo_r], [dst_r], xo_r, store=True)
                        else:
                            kb.dma("sp", dst_ap[128 * a_ + 1:128 * a_ + 129, :], xo[:], [xo_r], [dst_r], xo_r, store=True)
                    else:
                        r0 = 128 * (31 - a_)
                        kb.dma("sp", dst_ap[r0:r0 + 128, :], xo[:], [xo_r], [dst_r], xo_r, store=True)
            kb.barrier()


def ret_proj_phase(kb, src, norm_g, w_in, gn_gain, ident_d, rope_cos, rope_sin, qk_s, v_s, sg_s, tag="ra"):
    nc = kb.nc
    src_ap, src_r = src
    qk_ap, qk_r = qk_s
    v_ap, v_r = v_s
    sg_ap, sg_r = sg_s
    with ExitStack() as es:
        ident = es.enter_context(nc.sbuf_tensor(tag + "_ident", [128, 128], BF16))
        ident_r = Res("ident")
        kb.dma("sp", ident[:], ident_d, [], [ident_r], ident_r)
        xT = es.enter_context(nc.sbuf_tensor(tag + "_xT", [128, KT, S], BF16))
        xT_r = Res("xT")
        with ExitStack() as es0:
            fr = Front(kb, es0, ident, ident_r, norm_g, tag + "f")
            x4 = [es0.enter_context(nc.sbuf_tensor(tag + "_x4%d" % i, [128, 4, D], F32)) for i in range(4)]
            x4_r = [Res("x4%d" % i) for i in range(4)]
            for T in range(S // 512):
                b = T % 4
                kb.dma("sp", x4[b][:], src_ap[T * 512:(T + 1) * 512, :].rearrange("(j p) d -> p j d", p=128),
                       [src_r], [x4_r[b]], x4_r[b])
                fr.norm_T(x4[b][:], x4_r[b], 4, xT, xT_r, T * 512)
            kb.barrier()
        with ExitStack() as es1:
            wqk = es1.enter_context(nc.sbuf_tensor(tag + "_wqk", [128, KT, 2048], BF16))
            wqk_r = Res("wqk")
            for hh in range(2):
                kb.dma("pool", wqk[:, :, hh * 1024:(hh + 1) * 1024],
                       w_in[:, hh * 1024:(hh + 1) * 1024].rearrange("(kt p) f -> p kt f", p=128),
                       [], [wqk_r], wqk_r)
            cs = [es1.enter_context(nc.sbuf_tensor(tag + "_cs%d" % i, [128, 512], F32)) for i in range(2)]
            sn = [es1.enter_context(nc.sbuf_tensor(tag + "_sn%d" % i, [128, 512], F32)) for i in range(2)]
            cs_r = [Res("cs%d" % i) for i in range(2)]
            sn_r = [Res("sn%d" % i) for i in range(2)]
            As = [es1.enter_context(nc.sbuf_tensor(tag + "_As%d" % i, [128, 512], F32)) for i in range(2)]
            Bs = [es1.enter_context(nc.sbuf_tensor(tag + "_Bs%d" % i, [128, 512], F32)) for i in range(2)]
            As_r = [Res("As%d" % i) for i in range(2)]
            Bs_r = [Res("Bs%d" % i) for i in range(2)]
            tt = [es1.enter_context(nc.sbuf_tensor(tag + "_tt%d" % i, [128, 512], F32)) for i in range(8)]
            tt_r = [Res("tt%d" % i) for i in range(8)]
            stg = [es1.enter_context(nc.sbuf_tensor(tag + "_stg%d" % i, [128, 4, 16, 128], BF16)) for i in range(2)]
            stg_r = [Res("stg%d" % i) for i in range(2)]
            psA = [es1.enter_context(nc.psum_tensor(tag + "_psA%d" % i, [128, 512], F32)) for i in range(2)]
            psB = [es1.enter_context(nc.psum_tensor(tag + "_psB%d" % i, [128, 512], F32)) for i in range(2)]
            psA_r = [Res("psA%d" % i) for i in range(2)]
            psB_r = [Res("psB%d" % i) for i in range(2)]
            n = 0
            for T in range(S // 512):
                tb = T % 2
                kb.dma("sp", cs[tb][:], rope_cos[:, T * 512:(T + 1) * 512], [], [cs_r[tb]], cs_r[tb])
                kb.dma("sp", sn[tb][:], rope_sin[:, T * 512:(T + 1) * 512], [], [sn_r[tb]], sn_r[tb])
                for which in range(2):
                    scale = 1.0 if which == 0 else 1.0 / 16.0
                    for h in range(4):
                        b = n % 2
                        n += 1
                        f1 = which * 8 + 2 * h
                        for (ft, psX, psX_r) in ((f1, psA[b], psA_r[b]), (f1 + 1, psB[b], psB_r[b])):
                            for kt in range(KT):
                                kb.op("pe", lambda e, kt=kt, ft=ft, psX=psX: e.matmul(
                                    out=psX[:], lhsT=wqk[:, kt, ft * 128:(ft + 1) * 128],
                                    rhs=xT[:, kt, T * 512:(T + 1) * 512], start=(kt == 0), stop=(kt == KT - 1)),
                                    reads=[wqk_r, xT_r], writes=[psX_r], signal=(kt == KT - 1))
                        kb.op("act", lambda e: e.activation(out=As[b][:], in_=psA[b][:], func=AF.Identity, scale=scale),
                              reads=[psA_r[b]], writes=[As_r[b]])
                        kb.op("act", lambda e: e.activation(out=Bs[b][:], in_=psB[b][:], func=AF.Identity, scale=scale),
                              reads=[psB_r[b]], writes=[Bs_r[b]])
                        t = [tt[b * 4 + i] for i in range(4)]
                        t_r = [tt_r[b * 4 + i] for i in range(4)]
                        kb.op("dve", lambda e: e.tensor_tensor(out=t[0][:], in0=As[b][:], in1=cs[tb][:], op=ALU.mult),
                              reads=[As_r[b], cs_r[tb]], writes=[t_r[0]])
                        kb.op("dve", lambda e: e.tensor_tensor(out=t[1][:], in0=Bs[b][:], in1=sn[tb][:], op=ALU.mult),
                              reads=[Bs_r[b], sn_r[tb]], writes=[t_r[1]])
                        kb.op("dve", lambda e: e.tensor_tensor(out=t[2][:], in0=As[b][:], in1=sn[tb][:], op=ALU.mult),
                              reads=[As_r[b], sn_r[tb]], writes=[t_r[2]])
                        kb.op("dve", lambda e: e.tensor_tensor(out=t[3][:], in0=Bs[b][:], in1=cs[tb][:], op=ALU.mult),
                              reads=[Bs_r[b], cs_r[tb]], writes=[t_r[3]])
                        kb.op("pool", lambda e: e.tensor_tensor(
                            out=stg[tb][:, :, f1, :], in0=t[0][:].rearrange("p (c t) -> p c t", c=4),
                            in1=t[1][:].rearrange("p (c t) -> p c t", c=4), op=ALU.subtract),
                            reads=[t_r[0], t_r[1]], writes=[stg_r[tb]])
                        kb.op("pool", lambda e: e.tensor_tensor(
                            out=stg[tb][:, :, f1 + 1, :], in0=t[2][:].rearrange("p (c t) -> p c t", c=4),
                            in1=t[3][:].rearrange("p (c t) -> p c t", c=4), op=ALU.add),
                            reads=[t_r[2], t_r[3]], writes=[stg_r[tb]])
                kb.dma("sp", qk_ap[T], stg[tb][:], [stg_r[tb]], [qk_r], stg_r[tb], store=True)
            kb.barrier()
        with ExitStack() as es2:
            wv = [es2.enter_context(nc.sbuf_tensor(tag + "_wv%d" % i, [128, KT, 512], BF16)) for i in range(2)]
            wv_r = [Res("wv%d" % i) for i in range(2)]
            gg = es2.enter_context(nc.sbuf_tensor(tag + "_gg", [128, 2048], F32))
            gg_r = Res("gg")
            kb.dma("sp", gg[:], bcast_row(gn_gain, 2048), [], [gg_r], gg_r)
            vst = [es2.enter_context(nc.sbuf_tensor(tag + "_vst%d" % i, [128, 4, 512], BF16)) for i in range(2)]
            vst_r = [Res("vst%d" % i) for i in range(2)]
            sgt = [es2.enter_context(nc.sbuf_tensor(tag + "_sgt%d" % i, [128, 512], F32)) for i in range(2)]
            sgt_r = [Res("sgt%d" % i) for i in range(2)]
            psV = [es2.enter_context(nc.psum_tensor(tag + "_psV%d" % i, [128, 512], F32)) for i in range(4)]
            psV_r = [Res("psV%d" % i) for i in range(4)]

            def load_wv(cb):
                b = cb % 2
                c0 = 2048 + cb * 512
                kb.dma("pool", wv[b][:], w_in[:, c0:c0 + 512].rearrange("(kt p) f -> p kt f", p=128),
                       [], [wv_r[b]], wv_r[b])

            load_wv(0)
            n = 0
            m = 0
            for cb in range(8):
                b = cb % 2
                if cb + 1 < 8:
                    load_wv(cb + 1)
                is_g = cb >= 4
                cc0 = (cb % 4) * 512
                for T in range(S // 512):
                    sb_ = m % 2
                    m += 1
                    for j in range(4):
                        pb = n % 4
                        n += 1
                        t0 = T * 512 + j * 128
                        for kt in range(KT):
                            kb.op("pe", lambda e, kt=kt, t0=t0, pb=pb: e.matmul(
                                out=psV[pb][:], lhsT=xT[:, kt, t0:t0 + 128], rhs=wv[b][:, kt, :],
                                start=(kt == 0), stop=(kt == KT - 1)),
                                reads=[xT_r, wv_r[b]], writes=[psV_r[pb]], signal=(kt == KT - 1))
                        if not is_g:
                            kb.op("act", lambda e, j=j, pb=pb: e.activation(out=vst[sb_][:, j, :], in_=psV[pb][:],
                                                                            func=AF.Copy),
                                  reads=[psV_r[pb]], writes=[vst_r[sb_]])
                        else:
                            gb = n % 2
                            kb.op("act", lambda e, pb=pb, gb=gb: e.activation(out=sgt[gb][:], in_=psV[pb][:],
                                                                              func=AF.Silu),
                                  reads=[psV_r[pb]], writes=[sgt_r[gb]])
                            kb.op("dve", lambda e, j=j, gb=gb: e.tensor_tensor(
                                out=vst[sb_][:, j, :], in0=sgt[gb][:], in1=gg[:, cc0:cc0 + 512], op=ALU.mult),
                                reads=[sgt_r[gb], gg_r], writes=[vst_r[sb_]])
                    dst_ap, dst_r = (sg_ap, sg_r) if is_g else (v_ap, v_r)
                    kb.dma("sp", dst_ap[T * 512:(T + 1) * 512, cc0:cc0 + 512].rearrange("(j p) f -> p j f", p=128),
                           vst[sb_][:], [vst_r[sb_]], [dst_r], vst_r[sb_], store=True)
            kb.barrier()


class RetConsts:
    def __init__(self, kb, es, decay_logit, tag):
        nc = kb.nc
        I32 = mybir.dt.int32

        def T(name, shape, dtype=F32):
            return es.enter_context(nc.sbuf_tensor(tag + "_" + name, shape, dtype)), Res(name)

        dl, dl_r = T("dl", [128, 8])
        kb.dma("sp", dl[:], bcast_row(decay_logit.rearrange("a b -> (a b)"), 8), [], [dl_r], dl_r)
        self.lg, self.lg_r = T("lg", [128, 8])
        nlg, nlg_r = T("nlg", [128, 8])
        lg127, lg127_r = T("lg127", [128, 8])
        self.g128, self.g128_r = T("g128", [128, 8])
        lg, lg_r = self.lg, self.lg_r
        kb.op("act", lambda e: e.activation(out=nlg[:], in_=dl[:], func=AF.Exp, scale=-1.0), reads=[dl_r], writes=[nlg_r])
        kb.op("dve", lambda e: e.tensor_scalar(out=nlg[:], in0=nlg[:], scalar1=1.0, scalar2=None, op0=ALU.add),
              reads=[nlg_r], writes=[nlg_r])
        kb.op("act", lambda e: e.activation(out=nlg[:], in_=nlg[:], func=AF.Ln), reads=[nlg_r], writes=[nlg_r])
        kb.op("dve", lambda e: e.tensor_scalar(out=lg[:], in0=nlg[:], scalar1=-1.0, scalar2=None, op0=ALU.mult),
              reads=[nlg_r], writes=[lg_r])
        kb.op("dve", lambda e: e.tensor_scalar(out=lg127[:], in0=lg[:], scalar1=127.0, scalar2=None, op0=ALU.mult),
              reads=[lg_r], writes=[lg127_r])
        kb.op("act", lambda e: e.activation(out=self.g128[:], in_=lg[:], func=AF.Exp, scale=128.0),
              reads=[lg_r], writes=[self.g128_r])
        ii, ii_r = T("ii", [128, 128], I32)
        dmf, dmf_r = T("dmf", [128, 128])
        imat, imat_r = T("imat", [128, 128])
        jmat, jmat_r = T("jmat", [128, 128])
        pos, pos_r = T("pos", [128, 128])
        neg, neg_r = T("neg", [128, 128])
        tmp, tmp_r = T("tmp", [128, 128])
        kb.op("pool", lambda e: e.iota(ii[:], pattern=[[1, 128]], base=0, channel_multiplier=-1), writes=[ii_r])
        kb.op("dve", lambda e: e.tensor_copy(out=dmf[:], in_=ii[:]), reads=[ii_r], writes=[dmf_r])
        kb.op("pool", lambda e: e.iota(ii[:], pattern=[[1, 128]], base=0, channel_multiplier=0), reads=[], writes=[ii_r])
        kb.op("dve", lambda e: e.tensor_copy(out=imat[:], in_=ii[:]), reads=[ii_r], writes=[imat_r])
        kb.op("pool", lambda e: e.iota(ii[:], pattern=[[0, 128]], base=0, channel_multiplier=1), reads=[], writes=[ii_r])
        kb.op("dve", lambda e: e.tensor_copy(out=jmat[:], in_=ii[:]), reads=[ii_r], writes=[jmat_r])
        kb.op("dve", lambda e: e.tensor_scalar(out=pos[:], in0=dmf[:], scalar1=0.0, scalar2=None, op0=ALU.max),
              reads=[dmf_r], writes=[pos_r])
        kb.op("dve", lambda e: e.tensor_tensor(out=neg[:], in0=pos[:], in1=dmf[:], op=ALU.subtract),
              reads=[pos_r, dmf_r], writes=[neg_r])
        self.Dc, self.Dc_r = T("Dc", [128, 4, 128])
        self.XF, self.XF_r = T("XF", [128, 8, 128])
        self.XB, self.XB_r = T("XB", [128, 8, 128])
        self.ZF, self.ZF_r = T("ZF", [128, 8, 128])
        self.ZB, self.ZB_r = T("ZB", [128, 8, 128])
        for h in range(4):
            f, b_ = h, 4 + h
            kb.op("dve", lambda e: e.tensor_scalar(out=tmp[:], in0=pos[:], scalar1=lg[:, f:f + 1], scalar2=None,
                                                   op0=ALU.mult), reads=[pos_r, lg_r], writes=[tmp_r])
            kb.op("dve", lambda e: e.scalar_tensor_tensor(out=tmp[:], in0=neg[:], scalar=lg[:, b_:b_ + 1], in1=tmp[:],
                                                          op0=ALU.mult, op1=ALU.add),
                  reads=[neg_r, lg_r, tmp_r], writes=[tmp_r])
            kb.op("act", lambda e: e.activation(out=self.Dc[:, h, :], in_=tmp[:], func=AF.Exp),
                  reads=[tmp_r], writes=[self.Dc_r])
            for ft in (2 * h, 2 * h + 1):
                kb.op("act", lambda e: e.activation(out=self.XF[:, ft, :], in_=imat[:], func=AF.Exp,
                                                    scale=lg[:, f:f + 1], bias=lg[:, f:f + 1]),
                      reads=[imat_r, lg_r], writes=[self.XF_r])
                kb.op("act", lambda e: e.activation(out=self.XB[:, ft, :], in_=imat[:], func=AF.Exp,
                                                    scale=nlg[:, b_:b_ + 1], bias=lg127[:, b_:b_ + 1]),
                      reads=[imat_r, nlg_r, lg127_r], writes=[self.XB_r])
                kb.op("act", lambda e: e.activation(out=self.ZF[:, ft, :], in_=jmat[:], func=AF.Exp,
                                                    scale=nlg[:, f:f + 1], bias=lg127[:, f:f + 1]),
                      reads=[jmat_r, nlg_r, lg127_r], writes=[self.ZF_r])
                kb.op("act", lambda e: e.activation(out=self.ZB[:, ft, :], in_=jmat[:], func=AF.Exp,
                                                    scale=lg[:, b_:b_ + 1], bias=lg[:, b_:b_ + 1]),
                      reads=[jmat_r, lg_r], writes=[self.ZB_r])


def ret_core_phase(kb, decay_logit, ident_d, qk_s, v_s, sg_s, sb_s, z_s, tag="rb"):
    nc = kb.nc
    qk_ap, qk_r = qk_s
    v_ap, v_r = v_s
    sg_ap, sg_r = sg_s
    sb_ap, sb_r = sb_s
    z_ap, z_r = z_s
    NCK = S // 128
    with ExitStack() as es:
        ident = es.enter_context(nc.sbuf_tensor(tag + "_ident", [128, 128], BF16))
        ident_r = Res("ident")
        kb.dma("sp", ident[:], ident_d, [], [ident_r], ident_r)
        rc = RetConsts(kb, es, decay_logit, tag + "c")
        S32 = es.enter_context(nc.sbuf_tensor(tag + "_S32", [128, 8, 512], F32))
        S32_r = [Res("S32_%d" % i) for i in range(8)]
        S16 = [es.enter_context(nc.sbuf_tensor(tag + "_S16%d" % i, [128, 8, 512], BF16)) for i in range(2)]
        S16_r = [[Res("S16%d_%d" % (i, k)) for k in range(8)] for i in range(2)]
        S16_st = [Res("S16st%d" % i) for i in range(2)]
        qk = [es.enter_context(nc.sbuf_tensor(tag + "_qk%d" % i, [128, 16, 128], BF16)) for i in range(3)]
        qk_t = [Res("qk%d" % i) for i in range(3)]
        vt = [es.enter_context(nc.sbuf_tensor(tag + "_v%d" % i, [128, 2048], BF16)) for i in range(3)]
        vt_r = [Res("v%d" % i) for i in range(3)]
        kz = [es.enter_context(nc.sbuf_tensor(tag + "_kz%d" % i, [128, 8, 128], BF16)) for i in range(2)]
        kz_r = [Res("kz%d" % i) for i in range(2)]
        psK = es.enter_context(nc.psum_tensor(tag + "_psK", [128, 1024], BF16))
        psK_r = Res("psK")
        psS = [es.enter_context(nc.psum_tensor(tag + "_psS%d" % i, [128, 512], F32)) for i in range(2)]
        psS_r = [Res("psS%d" % i) for i in range(2)]
        cnt = {"s": 0}

        def load_chunk(n, want_q):
            b = n % 3
            T, c = n // 4, n % 4
            if want_q:
                kb.dma("sp", qk[b][:], qk_ap[T, :, c], [qk_r], [qk_t[b]], qk_t[b])
            else:
                kb.dma("sp", qk[b][:, 8:16, :], qk_ap[T, :, c, 8:16, :], [qk_r], [qk_t[b]], qk_t[b])
            kb.dma("sp", vt[b][:], v_ap[n * 128:(n + 1) * 128, :], [v_r], [vt_r[b]], vt_r[b])

        def k_tokmajor(n, Z, Z_r, use_act=False):
            b = n % 2
            b3 = n % 3
            for ft in range(8):
                kb.op("pe", lambda e, ft=ft: e.transpose(out=psK[:, ft * 128:(ft + 1) * 128], in_=qk[b3][:, 8 + ft, :],
                                                         identity=ident[:]),
                      reads=[qk_t[b3], ident_r], writes=[psK_r], signal=(ft == 7))
            if use_act:
                for h in range(4):
                    kb.op("act", lambda e: e.activation(out=kz[b][:, 2 * h:2 * h + 2, :],
                                                        in_=psK[:, h * 256:(h + 1) * 256].rearrange("p (f d) -> p f d", f=2),
                                                        func=AF.Identity, scale=Z[:, 2 * h, 0:1]),
                          reads=[psK_r, Z_r], writes=[kz_r[b]])
            else:
                kb.op("dve", lambda e: e.tensor_tensor(out=kz[b][:], in0=psK[:].rearrange("p (f d) -> p f d", f=8),
                                                       in1=Z[:], op=ALU.mult),
                      reads=[psK_r, Z_r], writes=[kz_r[b]])

        def state_update(n, gcol, s16_out, s16_out_r):
            b = n % 2
            b3 = n % 3
            for h in range(4):
                for dt_ in range(2):
                    ft = 2 * h + dt_
                    pb = cnt["s"] % len(psS)
                    cnt["s"] += 1
                    kb.op("pe", lambda e: e.matmul(out=psS[pb][:], lhsT=kz[b][:, ft, :],
                                                   rhs=vt[b3][:, h * 512:(h + 1) * 512], start=True, stop=True),
                          reads=[kz_r[b], vt_r[b3]], writes=[psS_r[pb]])
                    kb.op("dve", lambda e: e.scalar_tensor_tensor(
                        out=S32[:, ft, :], in0=S32[:, ft, :], scalar=rc.g128[:, gcol + h:gcol + h + 1], in1=psS[pb][:],
                        op0=ALU.mult, op1=ALU.add), reads=[S32_r[ft], rc.g128_r, psS_r[pb]], writes=[S32_r[ft]])
                    if gcol == 4 and ft % 4 == 3:
                        kb.op("pool", lambda e: e.tensor_copy(out=s16_out[:, ft, :], in_=S32[:, ft, :]),
                              reads=[S32_r[ft]], writes=[s16_out_r[ft]])
                    else:
                        kb.op("act", lambda e: e.activation(out=s16_out[:, ft, :], in_=S32[:, ft, :], func=AF.Copy),
                              reads=[S32_r[ft]], writes=[s16_out_r[ft]])

        esb1 = ExitStack()
        for i in range(2, 6):
            psS.append(esb1.enter_context(nc.psum_tensor(tag + "_psS%d" % i, [128, 512], F32)))
            psS_r.append(Res("psS%d" % i))
        kb.op("dve", lambda e: e.memset(S32[:], 0.0), writes=S32_r)
        kb.op("pool", lambda e: e.memset(S16[1][:], 0.0), writes=S16_r[1])
        load_chunk(NCK - 1, False)
        load_chunk(NCK - 2, False)
        k_tokmajor(NCK - 1, rc.ZB, rc.ZB_r, use_act=True)
        for n in range(NCK - 1, -1, -1):
            if n - 2 >= 0:
                load_chunk(n - 2, False)
            cur = S16[n % 2]
            cur_r = S16_r[n % 2]
            kb.dma("sp", sb_ap[n], cur[:], cur_r, [sb_r], S16_st[n % 2], store=True)
            if n > 1:
                k_tokmajor(n - 1, rc.ZB, rc.ZB_r, use_act=True)
            if n > 0:
                state_update(n, 4, S16[(n + 1) % 2], S16_r[(n + 1) % 2])
        kb.barrier(recycle=False)
        del psS[2:]
        del psS_r[2:]
        esb1.close()
        with ExitStack() as es2:
            sgt = [es2.enter_context(nc.sbuf_tensor(tag + "_sg%d" % i, [128, 2048], BF16)) for i in range(2)]
            sgt_r = [Res("sg%d" % i) for i in range(2)]
            sbn = [es2.enter_context(nc.sbuf_tensor(tag + "_sbn%d" % i, [128, 8, 512], BF16)) for i in range(3)]
            sbn_r = [Res("sbn%d" % i) for i in range(3)]
            qf2 = [es2.enter_context(nc.sbuf_tensor(tag + "_qf%d" % i, [128, 8, 128], BF16)) for i in range(2)]
            qb2 = [es2.enter_context(nc.sbuf_tensor(tag + "_qb%d" % i, [128, 8, 128], BF16)) for i in range(2)]
            qf2_r = [Res("qf%d" % i) for i in range(2)]
            qb2_r = [Res("qb%d" % i) for i in range(2)]
            PT = es2.enter_context(nc.sbuf_tensor(tag + "_PT", [128, 4, 128], BF16))
            PT_r = Res("PT")
            yn = [es2.enter_context(nc.sbuf_tensor(tag + "_yn%d" % i, [128, 512], F32)) for i in range(2)]
            yn_r = [Res("yn%d" % i) for i in range(2)]
            zt = [es2.enter_context(nc.sbuf_tensor(tag + "_z%d" % i, [128, 2048], BF16)) for i in range(2)]
            zt_r = [Res("z%d" % i) for i in range(2)]
            st6 = es2.enter_context(nc.sbuf_tensor(tag + "_st6", [128, 4, 6], F32))
            st6_r = Res("st6")
            mv = es2.enter_context(nc.sbuf_tensor(tag + "_mv", [128, 4, 2], F32))
            mv_r = Res("mv")
            rs = es2.enter_context(nc.sbuf_tensor(tag + "_rs", [128, 4], F32))
            rs_r = Res("rs")
            nmr = es2.enter_context(nc.sbuf_tensor(tag + "_nmr", [128, 4], F32))
            nmr_r = Res("nmr")
            psSc = es2.enter_context(nc.psum_tensor(tag + "_psSc", [128, 512], F32))
            psSc_r = Res("psSc")
            psY = [es2.enter_context(nc.psum_tensor(tag + "_psY%d" % i, [128, 512], F32)) for i in range(4)]
            psY_r = [Res("psY%d" % i) for i in range(4)]

            ysb = [es2.enter_context(nc.sbuf_tensor(tag + "_ysb%d" % i, [128, 2048], F32)) for i in range(2)]
            ysb_r = [[Res("ysb%d_%d" % (i, h)) for h in range(4)] for i in range(2)]
            sg3 = [es2.enter_context(nc.sbuf_tensor(tag + "_sg3%d" % i, [128, 2048], BF16)) for i in range(4)]
            sg3_r = [Res("sg3%d" % i) for i in range(4)]

            def load_chunk2(n):
                b = n % 3
                load_chunk(n, True)
                kb.dma("sp", sg3[n % 4][:], sg_ap[n * 128:(n + 1) * 128, :], [sg_r], [sg3_r[n % 4]], sg3_r[n % 4])
                kb.dma("sp", sbn[b][:], sb_ap[n], [sb_r], [sbn_r[b]], sbn_r[b])

            def stage_q(n):
                b = n % 3
                kb.op("pool", lambda e: e.tensor_tensor(out=qf2[n % 2][:], in0=qk[b][:, 0:8, :], in1=rc.XF[:], op=ALU.mult),
                      reads=[qk_t[b], rc.XF_r], writes=[qf2_r[n % 2]])
                kb.op("pool", lambda e: e.tensor_tensor(out=qb2[n % 2][:], in0=qk[b][:, 0:8, :], in1=rc.XB[:], op=ALU.mult),
                      reads=[qk_t[b], rc.XB_r], writes=[qb2_r[n % 2]])

            def stage_x(n):
                b = n % 3
                yb2 = n % 2
                qf, qf_r, qb, qb_r = qf2[n % 2], qf2_r[n % 2], qb2[n % 2], qb2_r[n % 2]
                sf = S16[(n + 1) % 2]
                sf_r = S16_r[(n + 1) % 2]
                if n + 2 < NCK:
                    k_tokmajor(n + 1, rc.ZF, rc.ZF_r)
                if n + 1 < NCK:
                    state_update(n, 0, S16[n % 2], S16_r[n % 2])
                for h in range(4):
                    for dt_ in range(2):
                        kb.op("pe", lambda e: e.matmul(out=psSc[:, h * 128:(h + 1) * 128],
                                                       lhsT=qk[b][:, 8 + 2 * h + dt_, :], rhs=qk[b][:, 2 * h + dt_, :],
                                                       start=(dt_ == 0), stop=(dt_ == 1)),
                              reads=[qk_t[b]], writes=[psSc_r], signal=(h == 3 and dt_ == 1))
                kb.op("dve", lambda e: e.tensor_tensor(out=PT[:], in0=psSc[:].rearrange("p (h i) -> p h i", h=4),
                                                       in1=rc.Dc[:], op=ALU.mult),
                      reads=[psSc_r, rc.Dc_r], writes=[PT_r])
                for h in range(4):
                    kb.op("pe", lambda e: e.matmul(out=psY[h][:], lhsT=PT[:, h, :], rhs=vt[b][:, h * 512:(h + 1) * 512],
                                                   start=True, stop=False),
                          reads=[PT_r, vt_r[b]], writes=[psY_r[h]], signal=False)
                    for dt_ in range(2):
                        kb.op("pe", lambda e: e.matmul(out=psY[h][:], lhsT=qf[:, 2 * h + dt_, :], rhs=sf[:, 2 * h + dt_, :],
                                                       start=False, stop=False),
                              reads=[qf_r, sf_r[2 * h + dt_]], writes=[psY_r[h]], signal=False)
                    for dt_ in range(2):
                        kb.op("pe", lambda e: e.matmul(out=psY[h][:], lhsT=qb[:, 2 * h + dt_, :],
                                                       rhs=sbn[b][:, 2 * h + dt_, :], start=False, stop=(dt_ == 1)),
                              reads=[PT_r, vt_r[b], qf_r, sf_r[2 * h], sf_r[2 * h + 1], qb_r, sbn_r[b]],
                              writes=[psY_r[h]], signal=(dt_ == 1))
                    kb.op("act", lambda e: e.activation(out=ysb[yb2][:, h * 512:(h + 1) * 512], in_=psY[h][:], func=AF.Copy),
                          reads=[psY_r[h]], writes=[ysb_r[yb2][h]])

            ycnt = {"c": 0}

            def stage_z(n):
                b = n % 2
                for h in range(4):
                    kb.op("dve", lambda e: e.bn_stats(out=st6[:, h, :], in_=ysb[b][:, h * 512:(h + 1) * 512]),
                          reads=[ysb_r[b][h]], writes=[st6_r])
                    kb.op("dve", lambda e: e.bn_aggr(out=mv[:, h, :], in_=st6[:, h, :]), reads=[st6_r], writes=[mv_r])
                kb.op("dve", lambda e: e.tensor_scalar(out=rs[:], in0=mv[:, :, 1], scalar1=EPS, scalar2=None, op0=ALU.add),
                      reads=[mv_r], writes=[rs_r])
                kb.op("act", lambda e: e.activation(out=rs[:], in_=rs[:], func=AF.Sqrt), reads=[rs_r], writes=[rs_r])
                kb.op("dve", lambda e: e.reciprocal(out=rs[:], in_=rs[:]), reads=[rs_r], writes=[rs_r])
                kb.op("dve", lambda e: e.scalar_tensor_tensor(out=nmr[:], in0=mv[:, :, 0], scalar=-1.0, in1=rs[:],
                                                              op0=ALU.mult, op1=ALU.mult),
                      reads=[mv_r, rs_r], writes=[nmr_r])
                for h in range(4):
                    yb = ycnt["c"] % 2
                    ycnt["c"] += 1
                    kb.op("act", lambda e: e.activation(out=yn[yb][:], in_=ysb[b][:, h * 512:(h + 1) * 512], func=AF.Identity,
                                                        scale=rs[:, h:h + 1], bias=nmr[:, h:h + 1]),
                          reads=[ysb_r[b][h], rs_r, nmr_r], writes=[yn_r[yb]])
                    kb.op("pool", lambda e: e.tensor_tensor(out=zt[b][:, h * 512:(h + 1) * 512], in0=yn[yb][:],
                                                            in1=sg3[n % 4][:, h * 512:(h + 1) * 512], op=ALU.mult),
                          reads=[yn_r[yb], sg3_r[n % 4]], writes=[zt_r[b]])
                kb.dma("sp", z_ap[n * 128:(n + 1) * 128, :], zt[b][:], [zt_r[b]], [z_r], zt_r[b], store=True)

            kb.op("dve", lambda e: e.memset(S32[:], 0.0), reads=[], writes=S32_r)
            kb.op("pool", lambda e: e.memset(S16[1][:], 0.0), reads=[], writes=S16_r[1])
            load_chunk2(0)
            load_chunk2(1)
            stage_q(0)
            k_tokmajor(0, rc.ZF, rc.ZF_r)
            for n in range(NCK):
                if n + 2 < NCK:
                    load_chunk2(n + 2)
                if n + 1 < NCK:
                    stage_q(n + 1)
                stage_x(n)
                if n >= 1:
                    stage_z(n - 1)
            stage_z(NCK - 1)
            kb.barrier()


def ret_out_phase(kb, src, dst, z_s, w_out, ident_d, tag="rc"):
    nc = kb.nc
    src_ap, src_r = src
    dst_ap, dst_r = dst
    z_ap, z_r = z_s
    with ExitStack() as es:
        ident = es.enter_context(nc.sbuf_tensor(tag + "_ident", [128, 128], BF16))
        ident_r = Res("ident")
        kb.dma("sp", ident[:], ident_d, [], [ident_r], ident_r)
        wo = es.enter_context(nc.sbuf_tensor(tag + "_wo", [128, 16, D], BF16))
        wo_r = Res("wo")
        for hh in range(2):
            kb.dma("pool", wo[:, hh * 8:(hh + 1) * 8, :],
                   w_out[hh * 1024:(hh + 1) * 1024, :].rearrange("(et p) d -> p et d", p=128), [], [wo_r], wo_r)
        zt = [es.enter_context(nc.sbuf_tensor(tag + "_z%d" % i, [128, 2048], BF16)) for i in range(2)]
        zt_r = [Res("z%d" % i) for i in range(2)]
        xt = [es.enter_context(nc.sbuf_tensor(tag + "_x%d" % i, [128, D], F32)) for i in range(4)]
        xt_r = [Res("x%d" % i) for i in range(4)]
        zT = [es.enter_context(nc.sbuf_tensor(tag + "_zT%d" % i, [128, 16, 128], BF16)) for i in range(2)]
        zT_r = [Res("zT%d" % i) for i in range(2)]
        psT = [es.enter_context(nc.psum_tensor(tag + "_psT%d" % i, [128, 1024], BF16)) for i in range(2)]
        psT_r = [Res("psT%d" % i) for i in range(2)]
        psO = [es.enter_context(nc.psum_tensor(tag + "_psO%d" % i, [128, 512], F32)) for i in range(4)]
        psO_r = [Res("psO%d" % i) for i in range(4)]

        def load(t):
            b = t % 2
            kb.dma("sp", zt[b][:], z_ap[t * 128:(t + 1) * 128, :], [z_r], [zt_r[b]], zt_r[b])
            kb.dma("sp", xt[t % 4][:], src_ap[t * 128:(t + 1) * 128, :], [src_r], [xt_r[t % 4]], xt_r[t % 4])

        def transp(t):
            b = t % 2
            for hh in range(2):
                for e8 in range(8):
                    et = hh * 8 + e8
                    kb.op("pe", lambda e: e.transpose(out=psT[hh][:, e8 * 128:(e8 + 1) * 128],
                                                      in_=zt[b][:, et * 128:(et + 1) * 128], identity=ident[:]),
                          reads=[zt_r[b], ident_r], writes=[psT_r[hh]], signal=(e8 == 7))
                kb.op("act", lambda e: e.activation(out=zT[b][:, hh * 8:(hh + 1) * 8, :],
                                                    in_=psT[hh][:].rearrange("p (k t) -> p k t", k=8), func=AF.Copy),
                      reads=[psT_r[hh]], writes=[zT_r[b]])

        load(0)
        load(1)
        transp(0)
        load(2)
        for t in range(S // 128):
            b = t % 2
            xb = t % 4
            if t + 1 < S // 128:
                transp(t + 1)
                if t + 3 < S // 128:
                    load(t + 3)
            for dh in range(2):
                pb = (t % 2) * 2 + dh
                for et in range(16):
                    kb.op("pe", lambda e: e.matmul(out=psO[pb][:], lhsT=zT[b][:, et, :],
                                                   rhs=wo[:, et, dh * 512:(dh + 1) * 512],
                                                   start=(et == 0), stop=(et == 15)),
                          reads=[zT_r[b], wo_r], writes=[psO_r[pb]], signal=(et == 15))
                kb.op("dve", lambda e: e.tensor_tensor(out=xt[xb][:, dh * 512:(dh + 1) * 512], in0=psO[pb][:],
                                                       in1=xt[xb][:, dh * 512:(dh + 1) * 512], op=ALU.add),
                      reads=[psO_r[pb], xt_r[xb]], writes=[xt_r[xb]])
            kb.dma("sp", dst_ap[t * 128:(t + 1) * 128, :], xt[xb][:], [xt_r[xb]], [dst_r], xt_r[xb], store=True)
        kb.barrier()


def host_consts():
    c = {}
    c["ident"] = np.eye(128, dtype=np.float32).astype(ml_dtypes.bfloat16)
    c["identf"] = np.eye(128, dtype=np.float32)
    half = 128
    inv = (np.float32(10000.0) ** (-np.arange(half, dtype=np.float32) / np.float32(half))).astype(np.float32)
    ang = (np.arange(S, dtype=np.float32)[:, None] * inv[None, :]).astype(np.float32)
    c["rope_cos"] = np.ascontiguousarray(np.cos(ang).astype(np.float32).T)
    c["rope_sin"] = np.ascontiguousarray(np.sin(ang).astype(np.float32).T)
    s_idx = (np.arange(32)[None, :, None] * 128 + np.arange(128)[:, None, None]).astype(np.int64)
    k_idx = ((128 * (np.arange(17)[:, None] - 1) + 1 + np.arange(128)[None, :]) % S).astype(np.int64)
    m = (s_idx[None] * k_idx[:, None, None, :]) % S
    th = m.astype(np.float64) * (2.0 * np.pi / S)
    c["dftc"] = np.cos(th).astype(np.float32).astype(ml_dtypes.bfloat16)
    c["dfts"] = np.sin(th).astype(np.float32).astype(ml_dtypes.bfloat16)
    c["rev"] = np.ascontiguousarray(np.eye(128, dtype=np.float32)[:, ::-1]).astype(ml_dtypes.bfloat16)
    cidx = (np.arange(2)[None, :, None] * 128 + np.arange(128)[:, None, None]).astype(np.int64)
    m2 = (cidx * np.arange(256, dtype=np.int64)[None, None, :]) % 256
    th2 = m2.astype(np.float64) * (2.0 * np.pi / 256)
    c["cc"] = np.cos(th2).astype(np.float32).astype(ml_dtypes.bfloat16)
    c["nsc"] = (-np.sin(th2)).astype(np.float32).astype(ml_dtypes.bfloat16)
    c["sc"] = np.sin(th2).astype(np.float32).astype(ml_dtypes.bfloat16)
    return c


ALL_PHASES = ("ret", "ffn0", "fnet", "moe")
SPARSE_MOE = True


def build(phases=ALL_PHASES):
    nc = bass.Bass("TRN2", target_bir_lowering=False)

    def din(name, shape, dtype=F32):
        return nc.dram_tensor(name, list(shape), dtype, kind="ExternalInput").ap()

    def dscr(name, shape, dtype):
        return nc.dram_tensor(name, list(shape), dtype, kind="Internal").ap(), Res(name, accum=True)

    x = din("x", [S, D])
    mix_norm = din("mix_norm", [2, D])
    ffn_norm = din("ffn_norm", [2, D])
    ident = din("ident", [128, 128], BF16)
    identf = din("identf", [128, 128], F32)
    if "ret" in phases:
        w_in = din("ret_w_in", [D, 6144])
        decay = din("ret_decay_logit", [2, 4])
        gn_gain = din("ret_gn_gain", [2048])
        w_out = din("ret_w_out", [2048, D])
        rope_cos = din("rope_cos", [128, S])
        rope_sin = din("rope_sin", [128, S])
    if "ffn0" in phases:
        dwg = din("dense_w_gate", [D, 2816])
        dwu = din("dense_w_up", [D, 2816])
        dwd = din("dense_w_down", [2816, D])
    if "fnet" in phases:
        w_fnet = din("fnet_w_out", [D, D])
        dftc = din("dftc", [17, 128, 32, 128], BF16)
        dfts = din("dfts", [17, 128, 32, 128], BF16)
        cc = din("cc", [128, 2, 256], BF16)
        nsc = din("nsc", [128, 2, 256], BF16)
        scp = din("sc", [128, 2, 256], BF16)
        rev = din("rev", [128, 128], BF16)
    if "moe" in phases:
        router = din("moe_router", [D, 8])
        mwg = din("moe_w_gate", [8, D, 3584])
        mwu = din("moe_w_up", [8, D, 3584])
        mwd = din("moe_w_down", [8, 3584, D])
        final_norm = din("final_norm", [D])
    y = nc.dram_tensor("y", [S, D], F32, kind="ExternalOutput").ap()
    y_r = Res("y", accum=True)
    with ExitStack() as es:
        kb = KB(nc, es)
        cur = (x, Res("x", accum=True))
        order = [p for p in ALL_PHASES if p in phases]
        for p in order:
            last = p == order[-1]
            nxt = (y, y_r) if last else dscr("h_" + p, [S, D], F32)
            if p == "ret":
                qk_s = dscr("qk_s", [8, 128, 4, 16, 128], BF16)
                v_s = dscr("v_s", [S, 2048], BF16)
                sg_s = dscr("sg_s", [S, 2048], BF16)
                sb_s = dscr("sb_s", [32, 128, 8, 512], BF16)
                z_s = dscr("z_s", [S, 2048], BF16)
                ret_proj_phase(kb, cur, mix_norm[0], w_in, gn_gain, ident, rope_cos, rope_sin, qk_s, v_s, sg_s)
                ret_core_phase(kb, decay, ident, qk_s, v_s, sg_s, sb_s, z_s)
                ret_out_phase(kb, cur, nxt, z_s, w_out, ident)
            elif p == "ffn0":
                ffn_phase(kb, cur, nxt, ffn_norm[0], [(dwg, dwu, dwd)], ident, tag="f0", chunk_sizes=[6, 6, 5, 5], nbuf=2)
            elif p == "fnet":
                fourier_phase(kb, cur, nxt, mix_norm[1], w_fnet, ident, dftc, dfts, cc, nsc, scp, rev)
            elif p == "moe":
                if SPARSE_MOE:
                    xg_s = dscr("xg_s", [NT_SLOT * BS, D], BF16)
                    o_s = dscr("o_s", [NT_SLOT * BS, D], F32)
                    moe_sparse_phase(kb, cur, nxt, ffn_norm[1], router, mwg, mwu, mwd, final_norm, ident, identf,
                                     xg_s, o_s)
                else:
                    ffn_phase(kb, cur, nxt, ffn_norm[1], [(mwg[e], mwu[e], mwd[e]) for e in range(8)], ident,
                              router=router, final_g=final_norm, FC=7, tag="f1")
            cur = nxt
        kb.final_wait("sp")
    return nc


_CONSTS = None
PHASE_INPUTS = {
    "ret": ["ret_w_in", "ret_decay_logit", "ret_gn_gain", "ret_w_out"],
    "ffn0": ["dense_w_gate", "dense_w_up", "dense_w_down"],
    "fnet": ["fnet_w_out"],
    "moe": ["moe_router", "moe_w_gate", "moe_w_up", "moe_w_down", "final_norm"],
}
PHASE_CONSTS = {"ret": ["rope_cos", "rope_sin"], "ffn0": [], "fnet": ["dftc", "dfts", "cc", "nsc", "sc", "rev"], "moe": []}


def make_in_maps(inputs, phases=ALL_PHASES, cores=range(NCORES), x_override=None):
    global _CONSTS
    if _CONSTS is None:
        _CONSTS = host_consts()
    shared = {"mix_norm": np.ascontiguousarray(inputs["mix_norm"], dtype=np.float32),
              "ffn_norm": np.ascontiguousarray(inputs["ffn_norm"], dtype=np.float32),
              "ident": _CONSTS["ident"], "identf": _CONSTS["identf"]}
    for p in phases:
        for k in PHASE_INPUTS[p]:
            a = np.asarray(inputs[k], dtype=np.float32)
            if k != "final_norm":
                a = a[0]
            shared[k] = np.ascontiguousarray(a)
        for k in PHASE_CONSTS[p]:
            shared[k] = _CONSTS[k]
    maps = []
    for c in cores:
        m = dict(shared)
        m["x"] = np.ascontiguousarray(inputs["x"][c] if x_override is None else x_override[c], dtype=np.float32)
        maps.append(m)
    return maps


def kernel(**inputs):
    nc = build(ALL_PHASES)
    maps = make_in_maps(inputs)
    res = run_bass_kernel_spmd(nc, maps, core_ids=list(range(NCORES)))
    return np.stack([np.asarray(r["y"], dtype=np.float32) for r in res.results], axis=0)


NT_SLOT = 24
BS = 512


def bc_last(ap, n):
    return bass.AP(ap.tensor, ap.offset, [list(a) for a in ap.ap] + [[0, n]])


def bc_mid(ap2, n):
    a = [list(v) for v in ap2.ap]
    return bass.AP(ap2.tensor, ap2.offset, [a[0], [0, n]] + a[1:])


def moe_sparse_phase(kb, src, dst, norm_g, router, mwg, mwu, mwd, final_g, ident_d, identf_d, xg_s, o_s, tag="ms"):
    nc = kb.nc
    I32 = mybir.dt.int32
    src_ap, src_r = src
    dst_ap, dst_r = dst
    xg_ap, xg_r = xg_s
    o_ap, o_r = o_s
    NTT = S // 128
    FC = 7
    NCH = 4
    with ExitStack() as es:
        ident = es.enter_context(nc.sbuf_tensor(tag + "_ident", [128, 128], BF16))
        ident_r = Res("ident")
        kb.dma("sp", ident[:], ident_d, [], [ident_r], ident_r)
        p1i = es.enter_context(nc.sbuf_tensor(tag + "_p1i", [128, NTT], I32))
        p2i = es.enter_context(nc.sbuf_tensor(tag + "_p2i", [128, NTT], I32))
        g1 = es.enter_context(nc.sbuf_tensor(tag + "_g1", [128, NTT], F32))
        g2 = es.enter_context(nc.sbuf_tensor(tag + "_g2", [128, NTT], F32))
        p1i_r, p2i_r, g1_r, g2_r = Res("p1i"), Res("p2i"), Res("g1"), Res("g2")
        idxg = es.enter_context(nc.sbuf_tensor(tag + "_idxg", [128, NT_SLOT, 32], I32))
        idxd = es.enter_context(nc.sbuf_tensor(tag + "_idxd", [128, NT_SLOT, 28], I32))
        idxg_r, idxd_r = Res("idxg"), Res("idxd")

        with ExitStack() as es0:
            def T(name, shape, dtype=F32):
                return es0.enter_context(nc.sbuf_tensor(tag + "_" + name, shape, dtype)), Res(name)

            fr = Front(kb, es0, ident, ident_r, norm_g, tag + "f", with_T=False)
            identf, identf_r = T("identf", [128, 128])
            kb.dma("sp", identf[:], identf_d, [], [identf_r], identf_r)
            wr, wr_r = T("wr", [128, KT, 8])
            kb.dma("sp", wr[:], router.rearrange("(kt p) e -> p kt e", p=128), [], [wr_r], wr_r)
            hn16, hn16_r = T("hn16", [128, NTT, D], BF16)
            x4 = [es0.enter_context(nc.sbuf_tensor(tag + "_x4%d" % i, [128, 4, D], F32)) for i in range(2)]
            x4_r = [Res("x4%d" % i) for i in range(2)]
            hn32 = [es0.enter_context(nc.sbuf_tensor(tag + "_hn32%d" % i, [128, D], F32)) for i in range(2)]
            hn32_r = [Res("hn32%d" % i) for i in range(2)]
            hT32 = [es0.enter_context(nc.sbuf_tensor(tag + "_hT32%d" % i, [128, KT, 128], F32)) for i in range(2)]
            hT32_r = [Res("hT32%d" % i) for i in range(2)]
            L, L_r = T("L", [128, NTT, 8])
            psT = [es0.enter_context(nc.psum_tensor(tag + "_psT%d" % i, [128, D], F32)) for i in range(2)]
            psT_r = [Res("psT%d" % i) for i in range(2)]
            psL = [es0.enter_context(nc.psum_tensor(tag + "_psL%d" % i, [128, 512], F32)) for i in range(2)]
            psL_r = [Res("psL%d" % i) for i in range(2)]
            zero, zero_r = T("zero", [128, 4, D], BF16)
            kb.op("pool", lambda e: e.memset(zero[:], 0.0), writes=[zero_r])
            n = 0
            for G in range(S // 512):
                b = G % 2
                kb.dma("sp", x4[b][:], src_ap[G * 512:(G + 1) * 512, :].rearrange("(j p) d -> p j d", p=128),
                       [src_r], [x4_r[b]], x4_r[b])
                for t in range(G * 3, G * 3 + 3):
                    kb.dma("sp", xg_ap[t * BS:(t + 1) * BS, :].rearrange("(j p) d -> p j d", p=128), zero[:],
                           [zero_r], [xg_r], zero_r, store=True)
                fr.rstd(x4[b][:], x4_r[b], 4)
                for j in range(4):
                    tt = G * 4 + j
                    hb = n % 2
                    n += 1
                    fr.norm_to(hn32[hb][:], hn32_r[hb], x4[b][:, j, :], x4_r[b], j)
                    kb.op("act", lambda e: e.activation(out=hn16[:, tt, :], in_=hn32[hb][:], func=AF.Copy),
                          reads=[hn32_r[hb]], writes=[hn16_r])
                    for kt in range(KT):
                        kb.op("pe", lambda e: e.transpose(out=psT[hb][:, kt * 128:(kt + 1) * 128],
                                                          in_=hn32[hb][:, kt * 128:(kt + 1) * 128], identity=identf[:]),
                              reads=[hn32_r[hb], identf_r], writes=[psT_r[hb]], signal=(kt == KT - 1))
                    kb.op("act", lambda e: e.activation(out=hT32[hb][:], in_=psT[hb][:].rearrange("p (k t) -> p k t", k=KT),
                                                        func=AF.Copy), reads=[psT_r[hb]], writes=[hT32_r[hb]])
                    for kt in range(KT):
                        kb.op("pe", lambda e: e.matmul(out=psL[hb][:, 0:8], lhsT=hT32[hb][:, kt, :], rhs=wr[:, kt, :],
                                                       start=(kt == 0), stop=(kt == KT - 1)),
                              reads=[hT32_r[hb], wr_r], writes=[psL_r[hb]], signal=(kt == KT - 1))
                    kb.op("dve", lambda e: e.tensor_copy(out=L[:, tt, :], in_=psL[hb][:, 0:8]),
                          reads=[psL_r[hb]], writes=[L_r])
            m1, m1_r = T("m1", [128, NTT])
            m2, m2_r = T("m2", [128, NTT])
            eq, eq_r = T("eq", [128, NTT, 8])
            L2, L2_r = T("L2", [128, NTT, 8])
            mask, mask_r = T("mask", [128, NTT, 8])
            ex, ex_r = T("ex", [128, NTT, 8])
            den, den_r = T("den", [128, NTT])
            gts, gts_r = T("gts", [128, NTT, 8])
            kb.op("dve", lambda e: e.tensor_reduce(out=m1[:], in_=L[:], axis=AX.X, op=ALU.max), reads=[L_r], writes=[m1_r])
            kb.op("dve", lambda e: e.tensor_tensor(out=eq[:], in0=L[:], in1=bc_last(m1[:], 8), op=ALU.is_equal),
                  reads=[L_r, m1_r], writes=[eq_r])
            kb.op("dve", lambda e: e.scalar_tensor_tensor(out=L2[:], in0=eq[:], scalar=-1.0e30, in1=L[:],
                                                          op0=ALU.mult, op1=ALU.add), reads=[eq_r, L_r], writes=[L2_r])
            kb.op("dve", lambda e: e.tensor_reduce(out=m2[:], in_=L2[:], axis=AX.X, op=ALU.max), reads=[L2_r], writes=[m2_r])
            kb.op("dve", lambda e: e.tensor_tensor(out=mask[:], in0=L[:], in1=bc_last(m2[:], 8), op=ALU.is_ge),
                  reads=[L_r, m2_r], writes=[mask_r])
            kb.op("dve", lambda e: e.tensor_tensor(out=ex[:], in0=L[:], in1=bc_last(m1[:], 8), op=ALU.subtract),
                  reads=[L_r, m1_r], writes=[ex_r])
            kb.op("act", lambda e: e.activation(out=ex[:], in_=ex[:], func=AF.Exp), reads=[ex_r], writes=[ex_r])
            kb.op("dve", lambda e: e.tensor_tensor(out=ex[:], in0=ex[:], in1=mask[:], op=ALU.mult),
                  reads=[ex_r, mask_r], writes=[ex_r])
            kb.op("dve", lambda e: e.tensor_reduce(out=den[:], in_=ex[:], axis=AX.X, op=ALU.add), reads=[ex_r], writes=[den_r])
            kb.op("dve", lambda e: e.reciprocal(out=den[:], in_=den[:]), reads=[den_r], writes=[den_r])
            kb.op("dve", lambda e: e.tensor_tensor(out=gts[:], in0=ex[:], in1=bc_last(den[:], 8), op=ALU.mult),
                  reads=[ex_r, den_r], writes=[gts_r])
            maskb, maskb_r = T("maskb", [128, NTT * 8], BF16)
            kb.op("dve", lambda e: e.tensor_copy(out=maskb[:], in_=mask[:].rearrange("p t e -> p (t e)")),
                  reads=[mask_r], writes=[maskb_r])
            ii, ii_r = T("ii", [128, 128], I32)
            dmf, dmf_r = T("dmf", [128, 128])
            Lt, Lt_r = T("Lt", [128, 128], BF16)
            ones, ones_r = T("ones", [128, 128], BF16)
            kb.op("pool", lambda e: e.iota(ii[:], pattern=[[1, 128]], base=0, channel_multiplier=-1), writes=[ii_r])
            kb.op("dve", lambda e: e.tensor_copy(out=dmf[:], in_=ii[:]), reads=[ii_r], writes=[dmf_r])
            kb.op("dve", lambda e: e.tensor_scalar(out=Lt[:], in0=dmf[:], scalar1=0.0, scalar2=None, op0=ALU.is_gt),
                  reads=[dmf_r], writes=[Lt_r])
            kb.op("pool", lambda e: e.memset(ones[:], 1.0), writes=[ones_r])
            kb.op("pe", lambda e: e.matmul(out=psL[0][:, 0:256], lhsT=Lt[:], rhs=maskb[:], start=True, stop=True),
                  reads=[Lt_r, maskb_r], writes=[psL_r[0]])
            kb.op("pe", lambda e: e.matmul(out=psL[1][:, 0:256], lhsT=ones[:], rhs=maskb[:], start=True, stop=True),
                  reads=[ones_r, maskb_r], writes=[psL_r[1]])
            within, within_r = T("within", [128, NTT, 8])
            tot, tot_r = T("tot", [128, NTT, 8])
            off, off_r = T("off", [128, NTT, 8])
            kb.op("dve", lambda e: e.tensor_copy(out=within[:].rearrange("p t e -> p (t e)"), in_=psL[0][:, 0:256]),
                  reads=[psL_r[0]], writes=[within_r])
            kb.op("dve", lambda e: e.tensor_copy(out=tot[:].rearrange("p t e -> p (t e)"), in_=psL[1][:, 0:256]),
                  reads=[psL_r[1]], writes=[tot_r])
            kb.op("dve", lambda e: e.memset(off[:, 0, :], 0.0), writes=[off_r])
            for t in range(1, NTT):
                kb.op("dve", lambda e: e.tensor_tensor(out=off[:, t, :], in0=off[:, t - 1, :], in1=tot[:, t - 1, :], op=ALU.add),
                      reads=[off_r, tot_r], writes=[off_r])
            cntp, cntp_r = T("cntp", [128, 8])
            pc, pc_r = T("pc", [128, 8])
            poff, poff_r = T("poff", [128, 8])
            pend, pend_r = T("pend", [128, 8])
            kb.op("dve", lambda e: e.tensor_tensor(out=cntp[:], in0=off[:, NTT - 1, :], in1=tot[:, NTT - 1, :], op=ALU.add),
                  reads=[off_r, tot_r], writes=[cntp_r])
            kb.op("dve", lambda e: e.memset(pc[:], 0.0), writes=[pc_r])
            for k_ in range(8):
                kb.op("dve", lambda e: e.scalar_tensor_tensor(out=pc[:], in0=cntp[:], scalar=float(BS * k_), in1=pc[:],
                                                              op0=ALU.is_gt, op1=ALU.add),
                      reads=[cntp_r, pc_r], writes=[pc_r])
            kb.op("dve", lambda e: e.tensor_scalar(out=pc[:], in0=pc[:], scalar1=float(BS), scalar2=None, op0=ALU.mult),
                  reads=[pc_r], writes=[pc_r])
            kb.op("dve", lambda e: e.memset(poff[:, 0:1], 0.0), writes=[poff_r])
            for e_ in range(1, 8):
                kb.op("dve", lambda e: e.tensor_tensor(out=poff[:, e_:e_ + 1], in0=poff[:, e_ - 1:e_], in1=pc[:, e_ - 1:e_],
                                                       op=ALU.add), reads=[poff_r, pc_r], writes=[poff_r])
            kb.op("dve", lambda e: e.tensor_tensor(out=pend[:], in0=poff[:], in1=pc[:], op=ALU.add),
                  reads=[poff_r, pc_r], writes=[pend_r])
            ms, ms_r = T("ms", [128, NTT, 8])
            kb.op("dve", lambda e: e.tensor_tensor(out=ms[:], in0=within[:], in1=off[:], op=ALU.add),
                  reads=[within_r, off_r], writes=[ms_r])
            kb.op("dve", lambda e: e.scalar_tensor_tensor(out=ms[:], in0=ms[:], scalar=1.0, in1=bc_mid(poff[:], NTT),
                                                          op0=ALU.add, op1=ALU.add), reads=[ms_r, poff_r], writes=[ms_r])
            kb.op("dve", lambda e: e.tensor_tensor(out=ms[:], in0=ms[:], in1=mask[:], op=ALU.mult),
                  reads=[ms_r, mask_r], writes=[ms_r])
            pa, pa_r = T("pa", [128, NTT])
            pb_, pb_r = T("pb", [128, NTT])
            kb.op("dve", lambda e: e.tensor_reduce(out=pa[:], in_=ms[:], axis=AX.X, op=ALU.max), reads=[ms_r], writes=[pa_r])
            kb.op("dve", lambda e: e.tensor_reduce(out=pb_[:], in_=ms[:], axis=AX.X, op=ALU.add), reads=[ms_r], writes=[pb_r])
            kb.op("dve", lambda e: e.tensor_tensor(out=pb_[:], in0=pb_[:], in1=pa[:], op=ALU.subtract),
                  reads=[pb_r, pa_r], writes=[pb_r])
            kb.op("dve", lambda e: e.tensor_tensor(out=eq[:], in0=ms[:], in1=bc_last(pa[:], 8), op=ALU.is_equal),
                  reads=[ms_r, pa_r], writes=[eq_r])
            kb.op("dve", lambda e: e.tensor_tensor(out=eq[:], in0=eq[:], in1=gts[:], op=ALU.mult),
                  reads=[eq_r, gts_r], writes=[eq_r])
            kb.op("dve", lambda e: e.tensor_reduce(out=g1[:], in_=eq[:], axis=AX.X, op=ALU.add), reads=[eq_r], writes=[g1_r])
            kb.op("dve", lambda e: e.tensor_scalar(out=g2[:], in0=g1[:], scalar1=-1.0, scalar2=1.0, op0=ALU.mult, op1=ALU.add),
                  reads=[g1_r], writes=[g2_r])
            kb.op("dve", lambda e: e.tensor_scalar(out=pa[:], in0=pa[:], scalar1=-1.0, scalar2=None, op0=ALU.add),
                  reads=[pa_r], writes=[pa_r])
            kb.op("dve", lambda e: e.tensor_scalar(out=pb_[:], in0=pb_[:], scalar1=-1.0, scalar2=None, op0=ALU.add),
                  reads=[pb_r], writes=[pb_r])
            kb.op("dve", lambda e: e.tensor_copy(out=p1i[:], in_=pa[:]), reads=[pa_r], writes=[p1i_r])
            kb.op("dve", lambda e: e.tensor_copy(out=p2i[:], in_=pb_[:]), reads=[pb_r], writes=[p2i_r])
            cmp, cmp_r = T("cmp", [128, NT_SLOT, 8])
            et, et_r = T("et", [128, NT_SLOT])
            for t in range(NT_SLOT):
                kb.op("dve", lambda e: e.tensor_scalar(out=cmp[:, t, :], in0=pend[:], scalar1=float(BS * t), scalar2=None,
                                                       op0=ALU.is_le), reads=[pend_r], writes=[cmp_r])
            kb.op("dve", lambda e: e.tensor_reduce(out=et[:], in_=cmp[:], axis=AX.X, op=ALU.add), reads=[cmp_r], writes=[et_r])
            kb.op("dve", lambda e: e.tensor_scalar(out=et[:], in0=et[:], scalar1=7.0, scalar2=None, op0=ALU.min),
                  reads=[et_r], writes=[et_r])
            sgi, sgi_r = T("sgi", [128, 32], I32)
            sdi, sdi_r = T("sdi", [128, 28], I32)
            sgf, sgf_r = T("sgf", [128, 32])
            sdf, sdf_r = T("sdf", [128, 28])
            kb.op("pool", lambda e: e.iota(sgi[:], pattern=[[512, 8], [1, 4]], base=0, channel_multiplier=4), writes=[sgi_r])
            kb.op("pool", lambda e: e.iota(sdi[:], pattern=[[896, 4], [128, 7]], base=0, channel_multiplier=1), writes=[sdi_r])
            kb.op("dve", lambda e: e.tensor_copy(out=sgf[:], in_=sgi[:]), reads=[sgi_r], writes=[sgf_r])
            kb.op("dve", lambda e: e.tensor_copy(out=sdf[:], in_=sdi[:]), reads=[sdi_r], writes=[sdf_r])
            igf, igf_r = T("igf", [128, NT_SLOT, 32])
            idf, idf_r = T("idf", [128, NT_SLOT, 28])
            etg, etg_r = T("etg", [128, NT_SLOT])
            etd, etd_r = T("etd", [128, NT_SLOT])
            kb.op("dve", lambda e: e.tensor_scalar(out=etg[:], in0=et[:], scalar1=4096.0, scalar2=None, op0=ALU.mult),
                  reads=[et_r], writes=[etg_r])
            kb.op("dve", lambda e: e.tensor_scalar(out=etd[:], in0=et[:], scalar1=3584.0, scalar2=None, op0=ALU.mult),
                  reads=[et_r], writes=[etd_r])
            for t in range(NT_SLOT):
                kb.op("dve", lambda e: e.tensor_scalar(out=igf[:, t, :], in0=sgf[:], scalar1=etg[:, t:t + 1], scalar2=None,
                                                       op0=ALU.add), reads=[etg_r, sgf_r], writes=[igf_r])
                kb.op("dve", lambda e: e.tensor_scalar(out=idf[:, t, :], in0=sdf[:], scalar1=etd[:, t:t + 1], scalar2=None,
                                                       op0=ALU.add), reads=[etd_r, sdf_r], writes=[idf_r])
            kb.op("dve", lambda e: e.tensor_copy(out=idxg[:], in_=igf[:]), reads=[igf_r], writes=[idxg_r])
            kb.op("dve", lambda e: e.tensor_copy(out=idxd[:], in_=idf[:]), reads=[idf_r], writes=[idxd_r])
            kb._wait("pool", kb._deps([xg_r], []))
            for tt in range(NTT):
                for (pi, pi_r) in ((p1i, p1i_r), (p2i, p2i_r)):
                    kb._wait("pool", kb._deps([hn16_r, pi_r], []))
                    if hn16_r.ssem is None:
                        hn16_r.ssem = kb.get_sem(fresh=True)
                    sem = hn16_r.ssem
                    ins = nc.gpsimd.indirect_dma_start(out=xg_ap, out_offset=bass.IndirectOffsetOnAxis(ap=pi[:, tt:tt + 1], axis=0),
                                                       in_=hn16[:, tt, :], in_offset=None)
                    ins.then_inc(sem, 16)
                    kb.cnt[sem] += 16
                    kb._register(sem, kb.cnt[sem], [hn16_r, pi_r], [xg_r])
            kb.barrier()
        with ExitStack() as es1:
            xgt = [es1.enter_context(nc.sbuf_tensor(tag + "_xgt%d" % i, [128, 4, D], BF16)) for i in range(2)]
            xgt_r = [Res("xgt%d" % i) for i in range(2)]
            xT = [es1.enter_context(nc.sbuf_tensor(tag + "_xT%d" % i, [128, KT, BS], BF16)) for i in range(2)]
            xT_r = [Res("xT%d" % i) for i in range(2)]
            wg = [es1.enter_context(nc.sbuf_tensor(tag + "_wg%d" % i, [128, KT, FC * 128], BF16)) for i in range(2)]
            wu = [es1.enter_context(nc.sbuf_tensor(tag + "_wu%d" % i, [128, KT, FC * 128], BF16)) for i in range(2)]
            wd = [es1.enter_context(nc.sbuf_tensor(tag + "_wd%d" % i, [128, FC, D], BF16)) for i in range(2)]
            wg_r = [Res("wg%d" % i) for i in range(2)]
            wu_r = [Res("wu%d" % i) for i in range(2)]
            wd_r = [Res("wd%d" % i) for i in range(2)]
            actT = [es1.enter_context(nc.sbuf_tensor(tag + "_actT%d" % i, [128, FC, BS], BF16)) for i in range(2)]
            actT_r = [Res("actT%d" % i) for i in range(2)]
            sg = [es1.enter_context(nc.sbuf_tensor(tag + "_sg%d" % i, [128, 512], F32)) for i in range(2)]
            sg_r = [Res("sg%d" % i) for i in range(2)]
            oacc = [es1.enter_context(nc.sbuf_tensor(tag + "_oacc%d" % i, [128, 4, D], F32)) for i in range(2)]
            oacc_r = [Res("oacc%d" % i) for i in range(2)]
            psT = [es1.enter_context(nc.psum_tensor(tag + "_psX%d" % i, [128, D], BF16)) for i in range(2)]
            psT_r = [Res("psX%d" % i) for i in range(2)]
            psG = [es1.enter_context(nc.psum_tensor(tag + "_psG%d" % i, [128, 512], F32)) for i in range(2)]
            psU = [es1.enter_context(nc.psum_tensor(tag + "_psU%d" % i, [128, 512], F32)) for i in range(2)]
            psO = [es1.enter_context(nc.psum_tensor(tag + "_psO%d" % i, [128, 512], F32)) for i in range(2)]
            psG_r = [Res("psG%d" % i) for i in range(2)]
            psU_r = [Res("psU%d" % i) for i in range(2)]
            psO_r = [Res("psO%d" % i) for i in range(2)]
            wg_flat = mwg.rearrange("e d (c f) -> (e d c) f", f=FC * 128)
            wu_flat = mwu.rearrange("e d (c f) -> (e d c) f", f=FC * 128)
            wd_flat = mwd.rearrange("e f d -> (e f) d")

            def gather(dst_ap_, dst_r_, src_flat, idx_ap, idx_r):
                kb._wait("pool", kb._deps([idx_r], []))
                if dst_r_.lsem is None:
                    dst_r_.lsem = kb.get_sem(fresh=True)
                sem = dst_r_.lsem
                ins = nc.gpsimd.indirect_dma_start(out=dst_ap_, out_offset=None, in_=src_flat,
                                                   in_offset=bass.IndirectOffsetOnAxis(ap=idx_ap, axis=0))
                ins.then_inc(sem, 16)
                kb.cnt[sem] += 16
                dst_r_.writes[sem] = kb.cnt[sem]
                idx_r.reads[sem] = kb.cnt[sem]

            wseq = [(t, c) for t in range(NT_SLOT) for c in range(NCH)]

            def load_w(i):
                t, c = wseq[i]
                b = i % 2
                for r_ in (wg_r[b], wu_r[b], wd_r[b]):
                    kb._wait("pool", kb._deps([], [r_]))
                    r_.writes = {}
                    r_.reads = {}
                for kt in range(KT):
                    gather(wg[b][:, kt, :], wg_r[b], wg_flat, idxg[:, t, kt * 4 + c:kt * 4 + c + 1], idxg_r)
                    gather(wu[b][:, kt, :], wu_r[b], wu_flat, idxg[:, t, kt * 4 + c:kt * 4 + c + 1], idxg_r)
                for fl in range(FC):
                    gather(wd[b][:, fl, :], wd_r[b], wd_flat, idxd[:, t, c * 7 + fl:c * 7 + fl + 1], idxd_r)

            def load_x(t):
                b = t % 2
                kb.dma("sp", xgt[b][:], xg_ap[t * BS:(t + 1) * BS, :].rearrange("(j p) d -> p j d", p=128),
                       [xg_r], [xgt_r[b]], xgt_r[b])

            load_x(0)
            load_w(0)
            wi = 0
            gcount = 0
            ocount = 0
            tcount = 0
            for t in range(NT_SLOT):
                xb = t % 2
                if t + 1 < NT_SLOT:
                    load_x(t + 1)
                for j in range(4):
                    tb = tcount % 2
                    tcount += 1
                    for kt in range(KT):
                        kb.op("pe", lambda e: e.transpose(out=psT[tb][:, kt * 128:(kt + 1) * 128],
                                                          in_=xgt[xb][:, j, kt * 128:(kt + 1) * 128], identity=ident[:]),
                              reads=[xgt_r[xb], ident_r], writes=[psT_r[tb]], signal=(kt == KT - 1))
                    kb.op("act", lambda e: e.activation(out=xT[xb][:, :, j * 128:(j + 1) * 128],
                                                        in_=psT[tb][:].rearrange("p (k t) -> p k t", k=KT), func=AF.Copy),
                          reads=[psT_r[tb]], writes=[xT_r[xb]])
                ob = t % 2
                for c in range(NCH):
                    b = wi % 2
                    if wi + 1 < len(wseq):
                        load_w(wi + 1)
                    ab = wi % 2
                    for fl in range(FC):
                        gb = gcount % 2
                        gcount += 1
                        for (wt, wt_r, pst, pst_r) in ((wg[b], wg_r[b], psG[gb], psG_r[gb]), (wu[b], wu_r[b], psU[gb], psU_r[gb])):
                            for kt in range(KT):
                                kb.op("pe", lambda e: e.matmul(out=pst[:], lhsT=wt[:, kt, fl * 128:(fl + 1) * 128],
                                                               rhs=xT[xb][:, kt, :], start=(kt == 0), stop=(kt == KT - 1)),
                                      reads=[wt_r, xT_r[xb]], writes=[pst_r], signal=(kt == KT - 1))
                        kb.op("act", lambda e: e.activation(out=sg[gb][:], in_=psG[gb][:], func=AF.Silu),
                              reads=[psG_r[gb]], writes=[sg_r[gb]])
                        kb.op("dve", lambda e: e.tensor_tensor(out=actT[ab][:, fl, :], in0=sg[gb][:], in1=psU[gb][:], op=ALU.mult),
                              reads=[sg_r[gb], psU_r[gb]], writes=[actT_r[ab]])
                    for j in range(4):
                        for dh in range(2):
                            pb2 = ocount % 2
                            ocount += 1
                            for fl in range(FC):
                                kb.op("pe", lambda e: e.matmul(out=psO[pb2][:], lhsT=actT[ab][:, fl, j * 128:(j + 1) * 128],
                                                               rhs=wd[b][:, fl, dh * 512:(dh + 1) * 512],
                                                               start=(fl == 0), stop=(fl == FC - 1)),
                                      reads=[actT_r[ab], wd_r[b]], writes=[psO_r[pb2]], signal=(fl == FC - 1))
                            if c == 0:
                                kb.op("act", lambda e: e.activation(out=oacc[ob][:, j, dh * 512:(dh + 1) * 512], in_=psO[pb2][:],
                                                                    func=AF.Copy), reads=[psO_r[pb2]], writes=[oacc_r[ob]])
                            else:
                                kb.op("dve", lambda e: e.tensor_tensor(out=oacc[ob][:, j, dh * 512:(dh + 1) * 512], in0=psO[pb2][:],
                                                                       in1=oacc[ob][:, j, dh * 512:(dh + 1) * 512], op=ALU.add),
                                      reads=[psO_r[pb2], oacc_r[ob]], writes=[oacc_r[ob]])
                    wi += 1
                kb.dma("sp", o_ap[t * BS:(t + 1) * BS, :].rearrange("(j p) d -> p j d", p=128), oacc[ob][:],
                       [oacc_r[ob]], [o_r], oacc_r[ob], store=True)
            kb.barrier()
        with ExitStack() as es2:
            fr2 = Front(kb, es2, ident, ident_r, final_g, tag + "g", with_T=False)
            x4 = [es2.enter_context(nc.sbuf_tensor(tag + "_y4%d" % i, [128, 4, D], F32)) for i in range(3)]
            x4_r = [Res("y4%d" % i) for i in range(3)]
            ga = [es2.enter_context(nc.sbuf_tensor(tag + "_ga%d" % i, [128, D], F32)) for i in range(8)]
            ga_r = [Res("ga%d" % i) for i in range(8)]
            n = 0

            def loadF(G):
                kb.dma("sp", x4[G % 3][:], src_ap[G * 512:(G + 1) * 512, :].rearrange("(j p) d -> p j d", p=128),
                       [src_r], [x4_r[G % 3]], x4_r[G % 3])

            loadF(0)
            loadF(1)
            for G in range(S // 512):
                b = G % 3
                if G + 2 < S // 512:
                    loadF(G + 2)
                for j in range(4):
                    tt = G * 4 + j
                    for (pi, pi_r, gg, gg_r) in ((p1i, p1i_r, g1, g1_r), (p2i, p2i_r, g2, g2_r)):
                        gb = n % 8
                        n += 1
                        kb._wait("pool", kb._deps([o_r, pi_r], [ga_r[gb]]))
                        if ga_r[gb].lsem is None:
                            ga_r[gb].lsem = kb.get_sem(fresh=True)
                        sem = ga_r[gb].lsem
                        ins = nc.gpsimd.indirect_dma_start(out=ga[gb][:], out_offset=None, in_=o_ap,
                                                           in_offset=bass.IndirectOffsetOnAxis(ap=pi[:, tt:tt + 1], axis=0))
                        ins.then_inc(sem, 16)
                        kb.cnt[sem] += 16
                        kb._register(sem, kb.cnt[sem], [o_r, pi_r], [ga_r[gb]])
                        kb.op("dve", lambda e: e.scalar_tensor_tensor(out=x4[b][:, j, :], in0=ga[gb][:], scalar=gg[:, tt:tt + 1],
                                                                      in1=x4[b][:, j, :], op0=ALU.mult, op1=ALU.add),
                              reads=[ga_r[gb], gg_r, x4_r[b]], writes=[x4_r[b]])
                fr2.rstd(x4[b][:], x4_r[b], 4)
                for j in range(4):
                    fr2.norm_to(x4[b][:, j, :], x4_r[b], x4[b][:, j, :], x4_r[b], j)
                kb.dma("sp", dst_ap[G * 512:(G + 1) * 512, :].rearrange("(j p) d -> p j d", p=128), x4[b][:],
                       [x4_r[b]], [dst_r], x4_r[b], store=True)
            kb.barrier()
```

```python
import numpy as np
import ml_dtypes
from contextlib import ExitStack
import concourse.bass as bass
import concourse.mybir as mybir
from concourse.bass_utils import run_bass_kernel_spmd

F32 = mybir.dt.float32
BF16 = mybir.dt.bfloat16
ALU = mybir.AluOpType
AF = mybir.ActivationFunctionType
AX = mybir.AxisListType

D = 1024
S = 4096
NCORES = 8
EPS = 1e-6
KT = D // 128


class Res:
    __slots__ = ("name", "writes", "reads", "accum", "lsem", "ssem")

    def __init__(self, name, accum=False):
        self.name = name
        self.writes = {}
        self.reads = {}
        self.accum = accum
        self.lsem = None
        self.ssem = None


class KB:
    def __init__(self, nc, es):
        self.nc = nc
        self.es = es
        self.engs = {"pe": nc.tensor, "act": nc.scalar, "dve": nc.vector, "pool": nc.gpsimd, "sp": nc.sync}
        self.esem = {}
        self.cnt = {}
        self.waited = {n: {} for n in self.engs}
        self.free_sems = []
        self.phase_sems = []
        self.nsem = 0
        for n in self.engs:
            s = es.enter_context(nc.semaphore("es_" + n))
            self.esem[n] = s
            self.cnt[s] = 0

    def get_sem(self, fresh=False):
        if self.free_sems and not fresh:
            s = self.free_sems.pop()
        else:
            s = self.es.enter_context(self.nc.semaphore("ds%d" % self.nsem))
            self.nsem += 1
            self.cnt[s] = 0
        if not fresh:
            self.phase_sems.append(s)
        return s

    def _wait(self, eng, evs):
        w = self.waited[eng]
        e = self.engs[eng]
        for sem, val in evs.items():
            if w.get(sem, 0) < val:
                e.wait_ge(sem, val)
                w[sem] = val

    @staticmethod
    def _deps(reads, writes):
        evs = {}
        for r in reads:
            for s, v in r.writes.items():
                if evs.get(s, 0) < v:
                    evs[s] = v
        for wr in writes:
            if not wr.accum:
                for s, v in wr.writes.items():
                    if evs.get(s, 0) < v:
                        evs[s] = v
            for s, v in wr.reads.items():
                if evs.get(s, 0) < v:
                    evs[s] = v
        return evs

    @staticmethod
    def _register(sem, v, reads, writes):
        for r in reads:
            r.reads[sem] = v
        for w in writes:
            if w.accum:
                w.writes[sem] = v
            else:
                w.writes = {sem: v}
                w.reads = {}

    def op(self, eng, fn, reads=(), writes=(), signal=True):
        self._wait(eng, self._deps(reads, writes))
        ins = fn(self.engs[eng])
        if signal:
            sem = self.esem[eng]
            self.cnt[sem] += 1
            ins.then_inc(sem, 1)
            self._register(sem, self.cnt[sem], reads, writes)
        return ins

    def dma(self, q, out, in_, reads, writes, own, store=False):
        self._wait(q, self._deps(reads, writes))
        if store:
            if own.ssem is None:
                own.ssem = self.get_sem(fresh=(q == "pool"))
            sem = own.ssem
        else:
            if own.lsem is None:
                own.lsem = self.get_sem(fresh=(q == "pool"))
            sem = own.lsem
        ins = self.engs[q].dma_start(out=out, in_=in_)
        ins.then_inc(sem, 16)
        self.cnt[sem] += 16
        self._register(sem, self.cnt[sem], reads, writes)
        return ins

    def barrier(self, recycle=True):
        allev = {s: c for s, c in self.cnt.items() if c > 0}
        for n in self.engs:
            self._wait(n, allev)
        if recycle:
            self.free_sems.extend(self.phase_sems)
            self.phase_sems = []

    def final_wait(self, eng="sp"):
        allev = {s: c for s, c in self.cnt.items() if c > 0}
        self._wait(eng, allev)


def bcast_row(ap1d, n):
    return ap1d.rearrange("(o n) -> o n", o=1).partition_broadcast(128)


class Front:
    def __init__(self, kb, es, ident, ident_r, gvec_ap, tag, with_T=True):
        nc = kb.nc
        self.kb = kb
        self.ident = ident
        self.ident_r = ident_r
        self.g = es.enter_context(nc.sbuf_tensor(tag + "_g", [128, D], F32))
        self.g_r = Res(tag + "_g")
        kb.dma("sp", self.g[:], bcast_row(gvec_ap, D), [], [self.g_r], self.g_r)
        self.junk = es.enter_context(nc.sbuf_tensor(tag + "_junk", [128, D], BF16))
        self.junk_r = Res(tag + "_junk")
        self.NR = 4
        self.ss_l = [es.enter_context(nc.sbuf_tensor(tag + "_ss%d" % i, [128, 8], F32)) for i in range(self.NR)]
        self.ss_rl = [Res(tag + "_ss%d" % i) for i in range(self.NR)]
        self.rs_l = [es.enter_context(nc.sbuf_tensor(tag + "_rs%d" % i, [128, 8], F32)) for i in range(self.NR)]
        self.rs_rl = [Res(tag + "_rs%d" % i) for i in range(self.NR)]
        self.cur = 0
        self.n = 0
        if not with_T:
            return
        self.hn = [es.enter_context(nc.sbuf_tensor(tag + "_hn%d" % i, [128, D], BF16)) for i in range(2)]
        self.hn_r = [Res(tag + "_hn%d" % i) for i in range(2)]
        self.ps = [es.enter_context(nc.psum_tensor(tag + "_pst%d" % i, [128, D], BF16)) for i in range(2)]
        self.ps_r = [Res(tag + "_pst%d" % i) for i in range(2)]

    @property
    def ss(self):
        return self.ss_l[self.cur]

    @property
    def ss_r(self):
        return self.ss_rl[self.cur]

    @property
    def rs(self):
        return self.rs_l[self.cur]

    @property
    def rs_r(self):
        return self.rs_rl[self.cur]

    def rstd(self, x3, x_r, nj):
        kb = self.kb
        self.cur = (self.cur + 1) % self.NR
        for j in range(nj):
            kb.op("act", lambda e, j=j: e.activation(out=self.junk[:], in_=x3[:, j, :], func=AF.Square,
                                                     accum_out=self.ss[:, j:j + 1]),
                  reads=[x_r], writes=[self.junk_r, self.ss_r])
        kb.op("dve", lambda e: e.tensor_scalar(out=self.rs[:, 0:nj], in0=self.ss[:, 0:nj], scalar1=1.0 / D,
                                                scalar2=EPS, op0=ALU.mult, op1=ALU.add),
              reads=[self.ss_r], writes=[self.rs_r])
        kb.op("act", lambda e: e.activation(out=self.rs[:, 0:nj], in_=self.rs[:, 0:nj], func=AF.Sqrt),
              reads=[self.rs_r], writes=[self.rs_r])
        kb.op("dve", lambda e: e.reciprocal(out=self.rs[:, 0:nj], in_=self.rs[:, 0:nj]),
              reads=[self.rs_r], writes=[self.rs_r])

    def norm_to(self, out_ap, out_r, x2, x_r, j):
        self.kb.op("dve", lambda e: e.scalar_tensor_tensor(out=out_ap, in0=x2, scalar=self.rs[:, j:j + 1],
                                                            in1=self.g[:], op0=ALU.mult, op1=ALU.mult),
                   reads=[x_r, self.rs_r, self.g_r], writes=[out_r])

    def norm_T(self, x3, x_r, nj, xT, xT_r, tok0):
        kb = self.kb
        self.rstd(x3, x_r, nj)
        for j in range(nj):
            b = self.n % 2
            self.n += 1
            hn, hn_r, ps, ps_r = self.hn[b], self.hn_r[b], self.ps[b], self.ps_r[b]
            self.norm_to(hn[:], hn_r, x3[:, j, :], x_r, j)
            for kt in range(KT):
                last = kt == KT - 1
                kb.op("pe", lambda e, kt=kt: e.transpose(out=ps[:, kt * 128:(kt + 1) * 128],
                                                         in_=hn[:, kt * 128:(kt + 1) * 128], identity=self.ident[:]),
                      reads=[hn_r, self.ident_r], writes=[ps_r], signal=last)
            t0 = tok0 + j * 128
            kb.op("act", lambda e, t0=t0: e.activation(out=xT[:, :, t0:t0 + 128],
                                                       in_=ps[:].rearrange("p (k t) -> p k t", k=KT),
                                                       func=AF.Copy),
                  reads=[ps_r], writes=[xT_r])


def ffn_phase(kb, src, dst, norm_g, experts, ident_d, router=None, final_g=None, FC=7, tag="ffn", chunk_sizes=None, nbuf=1):
    nc = kb.nc
    src_ap, src_r = src
    dst_ap, dst_r = dst
    F = experts[0][0].shape[1]
    NFT = F // 128
    if chunk_sizes is None:
        assert NFT % FC == 0
        chunk_sizes = [FC] * (NFT // FC)
    assert sum(chunk_sizes) == NFT
    FC = max(chunk_sizes)
    NCH = len(chunk_sizes)
    choff = [sum(chunk_sizes[:i]) for i in range(NCH)]
    TT = 1024
    NJ = TT // 128
    NE = len(experts)
    with ExitStack() as es:
        ident = es.enter_context(nc.sbuf_tensor(tag + "_ident", [128, 128], BF16))
        ident_r = Res("ident")
        kb.dma("sp", ident[:], ident_d, [], [ident_r], ident_r)
        fr = Front(kb, es, ident, ident_r, norm_g, tag + "f")
        if final_g is not None:
            gf = es.enter_context(nc.sbuf_tensor(tag + "_gf", [128, D], F32))
            gf_r = Res("gf")
            kb.dma("sp", gf[:], bcast_row(final_g, D), [], [gf_r], gf_r)
        assert nbuf == 1 or router is None
        accs = [es.enter_context(nc.sbuf_tensor(tag + "_acc%d" % i, [128, NJ, D], F32)) for i in range(nbuf)]
        accs_r = [Res("acc%d" % i) for i in range(nbuf)]
        xTs = [es.enter_context(nc.sbuf_tensor(tag + "_xT%d" % i, [128, KT, TT], BF16)) for i in range(nbuf)]
        xTs_r = [Res("xT%d" % i) for i in range(nbuf)]
        actT = [es.enter_context(nc.sbuf_tensor(tag + "_actT%d" % i, [128, FC, TT], BF16)) for i in range(2)]
        actT_r = [Res("actT%d" % i) for i in range(2)]
        wg = [es.enter_context(nc.sbuf_tensor(tag + "_wg%d" % i, [128, KT, FC * 128], BF16)) for i in range(2)]
        wu = [es.enter_context(nc.sbuf_tensor(tag + "_wu%d" % i, [128, KT, FC * 128], BF16)) for i in range(2)]
        wd = [es.enter_context(nc.sbuf_tensor(tag + "_wd%d" % i, [128, FC, D], BF16)) for i in range(2)]
        wg_r = [Res("wg%d" % i) for i in range(2)]
        wu_r = [Res("wu%d" % i) for i in range(2)]
        wd_r = [Res("wd%d" % i) for i in range(2)]
        sg = [es.enter_context(nc.sbuf_tensor(tag + "_sg%d" % i, [128, 512], F32)) for i in range(2)]
        sg_r = [Res("sg%d" % i) for i in range(2)]
        psG = [es.enter_context(nc.psum_tensor(tag + "_psG%d" % i, [128, 512], F32)) for i in range(2)]
        psU = [es.enter_context(nc.psum_tensor(tag + "_psU%d" % i, [128, 512], F32)) for i in range(2)]
        psG_r = [Res("psG%d" % i) for i in range(2)]
        psU_r = [Res("psU%d" % i) for i in range(2)]
        psO = [es.enter_context(nc.psum_tensor(tag + "_psO%d" % i, [128, 512], F32)) for i in range(2)]
        psO_r = [Res("psO%d" % i) for i in range(2)]
        if router is not None:
            wr = es.enter_context(nc.sbuf_tensor(tag + "_wr", [128, KT, 8], BF16))
            wr_r = Res("wr")
            kb.dma("pool", wr[:], router.rearrange("(kt p) e -> p kt e", p=128), [], [wr_r], wr_r)
            gates = es.enter_context(nc.sbuf_tensor(tag + "_gates", [128, NJ, 8], F32))
            gates_r = Res("gates")
            lg = es.enter_context(nc.sbuf_tensor(tag + "_lg", [128, 8], F32))
            lg_r = Res("lg")
            mx = es.enter_context(nc.sbuf_tensor(tag + "_mx", [128, 8], F32))
            mx_r = Res("mx")
            msk = es.enter_context(nc.sbuf_tensor(tag + "_msk", [128, 8], F32))
            msk_r = Res("msk")
            ex = es.enter_context(nc.sbuf_tensor(tag + "_ex", [128, 8], F32))
            ex_r = Res("ex")
            sm = es.enter_context(nc.sbuf_tensor(tag + "_sm", [128, 2], F32))
            sm_r = Res("sm")

        wseq = [(T, e, ch) for T in range(S // TT) for e in range(NE) for ch in range(NCH)]
        state = {"loaded": 0}

        def load_w(i):
            T, e, ch = wseq[i]
            b = i % 2
            wg_d, wu_d, wd_d = experts[e]
            f0 = choff[ch] * 128
            cw = chunk_sizes[ch] * 128
            kb.dma("pool", wg[b][:, :, 0:cw], wg_d[:, f0:f0 + cw].rearrange("(kt p) f -> p kt f", p=128),
                   [], [wg_r[b]], wg_r[b])
            kb.dma("pool", wu[b][:, :, 0:cw], wu_d[:, f0:f0 + cw].rearrange("(kt p) f -> p kt f", p=128),
                   [], [wu_r[b]], wu_r[b])
            kb.dma("pool", wd[b][:, 0:chunk_sizes[ch], :], wd_d[f0:f0 + cw, :].rearrange("(ft p) d -> p ft d", p=128),
                   [], [wd_r[b]], wd_r[b])

        load_w(0)
        wi = 0
        gcount = 0
        ocount = 0
        NT = S // TT

        def front(T):
            t0 = T * TT
            acc, acc_r = accs[T % nbuf], accs_r[T % nbuf]
            xT, xT_r = [xTs[T % nbuf]], [xTs_r[T % nbuf]]
            nonlocal ocount
            for hf in range(2):
                kb.dma("sp", acc[:, hf * 4:(hf + 1) * 4, :],
                       src_ap[t0 + hf * 512:t0 + (hf + 1) * 512, :].rearrange("(j p) d -> p j d", p=128),
                       [src_r], [acc_r], acc_r)
            for hf in range(2):
                fr.norm_T(acc[:, hf * 4:(hf + 1) * 4, :], acc_r, 4, xT[0], xT_r[0], hf * 512)
            if router is not None:
                for j in range(NJ):
                    pl = psO[ocount % 2]
                    pl_r = psO_r[ocount % 2]
                    ocount += 1
                    for kt in range(KT):
                        kb.op("pe", lambda e, kt=kt, j=j: e.matmul(out=pl[:, 0:8], lhsT=xT[0][:, kt, j * 128:(j + 1) * 128],
                                                                  rhs=wr[:, kt, :], start=(kt == 0), stop=(kt == KT - 1)),
                              reads=[xT_r[0], wr_r], writes=[pl_r], signal=(kt == KT - 1))
                    kb.op("act", lambda e: e.activation(out=lg[:], in_=pl[:, 0:8], func=AF.Copy),
                          reads=[pl_r], writes=[lg_r])
                    kb.op("dve", lambda e: e.max(out=mx[:], in_=lg[:]), reads=[lg_r], writes=[mx_r])
                    kb.op("dve", lambda e: e.tensor_scalar(out=msk[:], in0=lg[:], scalar1=mx[:, 1:2], scalar2=None,
                                                           op0=ALU.is_ge), reads=[lg_r, mx_r], writes=[msk_r])
                    kb.op("dve", lambda e: e.tensor_scalar(out=ex[:], in0=lg[:], scalar1=mx[:, 0:1], scalar2=None,
                                                           op0=ALU.subtract), reads=[lg_r, mx_r], writes=[ex_r])
                    kb.op("act", lambda e: e.activation(out=ex[:], in_=ex[:], func=AF.Exp), reads=[ex_r], writes=[ex_r])
                    kb.op("dve", lambda e: e.tensor_tensor(out=ex[:], in0=ex[:], in1=msk[:], op=ALU.mult),
                          reads=[ex_r, msk_r], writes=[ex_r])
                    kb.op("dve", lambda e: e.reduce_sum(out=sm[:, 0:1], in_=ex[:], axis=AX.X), reads=[ex_r], writes=[sm_r])
                    kb.op("dve", lambda e: e.reciprocal(out=sm[:, 1:2], in_=sm[:, 0:1]), reads=[sm_r], writes=[sm_r])
                    kb.op("dve", lambda e, j=j: e.tensor_scalar(out=gates[:, j, :], in0=ex[:], scalar1=sm[:, 1:2],
                                                                scalar2=None, op0=ALU.mult),
                          reads=[ex_r, sm_r], writes=[gates_r])
        front(0)
        for T in range(NT):
            t0 = T * TT
            acc, acc_r = accs[T % nbuf], accs_r[T % nbuf]
            xT, xT_r = [xTs[T % nbuf]], [xTs_r[T % nbuf]]
            for e_i in range(NE):
                for ch in range(NCH):
                    if nbuf == 2 and e_i == 0 and ch == min(1, NCH - 1) and T + 1 < NT:
                        front(T + 1)
                    b = wi % 2
                    if wi + 1 < len(wseq):
                        load_w(wi + 1)
                    ab = wi % 2
                    CS = chunk_sizes[ch]
                    for fl in range(CS):
                        for hf in range(2):
                            gb = gcount % 2
                            gcount += 1
                            for (wt, wt_r, pst, pst_r) in ((wg[b], wg_r[b], psG[gb], psG_r[gb]),
                                                           (wu[b], wu_r[b], psU[gb], psU_r[gb])):
                                for kt in range(KT):
                                    kb.op("pe", lambda e, kt=kt, wt=wt, pst=pst, fl=fl, hf=hf: e.matmul(
                                        out=pst[:], lhsT=wt[:, kt, fl * 128:(fl + 1) * 128],
                                        rhs=xT[0][:, kt, hf * 512:(hf + 1) * 512],
                                        start=(kt == 0), stop=(kt == KT - 1)),
                                        reads=[wt_r, xT_r[0]], writes=[pst_r], signal=(kt == KT - 1))
                            kb.op("act", lambda e, gb=gb: e.activation(out=sg[gb][:], in_=psG[gb][:], func=AF.Silu),
                                  reads=[psG_r[gb]], writes=[sg_r[gb]])
                            kb.op("dve", lambda e, gb=gb, fl=fl, hf=hf, ab=ab: e.tensor_tensor(
                                out=actT[ab][:, fl, hf * 512:(hf + 1) * 512], in0=sg[gb][:], in1=psU[gb][:], op=ALU.mult),
                                reads=[sg_r[gb], psU_r[gb]], writes=[actT_r[ab]])
                    for j in range(NJ):
                        for dh in range(2):
                            ob = ocount % 2
                            ocount += 1
                            for fl in range(CS):
                                kb.op("pe", lambda e, fl=fl, j=j, dh=dh, ob=ob, ab=ab, b=b: e.matmul(
                                    out=psO[ob][:], lhsT=actT[ab][:, fl, j * 128:(j + 1) * 128],
                                    rhs=wd[b][:, fl, dh * 512:(dh + 1) * 512],
                                    start=(fl == 0), stop=(fl == CS - 1)),
                                    reads=[actT_r[ab], wd_r[b]], writes=[psO_r[ob]], signal=(fl == CS - 1))
                            if router is not None:
                                kb.op("dve", lambda e, j=j, dh=dh, ob=ob, e_i=e_i: e.scalar_tensor_tensor(
                                    out=acc[:, j, dh * 512:(dh + 1) * 512], in0=psO[ob][:],
                                    scalar=gates[:, j, e_i:e_i + 1], in1=acc[:, j, dh * 512:(dh + 1) * 512],
                                    op0=ALU.mult, op1=ALU.add),
                                    reads=[psO_r[ob], gates_r, acc_r], writes=[acc_r])
                            else:
                                kb.op("dve", lambda e, j=j, dh=dh, ob=ob: e.tensor_tensor(
                                    out=acc[:, j, dh * 512:(dh + 1) * 512], in0=psO[ob][:],
                                    in1=acc[:, j, dh * 512:(dh + 1) * 512], op=ALU.add),
                                    reads=[psO_r[ob], acc_r], writes=[acc_r])
                    wi += 1
            if final_g is not None:
                fr.rstd(acc[:], acc_r, NJ)
                for j in range(NJ):
                    kb.op("dve", lambda e, j=j: e.scalar_tensor_tensor(out=acc[:, j, :], in0=acc[:, j, :],
                                                                       scalar=fr.rs[:, j:j + 1], in1=gf[:],
                                                                       op0=ALU.mult, op1=ALU.mult),
                          reads=[acc_r, fr.rs_r, gf_r], writes=[acc_r])
            for hf in range(2):
                kb.dma("sp", dst_ap[t0 + hf * 512:t0 + (hf + 1) * 512, :].rearrange("(j p) d -> p j d", p=128),
                       acc[:, hf * 4:(hf + 1) * 4, :], [acc_r], [dst_r], acc_r, store=True)
            if nbuf == 1 and T + 1 < NT:
                front(T + 1)
        kb.barrier()


def fourier_phase(kb, src, dst, norm_g, w_fnet, ident_d, dftc, dfts, cc_d, nsc_d, sc_d, rev_d, tag="fn"):
    nc = kb.nc
    src_ap, src_r = src
    dst_ap, dst_r = dst
    NST = S // 128
    with ExitStack() as es:
        ident = es.enter_context(nc.sbuf_tensor(tag + "_ident", [128, 128], BF16))
        ident_r = Res("ident")
        kb.dma("sp", ident[:], ident_d, [], [ident_r], ident_r)
        hnS = es.enter_context(nc.sbuf_tensor(tag + "_hnS", [128, NST, D], BF16))
        hnS_r = Res("hnS")
        wf = es.enter_context(nc.sbuf_tensor(tag + "_wf", [128, KT, D], BF16))
        wf_r = Res("wf")
        kb.dma("pool", wf[:], w_fnet.rearrange("(kt p) d -> p kt d", p=128), [], [wf_r], wf_r)
        cc = es.enter_context(nc.sbuf_tensor(tag + "_cc", [128, 2, 256], BF16))
        cc_r = Res("cc")
        kb.dma("sp", cc[:], cc_d, [], [cc_r], cc_r)
        nsc = es.enter_context(nc.sbuf_tensor(tag + "_nsc", [128, 2, 256], BF16))
        nsc_r = Res("nsc")
        kb.dma("sp", nsc[:], nsc_d, [], [nsc_r], nsc_r)
        with ExitStack() as es0:
            fr = Front(kb, es0, ident, ident_r, norm_g, tag + "f", with_T=False)
            x4 = [es0.enter_context(nc.sbuf_tensor(tag + "_x4%d" % i, [128, 4, D], F32)) for i in range(4)]
            x4_r = [Res("x4%d" % i) for i in range(4)]
            for T in range(S // 512):
                b = T % 4
                kb.dma("sp", x4[b][:], src_ap[T * 512:(T + 1) * 512, :].rearrange("(j p) d -> p j d", p=128),
                       [src_r], [x4_r[b]], x4_r[b])
                fr.rstd(x4[b][:], x4_r[b], 4)
                for j in range(4):
                    fr.norm_to(hnS[:, T * 4 + j, :], hnS_r, x4[b][:, j, :], x4_r[b], j)
            kb.barrier()
        with ExitStack() as es1:
            tc = [es1.enter_context(nc.sbuf_tensor(tag + "_tc%d" % i, [128, NST, 128], BF16)) for i in range(2)]
            ts = [es1.enter_context(nc.sbuf_tensor(tag + "_ts%d" % i, [128, NST, 128], BF16)) for i in range(2)]
            tc_r = [Res("tc%d" % i) for i in range(2)]
            ts_r = [Res("ts%d" % i) for i in range(2)]
            rev = es1.enter_context(nc.sbuf_tensor(tag + "_rev", [128, 128], BF16))
            rev_r = Res("rev")
            kb.dma("sp", rev[:], rev_d, [], [rev_r], rev_r)
            sc = es1.enter_context(nc.sbuf_tensor(tag + "_sc", [128, 2, 256], BF16))
            sc_r = Res("sc")
            kb.dma("sp", sc[:], sc_d, [], [sc_r], sc_r)
            Pb = es1.enter_context(nc.sbuf_tensor(tag + "_Pb", [128, D], BF16))
            Qb = es1.enter_context(nc.sbuf_tensor(tag + "_Qb", [128, D], BF16))
            Pb_r, Qb_r = Res("Pb"), Res("Qb")
            PT = [es1.enter_context(nc.sbuf_tensor(tag + "_PT%d" % i, [128, KT, 128], BF16)) for i in range(2)]
            QT = [es1.enter_context(nc.sbuf_tensor(tag + "_QT%d" % i, [128, KT, 128], BF16)) for i in range(2)]
            PT_r = [Res("PT%d" % i) for i in range(2)]
            QT_r = [Res("QT%d" % i) for i in range(2)]
            YT = [es1.enter_context(nc.sbuf_tensor(tag + "_YT%d" % i, [128, KT, 128], BF16)) for i in range(2)]
            YT_r = [Res("YT%d" % i) for i in range(2)]
            xt = [es1.enter_context(nc.sbuf_tensor(tag + "_xt%d" % i, [128, D], F32)) for i in range(4)]
            xt_r = [Res("xt%d" % i) for i in range(4)]
            psP = [es1.enter_context(nc.psum_tensor(tag + "_psP%d" % i, [128, 512], F32)) for i in range(2)]
            psQ = [es1.enter_context(nc.psum_tensor(tag + "_psQ%d" % i, [128, 512], F32)) for i in range(2)]
            psP_r = [Res("psP%d" % i) for i in range(2)]
            psQ_r = [Res("psQ%d" % i) for i in range(2)]
            psT = [es1.enter_context(nc.psum_tensor(tag + "_psT%d" % i, [128, D], BF16)) for i in range(2)]
            psT_r = [Res("psT%d" % i) for i in range(2)]
            psY = es1.enter_context(nc.psum_tensor(tag + "_psY", [128, D], F32))
            psY_r = Res("psY")
            NSRC = 17

            def load_tab(ai):
                b = ai % 2
                kb.dma("sp", tc[b][:], dftc[ai], [], [tc_r[b]], tc_r[b])
                kb.dma("sp", ts[b][:], dfts[ai], [], [ts_r[b]], ts_r[b])

            def load_x(ai):
                a_ = ai - 1
                xd, xd_r = xt[(ai % 2) * 2], xt_r[(ai % 2) * 2]
                xm, xm_r = xt[(ai % 2) * 2 + 1], xt_r[(ai % 2) * 2 + 1]
                if a_ < 0:
                    kb.op("dve", lambda e: e.memset(xd[:], 0.0), writes=[xd_r])
                    kb.dma("sp", xd[127:128, :], src_ap[0:1, :], [src_r], [xd_r], xd_r)
                else:
                    kb.dma("sp", xd[:], src_ap[128 * a_ + 1:128 * a_ + 129, :], [src_r], [xd_r], xd_r)
                    r0 = 128 * (31 - a_)
                    kb.dma("sp", xm[:], src_ap[r0:r0 + 128, :], [src_r], [xm_r], xm_r)

            load_tab(0)
            load_x(0)
            for ai in range(NSRC):
                a_ = ai - 1
                b = ai % 2
                if ai + 1 < NSRC:
                    load_tab(ai + 1)
                    load_x(ai + 1)
                for (tab, tab_r, psX, psX_r) in ((tc[b], tc_r[b], psP, psP_r), (ts[b], ts_r[b], psQ, psQ_r)):
                    for ch in range(2):
                        for st in range(NST):
                            kb.op("pe", lambda e: e.matmul(
                                out=psX[ch][:], lhsT=tab[:, st, :], rhs=hnS[:, st, ch * 512:(ch + 1) * 512],
                                start=(st == 0), stop=(st == NST - 1)),
                                reads=[tab_r, hnS_r], writes=[psX_r[ch]], signal=(st == NST - 1))
                for (psX, psX_r, Xb, Xb_r) in ((psP, psP_r, Pb, Pb_r), (psQ, psQ_r, Qb, Qb_r)):
                    for ch in range(2):
                        kb.op("act", lambda e: e.activation(
                            out=Xb[:, ch * 512:(ch + 1) * 512], in_=psX[ch][:], func=AF.Copy),
                            reads=[psX_r[ch]], writes=[Xb_r])
                variants = [(0, ident, ident_r, nsc, nsc_r)]
                if a_ >= 0:
                    variants.append((1, rev, rev_r, sc, sc_r))
                for (mi, perm, perm_r, stab, stab_r) in variants:
                    for i, (Xb, Xb_r, XT, XT_r) in enumerate(((Pb, Pb_r, PT[mi], PT_r[mi]), (Qb, Qb_r, QT[mi], QT_r[mi]))):
                        for ct in range(KT):
                            kb.op("pe", lambda e: e.transpose(
                                out=psT[i][:, ct * 128:(ct + 1) * 128], in_=Xb[:, ct * 128:(ct + 1) * 128],
                                identity=perm[:]), reads=[Xb_r, perm_r], writes=[psT_r[i]], signal=(ct == KT - 1))
                        kb.op("dve", lambda e: e.tensor_copy(
                            out=XT[:], in_=psT[i][:].rearrange("p (k t) -> p k t", k=KT)),
                            reads=[psT_r[i]], writes=[XT_r])
                    for g in range(4):
                        for c2 in range(2):
                            o = (g * 2 + c2) * 128
                            n = 0
                            for (tabc, XT) in ((cc, PT[mi]), (stab, QT[mi])):
                                for ct in range(2):
                                    kb.op("pe", lambda e: e.matmul(
                                        out=psY[:, o:o + 128], lhsT=tabc[:, ct, c2 * 128:(c2 + 1) * 128],
                                        rhs=XT[:, g * 2 + ct, :], start=(n == 0), stop=(n == 3)),
                                        reads=[cc_r, stab_r, PT_r[mi], QT_r[mi]], writes=[psY_r],
                                        signal=(n == 3 and g == 3 and c2 == 1))
                                    n += 1
                    kb.op("act", lambda e: e.activation(out=YT[mi][:], in_=psY[:].rearrange("p (k t) -> p k t", k=KT),
                                                        func=AF.Identity, scale=1.0 / 1024.0),
                          reads=[psY_r], writes=[YT_r[mi]])
                    xo, xo_r = xt[(ai % 2) * 2 + mi], xt_r[(ai % 2) * 2 + mi]
                    for dh in range(2):
                        for ft in range(KT):
                            kb.op("pe", lambda e: e.matmul(
                                out=psP[dh][:], lhsT=YT[mi][:, ft, :], rhs=wf[:, ft, dh * 512:(dh + 1) * 512],
                                start=(ft == 0), stop=(ft == KT - 1)),
                                reads=[YT_r[mi], wf_r], writes=[psP_r[dh]], signal=(ft == KT - 1))
                        kb.op("dve", lambda e: e.tensor_tensor(
                            out=xo[:, dh * 512:(dh + 1) * 512], in0=psP[dh][:], in1=xo[:, dh * 512:(dh + 1) * 512],
                            op=ALU.add), reads=[psP_r[dh], xo_r], writes=[xo_r])
                    if mi == 0:
                        if a_ < 0:
                            kb.dma("sp", dst_ap[0:1, :], xo[127:128, :], [xo_r], [dst_r], xo_r, store=True)
                        elif a_ == 15:
                            kb.dma("sp", dst_ap[128 * a_ + 1:128 * a_ + 128, :], xo[0:127, :], [xo_r], [dst_r], xo_r, store=True)
                        else:
                            kb.dma("sp", dst_ap[128 * a_ + 1:128 * a_ + 129, :], xo[:], [xo_r], [dst_r], xo_r, store=True)
                    else:
                        r0 = 128 * (31 - a_)
                        kb.dma("sp", dst_ap[r0:r0 + 128, :], xo[:], [xo_r], [dst_r], xo_r, store=True)
            kb.barrier()


def ret_proj_phase(kb, src, norm_g, w_in, gn_gain, ident_d, rope_cos, rope_sin, qk_s, v_s, sg_s, tag="ra"):
    nc = kb.nc
    src_ap, src_r = src
    qk_ap, qk_r = qk_s
    v_ap, v_r = v_s
    sg_ap, sg_r = sg_s
    with ExitStack() as es:
        ident = es.enter_context(nc.sbuf_tensor(tag + "_ident", [128, 128], BF16))
        ident_r = Res("ident")
        kb.dma("sp", ident[:], ident_d, [], [ident_r], ident_r)
        xT = es.enter_context(nc.sbuf_tensor(tag + "_xT", [128, KT, S], BF16))
        xT_r = Res("xT")
        with ExitStack() as es0:
            fr = Front(kb, es0, ident, ident_r, norm_g, tag + "f")
            x4 = [es0.enter_context(nc.sbuf_tensor(tag + "_x4%d" % i, [128, 4, D], F32)) for i in range(4)]
            x4_r = [Res("x4%d" % i) for i in range(4)]
            for T in range(S // 512):
                b = T % 4
                kb.dma("sp", x4[b][:], src_ap[T * 512:(T + 1) * 512, :].rearrange("(j p) d -> p j d", p=128),
                       [src_r], [x4_r[b]], x4_r[b])
                fr.norm_T(x4[b][:], x4_r[b], 4, xT, xT_r, T * 512)
            kb.barrier()
        with ExitStack() as es1:
            wqk = es1.enter_context(nc.sbuf_tensor(tag + "_wqk", [128, KT, 2048], BF16))
            wqk_r = Res("wqk")
            for hh in range(2):
                kb.dma("pool", wqk[:, :, hh * 1024:(hh + 1) * 1024],
                       w_in[:, hh * 1024:(hh + 1) * 1024].rearrange("(kt p) f -> p kt f", p=128),
                       [], [wqk_r], wqk_r)
            cs = [es1.enter_context(nc.sbuf_tensor(tag + "_cs%d" % i, [128, 512], F32)) for i in range(2)]
            sn = [es1.enter_context(nc.sbuf_tensor(tag + "_sn%d" % i, [128, 512], F32)) for i in range(2)]
            cs_r = [Res("cs%d" % i) for i in range(2)]
            sn_r = [Res("sn%d" % i) for i in range(2)]
            As = [es1.enter_context(nc.sbuf_tensor(tag + "_As%d" % i, [128, 512], F32)) for i in range(2)]
            Bs = [es1.enter_context(nc.sbuf_tensor(tag + "_Bs%d" % i, [128, 512], F32)) for i in range(2)]
            As_r = [Res("As%d" % i) for i in range(2)]
            Bs_r = [Res("Bs%d" % i) for i in range(2)]
            tt = [es1.enter_context(nc.sbuf_tensor(tag + "_tt%d" % i, [128, 512], F32)) for i in range(8)]
            tt_r = [Res("tt%d" % i) for i in range(8)]
            stg = [es1.enter_context(nc.sbuf_tensor(tag + "_stg%d" % i, [128, 4, 16, 128], BF16)) for i in range(2)]
            stg_r = [Res("stg%d" % i) for i in range(2)]
            psA = [es1.enter_context(nc.psum_tensor(tag + "_psA%d" % i, [128, 512], F32)) for i in range(2)]
            psB = [es1.enter_context(nc.psum_tensor(tag + "_psB%d" % i, [128, 512], F32)) for i in range(2)]
            psA_r = [Res("psA%d" % i) for i in range(2)]
            psB_r = [Res("psB%d" % i) for i in range(2)]
            n = 0
            for T in range(S // 512):
                tb = T % 2
                kb.dma("sp", cs[tb][:], rope_cos[:, T * 512:(T + 1) * 512], [], [cs_r[tb]], cs_r[tb])
                kb.dma("sp", sn[tb][:], rope_sin[:, T * 512:(T + 1) * 512], [], [sn_r[tb]], sn_r[tb])
                for which in range(2):
                    scale = 1.0 if which == 0 else 1.0 / 16.0
                    for h in range(4):
                        b = n % 2
                        n += 1
                        f1 = which * 8 + 2 * h
                        for (ft, psX, psX_r) in ((f1, psA[b], psA_r[b]), (f1 + 1, psB[b], psB_r[b])):
                            for kt in range(KT):
                                kb.op("pe", lambda e, kt=kt, ft=ft, psX=psX: e.matmul(
                                    out=psX[:], lhsT=wqk[:, kt, ft * 128:(ft + 1) * 128],
                                    rhs=xT[:, kt, T * 512:(T + 1) * 512], start=(kt == 0), stop=(kt == KT - 1)),
                                    reads=[wqk_r, xT_r], writes=[psX_r], signal=(kt == KT - 1))
                        kb.op("act", lambda e: e.activation(out=As[b][:], in_=psA[b][:], func=AF.Identity, scale=scale),
                              reads=[psA_r[b]], writes=[As_r[b]])
                        kb.op("act", lambda e: e.activation(out=Bs[b][:], in_=psB[b][:], func=AF.Identity, scale=scale),
                              reads=[psB_r[b]], writes=[Bs_r[b]])
                        t = [tt[b * 4 + i] for i in range(4)]
                        t_r = [tt_r[b * 4 + i] for i in range(4)]
                        kb.op("dve", lambda e: e.tensor_tensor(out=t[0][:], in0=As[b][:], in1=cs[tb][:], op=ALU.mult),
                              reads=[As_r[b], cs_r[tb]], writes=[t_r[0]])
                        kb.op("dve", lambda e: e.tensor_tensor(out=t[1][:], in0=Bs[b][:], in1=sn[tb][:], op=ALU.mult),
                              reads=[Bs_r[b], sn_r[tb]], writes=[t_r[1]])
                        kb.op("dve", lambda e: e.tensor_tensor(out=t[2][:], in0=As[b][:], in1=sn[tb][:], op=ALU.mult),
                              reads=[As_r[b], sn_r[tb]], writes=[t_r[2]])
                        kb.op("dve", lambda e: e.tensor_tensor(out=t[3][:], in0=Bs[b][:], in1=cs[tb][:], op=ALU.mult),
                              reads=[Bs_r[b], cs_r[tb]], writes=[t_r[3]])
                        kb.op("pool", lambda e: e.tensor_tensor(
                            out=stg[tb][:, :, f1, :], in0=t[0][:].rearrange("p (c t) -> p c t", c=4),
                            in1=t[1][:].rearrange("p (c t) -> p c t", c=4), op=ALU.subtract),
                            reads=[t_r[0], t_r[1]], writes=[stg_r[tb]])
                        kb.op("pool", lambda e: e.tensor_tensor(
                            out=stg[tb][:, :, f1 + 1, :], in0=t[2][:].rearrange("p (c t) -> p c t", c=4),
                            in1=t[3][:].rearrange("p (c t) -> p c t", c=4), op=ALU.add),
                            reads=[t_r[2], t_r[3]], writes=[stg_r[tb]])
                kb.dma("sp", qk_ap[T], stg[tb][:], [stg_r[tb]], [qk_r], stg_r[tb], store=True)
            kb.barrier()
        with ExitStack() as es2:
            wv = [es2.enter_context(nc.sbuf_tensor(tag + "_wv%d" % i, [128, KT, 512], BF16)) for i in range(2)]
            wv_r = [Res("wv%d" % i) for i in range(2)]
            gg = es2.enter_context(nc.sbuf_tensor(tag + "_gg", [128, 2048], F32))
            gg_r = Res("gg")
            kb.dma("sp", gg[:], bcast_row(gn_gain, 2048), [], [gg_r], gg_r)
            vst = [es2.enter_context(nc.sbuf_tensor(tag + "_vst%d" % i, [128, 4, 512], BF16)) for i in range(2)]
            vst_r = [Res("vst%d" % i) for i in range(2)]
            sgt = [es2.enter_context(nc.sbuf_tensor(tag + "_sgt%d" % i, [128, 512], F32)) for i in range(2)]
            sgt_r = [Res("sgt%d" % i) for i in range(2)]
            psV = [es2.enter_context(nc.psum_tensor(tag + "_psV%d" % i, [128, 512], F32)) for i in range(4)]
            psV_r = [Res("psV%d" % i) for i in range(4)]

            def load_wv(cb):
                b = cb % 2
                c0 = 2048 + cb * 512
                kb.dma("pool", wv[b][:], w_in[:, c0:c0 + 512].rearrange("(kt p) f -> p kt f", p=128),
                       [], [wv_r[b]], wv_r[b])

            load_wv(0)
            n = 0
            m = 0
            for cb in range(8):
                b = cb % 2
                if cb + 1 < 8:
                    load_wv(cb + 1)
                is_g = cb >= 4
                cc0 = (cb % 4) * 512
                for T in range(S // 512):
                    sb_ = m % 2
                    m += 1
                    for j in range(4):
                        pb = n % 4
                        n += 1
                        t0 = T * 512 + j * 128
                        for kt in range(KT):
                            kb.op("pe", lambda e, kt=kt, t0=t0, pb=pb: e.matmul(
                                out=psV[pb][:], lhsT=xT[:, kt, t0:t0 + 128], rhs=wv[b][:, kt, :],
                                start=(kt == 0), stop=(kt == KT - 1)),
                                reads=[xT_r, wv_r[b]], writes=[psV_r[pb]], signal=(kt == KT - 1))
                        if not is_g:
                            kb.op("act", lambda e, j=j, pb=pb: e.activation(out=vst[sb_][:, j, :], in_=psV[pb][:],
                                                                            func=AF.Copy),
                                  reads=[psV_r[pb]], writes=[vst_r[sb_]])
                        else:
                            gb = n % 2
                            kb.op("act", lambda e, pb=pb, gb=gb: e.activation(out=sgt[gb][:], in_=psV[pb][:],
                                                                              func=AF.Silu),
                                  reads=[psV_r[pb]], writes=[sgt_r[gb]])
                            kb.op("dve", lambda e, j=j, gb=gb: e.tensor_tensor(
                                out=vst[sb_][:, j, :], in0=sgt[gb][:], in1=gg[:, cc0:cc0 + 512], op=ALU.mult),
                                reads=[sgt_r[gb], gg_r], writes=[vst_r[sb_]])
                    dst_ap, dst_r = (sg_ap, sg_r) if is_g else (v_ap, v_r)
                    kb.dma("sp", dst_ap[T * 512:(T + 1) * 512, cc0:cc0 + 512].rearrange("(j p) f -> p j f", p=128),
                           vst[sb_][:], [vst_r[sb_]], [dst_r], vst_r[sb_], store=True)
            kb.barrier()


class RetConsts:
    def __init__(self, kb, es, decay_logit, tag):
        nc = kb.nc
        I32 = mybir.dt.int32

        def T(name, shape, dtype=F32):
            return es.enter_context(nc.sbuf_tensor(tag + "_" + name, shape, dtype)), Res(name)

        dl, dl_r = T("dl", [128, 8])
        kb.dma("sp", dl[:], bcast_row(decay_logit.rearrange("a b -> (a b)"), 8), [], [dl_r], dl_r)
        self.lg, self.lg_r = T("lg", [128, 8])
        nlg, nlg_r = T("nlg", [128, 8])
        lg127, lg127_r = T("lg127", [128, 8])
        self.g128, self.g128_r = T("g128", [128, 8])
        lg, lg_r = self.lg, self.lg_r
        kb.op("act", lambda e: e.activation(out=nlg[:], in_=dl[:], func=AF.Exp, scale=-1.0), reads=[dl_r], writes=[nlg_r])
        kb.op("dve", lambda e: e.tensor_scalar(out=nlg[:], in0=nlg[:], scalar1=1.0, scalar2=None, op0=ALU.add),
              reads=[nlg_r], writes=[nlg_r])
        kb.op("act", lambda e: e.activation(out=nlg[:], in_=nlg[:], func=AF.Ln), reads=[nlg_r], writes=[nlg_r])
        kb.op("dve", lambda e: e.tensor_scalar(out=lg[:], in0=nlg[:], scalar1=-1.0, scalar2=None, op0=ALU.mult),
              reads=[nlg_r], writes=[lg_r])
        kb.op("dve", lambda e: e.tensor_scalar(out=lg127[:], in0=lg[:], scalar1=127.0, scalar2=None, op0=ALU.mult),
              reads=[lg_r], writes=[lg127_r])
        kb.op("act", lambda e: e.activation(out=self.g128[:], in_=lg[:], func=AF.Exp, scale=128.0),
              reads=[lg_r], writes=[self.g128_r])
        ii, ii_r = T("ii", [128, 128], I32)
        dmf, dmf_r = T("dmf", [128, 128])
        imat, imat_r = T("imat", [128, 128])
        jmat, jmat_r = T("jmat", [128, 128])
        pos, pos_r = T("pos", [128, 128])
        neg, neg_r = T("neg", [128, 128])
        tmp, tmp_r = T("tmp", [128, 128])
        kb.op("pool", lambda e: e.iota(ii[:], pattern=[[1, 128]], base=0, channel_multiplier=-1), writes=[ii_r])
        kb.op("dve", lambda e: e.tensor_copy(out=dmf[:], in_=ii[:]), reads=[ii_r], writes=[dmf_r])
        kb.op("pool", lambda e: e.iota(ii[:], pattern=[[1, 128]], base=0, channel_multiplier=0), reads=[], writes=[ii_r])
        kb.op("dve", lambda e: e.tensor_copy(out=imat[:], in_=ii[:]), reads=[ii_r], writes=[imat_r])
        kb.op("pool", lambda e: e.iota(ii[:], pattern=[[0, 128]], base=0, channel_multiplier=1), reads=[], writes=[ii_r])
        kb.op("dve", lambda e: e.tensor_copy(out=jmat[:], in_=ii[:]), reads=[ii_r], writes=[jmat_r])
        kb.op("dve", lambda e: e.tensor_scalar(out=pos[:], in0=dmf[:], scalar1=0.0, scalar2=None, op0=ALU.max),
              reads=[dmf_r], writes=[pos_r])
        kb.op("dve", lambda e: e.tensor_tensor(out=neg[:], in0=pos[:], in1=dmf[:], op=ALU.subtract),
              reads=[pos_r, dmf_r], writes=[neg_r])
        self.Dc, self.Dc_r = T("Dc", [128, 4, 128])
        self.XF, self.XF_r = T("XF", [128, 8, 128])
        self.XB, self.XB_r = T("XB", [128, 8, 128])
        self.ZF, self.ZF_r = T("ZF", [128, 8, 128])
        self.ZB, self.ZB_r = T("ZB", [128, 8, 128])
        for h in range(4):
            f, b_ = h, 4 + h
            kb.op("dve", lambda e: e.tensor_scalar(out=tmp[:], in0=pos[:], scalar1=lg[:, f:f + 1], scalar2=None,
                                                   op0=ALU.mult), reads=[pos_r, lg_r], writes=[tmp_r])
            kb.op("dve", lambda e: e.scalar_tensor_tensor(out=tmp[:], in0=neg[:], scalar=lg[:, b_:b_ + 1], in1=tmp[:],
                                                          op0=ALU.mult, op1=ALU.add),
                  reads=[neg_r, lg_r, tmp_r], writes=[tmp_r])
            kb.op("act", lambda e: e.activation(out=self.Dc[:, h, :], in_=tmp[:], func=AF.Exp),
                  reads=[tmp_r], writes=[self.Dc_r])
            for ft in (2 * h, 2 * h + 1):
                kb.op("act", lambda e: e.activation(out=self.XF[:, ft, :], in_=imat[:], func=AF.Exp,
                                                    scale=lg[:, f:f + 1], bias=lg[:, f:f + 1]),
                      reads=[imat_r, lg_r], writes=[self.XF_r])
                kb.op("act", lambda e: e.activation(out=self.XB[:, ft, :], in_=imat[:], func=AF.Exp,
                                                    scale=nlg[:, b_:b_ + 1], bias=lg127[:, b_:b_ + 1]),
                      reads=[imat_r, nlg_r, lg127_r], writes=[self.XB_r])
                kb.op("act", lambda e: e.activation(out=self.ZF[:, ft, :], in_=jmat[:], func=AF.Exp,
                                                    scale=nlg[:, f:f + 1], bias=lg127[:, f:f + 1]),
                      reads=[jmat_r, nlg_r, lg127_r], writes=[self.ZF_r])
                kb.op("act", lambda e: e.activation(out=self.ZB[:, ft, :], in_=jmat[:], func=AF.Exp,
                                                    scale=lg[:, b_:b_ + 1], bias=lg[:, b_:b_ + 1]),
                      reads=[jmat_r, lg_r], writes=[self.ZB_r])


def ret_core_phase(kb, decay_logit, ident_d, qk_s, v_s, sg_s, sb_s, z_s, tag="rb"):
    nc = kb.nc
    qk_ap, qk_r = qk_s
    v_ap, v_r = v_s
    sg_ap, sg_r = sg_s
    sb_ap, sb_r = sb_s
    z_ap, z_r = z_s
    NCK = S // 128
    with ExitStack() as es:
        ident = es.enter_context(nc.sbuf_tensor(tag + "_ident", [128, 128], BF16))
        ident_r = Res("ident")
        kb.dma("sp", ident[:], ident_d, [], [ident_r], ident_r)
        rc = RetConsts(kb, es, decay_logit, tag + "c")
        S32 = es.enter_context(nc.sbuf_tensor(tag + "_S32", [128, 8, 512], F32))
        S32_r = [Res("S32_%d" % i) for i in range(8)]
        S16 = [es.enter_context(nc.sbuf_tensor(tag + "_S16%d" % i, [128, 8, 512], BF16)) for i in range(2)]
        S16_r = [[Res("S16%d_%d" % (i, k)) for k in range(8)] for i in range(2)]
        S16_st = [Res("S16st%d" % i) for i in range(2)]
        qk = [es.enter_context(nc.sbuf_tensor(tag + "_qk%d" % i, [128, 16, 128], BF16)) for i in range(3)]
        qk_t = [Res("qk%d" % i) for i in range(3)]
        vt = [es.enter_context(nc.sbuf_tensor(tag + "_v%d" % i, [128, 2048], BF16)) for i in range(3)]
        vt_r = [Res("v%d" % i) for i in range(3)]
        kz = [es.enter_context(nc.sbuf_tensor(tag + "_kz%d" % i, [128, 8, 128], BF16)) for i in range(2)]
        kz_r = [Res("kz%d" % i) for i in range(2)]
        psK = es.enter_context(nc.psum_tensor(tag + "_psK", [128, 1024], BF16))
        psK_r = Res("psK")
        psS = [es.enter_context(nc.psum_tensor(tag + "_psS%d" % i, [128, 512], F32)) for i in range(2)]
        psS_r = [Res("psS%d" % i) for i in range(2)]
        cnt = {"s": 0}

        def load_chunk(n, want_q):
            b = n % 3
            T, c = n // 4, n % 4
            if want_q:
                kb.dma("sp", qk[b][:], qk_ap[T, :, c], [qk_r], [qk_t[b]], qk_t[b])
            else:
                kb.dma("sp", qk[b][:, 8:16, :], qk_ap[T, :, c, 8:16, :], [qk_r], [qk_t[b]], qk_t[b])
            kb.dma("sp", vt[b][:], v_ap[n * 128:(n + 1) * 128, :], [v_r], [vt_r[b]], vt_r[b])

        def k_tokmajor(n, Z, Z_r, use_act=False):
            b = n % 2
            b3 = n % 3
            for ft in range(8):
                kb.op("pe", lambda e, ft=ft: e.transpose(out=psK[:, ft * 128:(ft + 1) * 128], in_=qk[b3][:, 8 + ft, :],
                                                         identity=ident[:]),
                      reads=[qk_t[b3], ident_r], writes=[psK_r], signal=(ft == 7))
            if use_act:
                for h in range(4):
                    kb.op("act", lambda e: e.activation(out=kz[b][:, 2 * h:2 * h + 2, :],
                                                        in_=psK[:, h * 256:(h + 1) * 256].rearrange("p (f d) -> p f d", f=2),
                                                        func=AF.Identity, scale=Z[:, 2 * h, 0:1]),
                          reads=[psK_r, Z_r], writes=[kz_r[b]])
            else:
                kb.op("dve", lambda e: e.tensor_tensor(out=kz[b][:], in0=psK[:].rearrange("p (f d) -> p f d", f=8),
                                                       in1=Z[:], op=ALU.mult),
                      reads=[psK_r, Z_r], writes=[kz_r[b]])

        def state_update(n, gcol, s16_out, s16_out_r):
            b = n % 2
            b3 = n % 3
            for h in range(4):
                for dt_ in range(2):
                    ft = 2 * h + dt_
                    pb = cnt["s"] % len(psS)
                    cnt["s"] += 1
                    kb.op("pe", lambda e: e.matmul(out=psS[pb][:], lhsT=kz[b][:, ft, :],
                                                   rhs=vt[b3][:, h * 512:(h + 1) * 512], start=True, stop=True),
                          reads=[kz_r[b], vt_r[b3]], writes=[psS_r[pb]])
                    kb.op("dve", lambda e: e.scalar_tensor_tensor(
                        out=S32[:, ft, :], in0=S32[:, ft, :], scalar=rc.g128[:, gcol + h:gcol + h + 1], in1=psS[pb][:],
                        op0=ALU.mult, op1=ALU.add), reads=[S32_r[ft], rc.g128_r, psS_r[pb]], writes=[S32_r[ft]])
                    if gcol == 4 and ft % 4 == 3:
                        kb.op("pool", lambda e: e.tensor_copy(out=s16_out[:, ft, :], in_=S32[:, ft, :]),
                              reads=[S32_r[ft]], writes=[s16_out_r[ft]])
                    else:
                        kb.op("act", lambda e: e.activation(out=s16_out[:, ft, :], in_=S32[:, ft, :], func=AF.Copy),
                              reads=[S32_r[ft]], writes=[s16_out_r[ft]])

        esb1 = ExitStack()
        for i in range(2, 6):
            psS.append(esb1.enter_context(nc.psum_tensor(tag + "_psS%d" % i, [128, 512], F32)))
            psS_r.append(Res("psS%d" % i))
        kb.op("dve", lambda e: e.memset(S32[:], 0.0), writes=S32_r)
        kb.op("pool", lambda e: e.memset(S16[1][:], 0.0), writes=S16_r[1])
        load_chunk(NCK - 1, False)
        load_chunk(NCK - 2, False)
        k_tokmajor(NCK - 1, rc.ZB, rc.ZB_r, use_act=True)
        for n in range(NCK - 1, -1, -1):
            if n - 2 >= 0:
                load_chunk(n - 2, False)
            cur = S16[n % 2]
            cur_r = S16_r[n % 2]
            kb.dma("sp", sb_ap[n], cur[:], cur_r, [sb_r], S16_st[n % 2], store=True)
            if n > 1:
                k_tokmajor(n - 1, rc.ZB, rc.ZB_r, use_act=True)
            if n > 0:
                state_update(n, 4, S16[(n + 1) % 2], S16_r[(n + 1) % 2])
        kb.barrier(recycle=False)
        del psS[2:]
        del psS_r[2:]
        esb1.close()
        with ExitStack() as es2:
            sgt = [es2.enter_context(nc.sbuf_tensor(tag + "_sg%d" % i, [128, 2048], BF16)) for i in range(2)]
            sgt_r = [Res("sg%d" % i) for i in range(2)]
            sbn = [es2.enter_context(nc.sbuf_tensor(tag + "_sbn%d" % i, [128, 8, 512], BF16)) for i in range(3)]
            sbn_r = [Res("sbn%d" % i) for i in range(3)]
            qf2 = [es2.enter_context(nc.sbuf_tensor(tag + "_qf%d" % i, [128, 8, 128], BF16)) for i in range(2)]
            qb2 = [es2.enter_context(nc.sbuf_tensor(tag + "_qb%d" % i, [128, 8, 128], BF16)) for i in range(2)]
            qf2_r = [Res("qf%d" % i) for i in range(2)]
            qb2_r = [Res("qb%d" % i) for i in range(2)]
            PT = es2.enter_context(nc.sbuf_tensor(tag + "_PT", [128, 4, 128], BF16))
            PT_r = Res("PT")
            yn = [es2.enter_context(nc.sbuf_tensor(tag + "_yn%d" % i, [128, 512], F32)) for i in range(2)]
            yn_r = [Res("yn%d" % i) for i in range(2)]
            zt = [es2.enter_context(nc.sbuf_tensor(tag + "_z%d" % i, [128, 2048], BF16)) for i in range(2)]
            zt_r = [Res("z%d" % i) for i in range(2)]
            st6 = es2.enter_context(nc.sbuf_tensor(tag + "_st6", [128, 4, 6], F32))
            st6_r = Res("st6")
            mv = es2.enter_context(nc.sbuf_tensor(tag + "_mv", [128, 4, 2], F32))
            mv_r = Res("mv")
            rs = es2.enter_context(nc.sbuf_tensor(tag + "_rs", [128, 4], F32))
            rs_r = Res("rs")
            nmr = es2.enter_context(nc.sbuf_tensor(tag + "_nmr", [128, 4], F32))
            nmr_r = Res("nmr")
            psSc = es2.enter_context(nc.psum_tensor(tag + "_psSc", [128, 512], F32))
            psSc_r = Res("psSc")
            psY2 = [es2.enter_context(nc.psum_tensor(tag + "_psY%d" % i, [128, 512], F32)) for i in range(2)]
            psY2_r = [Res("psY%d" % i) for i in range(2)]
            psY = [psY2[h % 2] for h in range(4)]
            psY_r = [psY2_r[h % 2] for h in range(4)]
            for i in range(2, 4):
                psS.append(es2.enter_context(nc.psum_tensor(tag + "_psSf%d" % i, [128, 512], F32)))
                psS_r.append(Res("psSf%d" % i))

            ysb = [es2.enter_context(nc.sbuf_tensor(tag + "_ysb%d" % i, [128, 2048], F32)) for i in range(2)]
            ysb_r = [[Res("ysb%d_%d" % (i, h)) for h in range(4)] for i in range(2)]
            sg3 = [es2.enter_context(nc.sbuf_tensor(tag + "_sg3%d" % i, [128, 2048], BF16)) for i in range(4)]
            sg3_r = [Res("sg3%d" % i) for i in range(4)]

            def load_chunk2(n):
                b = n % 3
                load_chunk(n, True)
                kb.dma("sp", sg3[n % 4][:], sg_ap[n * 128:(n + 1) * 128, :], [sg_r], [sg3_r[n % 4]], sg3_r[n % 4])
                kb.dma("sp", sbn[b][:], sb_ap[n], [sb_r], [sbn_r[b]], sbn_r[b])

            def stage_q(n):
                b = n % 3
                kb.op("pool", lambda e: e.tensor_tensor(out=qf2[n % 2][:], in0=qk[b][:, 0:8, :], in1=rc.XF[:], op=ALU.mult),
                      reads=[qk_t[b], rc.XF_r], writes=[qf2_r[n % 2]])
                kb.op("pool", lambda e: e.tensor_tensor(out=qb2[n % 2][:], in0=qk[b][:, 0:8, :], in1=rc.XB[:], op=ALU.mult),
                      reads=[qk_t[b], rc.XB_r], writes=[qb2_r[n % 2]])

            def stage_x(n):
                b = n % 3
                yb2 = n % 2
                qf, qf_r, qb, qb_r = qf2[n % 2], qf2_r[n % 2], qb2[n % 2], qb2_r[n % 2]
                sf = S16[(n + 1) % 2]
                sf_r = S16_r[(n + 1) % 2]
                if n + 2 < NCK:
                    k_tokmajor(n + 1, rc.ZF, rc.ZF_r)
                if n + 1 < NCK:
                    state_update(n, 0, S16[n % 2], S16_r[n % 2])
                for h in range(4):
                    for dt_ in range(2):
                        kb.op("pe", lambda e: e.matmul(out=psSc[:, h * 128:(h + 1) * 128],
                                                       lhsT=qk[b][:, 8 + 2 * h + dt_, :], rhs=qk[b][:, 2 * h + dt_, :],
                                                       start=(dt_ == 0), stop=(dt_ == 1)),
                              reads=[qk_t[b]], writes=[psSc_r], signal=(h == 3 and dt_ == 1))
                kb.op("dve", lambda e: e.tensor_tensor(out=PT[:], in0=psSc[:].rearrange("p (h i) -> p h i", h=4),
                                                       in1=rc.Dc[:], op=ALU.mult),
                      reads=[psSc_r, rc.Dc_r], writes=[PT_r])
                for h in range(4):
                    kb.op("pe", lambda e: e.matmul(out=psY[h][:], lhsT=PT[:, h, :], rhs=vt[b][:, h * 512:(h + 1) * 512],
                                                   start=True, stop=False),
                          reads=[PT_r, vt_r[b]], writes=[psY_r[h]], signal=False)
                    for dt_ in range(2):
                        kb.op("pe", lambda e: e.matmul(out=psY[h][:], lhsT=qf[:, 2 * h + dt_, :], rhs=sf[:, 2 * h + dt_, :],
                                                       start=False, stop=False),
                              reads=[qf_r, sf_r[2 * h + dt_]], writes=[psY_r[h]], signal=False)
                    for dt_ in range(2):
                        kb.op("pe", lambda e: e.matmul(out=psY[h][:], lhsT=qb[:, 2 * h + dt_, :],
                                                       rhs=sbn[b][:, 2 * h + dt_, :], start=False, stop=(dt_ == 1)),
                              reads=[PT_r, vt_r[b], qf_r, sf_r[2 * h], sf_r[2 * h + 1], qb_r, sbn_r[b]],
                              writes=[psY_r[h]], signal=(dt_ == 1))
                    kb.op("act", lambda e: e.activation(out=ysb[yb2][:, h * 512:(h + 1) * 512], in_=psY[h][:], func=AF.Copy),
                          reads=[psY_r[h]], writes=[ysb_r[yb2][h]])

            ycnt = {"c": 0}

            def stage_z(n):
                b = n % 2
                for h in range(4):
                    kb.op("dve", lambda e: e.bn_stats(out=st6[:, h, :], in_=ysb[b][:, h * 512:(h + 1) * 512]),
                          reads=[ysb_r[b][h]], writes=[st6_r])
                    kb.op("dve", lambda e: e.bn_aggr(out=mv[:, h, :], in_=st6[:, h, :]), reads=[st6_r], writes=[mv_r])
                kb.op("dve", lambda e: e.tensor_scalar(out=rs[:], in0=mv[:, :, 1], scalar1=EPS, scalar2=None, op0=ALU.add),
                      reads=[mv_r], writes=[rs_r])
                kb.op("act", lambda e: e.activation(out=rs[:], in_=rs[:], func=AF.Sqrt), reads=[rs_r], writes=[rs_r])
                kb.op("dve", lambda e: e.reciprocal(out=rs[:], in_=rs[:]), reads=[rs_r], writes=[rs_r])
                kb.op("dve", lambda e: e.scalar_tensor_tensor(out=nmr[:], in0=mv[:, :, 0], scalar=-1.0, in1=rs[:],
                                                              op0=ALU.mult, op1=ALU.mult),
                      reads=[mv_r, rs_r], writes=[nmr_r])
                for h in range(4):
                    yb = ycnt["c"] % 2
                    ycnt["c"] += 1
                    kb.op("act", lambda e: e.activation(out=yn[yb][:], in_=ysb[b][:, h * 512:(h + 1) * 512], func=AF.Identity,
                                                        scale=rs[:, h:h + 1], bias=nmr[:, h:h + 1]),
                          reads=[ysb_r[b][h], rs_r, nmr_r], writes=[yn_r[yb]])
                    kb.op("pool", lambda e: e.tensor_tensor(out=zt[b][:, h * 512:(h + 1) * 512], in0=yn[yb][:],
                                                            in1=sg3[n % 4][:, h * 512:(h + 1) * 512], op=ALU.mult),
                          reads=[yn_r[yb], sg3_r[n % 4]], writes=[zt_r[b]])
                kb.dma("sp", z_ap[n * 128:(n + 1) * 128, :], zt[b][:], [zt_r[b]], [z_r], zt_r[b], store=True)

            kb.op("dve", lambda e: e.memset(S32[:], 0.0), reads=[], writes=S32_r)
            kb.op("pool", lambda e: e.memset(S16[1][:], 0.0), reads=[], writes=S16_r[1])
            load_chunk2(0)
            load_chunk2(1)
            stage_q(0)
            k_tokmajor(0, rc.ZF, rc.ZF_r)
            for n in range(NCK):
                if n + 2 < NCK:
                    load_chunk2(n + 2)
                if n + 1 < NCK:
                    stage_q(n + 1)
                stage_x(n)
                if n >= 1:
                    stage_z(n - 1)
            stage_z(NCK - 1)
            kb.barrier()


def ret_out_phase(kb, src, dst, z_s, w_out, ident_d, tag="rc"):
    nc = kb.nc
    src_ap, src_r = src
    dst_ap, dst_r = dst
    z_ap, z_r = z_s
    with ExitStack() as es:
        ident = es.enter_context(nc.sbuf_tensor(tag + "_ident", [128, 128], BF16))
        ident_r = Res("ident")
        kb.dma("sp", ident[:], ident_d, [], [ident_r], ident_r)
        wo = es.enter_context(nc.sbuf_tensor(tag + "_wo", [128, 16, D], BF16))
        wo_r = Res("wo")
        for hh in range(2):
            kb.dma("pool", wo[:, hh * 8:(hh + 1) * 8, :],
                   w_out[hh * 1024:(hh + 1) * 1024, :].rearrange("(et p) d -> p et d", p=128), [], [wo_r], wo_r)
        zt = [es.enter_context(nc.sbuf_tensor(tag + "_z%d" % i, [128, 2048], BF16)) for i in range(2)]
        zt_r = [Res("z%d" % i) for i in range(2)]
        xt = [es.enter_context(nc.sbuf_tensor(tag + "_x%d" % i, [128, D], F32)) for i in range(4)]
        xt_r = [Res("x%d" % i) for i in range(4)]
        zT = [es.enter_context(nc.sbuf_tensor(tag + "_zT%d" % i, [128, 16, 128], BF16)) for i in range(2)]
        zT_r = [Res("zT%d" % i) for i in range(2)]
        psT = [es.enter_context(nc.psum_tensor(tag + "_psT%d" % i, [128, 1024], BF16)) for i in range(2)]
        psT_r = [Res("psT%d" % i) for i in range(2)]
        psO = [es.enter_context(nc.psum_tensor(tag + "_psO%d" % i, [128, 512], F32)) for i in range(4)]
        psO_r = [Res("psO%d" % i) for i in range(4)]

        def load(t):
            b = t % 2
            kb.dma("sp", zt[b][:], z_ap[t * 128:(t + 1) * 128, :], [z_r], [zt_r[b]], zt_r[b])
            kb.dma("sp", xt[t % 4][:], src_ap[t * 128:(t + 1) * 128, :], [src_r], [xt_r[t % 4]], xt_r[t % 4])

        def transp(t):
            b = t % 2
            for hh in range(2):
                for e8 in range(8):
                    et = hh * 8 + e8
                    kb.op("pe", lambda e: e.transpose(out=psT[hh][:, e8 * 128:(e8 + 1) * 128],
                                                      in_=zt[b][:, et * 128:(et + 1) * 128], identity=ident[:]),
                          reads=[zt_r[b], ident_r], writes=[psT_r[hh]], signal=(e8 == 7))
                kb.op("act", lambda e: e.activation(out=zT[b][:, hh * 8:(hh + 1) * 8, :],
                                                    in_=psT[hh][:].rearrange("p (k t) -> p k t", k=8), func=AF.Copy),
                      reads=[psT_r[hh]], writes=[zT_r[b]])

        load(0)
        load(1)
        transp(0)
        load(2)
        for t in range(S // 128):
            b = t % 2
            xb = t % 4
            if t + 1 < S // 128:
                transp(t + 1)
                if t + 3 < S // 128:
                    load(t + 3)
            for dh in range(2):
                pb = (t % 2) * 2 + dh
                for et in range(16):
                    kb.op("pe", lambda e: e.matmul(out=psO[pb][:], lhsT=zT[b][:, et, :],
                                                   rhs=wo[:, et, dh * 512:(dh + 1) * 512],
                                                   start=(et == 0), stop=(et == 15)),
                          reads=[zT_r[b], wo_r], writes=[psO_r[pb]], signal=(et == 15))
                kb.op("dve", lambda e: e.tensor_tensor(out=xt[xb][:, dh * 512:(dh + 1) * 512], in0=psO[pb][:],
                                                       in1=xt[xb][:, dh * 512:(dh + 1) * 512], op=ALU.add),
                      reads=[psO_r[pb], xt_r[xb]], writes=[xt_r[xb]])
            kb.dma("sp", dst_ap[t * 128:(t + 1) * 128, :], xt[xb][:], [xt_r[xb]], [dst_r], xt_r[xb], store=True)
        kb.barrier()


def host_consts():
    c = {}
    c["ident"] = np.eye(128, dtype=np.float32).astype(ml_dtypes.bfloat16)
    c["identf"] = np.eye(128, dtype=np.float32)
    half = 128
    inv = (np.float32(10000.0) ** (-np.arange(half, dtype=np.float32) / np.float32(half))).astype(np.float32)
    ang = (np.arange(S, dtype=np.float32)[:, None] * inv[None, :]).astype(np.float32)
    c["rope_cos"] = np.ascontiguousarray(np.cos(ang).astype(np.float32).T)
    c["rope_sin"] = np.ascontiguousarray(np.sin(ang).astype(np.float32).T)
    s_idx = (np.arange(32)[None, :, None] * 128 + np.arange(128)[:, None, None]).astype(np.int64)
    k_idx = ((128 * (np.arange(17)[:, None] - 1) + 1 + np.arange(128)[None, :]) % S).astype(np.int64)
    m = (s_idx[None] * k_idx[:, None, None, :]) % S
    th = m.astype(np.float64) * (2.0 * np.pi / S)
    c["dftc"] = np.cos(th).astype(np.float32).astype(ml_dtypes.bfloat16)
    c["dfts"] = np.sin(th).astype(np.float32).astype(ml_dtypes.bfloat16)
    c["rev"] = np.ascontiguousarray(np.eye(128, dtype=np.float32)[:, ::-1]).astype(ml_dtypes.bfloat16)
    cidx = (np.arange(2)[None, :, None] * 128 + np.arange(128)[:, None, None]).astype(np.int64)
    m2 = (cidx * np.arange(256, dtype=np.int64)[None, None, :]) % 256
    th2 = m2.astype(np.float64) * (2.0 * np.pi / 256)
    c["cc"] = np.cos(th2).astype(np.float32).astype(ml_dtypes.bfloat16)
    c["nsc"] = (-np.sin(th2)).astype(np.float32).astype(ml_dtypes.bfloat16)
    c["sc"] = np.sin(th2).astype(np.float32).astype(ml_dtypes.bfloat16)
    return c


ALL_PHASES = ("ret", "ffn0", "fnet", "moe")
SPARSE_MOE = True


def build(phases=ALL_PHASES):
    nc = bass.Bass("TRN2", target_bir_lowering=False)

    def din(name, shape, dtype=F32):
        return nc.dram_tensor(name, list(shape), dtype, kind="ExternalInput").ap()

    def dscr(name, shape, dtype):
        return nc.dram_tensor(name, list(shape), dtype, kind="Internal").ap(), Res(name, accum=True)

    x = din("x", [S, D])
    mix_norm = din("mix_norm", [2, D])
    ffn_norm = din("ffn_norm", [2, D])
    ident = din("ident", [128, 128], BF16)
    identf = din("identf", [128, 128], F32)
    if "ret" in phases:
        w_in = din("ret_w_in", [D, 6144])
        decay = din("ret_decay_logit", [2, 4])
        gn_gain = din("ret_gn_gain", [2048])
        w_out = din("ret_w_out", [2048, D])
        rope_cos = din("rope_cos", [128, S])
        rope_sin = din("rope_sin", [128, S])
    if "ffn0" in phases:
        dwg = din("dense_w_gate", [D, 2816])
        dwu = din("dense_w_up", [D, 2816])
        dwd = din("dense_w_down", [2816, D])
    if "fnet" in phases:
        w_fnet = din("fnet_w_out", [D, D])
        dftc = din("dftc", [17, 128, 32, 128], BF16)
        dfts = din("dfts", [17, 128, 32, 128], BF16)
        cc = din("cc", [128, 2, 256], BF16)
        nsc = din("nsc", [128, 2, 256], BF16)
        scp = din("sc", [128, 2, 256], BF16)
        rev = din("rev", [128, 128], BF16)
    if "moe" in phases:
        router = din("moe_router", [D, 8])
        mwg = din("moe_w_gate", [8, D, 3584])
        mwu = din("moe_w_up", [8, D, 3584])
        mwd = din("moe_w_down", [8, 3584, D])
        final_norm = din("final_norm", [D])
    y = nc.dram_tensor("y", [S, D], F32, kind="ExternalOutput").ap()
    y_r = Res("y", accum=True)
    with ExitStack() as es:
        kb = KB(nc, es)
        cur = (x, Res("x", accum=True))
        order = [p for p in ALL_PHASES if p in phases]
        for p in order:
            last = p == order[-1]
            nxt = (y, y_r) if last else dscr("h_" + p, [S, D], F32)
            if p == "ret":
                qk_s = dscr("qk_s", [8, 128, 4, 16, 128], BF16)
                v_s = dscr("v_s", [S, 2048], BF16)
                sg_s = dscr("sg_s", [S, 2048], BF16)
                sb_s = dscr("sb_s", [32, 128, 8, 512], BF16)
                z_s = dscr("z_s", [S, 2048], BF16)
                ret_proj_phase(kb, cur, mix_norm[0], w_in, gn_gain, ident, rope_cos, rope_sin, qk_s, v_s, sg_s)
                ret_core_phase(kb, decay, ident, qk_s, v_s, sg_s, sb_s, z_s)
                ret_out_phase(kb, cur, nxt, z_s, w_out, ident)
            elif p == "ffn0":
                ffn_phase(kb, cur, nxt, ffn_norm[0], [(dwg, dwu, dwd)], ident, tag="f0", chunk_sizes=[6, 6, 5, 5], nbuf=2)
            elif p == "fnet":
                fourier_phase(kb, cur, nxt, mix_norm[1], w_fnet, ident, dftc, dfts, cc, nsc, scp, rev)
            elif p == "moe":
                if SPARSE_MOE:
                    xg_s = dscr("xg_s", [NT_SLOT * BS, D], BF16)
                    o_s = dscr("o_s", [NT_SLOT * BS, D], F32)
                    moe_sparse_phase(kb, cur, nxt, ffn_norm[1], router, mwg, mwu, mwd, final_norm, ident, identf,
                                     xg_s, o_s)
                else:
                    ffn_phase(kb, cur, nxt, ffn_norm[1], [(mwg[e], mwu[e], mwd[e]) for e in range(8)], ident,
                              router=router, final_g=final_norm, FC=7, tag="f1")
            cur = nxt
        kb.final_wait("sp")
    return nc


_CONSTS = None
PHASE_INPUTS = {
    "ret": ["ret_w_in", "ret_decay_logit", "ret_gn_gain", "ret_w_out"],
    "ffn0": ["dense_w_gate", "dense_w_up", "dense_w_down"],
    "fnet": ["fnet_w_out"],
    "moe": ["moe_router", "moe_w_gate", "moe_w_up", "moe_w_down", "final_norm"],
}
PHASE_CONSTS = {"ret": ["rope_cos", "rope_sin"], "ffn0": [], "fnet": ["dftc", "dfts", "cc", "nsc", "sc", "rev"], "moe": []}


def make_in_maps(inputs, phases=ALL_PHASES, cores=range(NCORES), x_override=None):
    global _CONSTS
    if _CONSTS is None:
        _CONSTS = host_consts()
    shared = {"mix_norm": np.ascontiguousarray(inputs["mix_norm"], dtype=np.float32),
              "ffn_norm": np.ascontiguousarray(inputs["ffn_norm"], dtype=np.float32),
              "ident": _CONSTS["ident"], "identf": _CONSTS["identf"]}
    for p in phases:
        for k in PHASE_INPUTS[p]:
            a = np.asarray(inputs[k], dtype=np.float32)
            if k != "final_norm":
                a = a[0]
            shared[k] = np.ascontiguousarray(a)
        for k in PHASE_CONSTS[p]:
            shared[k] = _CONSTS[k]
    maps = []
    for c in cores:
        m = dict(shared)
        m["x"] = np.ascontiguousarray(inputs["x"][c] if x_override is None else x_override[c], dtype=np.float32)
        maps.append(m)
    return maps


def kernel(**inputs):
    nc = build(ALL_PHASES)
    maps = make_in_maps(inputs)
    res = run_bass_kernel_spmd(nc, maps, core_ids=list(range(NCORES)))
    return np.stack([np.asarray(r["y"], dtype=np.float32) for r in res.results], axis=0)


NT_SLOT = 24
BS = 512


def bc_last(ap, n):
    return bass.AP(ap.tensor, ap.offset, [list(a) for a in ap.ap] + [[0, n]])


def bc_mid(ap2, n):
    a = [list(v) for v in ap2.ap]
    return bass.AP(ap2.tensor, ap2.offset, [a[0], [0, n]] + a[1:])


def moe_sparse_phase(kb, src, dst, norm_g, router, mwg, mwu, mwd, final_g, ident_d, identf_d, xg_s, o_s, tag="ms"):
    nc = kb.nc
    I32 = mybir.dt.int32
    src_ap, src_r = src
    dst_ap, dst_r = dst
    xg_ap, xg_r = xg_s
    o_ap, o_r = o_s
    NTT = S // 128
    FC = 7
    NCH = 4
    with ExitStack() as es:
        ident = es.enter_context(nc.sbuf_tensor(tag + "_ident", [128, 128], BF16))
        ident_r = Res("ident")
        kb.dma("sp", ident[:], ident_d, [], [ident_r], ident_r)
        p1i = es.enter_context(nc.sbuf_tensor(tag + "_p1i", [128, NTT], I32))
        p2i = es.enter_context(nc.sbuf_tensor(tag + "_p2i", [128, NTT], I32))
        g1 = es.enter_context(nc.sbuf_tensor(tag + "_g1", [128, NTT], F32))
        g2 = es.enter_context(nc.sbuf_tensor(tag + "_g2", [128, NTT], F32))
        p1i_r, p2i_r, g1_r, g2_r = Res("p1i"), Res("p2i"), Res("g1"), Res("g2")
        idxg = es.enter_context(nc.sbuf_tensor(tag + "_idxg", [128, NT_SLOT, 32], I32))
        idxd = es.enter_context(nc.sbuf_tensor(tag + "_idxd", [128, NT_SLOT, 28], I32))
        idxg_r, idxd_r = Res("idxg"), Res("idxd")

        with ExitStack() as es0:
            def T(name, shape, dtype=F32):
                return es0.enter_context(nc.sbuf_tensor(tag + "_" + name, shape, dtype)), Res(name)

            fr = Front(kb, es0, ident, ident_r, norm_g, tag + "f", with_T=False)
            identf, identf_r = T("identf", [128, 128])
            kb.dma("sp", identf[:], identf_d, [], [identf_r], identf_r)
            wr, wr_r = T("wr", [128, KT, 8])
            kb.dma("sp", wr[:], router.rearrange("(kt p) e -> p kt e", p=128), [], [wr_r], wr_r)
            hn16, hn16_r = T("hn16", [128, NTT, D], BF16)
            x4 = [es0.enter_context(nc.sbuf_tensor(tag + "_x4%d" % i, [128, 4, D], F32)) for i in range(2)]
            x4_r = [Res("x4%d" % i) for i in range(2)]
            hn32 = [es0.enter_context(nc.sbuf_tensor(tag + "_hn32%d" % i, [128, D], F32)) for i in range(2)]
            hn32_r = [Res("hn32%d" % i) for i in range(2)]
            hT32 = [es0.enter_context(nc.sbuf_tensor(tag + "_hT32%d" % i, [128, KT, 128], F32)) for i in range(2)]
            hT32_r = [Res("hT32%d" % i) for i in range(2)]
            L, L_r = T("L", [128, NTT, 8])
            psT = [es0.enter_context(nc.psum_tensor(tag + "_psT%d" % i, [128, D], F32)) for i in range(2)]
            psT_r = [Res("psT%d" % i) for i in range(2)]
            psL = [es0.enter_context(nc.psum_tensor(tag + "_psL%d" % i, [128, 512], F32)) for i in range(2)]
            psL_r = [Res("psL%d" % i) for i in range(2)]
            zero, zero_r = T("zero", [128, 4, D], BF16)
            kb.op("pool", lambda e: e.memset(zero[:], 0.0), writes=[zero_r])
            n = 0
            for G in range(S // 512):
                b = G % 2
                kb.dma("sp", x4[b][:], src_ap[G * 512:(G + 1) * 512, :].rearrange("(j p) d -> p j d", p=128),
                       [src_r], [x4_r[b]], x4_r[b])
                for t in range(G * 3, G * 3 + 3):
                    kb.dma("sp", xg_ap[t * BS:(t + 1) * BS, :].rearrange("(j p) d -> p j d", p=128), zero[:],
                           [zero_r], [xg_r], zero_r, store=True)
                fr.rstd(x4[b][:], x4_r[b], 4)
                for j in range(4):
                    tt = G * 4 + j
                    hb = n % 2
                    n += 1
                    fr.norm_to(hn32[hb][:], hn32_r[hb], x4[b][:, j, :], x4_r[b], j)
                    kb.op("act", lambda e: e.activation(out=hn16[:, tt, :], in_=hn32[hb][:], func=AF.Copy),
                          reads=[hn32_r[hb]], writes=[hn16_r])
                    for kt in range(KT):
                        kb.op("pe", lambda e: e.transpose(out=psT[hb][:, kt * 128:(kt + 1) * 128],
                                                          in_=hn32[hb][:, kt * 128:(kt + 1) * 128], identity=identf[:]),
                              reads=[hn32_r[hb], identf_r], writes=[psT_r[hb]], signal=(kt == KT - 1))
                    kb.op("act", lambda e: e.activation(out=hT32[hb][:], in_=psT[hb][:].rearrange("p (k t) -> p k t", k=KT),
                                                        func=AF.Copy), reads=[psT_r[hb]], writes=[hT32_r[hb]])
                    for kt in range(KT):
                        kb.op("pe", lambda e: e.matmul(out=psL[hb][:, 0:8], lhsT=hT32[hb][:, kt, :], rhs=wr[:, kt, :],
                                                       start=(kt == 0), stop=(kt == KT - 1)),
                              reads=[hT32_r[hb], wr_r], writes=[psL_r[hb]], signal=(kt == KT - 1))
                    kb.op("dve", lambda e: e.tensor_copy(out=L[:, tt, :], in_=psL[hb][:, 0:8]),
                          reads=[psL_r[hb]], writes=[L_r])
            m1, m1_r = T("m1", [128, NTT])
            m2, m2_r = T("m2", [128, NTT])
            eq, eq_r = T("eq", [128, NTT, 8])
            L2, L2_r = T("L2", [128, NTT, 8])
            mask, mask_r = T("mask", [128, NTT, 8])
            ex, ex_r = T("ex", [128, NTT, 8])
            den, den_r = T("den", [128, NTT])
            gts, gts_r = T("gts", [128, NTT, 8])
            kb.op("dve", lambda e: e.tensor_reduce(out=m1[:], in_=L[:], axis=AX.X, op=ALU.max), reads=[L_r], writes=[m1_r])
            kb.op("dve", lambda e: e.tensor_tensor(out=eq[:], in0=L[:], in1=bc_last(m1[:], 8), op=ALU.is_equal),
                  reads=[L_r, m1_r], writes=[eq_r])
            kb.op("dve", lambda e: e.scalar_tensor_tensor(out=L2[:], in0=eq[:], scalar=-1.0e30, in1=L[:],
                                                          op0=ALU.mult, op1=ALU.add), reads=[eq_r, L_r], writes=[L2_r])
            kb.op("dve", lambda e: e.tensor_reduce(out=m2[:], in_=L2[:], axis=AX.X, op=ALU.max), reads=[L2_r], writes=[m2_r])
            kb.op("dve", lambda e: e.tensor_tensor(out=mask[:], in0=L[:], in1=bc_last(m2[:], 8), op=ALU.is_ge),
                  reads=[L_r, m2_r], writes=[mask_r])
            kb.op("dve", lambda e: e.tensor_tensor(out=ex[:], in0=L[:], in1=bc_last(m1[:], 8), op=ALU.subtract),
                  reads=[L_r, m1_r], writes=[ex_r])
            kb.op("act", lambda e: e.activation(out=ex[:], in_=ex[:], func=AF.Exp), reads=[ex_r], writes=[ex_r])
            kb.op("dve", lambda e: e.tensor_tensor(out=ex[:], in0=ex[:], in1=mask[:], op=ALU.mult),
                  reads=[ex_r, mask_r], writes=[ex_r])
            kb.op("dve", lambda e: e.tensor_reduce(out=den[:], in_=ex[:], axis=AX.X, op=ALU.add), reads=[ex_r], writes=[den_r])
            kb.op("dve", lambda e: e.reciprocal(out=den[:], in_=den[:]), reads=[den_r], writes=[den_r])
            kb.op("dve", lambda e: e.tensor_tensor(out=gts[:], in0=ex[:], in1=bc_last(den[:], 8), op=ALU.mult),
                  reads=[ex_r, den_r], writes=[gts_r])
            maskb, maskb_r = T("maskb", [128, NTT * 8], BF16)
            kb.op("dve", lambda e: e.tensor_copy(out=maskb[:], in_=mask[:].rearrange("p t e -> p (t e)")),
                  reads=[mask_r], writes=[maskb_r])
            ii, ii_r = T("ii", [128, 128], I32)
            dmf, dmf_r = T("dmf", [128, 128])
            Lt, Lt_r = T("Lt", [128, 128], BF16)
            ones, ones_r = T("ones", [128, 128], BF16)
            kb.op("pool", lambda e: e.iota(ii[:], pattern=[[1, 128]], base=0, channel_multiplier=-1), writes=[ii_r])
            kb.op("dve", lambda e: e.tensor_copy(out=dmf[:], in_=ii[:]), reads=[ii_r], writes=[dmf_r])
            kb.op("dve", lambda e: e.tensor_scalar(out=Lt[:], in0=dmf[:], scalar1=0.0, scalar2=None, op0=ALU.is_gt),
                  reads=[dmf_r], writes=[Lt_r])
            kb.op("pool", lambda e: e.memset(ones[:], 1.0), writes=[ones_r])
            kb.op("pe", lambda e: e.matmul(out=psL[0][:, 0:256], lhsT=Lt[:], rhs=maskb[:], start=True, stop=True),
                  reads=[Lt_r, maskb_r], writes=[psL_r[0]])
            kb.op("pe", lambda e: e.matmul(out=psL[1][:, 0:256], lhsT=ones[:], rhs=maskb[:], start=True, stop=True),
                  reads=[ones_r, maskb_r], writes=[psL_r[1]])
            within, within_r = T("within", [128, NTT, 8])
            tot, tot_r = T("tot", [128, NTT, 8])
            off, off_r = T("off", [128, NTT, 8])
            kb.op("dve", lambda e: e.tensor_copy(out=within[:].rearrange("p t e -> p (t e)"), in_=psL[0][:, 0:256]),
                  reads=[psL_r[0]], writes=[within_r])
            kb.op("dve", lambda e: e.tensor_copy(out=tot[:].rearrange("p t e -> p (t e)"), in_=psL[1][:, 0:256]),
                  reads=[psL_r[1]], writes=[tot_r])
            kb.op("dve", lambda e: e.memset(off[:, 0, :], 0.0), writes=[off_r])
            for t in range(1, NTT):
                kb.op("dve", lambda e: e.tensor_tensor(out=off[:, t, :], in0=off[:, t - 1, :], in1=tot[:, t - 1, :], op=ALU.add),
                      reads=[off_r, tot_r], writes=[off_r])
            cntp, cntp_r = T("cntp", [128, 8])
            pc, pc_r = T("pc", [128, 8])
            poff, poff_r = T("poff", [128, 8])
            pend, pend_r = T("pend", [128, 8])
            kb.op("dve", lambda e: e.tensor_tensor(out=cntp[:], in0=off[:, NTT - 1, :], in1=tot[:, NTT - 1, :], op=ALU.add),
                  reads=[off_r, tot_r], writes=[cntp_r])
            kb.op("dve", lambda e: e.memset(pc[:], 0.0), writes=[pc_r])
            for k_ in range(8):
                kb.op("dve", lambda e: e.scalar_tensor_tensor(out=pc[:], in0=cntp[:], scalar=float(BS * k_), in1=pc[:],
                                                              op0=ALU.is_gt, op1=ALU.add),
                      reads=[cntp_r, pc_r], writes=[pc_r])
            kb.op("dve", lambda e: e.tensor_scalar(out=pc[:], in0=pc[:], scalar1=float(BS), scalar2=None, op0=ALU.mult),
                  reads=[pc_r], writes=[pc_r])
            kb.op("dve", lambda e: e.memset(poff[:, 0:1], 0.0), writes=[poff_r])
            for e_ in range(1, 8):
                kb.op("dve", lambda e: e.tensor_tensor(out=poff[:, e_:e_ + 1], in0=poff[:, e_ - 1:e_], in1=pc[:, e_ - 1:e_],
                                                       op=ALU.add), reads=[poff_r, pc_r], writes=[poff_r])
            kb.op("dve", lambda e: e.tensor_tensor(out=pend[:], in0=poff[:], in1=pc[:], op=ALU.add),
                  reads=[poff_r, pc_r], writes=[pend_r])
            ms, ms_r = T("ms", [128, NTT, 8])
            kb.op("dve", lambda e: e.tensor_tensor(out=ms[:], in0=within[:], in1=off[:], op=ALU.add),
                  reads=[within_r, off_r], writes=[ms_r])
            kb.op("dve", lambda e: e.scalar_tensor_tensor(out=ms[:], in0=ms[:], scalar=1.0, in1=bc_mid(poff[:], NTT),
                                                          op0=ALU.add, op1=ALU.add), reads=[ms_r, poff_r], writes=[ms_r])
            kb.op("dve", lambda e: e.tensor_tensor(out=ms[:], in0=ms[:], in1=mask[:], op=ALU.mult),
                  reads=[ms_r, mask_r], writes=[ms_r])
            pa, pa_r = T("pa", [128, NTT])
            pb_, pb_r = T("pb", [128, NTT])
            kb.op("dve", lambda e: e.tensor_reduce(out=pa[:], in_=ms[:], axis=AX.X, op=ALU.max), reads=[ms_r], writes=[pa_r])
            kb.op("dve", lambda e: e.tensor_reduce(out=pb_[:], in_=ms[:], axis=AX.X, op=ALU.add), reads=[ms_r], writes=[pb_r])
            kb.op("dve", lambda e: e.tensor_tensor(out=pb_[:], in0=pb_[:], in1=pa[:], op=ALU.subtract),
                  reads=[pb_r, pa_r], writes=[pb_r])
            kb.op("dve", lambda e: e.tensor_tensor(out=eq[:], in0=ms[:], in1=bc_last(pa[:], 8), op=ALU.is_equal),
                  reads=[ms_r, pa_r], writes=[eq_r])
            kb.op("dve", lambda e: e.tensor_tensor(out=eq[:], in0=eq[:], in1=gts[:], op=ALU.mult),
                  reads=[eq_r, gts_r], writes=[eq_r])
            kb.op("dve", lambda e: e.tensor_reduce(out=g1[:], in_=eq[:], axis=AX.X, op=ALU.add), reads=[eq_r], writes=[g1_r])
            kb.op("dve", lambda e: e.tensor_scalar(out=g2[:], in0=g1[:], scalar1=-1.0, scalar2=1.0, op0=ALU.mult, op1=ALU.add),
                  reads=[g1_r], writes=[g2_r])
            kb.op("dve", lambda e: e.tensor_scalar(out=pa[:], in0=pa[:], scalar1=-1.0, scalar2=None, op0=ALU.add),
                  reads=[pa_r], writes=[pa_r])
            kb.op("dve", lambda e: e.tensor_scalar(out=pb_[:], in0=pb_[:], scalar1=-1.0, scalar2=None, op0=ALU.add),
                  reads=[pb_r], writes=[pb_r])
            kb.op("dve", lambda e: e.tensor_copy(out=p1i[:], in_=pa[:]), reads=[pa_r], writes=[p1i_r])
            kb.op("dve", lambda e: e.tensor_copy(out=p2i[:], in_=pb_[:]), reads=[pb_r], writes=[p2i_r])
            cmp, cmp_r = T("cmp", [128, NT_SLOT, 8])
            et, et_r = T("et", [128, NT_SLOT])
            for t in range(NT_SLOT):
                kb.op("dve", lambda e: e.tensor_scalar(out=cmp[:, t, :], in0=pend[:], scalar1=float(BS * t), scalar2=None,
                                                       op0=ALU.is_le), reads=[pend_r], writes=[cmp_r])
            kb.op("dve", lambda e: e.tensor_reduce(out=et[:], in_=cmp[:], axis=AX.X, op=ALU.add), reads=[cmp_r], writes=[et_r])
            kb.op("dve", lambda e: e.tensor_scalar(out=et[:], in0=et[:], scalar1=7.0, scalar2=None, op0=ALU.min),
                  reads=[et_r], writes=[et_r])
            sgi, sgi_r = T("sgi", [128, 32], I32)
            sdi, sdi_r = T("sdi", [128, 28], I32)
            sgf, sgf_r = T("sgf", [128, 32])
            sdf, sdf_r = T("sdf", [128, 28])
            kb.op("pool", lambda e: e.iota(sgi[:], pattern=[[512, 8], [1, 4]], base=0, channel_multiplier=4), writes=[sgi_r])
            kb.op("pool", lambda e: e.iota(sdi[:], pattern=[[896, 4], [128, 7]], base=0, channel_multiplier=1), writes=[sdi_r])
            kb.op("dve", lambda e: e.tensor_copy(out=sgf[:], in_=sgi[:]), reads=[sgi_r], writes=[sgf_r])
            kb.op("dve", lambda e: e.tensor_copy(out=sdf[:], in_=sdi[:]), reads=[sdi_r], writes=[sdf_r])
            igf, igf_r = T("igf", [128, NT_SLOT, 32])
            idf, idf_r = T("idf", [128, NT_SLOT, 28])
            etg, etg_r = T("etg", [128, NT_SLOT])
            etd, etd_r = T("etd", [128, NT_SLOT])
            kb.op("dve", lambda e: e.tensor_scalar(out=etg[:], in0=et[:], scalar1=4096.0, scalar2=None, op0=ALU.mult),
                  reads=[et_r], writes=[etg_r])
            kb.op("dve", lambda e: e.tensor_scalar(out=etd[:], in0=et[:], scalar1=3584.0, scalar2=None, op0=ALU.mult),
                  reads=[et_r], writes=[etd_r])
            for t in range(NT_SLOT):
                kb.op("dve", lambda e: e.tensor_scalar(out=igf[:, t, :], in0=sgf[:], scalar1=etg[:, t:t + 1], scalar2=None,
                                                       op0=ALU.add), reads=[etg_r, sgf_r], writes=[igf_r])
                kb.op("dve", lambda e: e.tensor_scalar(out=idf[:, t, :], in0=sdf[:], scalar1=etd[:, t:t + 1], scalar2=None,
                                                       op0=ALU.add), reads=[etd_r, sdf_r], writes=[idf_r])
            kb.op("dve", lambda e: e.tensor_copy(out=idxg[:], in_=igf[:]), reads=[igf_r], writes=[idxg_r])
            kb.op("dve", lambda e: e.tensor_copy(out=idxd[:], in_=idf[:]), reads=[idf_r], writes=[idxd_r])
            kb._wait("pool", kb._deps([xg_r], []))
            for tt in range(NTT):
                for (pi, pi_r) in ((p1i, p1i_r), (p2i, p2i_r)):
                    kb._wait("pool", kb._deps([hn16_r, pi_r], []))
                    if hn16_r.ssem is None:
                        hn16_r.ssem = kb.get_sem(fresh=True)
                    sem = hn16_r.ssem
                    ins = nc.gpsimd.indirect_dma_start(out=xg_ap, out_offset=bass.IndirectOffsetOnAxis(ap=pi[:, tt:tt + 1], axis=0),
                                                       in_=hn16[:, tt, :], in_offset=None)
                    ins.then_inc(sem, 16)
                    kb.cnt[sem] += 16
                    kb._register(sem, kb.cnt[sem], [hn16_r, pi_r], [xg_r])
            kb.barrier()
        with ExitStack() as es1:
            xgt = [es1.enter_context(nc.sbuf_tensor(tag + "_xgt%d" % i, [128, 4, D], BF16)) for i in range(2)]
            xgt_r = [Res("xgt%d" % i) for i in range(2)]
            xT = [es1.enter_context(nc.sbuf_tensor(tag + "_xT%d" % i, [128, KT, BS], BF16)) for i in range(2)]
            xT_r = [Res("xT%d" % i) for i in range(2)]
            wg = [es1.enter_context(nc.sbuf_tensor(tag + "_wg%d" % i, [128, KT, FC * 128], BF16)) for i in range(2)]
            wu = [es1.enter_context(nc.sbuf_tensor(tag + "_wu%d" % i, [128, KT, FC * 128], BF16)) for i in range(2)]
            wd = [es1.enter_context(nc.sbuf_tensor(tag + "_wd%d" % i, [128, FC, D], BF16)) for i in range(2)]
            wg_r = [Res("wg%d" % i) for i in range(2)]
            wu_r = [Res("wu%d" % i) for i in range(2)]
            wd_r = [Res("wd%d" % i) for i in range(2)]
            actT = [es1.enter_context(nc.sbuf_tensor(tag + "_actT%d" % i, [128, FC, BS], BF16)) for i in range(2)]
            actT_r = [Res("actT%d" % i) for i in range(2)]
            sg = [es1.enter_context(nc.sbuf_tensor(tag + "_sg%d" % i, [128, 512], F32)) for i in range(2)]
            sg_r = [Res("sg%d" % i) for i in range(2)]
            oacc = [es1.enter_context(nc.sbuf_tensor(tag + "_oacc%d" % i, [128, 4, D], F32)) for i in range(2)]
            oacc_r = [Res("oacc%d" % i) for i in range(2)]
            psT = [es1.enter_context(nc.psum_tensor(tag + "_psX%d" % i, [128, D], BF16)) for i in range(2)]
            psT_r = [Res("psX%d" % i) for i in range(2)]
            psG = [es1.enter_context(nc.psum_tensor(tag + "_psG%d" % i, [128, 512], F32)) for i in range(2)]
            psU = [es1.enter_context(nc.psum_tensor(tag + "_psU%d" % i, [128, 512], F32)) for i in range(2)]
            psO = [es1.enter_context(nc.psum_tensor(tag + "_psO%d" % i, [128, 512], F32)) for i in range(2)]
            psG_r = [Res("psG%d" % i) for i in range(2)]
            psU_r = [Res("psU%d" % i) for i in range(2)]
            psO_r = [Res("psO%d" % i) for i in range(2)]
            wg_flat = mwg.rearrange("e d (c f) -> (e d c) f", f=FC * 128)
            wu_flat = mwu.rearrange("e d (c f) -> (e d c) f", f=FC * 128)
            wd_flat = mwd.rearrange("e f d -> (e f) d")

            def gather(dst_ap_, dst_r_, src_flat, idx_ap, idx_r):
                kb._wait("pool", kb._deps([idx_r], []))
                if dst_r_.lsem is None:
                    dst_r_.lsem = kb.get_sem(fresh=True)
                sem = dst_r_.lsem
                ins = nc.gpsimd.indirect_dma_start(out=dst_ap_, out_offset=None, in_=src_flat,
                                                   in_offset=bass.IndirectOffsetOnAxis(ap=idx_ap, axis=0))
                ins.then_inc(sem, 16)
                kb.cnt[sem] += 16
                dst_r_.writes[sem] = kb.cnt[sem]
                idx_r.reads[sem] = kb.cnt[sem]

            wseq = [(t, c) for t in range(NT_SLOT) for c in range(NCH)]

            def load_w(i):
                t, c = wseq[i]
                b = i % 2
                for r_ in (wg_r[b], wu_r[b], wd_r[b]):
                    kb._wait("pool", kb._deps([], [r_]))
                    r_.writes = {}
                    r_.reads = {}
                for kt in range(KT):
                    gather(wg[b][:, kt, :], wg_r[b], wg_flat, idxg[:, t, kt * 4 + c:kt * 4 + c + 1], idxg_r)
                    gather(wu[b][:, kt, :], wu_r[b], wu_flat, idxg[:, t, kt * 4 + c:kt * 4 + c + 1], idxg_r)
                for fl in range(FC):
                    gather(wd[b][:, fl, :], wd_r[b], wd_flat, idxd[:, t, c * 7 + fl:c * 7 + fl + 1], idxd_r)

            def load_x(t):
                b = t % 2
                kb.dma("sp", xgt[b][:], xg_ap[t * BS:(t + 1) * BS, :].rearrange("(j p) d -> p j d", p=128),
                       [xg_r], [xgt_r[b]], xgt_r[b])

            load_x(0)
            load_w(0)
            wi = 0
            gcount = 0
            ocount = 0
            tcount = 0
            for t in range(NT_SLOT):
                xb = t % 2
                if t + 1 < NT_SLOT:
                    load_x(t + 1)
                for j in range(4):
                    tb = tcount % 2
                    tcount += 1
                    for kt in range(KT):
                        kb.op("pe", lambda e: e.transpose(out=psT[tb][:, kt * 128:(kt + 1) * 128],
                                                          in_=xgt[xb][:, j, kt * 128:(kt + 1) * 128], identity=ident[:]),
                              reads=[xgt_r[xb], ident_r], writes=[psT_r[tb]], signal=(kt == KT - 1))
                    kb.op("act", lambda e: e.activation(out=xT[xb][:, :, j * 128:(j + 1) * 128],
                                                        in_=psT[tb][:].rearrange("p (k t) -> p k t", k=KT), func=AF.Copy),
                          reads=[psT_r[tb]], writes=[xT_r[xb]])
                ob = t % 2
                for c in range(NCH):
                    b = wi % 2
                    if wi + 1 < len(wseq):
                        load_w(wi + 1)
                    ab = wi % 2
                    for fl in range(FC):
                        gb = gcount % 2
                        gcount += 1
                        for (wt, wt_r, pst, pst_r) in ((wg[b], wg_r[b], psG[gb], psG_r[gb]), (wu[b], wu_r[b], psU[gb], psU_r[gb])):
                            for kt in range(KT):
                                kb.op("pe", lambda e: e.matmul(out=pst[:], lhsT=wt[:, kt, fl * 128:(fl + 1) * 128],
                                                               rhs=xT[xb][:, kt, :], start=(kt == 0), stop=(kt == KT - 1)),
                                      reads=[wt_r, xT_r[xb]], writes=[pst_r], signal=(kt == KT - 1))
                        kb.op("act", lambda e: e.activation(out=sg[gb][:], in_=psG[gb][:], func=AF.Silu),
                              reads=[psG_r[gb]], writes=[sg_r[gb]])
                        kb.op("dve", lambda e: e.tensor_tensor(out=actT[ab][:, fl, :], in0=sg[gb][:], in1=psU[gb][:], op=ALU.mult),
                              reads=[sg_r[gb], psU_r[gb]], writes=[actT_r[ab]])
                    for j in range(4):
                        for dh in range(2):
                            pb2 = ocount % 2
                            ocount += 1
                            for fl in range(FC):
                                kb.op("pe", lambda e: e.matmul(out=psO[pb2][:], lhsT=actT[ab][:, fl, j * 128:(j + 1) * 128],
                                                               rhs=wd[b][:, fl, dh * 512:(dh + 1) * 512],
                                                               start=(fl == 0), stop=(fl == FC - 1)),
                                      reads=[actT_r[ab], wd_r[b]], writes=[psO_r[pb2]], signal=(fl == FC - 1))
                            if c == 0:
                                kb.op("act", lambda e: e.activation(out=oacc[ob][:, j, dh * 512:(dh + 1) * 512], in_=psO[pb2][:],
                                                                    func=AF.Copy), reads=[psO_r[pb2]], writes=[oacc_r[ob]])
                            else:
                                kb.op("dve", lambda e: e.tensor_tensor(out=oacc[ob][:, j, dh * 512:(dh + 1) * 512], in0=psO[pb2][:],
                                                                       in1=oacc[ob][:, j, dh * 512:(dh + 1) * 512], op=ALU.add),
                                      reads=[psO_r[pb2], oacc_r[ob]], writes=[oacc_r[ob]])
                    wi += 1
                kb.dma("sp", o_ap[t * BS:(t + 1) * BS, :].rearrange("(j p) d -> p j d", p=128), oacc[ob][:],
                       [oacc_r[ob]], [o_r], oacc_r[ob], store=True)
            kb.barrier()
        with ExitStack() as es2:
            fr2 = Front(kb, es2, ident, ident_r, final_g, tag + "g", with_T=False)
            x4 = [es2.enter_context(nc.sbuf_tensor(tag + "_y4%d" % i, [128, 4, D], F32)) for i in range(3)]
            x4_r = [Res("y4%d" % i) for i in range(3)]
            ga = [es2.enter_context(nc.sbuf_tensor(tag + "_ga%d" % i, [128, D], F32)) for i in range(8)]
            ga_r = [Res("ga%d" % i) for i in range(8)]
            n = 0

            def loadF(G):
                kb.dma("sp", x4[G % 3][:], src_ap[G * 512:(G + 1) * 512, :].rearrange("(j p) d -> p j d", p=128),
                       [src_r], [x4_r[G % 3]], x4_r[G % 3])

            loadF(0)
            loadF(1)
            for G in range(S // 512):
                b = G % 3
                if G + 2 < S // 512:
                    loadF(G + 2)
                for j in range(4):
                    tt = G * 4 + j
                    for (pi, pi_r, gg, gg_r) in ((p1i, p1i_r, g1, g1_r), (p2i, p2i_r, g2, g2_r)):
                        gb = n % 8
                        n += 1
                        kb._wait("pool", kb._deps([o_r, pi_r], [ga_r[gb]]))
                        if ga_r[gb].lsem is None:
                            ga_r[gb].lsem = kb.get_sem(fresh=True)
                        sem = ga_r[gb].lsem
                        ins = nc.gpsimd.indirect_dma_start(out=ga[gb][:], out_offset=None, in_=o_ap,
                                                           in_offset=bass.IndirectOffsetOnAxis(ap=pi[:, tt:tt + 1], axis=0))
                        ins.then_inc(sem, 16)
                        kb.cnt[sem] += 16
                        kb._register(sem, kb.cnt[sem], [o_r, pi_r], [ga_r[gb]])
                        kb.op("dve", lambda e: e.scalar_tensor_tensor(out=x4[b][:, j, :], in0=ga[gb][:], scalar=gg[:, tt:tt + 1],
                                                                      in1=x4[b][:, j, :], op0=ALU.mult, op1=ALU.add),
                              reads=[ga_r[gb], gg_r, x4_r[b]], writes=[x4_r[b]])
                fr2.rstd(x4[b][:], x4_r[b], 4)
                for j in range(4):
                    fr2.norm_to(x4[b][:, j, :], x4_r[b], x4[b][:, j, :], x4_r[b], j)
                kb.dma("sp", dst_ap[G * 512:(G + 1) * 512, :].rearrange("(j p) d -> p j d", p=128), x4[b][:],
                       [x4_r[b]], [dst_r], x4_r[b], store=True)
            kb.barrier()
```

```python
import numpy as np
import ml_dtypes
from contextlib import ExitStack
import concourse.bass as bass
import concourse.mybir as mybir
from concourse.bass_utils import run_bass_kernel_spmd

F32 = mybir.dt.float32
BF16 = mybir.dt.bfloat16
ALU = mybir.AluOpType
AF = mybir.ActivationFunctionType
AX = mybir.AxisListType

D = 1024
S = 4096
NCORES = 8
EPS = 1e-6
KT = D // 128


class Res:
    __slots__ = ("name", "writes", "reads", "accum", "lsem", "ssem")

    def __init__(self, name, accum=False):
        self.name = name
        self.writes = {}
        self.reads = {}
        self.accum = accum
        self.lsem = None
        self.ssem = None


class KB:
    def __init__(self, nc, es):
        self.nc = nc
        self.es = es
        self.engs = {"pe": nc.tensor, "act": nc.scalar, "dve": nc.vector, "pool": nc.gpsimd, "sp": nc.sync}
        self.esem = {}
        self.cnt = {}
        self.waited = {n: {} for n in self.engs}
        self.free_sems = []
        self.phase_sems = []
        self.nsem = 0
        for n in self.engs:
            s = es.enter_context(nc.semaphore("es_" + n))
            self.esem[n] = s
            self.cnt[s] = 0

    def get_sem(self, fresh=False):
        if self.free_sems and not fresh:
            s = self.free_sems.pop()
        else:
            s = self.es.enter_context(self.nc.semaphore("ds%d" % self.nsem))
            self.nsem += 1
            self.cnt[s] = 0
        if not fresh:
            self.phase_sems.append(s)
        return s

    def _wait(self, eng, evs):
        w = self.waited[eng]
        e = self.engs[eng]
        for sem, val in evs.items():
            if w.get(sem, 0) < val:
                e.wait_ge(sem, val)
                w[sem] = val

    @staticmethod
    def _deps(reads, writes):
        evs = {}
        for r in reads:
            for s, v in r.writes.items():
                if evs.get(s, 0) < v:
                    evs[s] = v
        for wr in writes:
            if not wr.accum:
                for s, v in wr.writes.items():
                    if evs.get(s, 0) < v:
                        evs[s] = v
            for s, v in wr.reads.items():
                if evs.get(s, 0) < v:
                    evs[s] = v
        return evs

    @staticmethod
    def _register(sem, v, reads, writes):
        for r in reads:
            r.reads[sem] = v
        for w in writes:
            if w.accum:
                w.writes[sem] = v
            else:
                w.writes = {sem: v}
                w.reads = {}

    def op(self, eng, fn, reads=(), writes=(), signal=True):
        self._wait(eng, self._deps(reads, writes))
        ins = fn(self.engs[eng])
        if signal:
            sem = self.esem[eng]
            self.cnt[sem] += 1
            ins.then_inc(sem, 1)
            self._register(sem, self.cnt[sem], reads, writes)
        return ins

    def dma(self, q, out, in_, reads, writes, own, store=False):
        self._wait(q, self._deps(reads, writes))
        if store:
            if own.ssem is None:
                own.ssem = self.get_sem(fresh=(q == "pool"))
            sem = own.ssem
        else:
            if own.lsem is None:
                own.lsem = self.get_sem(fresh=(q == "pool"))
            sem = own.lsem
        ins = self.engs[q].dma_start(out=out, in_=in_)
        ins.then_inc(sem, 16)
        self.cnt[sem] += 16
        self._register(sem, self.cnt[sem], reads, writes)
        return ins

    def barrier(self, recycle=True):
        allev = {s: c for s, c in self.cnt.items() if c > 0}
        for n in self.engs:
            self._wait(n, allev)
        if recycle:
            self.free_sems.extend(self.phase_sems)
            self.phase_sems = []

    def final_wait(self, eng="sp"):
        allev = {s: c for s, c in self.cnt.items() if c > 0}
        self._wait(eng, allev)


def bcast_row(ap1d, n):
    return ap1d.rearrange("(o n) -> o n", o=1).partition_broadcast(128)


class Front:
    def __init__(self, kb, es, ident, ident_r, gvec_ap, tag, with_T=True):
        nc = kb.nc
        self.kb = kb
        self.ident = ident
        self.ident_r = ident_r
        self.g = es.enter_context(nc.sbuf_tensor(tag + "_g", [128, D], F32))
        self.g_r = Res(tag + "_g")
        kb.dma("sp", self.g[:], bcast_row(gvec_ap, D), [], [self.g_r], self.g_r)
        self.junk = es.enter_context(nc.sbuf_tensor(tag + "_junk", [128, D], BF16))
        self.junk_r = Res(tag + "_junk")
        self.NR = 4
        self.ss_l = [es.enter_context(nc.sbuf_tensor(tag + "_ss%d" % i, [128, 8], F32)) for i in range(self.NR)]
        self.ss_rl = [Res(tag + "_ss%d" % i) for i in range(self.NR)]
        self.rs_l = [es.enter_context(nc.sbuf_tensor(tag + "_rs%d" % i, [128, 8], F32)) for i in range(self.NR)]
        self.rs_rl = [Res(tag + "_rs%d" % i) for i in range(self.NR)]
        self.cur = 0
        self.n = 0
        if not with_T:
            return
        self.hn = [es.enter_context(nc.sbuf_tensor(tag + "_hn%d" % i, [128, D], BF16)) for i in range(2)]
        self.hn_r = [Res(tag + "_hn%d" % i) for i in range(2)]
        self.ps = [es.enter_context(nc.psum_tensor(tag + "_pst%d" % i, [128, D], BF16)) for i in range(2)]
        self.ps_r = [Res(tag + "_pst%d" % i) for i in range(2)]

    @property
    def ss(self):
        return self.ss_l[self.cur]

    @property
    def ss_r(self):
        return self.ss_rl[self.cur]

    @property
    def rs(self):
        return self.rs_l[self.cur]

    @property
    def rs_r(self):
        return self.rs_rl[self.cur]

    def rstd(self, x3, x_r, nj):
        kb = self.kb
        self.cur = (self.cur + 1) % self.NR
        for j in range(nj):
            kb.op("act", lambda e, j=j: e.activation(out=self.junk[:], in_=x3[:, j, :], func=AF.Square,
                                                     accum_out=self.ss[:, j:j + 1]),
                  reads=[x_r], writes=[self.junk_r, self.ss_r])
        kb.op("dve", lambda e: e.tensor_scalar(out=self.rs[:, 0:nj], in0=self.ss[:, 0:nj], scalar1=1.0 / D,
                                                scalar2=EPS, op0=ALU.mult, op1=ALU.add),
              reads=[self.ss_r], writes=[self.rs_r])
        kb.op("act", lambda e: e.activation(out=self.rs[:, 0:nj], in_=self.rs[:, 0:nj], func=AF.Sqrt),
              reads=[self.rs_r], writes=[self.rs_r])
        kb.op("dve", lambda e: e.reciprocal(out=self.rs[:, 0:nj], in_=self.rs[:, 0:nj]),
              reads=[self.rs_r], writes=[self.rs_r])

    def norm_to(self, out_ap, out_r, x2, x_r, j):
        self.kb.op("dve", lambda e: e.scalar_tensor_tensor(out=out_ap, in0=x2, scalar=self.rs[:, j:j + 1],
                                                            in1=self.g[:], op0=ALU.mult, op1=ALU.mult),
                   reads=[x_r, self.rs_r, self.g_r], writes=[out_r])

    def norm_T(self, x3, x_r, nj, xT, xT_r, tok0):
        kb = self.kb
        self.rstd(x3, x_r, nj)
        for j in range(nj):
            b = self.n % 2
            self.n += 1
            hn, hn_r, ps, ps_r = self.hn[b], self.hn_r[b], self.ps[b], self.ps_r[b]
            self.norm_to(hn[:], hn_r, x3[:, j, :], x_r, j)
            for kt in range(KT):
                last = kt == KT - 1
                kb.op("pe", lambda e, kt=kt: e.transpose(out=ps[:, kt * 128:(kt + 1) * 128],
                                                         in_=hn[:, kt * 128:(kt + 1) * 128], identity=self.ident[:]),
                      reads=[hn_r, self.ident_r], writes=[ps_r], signal=last)
            t0 = tok0 + j * 128
            kb.op("act", lambda e, t0=t0: e.activation(out=xT[:, :, t0:t0 + 128],
                                                       in_=ps[:].rearrange("p (k t) -> p k t", k=KT),
                                                       func=AF.Copy),
                  reads=[ps_r], writes=[xT_r])


def ffn_phase(kb, src, dst, norm_g, experts, ident_d, router=None, final_g=None, FC=7, tag="ffn", chunk_sizes=None, nbuf=1):
    nc = kb.nc
    src_ap, src_r = src
    dst_ap, dst_r = dst
    F = experts[0][0].shape[1]
    NFT = F // 128
    if chunk_sizes is None:
        assert NFT % FC == 0
        chunk_sizes = [FC] * (NFT // FC)
    assert sum(chunk_sizes) == NFT
    FC = max(chunk_sizes)
    NCH = len(chunk_sizes)
    choff = [sum(chunk_sizes[:i]) for i in range(NCH)]
    TT = 1024
    NJ = TT // 128
    NE = len(experts)
    with ExitStack() as es:
        ident = es.enter_context(nc.sbuf_tensor(tag + "_ident", [128, 128], BF16))
        ident_r = Res("ident")
        kb.dma("sp", ident[:], ident_d, [], [ident_r], ident_r)
        fr = Front(kb, es, ident, ident_r, norm_g, tag + "f")
        if final_g is not None:
            gf = es.enter_context(nc.sbuf_tensor(tag + "_gf", [128, D], F32))
            gf_r = Res("gf")
            kb.dma("sp", gf[:], bcast_row(final_g, D), [], [gf_r], gf_r)
        assert nbuf == 1 or router is None
        accs = [es.enter_context(nc.sbuf_tensor(tag + "_acc%d" % i, [128, NJ, D], F32)) for i in range(nbuf)]
        accs_r = [Res("acc%d" % i) for i in range(nbuf)]
        xTs = [es.enter_context(nc.sbuf_tensor(tag + "_xT%d" % i, [128, KT, TT], BF16)) for i in range(nbuf)]
        xTs_r = [Res("xT%d" % i) for i in range(nbuf)]
        actT = [es.enter_context(nc.sbuf_tensor(tag + "_actT%d" % i, [128, FC, TT], BF16)) for i in range(2)]
        actT_r = [Res("actT%d" % i) for i in range(2)]
        wg = [es.enter_context(nc.sbuf_tensor(tag + "_wg%d" % i, [128, KT, FC * 128], BF16)) for i in range(2)]
        wu = [es.enter_context(nc.sbuf_tensor(tag + "_wu%d" % i, [128, KT, FC * 128], BF16)) for i in range(2)]
        wd = [es.enter_context(nc.sbuf_tensor(tag + "_wd%d" % i, [128, FC, D], BF16)) for i in range(2)]
        wg_r = [Res("wg%d" % i) for i in range(2)]
        wu_r = [Res("wu%d" % i) for i in range(2)]
        wd_r = [Res("wd%d" % i) for i in range(2)]
        sg = [es.enter_context(nc.sbuf_tensor(tag + "_sg%d" % i, [128, 512], F32)) for i in range(2)]
        sg_r = [Res("sg%d" % i) for i in range(2)]
        psG = [es.enter_context(nc.psum_tensor(tag + "_psG%d" % i, [128, 512], F32)) for i in range(2)]
        psU = [es.enter_context(nc.psum_tensor(tag + "_psU%d" % i, [128, 512], F32)) for i in range(2)]
        psG_r = [Res("psG%d" % i) for i in range(2)]
        psU_r = [Res("psU%d" % i) for i in range(2)]
        psO = [es.enter_context(nc.psum_tensor(tag + "_psO%d" % i, [128, 512], F32)) for i in range(2)]
        psO_r = [Res("psO%d" % i) for i in range(2)]
        if router is not None:
            wr = es.enter_context(nc.sbuf_tensor(tag + "_wr", [128, KT, 8], BF16))
            wr_r = Res("wr")
            kb.dma("pool", wr[:], router.rearrange("(kt p) e -> p kt e", p=128), [], [wr_r], wr_r)
            gates = es.enter_context(nc.sbuf_tensor(tag + "_gates", [128, NJ, 8], F32))
            gates_r = Res("gates")
            lg = es.enter_context(nc.sbuf_tensor(tag + "_lg", [128, 8], F32))
            lg_r = Res("lg")
            mx = es.enter_context(nc.sbuf_tensor(tag + "_mx", [128, 8], F32))
            mx_r = Res("mx")
            msk = es.enter_context(nc.sbuf_tensor(tag + "_msk", [128, 8], F32))
            msk_r = Res("msk")
            ex = es.enter_context(nc.sbuf_tensor(tag + "_ex", [128, 8], F32))
            ex_r = Res("ex")
            sm = es.enter_context(nc.sbuf_tensor(tag + "_sm", [128, 2], F32))
            sm_r = Res("sm")

        wseq = [(T, e, ch) for T in range(S // TT) for e in range(NE) for ch in range(NCH)]
        state = {"loaded": 0}

        def load_w(i):
            T, e, ch = wseq[i]
            b = i % 2
            wg_d, wu_d, wd_d = experts[e]
            f0 = choff[ch] * 128
            cw = chunk_sizes[ch] * 128
            kb.dma("pool", wg[b][:, :, 0:cw], wg_d[:, f0:f0 + cw].rearrange("(kt p) f -> p kt f", p=128),
                   [], [wg_r[b]], wg_r[b])
            kb.dma("pool", wu[b][:, :, 0:cw], wu_d[:, f0:f0 + cw].rearrange("(kt p) f -> p kt f", p=128),
                   [], [wu_r[b]], wu_r[b])
            kb.dma("pool", wd[b][:, 0:chunk_sizes[ch], :], wd_d[f0:f0 + cw, :].rearrange("(ft p) d -> p ft d", p=128),
                   [], [wd_r[b]], wd_r[b])

        load_w(0)
        wi = 0
        gcount = 0
        ocount = 0
        NT = S // TT

        def front(T):
            t0 = T * TT
            acc, acc_r = accs[T % nbuf], accs_r[T % nbuf]
            xT, xT_r = [xTs[T % nbuf]], [xTs_r[T % nbuf]]
            nonlocal ocount
            for hf in range(2):
                kb.dma("sp", acc[:, hf * 4:(hf + 1) * 4, :],
                       src_ap[t0 + hf * 512:t0 + (hf + 1) * 512, :].rearrange("(j p) d -> p j d", p=128),
                       [src_r], [acc_r], acc_r)
            for hf in range(2):
                fr.norm_T(acc[:, hf * 4:(hf + 1) * 4, :], acc_r, 4, xT[0], xT_r[0], hf * 512)
            if router is not None:
                for j in range(NJ):
                    pl = psO[ocount % 2]
                    pl_r = psO_r[ocount % 2]
                    ocount += 1
                    for kt in range(KT):
                        kb.op("pe", lambda e, kt=kt, j=j: e.matmul(out=pl[:, 0:8], lhsT=xT[0][:, kt, j * 128:(j + 1) * 128],
                                                                  rhs=wr[:, kt, :], start=(kt == 0), stop=(kt == KT - 1)),
                              reads=[xT_r[0], wr_r], writes=[pl_r], signal=(kt == KT - 1))
                    kb.op("act", lambda e: e.activation(out=lg[:], in_=pl[:, 0:8], func=AF.Copy),
                          reads=[pl_r], writes=[lg_r])
                    kb.op("dve", lambda e: e.max(out=mx[:], in_=lg[:]), reads=[lg_r], writes=[mx_r])
                    kb.op("dve", lambda e: e.tensor_scalar(out=msk[:], in0=lg[:], scalar1=mx[:, 1:2], scalar2=None,
                                                           op0=ALU.is_ge), reads=[lg_r, mx_r], writes=[msk_r])
                    kb.op("dve", lambda e: e.tensor_scalar(out=ex[:], in0=lg[:], scalar1=mx[:, 0:1], scalar2=None,
                                                           op0=ALU.subtract), reads=[lg_r, mx_r], writes=[ex_r])
                    kb.op("act", lambda e: e.activation(out=ex[:], in_=ex[:], func=AF.Exp), reads=[ex_r], writes=[ex_r])
                    kb.op("dve", lambda e: e.tensor_tensor(out=ex[:], in0=ex[:], in1=msk[:], op=ALU.mult),
                          reads=[ex_r, msk_r], writes=[ex_r])
                    kb.op("dve", lambda e: e.reduce_sum(out=sm[:, 0:1], in_=ex[:], axis=AX.X), reads=[ex_r], writes=[sm_r])
                    kb.op("dve", lambda e: e.reciprocal(out=sm[:, 1:2], in_=sm[:, 0:1]), reads=[sm_r], writes=[sm_r])
                    kb.op("dve", lambda e, j=j: e.tensor_scalar(out=gates[:, j, :], in0=ex[:], scalar1=sm[:, 1:2],
                                                                scalar2=None, op0=ALU.mult),
                          reads=[ex_r, sm_r], writes=[gates_r])
        front(0)
        for T in range(NT):
            t0 = T * TT
            acc, acc_r = accs[T % nbuf], accs_r[T % nbuf]
            xT, xT_r = [xTs[T % nbuf]], [xTs_r[T % nbuf]]
            for e_i in range(NE):
                for ch in range(NCH):
                    if nbuf == 2 and e_i == 0 and ch == min(1, NCH - 1) and T + 1 < NT:
                        front(T + 1)
                    b = wi % 2
                    if wi + 1 < len(wseq):
                        load_w(wi + 1)
                    ab = wi % 2
                    CS = chunk_sizes[ch]
                    for fl in range(CS):
                        for hf in range(2):
                            gb = gcount % 2
                            gcount += 1
                            for (wt, wt_r, pst, pst_r) in ((wg[b], wg_r[b], psG[gb], psG_r[gb]),
                                                           (wu[b], wu_r[b], psU[gb], psU_r[gb])):
                                for kt in range(KT):
                                    kb.op("pe", lambda e, kt=kt, wt=wt, pst=pst, fl=fl, hf=hf: e.matmul(
                                        out=pst[:], lhsT=wt[:, kt, fl * 128:(fl + 1) * 128],
                                        rhs=xT[0][:, kt, hf * 512:(hf + 1) * 512],
                                        start=(kt == 0), stop=(kt == KT - 1)),
                                        reads=[wt_r, xT_r[0]], writes=[pst_r], signal=(kt == KT - 1))
                            kb.op("act", lambda e, gb=gb: e.activation(out=sg[gb][:], in_=psG[gb][:], func=AF.Silu),
                                  reads=[psG_r[gb]], writes=[sg_r[gb]])
                            kb.op("dve", lambda e, gb=gb, fl=fl, hf=hf, ab=ab: e.tensor_tensor(
                                out=actT[ab][:, fl, hf * 512:(hf + 1) * 512], in0=sg[gb][:], in1=psU[gb][:], op=ALU.mult),
                                reads=[sg_r[gb], psU_r[gb]], writes=[actT_r[ab]])
                    for j in range(NJ):
                        for dh in range(2):
                            ob = ocount % 2
                            ocount += 1
                            for fl in range(CS):
                                kb.op("pe", lambda e, fl=fl, j=j, dh=dh, ob=ob, ab=ab, b=b: e.matmul(
                                    out=psO[ob][:], lhsT=actT[ab][:, fl, j * 128:(j + 1) * 128],
                                    rhs=wd[b][:, fl, dh * 512:(dh + 1) * 512],
                                    start=(fl == 0), stop=(fl == CS - 1)),
                                    reads=[actT_r[ab], wd_r[b]], writes=[psO_r[ob]], signal=(fl == CS - 1))
                            if router is not None:
                                kb.op("dve", lambda e, j=j, dh=dh, ob=ob, e_i=e_i: e.scalar_tensor_tensor(
                                    out=acc[:, j, dh * 512:(dh + 1) * 512], in0=psO[ob][:],
                                    scalar=gates[:, j, e_i:e_i + 1], in1=acc[:, j, dh * 512:(dh + 1) * 512],
                                    op0=ALU.mult, op1=ALU.add),
                                    reads=[psO_r[ob], gates_r, acc_r], writes=[acc_r])
                            else:
                                kb.op("dve", lambda e, j=j, dh=dh, ob=ob: e.tensor_tensor(
                                    out=acc[:, j, dh * 512:(dh + 1) * 512], in0=psO[ob][:],
                                    in1=acc[:, j, dh * 512:(dh + 1) * 512], op=ALU.add),
                                    reads=[psO_r[ob], acc_r], writes=[acc_r])
                    wi += 1
            if final_g is not None:
                fr.rstd(acc[:], acc_r, NJ)
                for j in range(NJ):
                    kb.op("dve", lambda e, j=j: e.scalar_tensor_tensor(out=acc[:, j, :], in0=acc[:, j, :],
                                                                       scalar=fr.rs[:, j:j + 1], in1=gf[:],
                                                                       op0=ALU.mult, op1=ALU.mult),
                          reads=[acc_r, fr.rs_r, gf_r], writes=[acc_r])
            for hf in range(2):
                kb.dma("sp", dst_ap[t0 + hf * 512:t0 + (hf + 1) * 512, :].rearrange("(j p) d -> p j d", p=128),
                       acc[:, hf * 4:(hf + 1) * 4, :], [acc_r], [dst_r], acc_r, store=True)
            if nbuf == 1 and T + 1 < NT:
                front(T + 1)
        kb.barrier()


def fourier_phase(kb, src, dst, norm_g, w_fnet, ident_d, dftc, dfts, cc_d, nsc_d, sc_d, rev_d, tag="fn"):
    nc = kb.nc
    src_ap, src_r = src
    dst_ap, dst_r = dst
    NST = S // 128
    with ExitStack() as es:
        ident = es.enter_context(nc.sbuf_tensor(tag + "_ident", [128, 128], BF16))
        ident_r = Res("ident")
        kb.dma("sp", ident[:], ident_d, [], [ident_r], ident_r)
        hnS = es.enter_context(nc.sbuf_tensor(tag + "_hnS", [128, NST, D], BF16))
        hnS_r = Res("hnS")
        wf = es.enter_context(nc.sbuf_tensor(tag + "_wf", [128, KT, D], BF16))
        wf_r = Res("wf")
        kb.dma("pool", wf[:], w_fnet.rearrange("(kt p) d -> p kt d", p=128), [], [wf_r], wf_r)
        cc = es.enter_context(nc.sbuf_tensor(tag + "_cc", [128, 2, 256], BF16))
        cc_r = Res("cc")
        kb.dma("sp", cc[:], cc_d, [], [cc_r], cc_r)
        nsc = es.enter_context(nc.sbuf_tensor(tag + "_nsc", [128, 2, 256], BF16))
        nsc_r = Res("nsc")
        kb.dma("sp", nsc[:], nsc_d, [], [nsc_r], nsc_r)
        with ExitStack() as es0:
            fr = Front(kb, es0, ident, ident_r, norm_g, tag + "f", with_T=False)
            x4 = [es0.enter_context(nc.sbuf_tensor(tag + "_x4%d" % i, [128, 4, D], F32)) for i in range(4)]
            x4_r = [Res("x4%d" % i) for i in range(4)]
            for T in range(S // 512):
                b = T % 4
                kb.dma("sp", x4[b][:], src_ap[T * 512:(T + 1) * 512, :].rearrange("(j p) d -> p j d", p=128),
                       [src_r], [x4_r[b]], x4_r[b])
                fr.rstd(x4[b][:], x4_r[b], 4)
                for j in range(4):
                    fr.norm_to(hnS[:, T * 4 + j, :], hnS_r, x4[b][:, j, :], x4_r[b], j)
            kb.barrier()
        with ExitStack() as es1:
            tc = [es1.enter_context(nc.sbuf_tensor(tag + "_tc%d" % i, [128, NST, 128], BF16)) for i in range(2)]
            ts = [es1.enter_context(nc.sbuf_tensor(tag + "_ts%d" % i, [128, NST, 128], BF16)) for i in range(2)]
            tc_r = [Res("tc%d" % i) for i in range(2)]
            ts_r = [Res("ts%d" % i) for i in range(2)]
            rev = es1.enter_context(nc.sbuf_tensor(tag + "_rev", [128, 128], BF16))
            rev_r = Res("rev")
            kb.dma("sp", rev[:], rev_d, [], [rev_r], rev_r)
            sc = es1.enter_context(nc.sbuf_tensor(tag + "_sc", [128, 2, 256], BF16))
            sc_r = Res("sc")
            kb.dma("sp", sc[:], sc_d, [], [sc_r], sc_r)
            Pb = es1.enter_context(nc.sbuf_tensor(tag + "_Pb", [128, D], BF16))
            Qb = es1.enter_context(nc.sbuf_tensor(tag + "_Qb", [128, D], BF16))
            Pb_r, Qb_r = Res("Pb"), Res("Qb")
            PT = [es1.enter_context(nc.sbuf_tensor(tag + "_PT%d" % i, [128, KT, 128], BF16)) for i in range(2)]
            QT = [es1.enter_context(nc.sbuf_tensor(tag + "_QT%d" % i, [128, KT, 128], BF16)) for i in range(2)]
            PT_r = [Res("PT%d" % i) for i in range(2)]
            QT_r = [Res("QT%d" % i) for i in range(2)]
            YT = [es1.enter_context(nc.sbuf_tensor(tag + "_YT%d" % i, [128, KT, 128], BF16)) for i in range(2)]
            YT_r = [Res("YT%d" % i) for i in range(2)]
            xt = [es1.enter_context(nc.sbuf_tensor(tag + "_xt%d" % i, [128, D], F32)) for i in range(4)]
            xt_r = [Res("xt%d" % i) for i in range(4)]
            psP = [es1.enter_context(nc.psum_tensor(tag + "_psP%d" % i, [128, 512], F32)) for i in range(2)]
            psQ = [es1.enter_context(nc.psum_tensor(tag + "_psQ%d" % i, [128, 512], F32)) for i in range(2)]
            psP_r = [Res("psP%d" % i) for i in range(2)]
            psQ_r = [Res("psQ%d" % i) for i in range(2)]
            psT = [es1.enter_context(nc.psum_tensor(tag + "_psT%d" % i, [128, D], BF16)) for i in range(2)]
            psT_r = [Res("psT%d" % i) for i in range(2)]
            psY = es1.enter_context(nc.psum_tensor(tag + "_psY", [128, D], F32))
            psY_r = Res("psY")
            NSRC = 17

            def load_tab(ai):
                b = ai % 2
                kb.dma("sp", tc[b][:], dftc[ai], [], [tc_r[b]], tc_r[b])
                kb.dma("sp", ts[b][:], dfts[ai], [], [ts_r[b]], ts_r[b])

            def load_x(ai):
                a_ = ai - 1
                xd, xd_r = xt[(ai % 2) * 2], xt_r[(ai % 2) * 2]
                xm, xm_r = xt[(ai % 2) * 2 + 1], xt_r[(ai % 2) * 2 + 1]
                if a_ < 0:
                    kb.op("dve", lambda e: e.memset(xd[:], 0.0), writes=[xd_r])
                    kb.dma("sp", xd[127:128, :], src_ap[0:1, :], [src_r], [xd_r], xd_r)
                else:
                    kb.dma("sp", xd[:], src_ap[128 * a_ + 1:128 * a_ + 129, :], [src_r], [xd_r], xd_r)
                    r0 = 128 * (31 - a_)
                    kb.dma("sp", xm[:], src_ap[r0:r0 + 128, :], [src_r], [xm_r], xm_r)

            load_tab(0)
            load_x(0)
            for ai in range(NSRC):
                a_ = ai - 1
                b = ai % 2
                if ai + 1 < NSRC:
                    load_tab(ai + 1)
                    load_x(ai + 1)
                for (tab, tab_r, psX, psX_r) in ((tc[b], tc_r[b], psP, psP_r), (ts[b], ts_r[b], psQ, psQ_r)):
                    for ch in range(2):
                        for st in range(NST):
                            kb.op("pe", lambda e: e.matmul(
                                out=psX[ch][:], lhsT=tab[:, st, :], rhs=hnS[:, st, ch * 512:(ch + 1) * 512],
                                start=(st == 0), stop=(st == NST - 1)),
                                reads=[tab_r, hnS_r], writes=[psX_r[ch]], signal=(st == NST - 1))
                for (psX, psX_r, Xb, Xb_r) in ((psP, psP_r, Pb, Pb_r), (psQ, psQ_r, Qb, Qb_r)):
                    for ch in range(2):
                        kb.op("act", lambda e: e.activation(
                            out=Xb[:, ch * 512:(ch + 1) * 512], in_=psX[ch][:], func=AF.Copy),
                            reads=[psX_r[ch]], writes=[Xb_r])
                variants = [(0, ident, ident_r, nsc, nsc_r)]
                if a_ >= 0:
                    variants.append((1, rev, rev_r, sc, sc_r))
                for (mi, perm, perm_r, stab, stab_r) in variants:
                    for i, (Xb, Xb_r, XT, XT_r) in enumerate(((Pb, Pb_r, PT[mi], PT_r[mi]), (Qb, Qb_r, QT[mi], QT_r[mi]))):
                        for ct in range(KT):
                            kb.op("pe", lambda e: e.transpose(
                                out=psT[i][:, ct * 128:(ct + 1) * 128], in_=Xb[:, ct * 128:(ct + 1) * 128],
                                identity=perm[:]), reads=[Xb_r, perm_r], writes=[psT_r[i]], signal=(ct == KT - 1))
                        kb.op("dve", lambda e: e.tensor_copy(
                            out=XT[:], in_=psT[i][:].rearrange("p (k t) -> p k t", k=KT)),
                            reads=[psT_r[i]], writes=[XT_r])
                    for g in range(4):
                        for c2 in range(2):
                            o = (g * 2 + c2) * 128
                            n = 0
                            for (tabc, XT) in ((cc, PT[mi]), (stab, QT[mi])):
                                for ct in range(2):
                                    kb.op("pe", lambda e: e.matmul(
                                        out=psY[:, o:o + 128], lhsT=tabc[:, ct, c2 * 128:(c2 + 1) * 128],
                                        rhs=XT[:, g * 2 + ct, :], start=(n == 0), stop=(n == 3)),
                                        reads=[cc_r, stab_r, PT_r[mi], QT_r[mi]], writes=[psY_r],
                                        signal=(n == 3 and g == 3 and c2 == 1))
                                    n += 1
                    kb.op("act", lambda e: e.activation(out=YT[mi][:], in_=psY[:].rearrange("p (k t) -> p k t", k=KT),
                                                        func=AF.Identity, scale=1.0 / 1024.0),
                          reads=[psY_r], writes=[YT_r[mi]])
                    xo, xo_r = xt[(ai % 2) * 2 + mi], xt_r[(ai % 2) * 2 + mi]
                    for dh in range(2):
                        for ft in range(KT):
                            kb.op("pe", lambda e: e.matmul(
                                out=psP[dh][:], lhsT=YT[mi][:, ft, :], rhs=wf[:, ft, dh * 512:(dh + 1) * 512],
                                start=(ft == 0), stop=(ft == KT - 1)),
                                reads=[YT_r[mi], wf_r], writes=[psP_r[dh]], signal=(ft == KT - 1))
                        kb.op("dve", lambda e: e.tensor_tensor(
                            out=xo[:, dh * 512:(dh + 1) * 512], in0=psP[dh][:], in1=xo[:, dh * 512:(dh + 1) * 512],
                            op=ALU.add), reads=[psP_r[dh], xo_r], writes=[xo_r])
                    if mi == 0:
                        if a_ < 0:
                            kb.dma("sp", dst_ap[0:1, :], xo[127:128, :], [xo_r], [dst_r], xo_r, store=True)
                        elif a_ == 15:
                            kb.dma("sp", dst_ap[128 * a_ + 1:128 * a_ + 128, :], xo[0:127, :], [xo_r], [dst_r], xo_r, store=True)
                        else:
                            kb.dma("sp", dst_ap[128 * a_ + 1:128 * a_ + 129, :], xo[:], [xo_r], [dst_r], xo_r, store=True)
                    else:
                        r0 = 128 * (31 - a_)
                        kb.dma("sp", dst_ap[r0:r0 + 128, :], xo[:], [xo_r], [dst_r], xo_r, store=True)
            kb.barrier()


def ret_proj_phase(kb, src, norm_g, w_in, gn_gain, ident_d, rope_cos, rope_sin, qk_s, v_s, sg_s, tag="ra"):
    nc = kb.nc
    src_ap, src_r = src
    qk_ap, qk_r = qk_s
    v_ap, v_r = v_s
    sg_ap, sg_r = sg_s
    with ExitStack() as es:
        ident = es.enter_context(nc.sbuf_tensor(tag + "_ident", [128, 128], BF16))
        ident_r = Res("ident")
        kb.dma("sp", ident[:], ident_d, [], [ident_r], ident_r)
        xT = es.enter_context(nc.sbuf_tensor(tag + "_xT", [128, KT, S], BF16))
        xT_r = Res("xT")
        with ExitStack() as es0:
            fr = Front(kb, es0, ident, ident_r, norm_g, tag + "f")
            x4 = [es0.enter_context(nc.sbuf_tensor(tag + "_x4%d" % i, [128, 4, D], F32)) for i in range(4)]
            x4_r = [Res("x4%d" % i) for i in range(4)]
            for T in range(S // 512):
                b = T % 4
                kb.dma("sp", x4[b][:], src_ap[T * 512:(T + 1) * 512, :].rearrange("(j p) d -> p j d", p=128),
                       [src_r], [x4_r[b]], x4_r[b])
                fr.norm_T(x4[b][:], x4_r[b], 4, xT, xT_r, T * 512)
            kb.barrier()
        with ExitStack() as es1:
            wqk = es1.enter_context(nc.sbuf_tensor(tag + "_wqk", [128, KT, 2048], BF16))
            wqk_r = Res("wqk")
            for hh in range(2):
                kb.dma("pool", wqk[:, :, hh * 1024:(hh + 1) * 1024],
                       w_in[:, hh * 1024:(hh + 1) * 1024].rearrange("(kt p) f -> p kt f", p=128),
                       [], [wqk_r], wqk_r)
            cs = [es1.enter_context(nc.sbuf_tensor(tag + "_cs%d" % i, [128, 512], F32)) for i in range(2)]
            sn = [es1.enter_context(nc.sbuf_tensor(tag + "_sn%d" % i, [128, 512], F32)) for i in range(2)]
            cs_r = [Res("cs%d" % i) for i in range(2)]
            sn_r = [Res("sn%d" % i) for i in range(2)]
            As = [es1.enter_context(nc.sbuf_tensor(tag + "_As%d" % i, [128, 512], F32)) for i in range(2)]
            Bs = [es1.enter_context(nc.sbuf_tensor(tag + "_Bs%d" % i, [128, 512], F32)) for i in range(2)]
            As_r = [Res("As%d" % i) for i in range(2)]
            Bs_r = [Res("Bs%d" % i) for i in range(2)]
            tt = [es1.enter_context(nc.sbuf_tensor(tag + "_tt%d" % i, [128, 512], F32)) for i in range(8)]
            tt_r = [Res("tt%d" % i) for i in range(8)]
            stg = [es1.enter_context(nc.sbuf_tensor(tag + "_stg%d" % i, [128, 4, 16, 128], BF16)) for i in range(2)]
            stg_r = [Res("stg%d" % i) for i in range(2)]
            psA = [es1.enter_context(nc.psum_tensor(tag + "_psA%d" % i, [128, 512], F32)) for i in range(2)]
            psB = [es1.enter_context(nc.psum_tensor(tag + "_psB%d" % i, [128, 512], F32)) for i in range(2)]
            psA_r = [Res("psA%d" % i) for i in range(2)]
            psB_r = [Res("psB%d" % i) for i in range(2)]
            n = 0
            for T in range(S // 512):
                tb = T % 2
                kb.dma("sp", cs[tb][:], rope_cos[:, T * 512:(T + 1) * 512], [], [cs_r[tb]], cs_r[tb])
                kb.dma("sp", sn[tb][:], rope_sin[:, T * 512:(T + 1) * 512], [], [sn_r[tb]], sn_r[tb])
                for which in range(2):
                    scale = 1.0 if which == 0 else 1.0 / 16.0
                    for h in range(4):
                        b = n % 2
                        n += 1
                        f1 = which * 8 + 2 * h
                        for (ft, psX, psX_r) in ((f1, psA[b], psA_r[b]), (f1 + 1, psB[b], psB_r[b])):
                            for kt in range(KT):
                                kb.op("pe", lambda e, kt=kt, ft=ft, psX=psX: e.matmul(
                                    out=psX[:], lhsT=wqk[:, kt, ft * 128:(ft + 1) * 128],
                                    rhs=xT[:, kt, T * 512:(T + 1) * 512], start=(kt == 0), stop=(kt == KT - 1)),
                                    reads=[wqk_r, xT_r], writes=[psX_r], signal=(kt == KT - 1))
                        kb.op("act", lambda e: e.activation(out=As[b][:], in_=psA[b][:], func=AF.Identity, scale=scale),
                              reads=[psA_r[b]], writes=[As_r[b]])
                        kb.op("act", lambda e: e.activation(out=Bs[b][:], in_=psB[b][:], func=AF.Identity, scale=scale),
                              reads=[psB_r[b]], writes=[Bs_r[b]])
                        t = [tt[b * 4 + i] for i in range(4)]
                        t_r = [tt_r[b * 4 + i] for i in range(4)]
                        kb.op("dve", lambda e: e.tensor_tensor(out=t[0][:], in0=As[b][:], in1=cs[tb][:], op=ALU.mult),
                              reads=[As_r[b], cs_r[tb]], writes=[t_r[0]])
                        kb.op("dve", lambda e: e.tensor_tensor(out=t[1][:], in0=Bs[b][:], in1=sn[tb][:], op=ALU.mult),
                              reads=[Bs_r[b], sn_r[tb]], writes=[t_r[1]])
                        kb.op("dve", lambda e: e.tensor_tensor(out=t[2][:], in0=As[b][:], in1=sn[tb][:], op=ALU.mult),
                              reads=[As_r[b], sn_r[tb]], writes=[t_r[2]])
                        kb.op("dve", lambda e: e.tensor_tensor(out=t[3][:], in0=Bs[b][:], in1=cs[tb][:], op=ALU.mult),
                              reads=[Bs_r[b], cs_r[tb]], writes=[t_r[3]])
                        kb.op("pool", lambda e: e.tensor_tensor(
                            out=stg[tb][:, :, f1, :], in0=t[0][:].rearrange("p (c t) -> p c t", c=4),
                            in1=t[1][:].rearrange("p (c t) -> p c t", c=4), op=ALU.subtract),
                            reads=[t_r[0], t_r[1]], writes=[stg_r[tb]])
                        kb.op("pool", lambda e: e.tensor_tensor(
                            out=stg[tb][:, :, f1 + 1, :], in0=t[2][:].rearrange("p (c t) -> p c t", c=4),
                            in1=t[3][:].rearrange("p (c t) -> p c t", c=4), op=ALU.add),
                            reads=[t_r[2], t_r[3]], writes=[stg_r[tb]])
                kb.dma("sp", qk_ap[T], stg[tb][:], [stg_r[tb]], [qk_r], stg_r[tb], store=True)
            kb.barrier()
        with ExitStack() as es2:
            wv = [es2.enter_context(nc.sbuf_tensor(tag + "_wv%d" % i, [128, KT, 512], BF16)) for i in range(2)]
            wv_r = [Res("wv%d" % i) for i in range(2)]
            gg = es2.enter_context(nc.sbuf_tensor(tag + "_gg", [128, 2048], F32))
            gg_r = Res("gg")
            kb.dma("sp", gg[:], bcast_row(gn_gain, 2048), [], [gg_r], gg_r)
            vst = [es2.enter_context(nc.sbuf_tensor(tag + "_vst%d" % i, [128, 4, 512], BF16)) for i in range(2)]
            vst_r = [Res("vst%d" % i) for i in range(2)]
            sgt = [es2.enter_context(nc.sbuf_tensor(tag + "_sgt%d" % i, [128, 512], F32)) for i in range(2)]
            sgt_r = [Res("sgt%d" % i) for i in range(2)]
            psV = [es2.enter_context(nc.psum_tensor(tag + "_psV%d" % i, [128, 512], F32)) for i in range(4)]
            psV_r = [Res("psV%d" % i) for i in range(4)]

            def load_wv(cb):
                b = cb % 2
                c0 = 2048 + cb * 512
                kb.dma("pool", wv[b][:], w_in[:, c0:c0 + 512].rearrange("(kt p) f -> p kt f", p=128),
                       [], [wv_r[b]], wv_r[b])

            load_wv(0)
            n = 0
            m = 0
            for cb in range(8):
                b = cb % 2
                if cb + 1 < 8:
                    load_wv(cb + 1)
                is_g = cb >= 4
                cc0 = (cb % 4) * 512
                for T in range(S // 512):
                    sb_ = m % 2
                    m += 1
                    for j in range(4):
                        pb = n % 4
                        n += 1
                        t0 = T * 512 + j * 128
                        for kt in range(KT):
                            kb.op("pe", lambda e, kt=kt, t0=t0, pb=pb: e.matmul(
                                out=psV[pb][:], lhsT=xT[:, kt, t0:t0 + 128], rhs=wv[b][:, kt, :],
                                start=(kt == 0), stop=(kt == KT - 1)),
                                reads=[xT_r, wv_r[b]], writes=[psV_r[pb]], signal=(kt == KT - 1))
                        if not is_g:
                            kb.op("act", lambda e, j=j, pb=pb: e.activation(out=vst[sb_][:, j, :], in_=psV[pb][:],
                                                                            func=AF.Copy),
                                  reads=[psV_r[pb]], writes=[vst_r[sb_]])
                        else:
                            gb = n % 2
                            kb.op("act", lambda e, pb=pb, gb=gb: e.activation(out=sgt[gb][:], in_=psV[pb][:],
                                                                              func=AF.Silu),
                                  reads=[psV_r[pb]], writes=[sgt_r[gb]])
                            kb.op("dve", lambda e, j=j, gb=gb: e.tensor_tensor(
                                out=vst[sb_][:, j, :], in0=sgt[gb][:], in1=gg[:, cc0:cc0 + 512], op=ALU.mult),
                                reads=[sgt_r[gb], gg_r], writes=[vst_r[sb_]])
                    dst_ap, dst_r = (sg_ap, sg_r) if is_g else (v_ap, v_r)
                    kb.dma("sp", dst_ap[T * 512:(T + 1) * 512, cc0:cc0 + 512].rearrange("(j p) f -> p j f", p=128),
                           vst[sb_][:], [vst_r[sb_]], [dst_r], vst_r[sb_], store=True)
            kb.barrier()


class RetConsts:
    def __init__(self, kb, es, decay_logit, tag):
        nc = kb.nc
        I32 = mybir.dt.int32

        def T(name, shape, dtype=F32):
            return es.enter_context(nc.sbuf_tensor(tag + "_" + name, shape, dtype)), Res(name)

        dl, dl_r = T("dl", [128, 8])
        kb.dma("sp", dl[:], bcast_row(decay_logit.rearrange("a b -> (a b)"), 8), [], [dl_r], dl_r)
        self.lg, self.lg_r = T("lg", [128, 8])
        nlg, nlg_r = T("nlg", [128, 8])
        lg127, lg127_r = T("lg127", [128, 8])
        self.g128, self.g128_r = T("g128", [128, 8])
        lg, lg_r = self.lg, self.lg_r
        kb.op("act", lambda e: e.activation(out=nlg[:], in_=dl[:], func=AF.Exp, scale=-1.0), reads=[dl_r], writes=[nlg_r])
        kb.op("dve", lambda e: e.tensor_scalar(out=nlg[:], in0=nlg[:], scalar1=1.0, scalar2=None, op0=ALU.add),
              reads=[nlg_r], writes=[nlg_r])
        kb.op("act", lambda e: e.activation(out=nlg[:], in_=nlg[:], func=AF.Ln), reads=[nlg_r], writes=[nlg_r])
        kb.op("dve", lambda e: e.tensor_scalar(out=lg[:], in0=nlg[:], scalar1=-1.0, scalar2=None, op0=ALU.mult),
              reads=[nlg_r], writes=[lg_r])
        kb.op("dve", lambda e: e.tensor_scalar(out=lg127[:], in0=lg[:], scalar1=127.0, scalar2=None, op0=ALU.mult),
              reads=[lg_r], writes=[lg127_r])
        kb.op("act", lambda e: e.activation(out=self.g128[:], in_=lg[:], func=AF.Exp, scale=128.0),
              reads=[lg_r], writes=[self.g128_r])
        ii, ii_r = T("ii", [128, 128], I32)
        dmf, dmf_r = T("dmf", [128, 128])
        imat, imat_r = T("imat", [128, 128])
        jmat, jmat_r = T("jmat", [128, 128])
        pos, pos_r = T("pos", [128, 128])
        neg, neg_r = T("neg", [128, 128])
        tmp, tmp_r = T("tmp", [128, 128])
        kb.op("pool", lambda e: e.iota(ii[:], pattern=[[1, 128]], base=0, channel_multiplier=-1), writes=[ii_r])
        kb.op("dve", lambda e: e.tensor_copy(out=dmf[:], in_=ii[:]), reads=[ii_r], writes=[dmf_r])
        kb.op("pool", lambda e: e.iota(ii[:], pattern=[[1, 128]], base=0, channel_multiplier=0), reads=[], writes=[ii_r])
        kb.op("dve", lambda e: e.tensor_copy(out=imat[:], in_=ii[:]), reads=[ii_r], writes=[imat_r])
        kb.op("pool", lambda e: e.iota(ii[:], pattern=[[0, 128]], base=0, channel_multiplier=1), reads=[], writes=[ii_r])
        kb.op("dve", lambda e: e.tensor_copy(out=jmat[:], in_=ii[:]), reads=[ii_r], writes=[jmat_r])
        kb.op("dve", lambda e: e.tensor_scalar(out=pos[:], in0=dmf[:], scalar1=0.0, scalar2=None, op0=ALU.max),
              reads=[dmf_r], writes=[pos_r])
        kb.op("dve", lambda e: e.tensor_tensor(out=neg[:], in0=pos[:], in1=dmf[:], op=ALU.subtract),
              reads=[pos_r, dmf_r], writes=[neg_r])
        self.Dc, self.Dc_r = T("Dc", [128, 4, 128])
        self.XF, self.XF_r = T("XF", [128, 8, 128])
        self.XB, self.XB_r = T("XB", [128, 8, 128])
        self.ZF, self.ZF_r = T("ZF", [128, 8, 128])
        self.ZB, self.ZB_r = T("ZB", [128, 8, 128])
        for h in range(4):
            f, b_ = h, 4 + h
            kb.op("dve", lambda e: e.tensor_scalar(out=tmp[:], in0=pos[:], scalar1=lg[:, f:f + 1], scalar2=None,
                                                   op0=ALU.mult), reads=[pos_r, lg_r], writes=[tmp_r])
            kb.op("dve", lambda e: e.scalar_tensor_tensor(out=tmp[:], in0=neg[:], scalar=lg[:, b_:b_ + 1], in1=tmp[:],
                                                          op0=ALU.mult, op1=ALU.add),
                  reads=[neg_r, lg_r, tmp_r], writes=[tmp_r])
            kb.op("act", lambda e: e.activation(out=self.Dc[:, h, :], in_=tmp[:], func=AF.Exp),
                  reads=[tmp_r], writes=[self.Dc_r])
            for ft in (2 * h, 2 * h + 1):
                kb.op("act", lambda e: e.activation(out=self.XF[:, ft, :], in_=imat[:], func=AF.Exp,
                                                    scale=lg[:, f:f + 1], bias=lg[:, f:f + 1]),
                      reads=[imat_r, lg_r], writes=[self.XF_r])
                kb.op("act", lambda e: e.activation(out=self.XB[:, ft, :], in_=imat[:], func=AF.Exp,
                                                    scale=nlg[:, b_:b_ + 1], bias=lg127[:, b_:b_ + 1]),
                      reads=[imat_r, nlg_r, lg127_r], writes=[self.XB_r])
                kb.op("act", lambda e: e.activation(out=self.ZF[:, ft, :], in_=jmat[:], func=AF.Exp,
                                                    scale=nlg[:, f:f + 1], bias=lg127[:, f:f + 1]),
                      reads=[jmat_r, nlg_r, lg127_r], writes=[self.ZF_r])
                kb.op("act", lambda e: e.activation(out=self.ZB[:, ft, :], in_=jmat[:], func=AF.Exp,
                                                    scale=lg[:, b_:b_ + 1], bias=lg[:, b_:b_ + 1]),
                      reads=[jmat_r, lg_r], writes=[self.ZB_r])


def ret_core_phase(kb, decay_logit, ident_d, qk_s, v_s, sg_s, sb_s, z_s, tag="rb"):
    nc = kb.nc
    qk_ap, qk_r = qk_s
    v_ap, v_r = v_s
    sg_ap, sg_r = sg_s
    sb_ap, sb_r = sb_s
    z_ap, z_r = z_s
    NCK = S // 128
    with ExitStack() as es:
        ident = es.enter_context(nc.sbuf_tensor(tag + "_ident", [128, 128], BF16))
        ident_r = Res("ident")
        kb.dma("sp", ident[:], ident_d, [], [ident_r], ident_r)
        rc = RetConsts(kb, es, decay_logit, tag + "c")
        S32 = es.enter_context(nc.sbuf_tensor(tag + "_S32", [128, 8, 512], F32))
        S32_r = [Res("S32_%d" % i) for i in range(8)]
        S16 = [es.enter_context(nc.sbuf_tensor(tag + "_S16%d" % i, [128, 8, 512], BF16)) for i in range(2)]
        S16_r = [[Res("S16%d_%d" % (i, k)) for k in range(8)] for i in range(2)]
        S16_st = [Res("S16st%d" % i) for i in range(2)]
        qk = [es.enter_context(nc.sbuf_tensor(tag + "_qk%d" % i, [128, 16, 128], BF16)) for i in range(3)]
        qk_t = [Res("qk%d" % i) for i in range(3)]
        vt = [es.enter_context(nc.sbuf_tensor(tag + "_v%d" % i, [128, 2048], BF16)) for i in range(3)]
        vt_r = [Res("v%d" % i) for i in range(3)]
        kz = [es.enter_context(nc.sbuf_tensor(tag + "_kz%d" % i, [128, 8, 128], BF16)) for i in range(2)]
        kz_r = [Res("kz%d" % i) for i in range(2)]
        psK = es.enter_context(nc.psum_tensor(tag + "_psK", [128, 1024], BF16))
        psK_r = Res("psK")
        psS = [es.enter_context(nc.psum_tensor(tag + "_psS%d" % i, [128, 512], F32)) for i in range(2)]
        psS_r = [Res("psS%d" % i) for i in range(2)]
        cnt = {"s": 0}

        def load_chunk(n, want_q):
            b = n % 3
            T, c = n // 4, n % 4
            if want_q:
                kb.dma("sp", qk[b][:], qk_ap[T, :, c], [qk_r], [qk_t[b]], qk_t[b])
            else:
                kb.dma("sp", qk[b][:, 8:16, :], qk_ap[T, :, c, 8:16, :], [qk_r], [qk_t[b]], qk_t[b])
            kb.dma("sp", vt[b][:], v_ap[n * 128:(n + 1) * 128, :], [v_r], [vt_r[b]], vt_r[b])

        def k_tokmajor(n, Z, Z_r, use_act=False):
            b = n % 2
            b3 = n % 3
            for ft in range(8):
                kb.op("pe", lambda e, ft=ft: e.transpose(out=psK[:, ft * 128:(ft + 1) * 128], in_=qk[b3][:, 8 + ft, :],
                                                         identity=ident[:]),
                      reads=[qk_t[b3], ident_r], writes=[psK_r], signal=(ft == 7))
            if use_act:
                for h in range(4):
                    kb.op("act", lambda e: e.activation(out=kz[b][:, 2 * h:2 * h + 2, :],
                                                        in_=psK[:, h * 256:(h + 1) * 256].rearrange("p (f d) -> p f d", f=2),
                                                        func=AF.Identity, scale=Z[:, 2 * h, 0:1]),
                          reads=[psK_r, Z_r], writes=[kz_r[b]])
            else:
                kb.op("dve", lambda e: e.tensor_tensor(out=kz[b][:], in0=psK[:].rearrange("p (f d) -> p f d", f=8),
                                                       in1=Z[:], op=ALU.mult),
                      reads=[psK_r, Z_r], writes=[kz_r[b]])

        def state_update(n, gcol, s16_out, s16_out_r):
            b = n % 2
            b3 = n % 3
            for h in range(4):
                for dt_ in range(2):
                    ft = 2 * h + dt_
                    pb = cnt["s"] % len(psS)
                    cnt["s"] += 1
                    kb.op("pe", lambda e: e.matmul(out=psS[pb][:], lhsT=kz[b][:, ft, :],
                                                   rhs=vt[b3][:, h * 512:(h + 1) * 512], start=True, stop=True),
                          reads=[kz_r[b], vt_r[b3]], writes=[psS_r[pb]])
                    kb.op("dve", lambda e: e.scalar_tensor_tensor(
                        out=S32[:, ft, :], in0=S32[:, ft, :], scalar=rc.g128[:, gcol + h:gcol + h + 1], in1=psS[pb][:],
                        op0=ALU.mult, op1=ALU.add), reads=[S32_r[ft], rc.g128_r, psS_r[pb]], writes=[S32_r[ft]])
                    if gcol == 4 and ft % 4 == 3:
                        kb.op("pool", lambda e: e.tensor_copy(out=s16_out[:, ft, :], in_=S32[:, ft, :]),
                              reads=[S32_r[ft]], writes=[s16_out_r[ft]])
                    else:
                        kb.op("act", lambda e: e.activation(out=s16_out[:, ft, :], in_=S32[:, ft, :], func=AF.Copy),
                              reads=[S32_r[ft]], writes=[s16_out_r[ft]])

        esb1 = ExitStack()
        for i in range(2, 6):
            psS.append(esb1.enter_context(nc.psum_tensor(tag + "_psS%d" % i, [128, 512], F32)))
            psS_r.append(Res("psS%d" % i))
        kb.op("dve", lambda e: e.memset(S32[:], 0.0), writes=S32_r)
        kb.op("pool", lambda e: e.memset(S16[1][:], 0.0), writes=S16_r[1])
        load_chunk(NCK - 1, False)
        load_chunk(NCK - 2, False)
        k_tokmajor(NCK - 1, rc.ZB, rc.ZB_r, use_act=True)
        for n in range(NCK - 1, -1, -1):
            if n - 2 >= 0:
                load_chunk(n - 2, False)
            cur = S16[n % 2]
            cur_r = S16_r[n % 2]
            kb.dma("sp", sb_ap[n], cur[:], cur_r, [sb_r], S16_st[n % 2], store=True)
            if n > 1:
                k_tokmajor(n - 1, rc.ZB, rc.ZB_r, use_act=True)
            if n > 0:
                state_update(n, 4, S16[(n + 1) % 2], S16_r[(n + 1) % 2])
        kb.barrier(recycle=False)
        del psS[2:]
        del psS_r[2:]
        esb1.close()
        with ExitStack() as es2:
            sgt = [es2.enter_context(nc.sbuf_tensor(tag + "_sg%d" % i, [128, 2048], BF16)) for i in range(2)]
            sgt_r = [Res("sg%d" % i) for i in range(2)]
            sbn = [es2.enter_context(nc.sbuf_tensor(tag + "_sbn%d" % i, [128, 8, 512], BF16)) for i in range(3)]
            sbn_r = [Res("sbn%d" % i) for i in range(3)]
            qf2 = [es2.enter_context(nc.sbuf_tensor(tag + "_qf%d" % i, [128, 8, 128], BF16)) for i in range(2)]
            qb2 = [es2.enter_context(nc.sbuf_tensor(tag + "_qb%d" % i, [128, 8, 128], BF16)) for i in range(2)]
            qf2_r = [Res("qf%d" % i) for i in range(2)]
            qb2_r = [Res("qb%d" % i) for i in range(2)]
            PT = es2.enter_context(nc.sbuf_tensor(tag + "_PT", [128, 4, 128], BF16))
            PT_r = Res("PT")
            yn = [es2.enter_context(nc.sbuf_tensor(tag + "_yn%d" % i, [128, 512], F32)) for i in range(2)]
            yn_r = [Res("yn%d" % i) for i in range(2)]
            zt = [es2.enter_context(nc.sbuf_tensor(tag + "_z%d" % i, [128, 2048], BF16)) for i in range(2)]
            zt_r = [Res("z%d" % i) for i in range(2)]
            st6 = es2.enter_context(nc.sbuf_tensor(tag + "_st6", [128, 4, 6], F32))
            st6_r = Res("st6")
            mv = es2.enter_context(nc.sbuf_tensor(tag + "_mv", [128, 4, 2], F32))
            mv_r = Res("mv")
            rs = es2.enter_context(nc.sbuf_tensor(tag + "_rs", [128, 4], F32))
            rs_r = Res("rs")
            nmr = es2.enter_context(nc.sbuf_tensor(tag + "_nmr", [128, 4], F32))
            nmr_r = Res("nmr")
            psSc = es2.enter_context(nc.psum_tensor(tag + "_psSc", [128, 512], F32))
            psSc_r = Res("psSc")
            psY2 = [es2.enter_context(nc.psum_tensor(tag + "_psY%d" % i, [128, 512], F32)) for i in range(2)]
            psY2_r = [Res("psY%d" % i) for i in range(2)]
            psY = [psY2[h % 2] for h in range(4)]
            psY_r = [psY2_r[h % 2] for h in range(4)]
            for i in range(2, 4):
                psS.append(es2.enter_context(nc.psum_tensor(tag + "_psSf%d" % i, [128, 512], F32)))
                psS_r.append(Res("psSf%d" % i))

            ysb = [es2.enter_context(nc.sbuf_tensor(tag + "_ysb%d" % i, [128, 2048], F32)) for i in range(2)]
            ysb_r = [[Res("ysb%d_%d" % (i, h)) for h in range(4)] for i in range(2)]
            sg3 = [es2.enter_context(nc.sbuf_tensor(tag + "_sg3%d" % i, [128, 2048], BF16)) for i in range(4)]
            sg3_r = [Res("sg3%d" % i) for i in range(4)]

            def load_chunk2(n):
                b = n % 3
                load_chunk(n, True)
                kb.dma("sp", sg3[n % 4][:], sg_ap[n * 128:(n + 1) * 128, :], [sg_r], [sg3_r[n % 4]], sg3_r[n % 4])
                kb.dma("sp", sbn[b][:], sb_ap[n], [sb_r], [sbn_r[b]], sbn_r[b])

            def stage_q(n):
                b = n % 3
                kb.op("pool", lambda e: e.tensor_tensor(out=qf2[n % 2][:], in0=qk[b][:, 0:8, :], in1=rc.XF[:], op=ALU.mult),
                      reads=[qk_t[b], rc.XF_r], writes=[qf2_r[n % 2]])
                kb.op("pool", lambda e: e.tensor_tensor(out=qb2[n % 2][:], in0=qk[b][:, 0:8, :], in1=rc.XB[:], op=ALU.mult),
                      reads=[qk_t[b], rc.XB_r], writes=[qb2_r[n % 2]])

            def stage_x(n):
                b = n % 3
                yb2 = n % 2
                qf, qf_r, qb, qb_r = qf2[n % 2], qf2_r[n % 2], qb2[n % 2], qb2_r[n % 2]
                sf = S16[(n + 1) % 2]
                sf_r = S16_r[(n + 1) % 2]
                if n + 2 < NCK:
                    k_tokmajor(n + 1, rc.ZF, rc.ZF_r)
                if n + 1 < NCK:
                    state_update(n, 0, S16[n % 2], S16_r[n % 2])
                for h in range(4):
                    for dt_ in range(2):
                        kb.op("pe", lambda e: e.matmul(out=psSc[:, h * 128:(h + 1) * 128],
                                                       lhsT=qk[b][:, 8 + 2 * h + dt_, :], rhs=qk[b][:, 2 * h + dt_, :],
                                                       start=(dt_ == 0), stop=(dt_ == 1)),
                              reads=[qk_t[b]], writes=[psSc_r], signal=(h == 3 and dt_ == 1))
                kb.op("dve", lambda e: e.tensor_tensor(out=PT[:], in0=psSc[:].rearrange("p (h i) -> p h i", h=4),
                                                       in1=rc.Dc[:], op=ALU.mult),
                      reads=[psSc_r, rc.Dc_r], writes=[PT_r])
                for h in range(4):
                    kb.op("pe", lambda e: e.matmul(out=psY[h][:], lhsT=PT[:, h, :], rhs=vt[b][:, h * 512:(h + 1) * 512],
                                                   start=True, stop=False),
                          reads=[PT_r, vt_r[b]], writes=[psY_r[h]], signal=False)
                    for dt_ in range(2):
                        kb.op("pe", lambda e: e.matmul(out=psY[h][:], lhsT=qf[:, 2 * h + dt_, :], rhs=sf[:, 2 * h + dt_, :],
                                                       start=False, stop=False),
                              reads=[qf_r, sf_r[2 * h + dt_]], writes=[psY_r[h]], signal=False)
                    for dt_ in range(2):
                        kb.op("pe", lambda e: e.matmul(out=psY[h][:], lhsT=qb[:, 2 * h + dt_, :],
                                                       rhs=sbn[b][:, 2 * h + dt_, :], start=False, stop=(dt_ == 1)),
                              reads=[PT_r, vt_r[b], qf_r, sf_r[2 * h], sf_r[2 * h + 1], qb_r, sbn_r[b]],
                              writes=[psY_r[h]], signal=(dt_ == 1))
                    kb.op("act", lambda e: e.activation(out=ysb[yb2][:, h * 512:(h + 1) * 512], in_=psY[h][:], func=AF.Copy),
                          reads=[psY_r[h]], writes=[ysb_r[yb2][h]])

            ycnt = {"c": 0}

            def stage_z(n):
                b = n % 2
                for h in range(4):
                    kb.op("dve", lambda e: e.bn_stats(out=st6[:, h, :], in_=ysb[b][:, h * 512:(h + 1) * 512]),
                          reads=[ysb_r[b][h]], writes=[st6_r])
                    kb.op("dve", lambda e: e.bn_aggr(out=mv[:, h, :], in_=st6[:, h, :]), reads=[st6_r], writes=[mv_r])
                kb.op("dve", lambda e: e.tensor_scalar(out=rs[:], in0=mv[:, :, 1], scalar1=EPS, scalar2=None, op0=ALU.add),
                      reads=[mv_r], writes=[rs_r])
                kb.op("act", lambda e: e.activation(out=rs[:], in_=rs[:], func=AF.Sqrt), reads=[rs_r], writes=[rs_r])
                kb.op("dve", lambda e: e.reciprocal(out=rs[:], in_=rs[:]), reads=[rs_r], writes=[rs_r])
                kb.op("dve", lambda e: e.scalar_tensor_tensor(out=nmr[:], in0=mv[:, :, 0], scalar=-1.0, in1=rs[:],
                                                              op0=ALU.mult, op1=ALU.mult),
                      reads=[mv_r, rs_r], writes=[nmr_r])
                for h in range(4):
                    yb = ycnt["c"] % 2
                    ycnt["c"] += 1
                    kb.op("act", lambda e: e.activation(out=yn[yb][:], in_=ysb[b][:, h * 512:(h + 1) * 512], func=AF.Identity,
                                                        scale=rs[:, h:h + 1], bias=nmr[:, h:h + 1]),
                          reads=[ysb_r[b][h], rs_r, nmr_r], writes=[yn_r[yb]])
                    kb.op("pool", lambda e: e.tensor_tensor(out=zt[b][:, h * 512:(h + 1) * 512], in0=yn[yb][:],
                                                            in1=sg3[n % 4][:, h * 512:(h + 1) * 512], op=ALU.mult),
                          reads=[yn_r[yb], sg3_r[n % 4]], writes=[zt_r[b]])
                kb.dma("sp", z_ap[n * 128:(n + 1) * 128, :], zt[b][:], [zt_r[b]], [z_r], zt_r[b], store=True)

            kb.op("dve", lambda e: e.memset(S32[:], 0.0), reads=[], writes=S32_r)
            kb.op("pool", lambda e: e.memset(S16[1][:], 0.0), reads=[], writes=S16_r[1])
            load_chunk2(0)
            load_chunk2(1)
            stage_q(0)
            k_tokmajor(0, rc.ZF, rc.ZF_r)
            for n in range(NCK):
                if n + 2 < NCK:
                    load_chunk2(n + 2)
                if n + 1 < NCK:
                    stage_q(n + 1)
                stage_x(n)
                if n >= 1:
                    stage_z(n - 1)
            stage_z(NCK - 1)
            kb.barrier()


def ret_out_phase(kb, src, dst, z_s, w_out, ident_d, tag="rc"):
    nc = kb.nc
    src_ap, src_r = src
    dst_ap, dst_r = dst
    z_ap, z_r = z_s
    with ExitStack() as es:
        ident = es.enter_context(nc.sbuf_tensor(tag + "_ident", [128, 128], BF16))
        ident_r = Res("ident")
        kb.dma("sp", ident[:], ident_d, [], [ident_r], ident_r)
        wo = es.enter_context(nc.sbuf_tensor(tag + "_wo", [128, 16, D], BF16))
        wo_r = Res("wo")
        for hh in range(2):
            kb.dma("pool", wo[:, hh * 8:(hh + 1) * 8, :],
                   w_out[hh * 1024:(hh + 1) * 1024, :].rearrange("(et p) d -> p et d", p=128), [], [wo_r], wo_r)
        zt = [es.enter_context(nc.sbuf_tensor(tag + "_z%d" % i, [128, 2048], BF16)) for i in range(2)]
        zt_r = [Res("z%d" % i) for i in range(2)]
        xt = [es.enter_context(nc.sbuf_tensor(tag + "_x%d" % i, [128, D], F32)) for i in range(4)]
        xt_r = [Res("x%d" % i) for i in range(4)]
        zT = [es.enter_context(nc.sbuf_tensor(tag + "_zT%d" % i, [128, 16, 128], BF16)) for i in range(2)]
        zT_r = [Res("zT%d" % i) for i in range(2)]
        psT = [es.enter_context(nc.psum_tensor(tag + "_psT%d" % i, [128, 1024], BF16)) for i in range(2)]
        psT_r = [Res("psT%d" % i) for i in range(2)]
        psO = [es.enter_context(nc.psum_tensor(tag + "_psO%d" % i, [128, 512], F32)) for i in range(4)]
        psO_r = [Res("psO%d" % i) for i in range(4)]

        def load(t):
            b = t % 2
            kb.dma("sp", zt[b][:], z_ap[t * 128:(t + 1) * 128, :], [z_r], [zt_r[b]], zt_r[b])
            kb.dma("sp", xt[t % 4][:], src_ap[t * 128:(t + 1) * 128, :], [src_r], [xt_r[t % 4]], xt_r[t % 4])

        def transp(t):
            b = t % 2
            for hh in range(2):
                for e8 in range(8):
                    et = hh * 8 + e8
                    kb.op("pe", lambda e: e.transpose(out=psT[hh][:, e8 * 128:(e8 + 1) * 128],
                                                      in_=zt[b][:, et * 128:(et + 1) * 128], identity=ident[:]),
                          reads=[zt_r[b], ident_r], writes=[psT_r[hh]], signal=(e8 == 7))
                kb.op("act", lambda e: e.activation(out=zT[b][:, hh * 8:(hh + 1) * 8, :],
                                                    in_=psT[hh][:].rearrange("p (k t) -> p k t", k=8), func=AF.Copy),
                      reads=[psT_r[hh]], writes=[zT_r[b]])

        load(0)
        load(1)
        transp(0)
        load(2)
        for t in range(S // 128):
            b = t % 2
            xb = t % 4
            if t + 1 < S // 128:
                transp(t + 1)
                if t + 3 < S // 128:
                    load(t + 3)
            for dh in range(2):
                pb = (t % 2) * 2 + dh
                for et in range(16):
                    kb.op("pe", lambda e: e.matmul(out=psO[pb][:], lhsT=zT[b][:, et, :],
                                                   rhs=wo[:, et, dh * 512:(dh + 1) * 512],
                                                   start=(et == 0), stop=(et == 15)),
                          reads=[zT_r[b], wo_r], writes=[psO_r[pb]], signal=(et == 15))
                kb.op("dve", lambda e: e.tensor_tensor(out=xt[xb][:, dh * 512:(dh + 1) * 512], in0=psO[pb][:],
                                                       in1=xt[xb][:, dh * 512:(dh + 1) * 512], op=ALU.add),
                      reads=[psO_r[pb], xt_r[xb]], writes=[xt_r[xb]])
            kb.dma("sp", dst_ap[t * 128:(t + 1) * 128, :], xt[xb][:], [xt_r[xb]], [dst_r], xt_r[xb], store=True)
        kb.barrier()


def host_consts():
    c = {}
    c["ident"] = np.eye(128, dtype=np.float32).astype(ml_dtypes.bfloat16)
    c["identf"] = np.eye(128, dtype=np.float32)
    half = 128
    inv = (np.float32(10000.0) ** (-np.arange(half, dtype=np.float32) / np.float32(half))).astype(np.float32)
    ang = (np.arange(S, dtype=np.float32)[:, None] * inv[None, :]).astype(np.float32)
    c["rope_cos"] = np.ascontiguousarray(np.cos(ang).astype(np.float32).T)
    c["rope_sin"] = np.ascontiguousarray(np.sin(ang).astype(np.float32).T)
    s_idx = (np.arange(32)[None, :, None] * 128 + np.arange(128)[:, None, None]).astype(np.int64)
    k_idx = ((128 * (np.arange(17)[:, None] - 1) + 1 + np.arange(128)[None, :]) % S).astype(np.int64)
    m = (s_idx[None] * k_idx[:, None, None, :]) % S
    th = m.astype(np.float64) * (2.0 * np.pi / S)
    c["dftc"] = np.cos(th).astype(np.float32).astype(ml_dtypes.bfloat16)
    c["dfts"] = np.sin(th).astype(np.float32).astype(ml_dtypes.bfloat16)
    c["rev"] = np.ascontiguousarray(np.eye(128, dtype=np.float32)[:, ::-1]).astype(ml_dtypes.bfloat16)
    cidx = (np.arange(2)[None, :, None] * 128 + np.arange(128)[:, None, None]).astype(np.int64)
    m2 = (cidx * np.arange(256, dtype=np.int64)[None, None, :]) % 256
    th2 = m2.astype(np.float64) * (2.0 * np.pi / 256)
    c["cc"] = np.cos(th2).astype(np.float32).astype(ml_dtypes.bfloat16)
    c["nsc"] = (-np.sin(th2)).astype(np.float32).astype(ml_dtypes.bfloat16)
    c["sc"] = np.sin(th2).astype(np.float32).astype(ml_dtypes.bfloat16)
    return c


ALL_PHASES = ("ret", "ffn0", "fnet", "moe")
SPARSE_MOE = True


def build(phases=ALL_PHASES):
    nc = bass.Bass("TRN2", target_bir_lowering=False)

    def din(name, shape, dtype=F32):
        return nc.dram_tensor(name, list(shape), dtype, kind="ExternalInput").ap()

    def dscr(name, shape, dtype):
        return nc.dram_tensor(name, list(shape), dtype, kind="Internal").ap(), Res(name, accum=True)

    x = din("x", [S, D])
    mix_norm = din("mix_norm", [2, D])
    ffn_norm = din("ffn_norm", [2, D])
    ident = din("ident", [128, 128], BF16)
    identf = din("identf", [128, 128], F32)
    if "ret" in phases:
        w_in = din("ret_w_in", [D, 6144])
        decay = din("ret_decay_logit", [2, 4])
        gn_gain = din("ret_gn_gain", [2048])
        w_out = din("ret_w_out", [2048, D])
        rope_cos = din("rope_cos", [128, S])
        rope_sin = din("rope_sin", [128, S])
    if "ffn0" in phases:
        dwg = din("dense_w_gate", [D, 2816])
        dwu = din("dense_w_up", [D, 2816])
        dwd = din("dense_w_down", [2816, D])
    if "fnet" in phases:
        w_fnet = din("fnet_w_out", [D, D])
        dftc = din("dftc", [17, 128, 32, 128], BF16)
        dfts = din("dfts", [17, 128, 32, 128], BF16)
        cc = din("cc", [128, 2, 256], BF16)
        nsc = din("nsc", [128, 2, 256], BF16)
        scp = din("sc", [128, 2, 256], BF16)
        rev = din("rev", [128, 128], BF16)
    if "moe" in phases:
        router = din("moe_router", [D, 8])
        mwg = din("moe_w_gate", [8, D, 3584])
        mwu = din("moe_w_up", [8, D, 3584])
        mwd = din("moe_w_down", [8, 3584, D])
        final_norm = din("final_norm", [D])
    y = nc.dram_tensor("y", [S, D], F32, kind="ExternalOutput").ap()
    y_r = Res("y", accum=True)
    with ExitStack() as es:
        kb = KB(nc, es)
        cur = (x, Res("x", accum=True))
        order = [p for p in ALL_PHASES if p in phases]
        for p in order:
            last = p == order[-1]
            nxt = (y, y_r) if last else dscr("h_" + p, [S, D], F32)
            if p == "ret":
                qk_s = dscr("qk_s", [8, 128, 4, 16, 128], BF16)
                v_s = dscr("v_s", [S, 2048], BF16)
                sg_s = dscr("sg_s", [S, 2048], BF16)
                sb_s = dscr("sb_s", [32, 128, 8, 512], BF16)
                z_s = dscr("z_s", [S, 2048], BF16)
                ret_proj_phase(kb, cur, mix_norm[0], w_in, gn_gain, ident, rope_cos, rope_sin, qk_s, v_s, sg_s)
                ret_core_phase(kb, decay, ident, qk_s, v_s, sg_s, sb_s, z_s)
                ret_out_phase(kb, cur, nxt, z_s, w_out, ident)
            elif p == "ffn0":
                ffn_phase(kb, cur, nxt, ffn_norm[0], [(dwg, dwu, dwd)], ident, tag="f0", chunk_sizes=[6, 6, 5, 5], nbuf=2)
            elif p == "fnet":
                fourier_phase(kb, cur, nxt, mix_norm[1], w_fnet, ident, dftc, dfts, cc, nsc, scp, rev)
            elif p == "moe":
                if SPARSE_MOE:
                    xg_s = dscr("xg_s", [NT_SLOT * BS, D], BF16)
                    o_s = dscr("o_s", [NT_SLOT * BS, D], F32)
                    moe_sparse_phase(kb, cur, nxt, ffn_norm[1], router, mwg, mwu, mwd, final_norm, ident, identf,
                                     xg_s, o_s)
                else:
                    ffn_phase(kb, cur, nxt, ffn_norm[1], [(mwg[e], mwu[e], mwd[e]) for e in range(8)], ident,
                              router=router, final_g=final_norm, FC=7, tag="f1")
            cur = nxt
        kb.final_wait("sp")
    return nc


_CONSTS = None
PHASE_INPUTS = {
    "ret": ["ret_w_in", "ret_decay_logit", "ret_gn_gain", "ret_w_out"],
    "ffn0": ["dense_w_gate", "dense_w_up", "dense_w_down"],
    "fnet": ["fnet_w_out"],
    "moe": ["moe_router", "moe_w_gate", "moe_w_up", "moe_w_down", "final_norm"],
}
PHASE_CONSTS = {"ret": ["rope_cos", "rope_sin"], "ffn0": [], "fnet": ["dftc", "dfts", "cc", "nsc", "sc", "rev"], "moe": []}


def make_in_maps(inputs, phases=ALL_PHASES, cores=range(NCORES), x_override=None):
    global _CONSTS
    if _CONSTS is None:
        _CONSTS = host_consts()
    shared = {"mix_norm": np.ascontiguousarray(inputs["mix_norm"], dtype=np.float32),
              "ffn_norm": np.ascontiguousarray(inputs["ffn_norm"], dtype=np.float32),
              "ident": _CONSTS["ident"], "identf": _CONSTS["identf"]}
    for p in phases:
        for k in PHASE_INPUTS[p]:
            a = np.asarray(inputs[k], dtype=np.float32)
            if k != "final_norm":
                a = a[0]
            shared[k] = np.ascontiguousarray(a)
        for k in PHASE_CONSTS[p]:
            shared[k] = _CONSTS[k]
    maps = []
    for c in cores:
        m = dict(shared)
        m["x"] = np.ascontiguousarray(inputs["x"][c] if x_override is None else x_override[c], dtype=np.float32)
        maps.append(m)
    return maps


def kernel(**inputs):
    nc = build(ALL_PHASES)
    maps = make_in_maps(inputs)
    res = run_bass_kernel_spmd(nc, maps, core_ids=list(range(NCORES)))
    return np.stack([np.asarray(r["y"], dtype=np.float32) for r in res.results], axis=0)


NT_SLOT = 24
BS = 512


def bc_last(ap, n):
    return bass.AP(ap.tensor, ap.offset, [list(a) for a in ap.ap] + [[0, n]])


def bc_mid(ap2, n):
    a = [list(v) for v in ap2.ap]
    return bass.AP(ap2.tensor, ap2.offset, [a[0], [0, n]] + a[1:])


def moe_sparse_phase(kb, src, dst, norm_g, router, mwg, mwu, mwd, final_g, ident_d, identf_d, xg_s, o_s, tag="ms"):
    nc = kb.nc
    I32 = mybir.dt.int32
    src_ap, src_r = src
    dst_ap, dst_r = dst
    xg_ap, xg_r = xg_s
    o_ap, o_r = o_s
    NTT = S // 128
    FC = 7
    NCH = 4
    with ExitStack() as es:
        ident = es.enter_context(nc.sbuf_tensor(tag + "_ident", [128, 128], BF16))
        ident_r = Res("ident")
        kb.dma("sp", ident[:], ident_d, [], [ident_r], ident_r)
        p1i = es.enter_context(nc.sbuf_tensor(tag + "_p1i", [128, NTT], I32))
        p2i = es.enter_context(nc.sbuf_tensor(tag + "_p2i", [128, NTT], I32))
        g1 = es.enter_context(nc.sbuf_tensor(tag + "_g1", [128, NTT], F32))
        g2 = es.enter_context(nc.sbuf_tensor(tag + "_g2", [128, NTT], F32))
        p1i_r, p2i_r, g1_r, g2_r = Res("p1i"), Res("p2i"), Res("g1"), Res("g2")
        idxg = es.enter_context(nc.sbuf_tensor(tag + "_idxg", [128, NT_SLOT, 32], I32))
        idxd = es.enter_context(nc.sbuf_tensor(tag + "_idxd", [128, NT_SLOT, 28], I32))
        idxg_r, idxd_r = Res("idxg"), Res("idxd")

        with ExitStack() as es0:
            def T(name, shape, dtype=F32):
                return es0.enter_context(nc.sbuf_tensor(tag + "_" + name, shape, dtype)), Res(name)

            fr = Front(kb, es0, ident, ident_r, norm_g, tag + "f", with_T=False)
            identf, identf_r = T("identf", [128, 128])
            kb.dma("sp", identf[:], identf_d, [], [identf_r], identf_r)
            wr, wr_r = T("wr", [128, KT, 8])
            kb.dma("sp", wr[:], router.rearrange("(kt p) e -> p kt e", p=128), [], [wr_r], wr_r)
            hn16, hn16_r = T("hn16", [128, NTT, D], BF16)
            x4 = [es0.enter_context(nc.sbuf_tensor(tag + "_x4%d" % i, [128, 4, D], F32)) for i in range(2)]
            x4_r = [Res("x4%d" % i) for i in range(2)]
            hn32 = [es0.enter_context(nc.sbuf_tensor(tag + "_hn32%d" % i, [128, D], F32)) for i in range(2)]
            hn32_r = [Res("hn32%d" % i) for i in range(2)]
            hT32 = [es0.enter_context(nc.sbuf_tensor(tag + "_hT32%d" % i, [128, KT, 128], F32)) for i in range(2)]
            hT32_r = [Res("hT32%d" % i) for i in range(2)]
            L, L_r = T("L", [128, NTT, 8])
            psT = [es0.enter_context(nc.psum_tensor(tag + "_psT%d" % i, [128, D], F32)) for i in range(2)]
            psT_r = [Res("psT%d" % i) for i in range(2)]
            psL = [es0.enter_context(nc.psum_tensor(tag + "_psL%d" % i, [128, 512], F32)) for i in range(2)]
            psL_r = [Res("psL%d" % i) for i in range(2)]
            zero, zero_r = T("zero", [128, 4, D], BF16)
            kb.op("pool", lambda e: e.memset(zero[:], 0.0), writes=[zero_r])
            n = 0
            pend_router = []

            def router_mm(tt, hb):
                for kt in range(KT):
                    kb.op("pe", lambda e: e.matmul(out=psL[hb][:, 0:8], lhsT=hT32[hb][:, kt, :], rhs=wr[:, kt, :],
                                                   start=(kt == 0), stop=(kt == KT - 1)),
                          reads=[hT32_r[hb], wr_r], writes=[psL_r[hb]], signal=(kt == KT - 1))
                kb.op("dve", lambda e: e.tensor_copy(out=L[:, tt, :], in_=psL[hb][:, 0:8]),
                      reads=[psL_r[hb]], writes=[L_r])

            for G in range(S // 512):
                b = G % 2
                kb.dma("sp", x4[b][:], src_ap[G * 512:(G + 1) * 512, :].rearrange("(j p) d -> p j d", p=128),
                       [src_r], [x4_r[b]], x4_r[b])
                for t in range(G * 3, G * 3 + 3):
                    kb.dma("sp", xg_ap[t * BS:(t + 1) * BS, :].rearrange("(j p) d -> p j d", p=128), zero[:],
                           [zero_r], [xg_r], zero_r, store=True)
                fr.rstd(x4[b][:], x4_r[b], 4)
                for j in range(4):
                    tt = G * 4 + j
                    hb = n % 2
                    n += 1
                    fr.norm_to(hn32[hb][:], hn32_r[hb], x4[b][:, j, :], x4_r[b], j)
                    kb.op("act", lambda e: e.activation(out=hn16[:, tt, :], in_=hn32[hb][:], func=AF.Copy),
                          reads=[hn32_r[hb]], writes=[hn16_r])
                    for kt in range(KT):
                        kb.op("pe", lambda e: e.transpose(out=psT[hb][:, kt * 128:(kt + 1) * 128],
                                                          in_=hn32[hb][:, kt * 128:(kt + 1) * 128], identity=identf[:]),
                              reads=[hn32_r[hb], identf_r], writes=[psT_r[hb]], signal=(kt == KT - 1))
                    kb.op("act", lambda e: e.activation(out=hT32[hb][:], in_=psT[hb][:].rearrange("p (k t) -> p k t", k=KT),
                                                        func=AF.Copy), reads=[psT_r[hb]], writes=[hT32_r[hb]])
                    if pend_router:
                        router_mm(*pend_router.pop())
                    pend_router.append((tt, hb))
            router_mm(*pend_router.pop())
            m1, m1_r = T("m1", [128, NTT])
            m2, m2_r = T("m2", [128, NTT])
            eq, eq_r = T("eq", [128, NTT, 8])
            L2, L2_r = T("L2", [128, NTT, 8])
            mask, mask_r = T("mask", [128, NTT, 8])
            ex, ex_r = T("ex", [128, NTT, 8])
            den, den_r = T("den", [128, NTT])
            gts, gts_r = T("gts", [128, NTT, 8])
            kb.op("dve", lambda e: e.tensor_reduce(out=m1[:], in_=L[:], axis=AX.X, op=ALU.max), reads=[L_r], writes=[m1_r])
            kb.op("dve", lambda e: e.tensor_tensor(out=eq[:], in0=L[:], in1=bc_last(m1[:], 8), op=ALU.is_equal),
                  reads=[L_r, m1_r], writes=[eq_r])
            kb.op("dve", lambda e: e.scalar_tensor_tensor(out=L2[:], in0=eq[:], scalar=-1.0e30, in1=L[:],
                                                          op0=ALU.mult, op1=ALU.add), reads=[eq_r, L_r], writes=[L2_r])
            kb.op("dve", lambda e: e.tensor_reduce(out=m2[:], in_=L2[:], axis=AX.X, op=ALU.max), reads=[L2_r], writes=[m2_r])
            kb.op("dve", lambda e: e.tensor_tensor(out=mask[:], in0=L[:], in1=bc_last(m2[:], 8), op=ALU.is_ge),
                  reads=[L_r, m2_r], writes=[mask_r])
            kb.op("dve", lambda e: e.tensor_tensor(out=ex[:], in0=L[:], in1=bc_last(m1[:], 8), op=ALU.subtract),
                  reads=[L_r, m1_r], writes=[ex_r])
            kb.op("act", lambda e: e.activation(out=ex[:], in_=ex[:], func=AF.Exp), reads=[ex_r], writes=[ex_r])
            kb.op("dve", lambda e: e.tensor_tensor(out=ex[:], in0=ex[:], in1=mask[:], op=ALU.mult),
                  reads=[ex_r, mask_r], writes=[ex_r])
            kb.op("dve", lambda e: e.tensor_reduce(out=den[:], in_=ex[:], axis=AX.X, op=ALU.add), reads=[ex_r], writes=[den_r])
            kb.op("dve", lambda e: e.reciprocal(out=den[:], in_=den[:]), reads=[den_r], writes=[den_r])
            kb.op("dve", lambda e: e.tensor_tensor(out=gts[:], in0=ex[:], in1=bc_last(den[:], 8), op=ALU.mult),
                  reads=[ex_r, den_r], writes=[gts_r])
            maskb, maskb_r = T("maskb", [128, NTT * 8], BF16)
            kb.op("dve", lambda e: e.tensor_copy(out=maskb[:], in_=mask[:].rearrange("p t e -> p (t e)")),
                  reads=[mask_r], writes=[maskb_r])
            ii, ii_r = T("ii", [128, 128], I32)
            dmf, dmf_r = T("dmf", [128, 128])
            Lt, Lt_r = T("Lt", [128, 128], BF16)
            ones, ones_r = T("ones", [128, 128], BF16)
            kb.op("pool", lambda e: e.iota(ii[:], pattern=[[1, 128]], base=0, channel_multiplier=-1), writes=[ii_r])
            kb.op("dve", lambda e: e.tensor_copy(out=dmf[:], in_=ii[:]), reads=[ii_r], writes=[dmf_r])
            kb.op("dve", lambda e: e.tensor_scalar(out=Lt[:], in0=dmf[:], scalar1=0.0, scalar2=None, op0=ALU.is_gt),
                  reads=[dmf_r], writes=[Lt_r])
            kb.op("pool", lambda e: e.memset(ones[:], 1.0), writes=[ones_r])
            kb.op("pe", lambda e: e.matmul(out=psL[0][:, 0:256], lhsT=Lt[:], rhs=maskb[:], start=True, stop=True),
                  reads=[Lt_r, maskb_r], writes=[psL_r[0]])
            kb.op("pe", lambda e: e.matmul(out=psL[1][:, 0:256], lhsT=ones[:], rhs=maskb[:], start=True, stop=True),
                  reads=[ones_r, maskb_r], writes=[psL_r[1]])
            within, within_r = T("within", [128, NTT, 8])
            tot, tot_r = T("tot", [128, NTT, 8])
            off, off_r = T("off", [128, NTT, 8])
            kb.op("dve", lambda e: e.tensor_copy(out=within[:].rearrange("p t e -> p (t e)"), in_=psL[0][:, 0:256]),
                  reads=[psL_r[0]], writes=[within_r])
            kb.op("dve", lambda e: e.tensor_copy(out=tot[:].rearrange("p t e -> p (t e)"), in_=psL[1][:, 0:256]),
                  reads=[psL_r[1]], writes=[tot_r])
            kb.op("dve", lambda e: e.memset(off[:, 0, :], 0.0), writes=[off_r])
            for t in range(1, NTT):
                kb.op("dve", lambda e: e.tensor_tensor(out=off[:, t, :], in0=off[:, t - 1, :], in1=tot[:, t - 1, :], op=ALU.add),
                      reads=[off_r, tot_r], writes=[off_r])
            cntp, cntp_r = T("cntp", [128, 8])
            pc, pc_r = T("pc", [128, 8])
            poff, poff_r = T("poff", [128, 8])
            pend, pend_r = T("pend", [128, 8])
            kb.op("dve", lambda e: e.tensor_tensor(out=cntp[:], in0=off[:, NTT - 1, :], in1=tot[:, NTT - 1, :], op=ALU.add),
                  reads=[off_r, tot_r], writes=[cntp_r])
            kb.op("dve", lambda e: e.memset(pc[:], 0.0), writes=[pc_r])
            for k_ in range(8):
                kb.op("dve", lambda e: e.scalar_tensor_tensor(out=pc[:], in0=cntp[:], scalar=float(BS * k_), in1=pc[:],
                                                              op0=ALU.is_gt, op1=ALU.add),
                      reads=[cntp_r, pc_r], writes=[pc_r])
            kb.op("dve", lambda e: e.tensor_scalar(out=pc[:], in0=pc[:], scalar1=float(BS), scalar2=None, op0=ALU.mult),
                  reads=[pc_r], writes=[pc_r])
            kb.op("dve", lambda e: e.memset(poff[:, 0:1], 0.0), writes=[poff_r])
            for e_ in range(1, 8):
                kb.op("dve", lambda e: e.tensor_tensor(out=poff[:, e_:e_ + 1], in0=poff[:, e_ - 1:e_], in1=pc[:, e_ - 1:e_],
                                                       op=ALU.add), reads=[poff_r, pc_r], writes=[poff_r])
            kb.op("dve", lambda e: e.tensor_tensor(out=pend[:], in0=poff[:], in1=pc[:], op=ALU.add),
                  reads=[poff_r, pc_r], writes=[pend_r])
            ms, ms_r = T("ms", [128, NTT, 8])
            kb.op("dve", lambda e: e.tensor_tensor(out=ms[:], in0=within[:], in1=off[:], op=ALU.add),
                  reads=[within_r, off_r], writes=[ms_r])
            kb.op("dve", lambda e: e.scalar_tensor_tensor(out=ms[:], in0=ms[:], scalar=1.0, in1=bc_mid(poff[:], NTT),
                                                          op0=ALU.add, op1=ALU.add), reads=[ms_r, poff_r], writes=[ms_r])
            kb.op("dve", lambda e: e.tensor_tensor(out=ms[:], in0=ms[:], in1=mask[:], op=ALU.mult),
                  reads=[ms_r, mask_r], writes=[ms_r])
            pa, pa_r = T("pa", [128, NTT])
            pb_, pb_r = T("pb", [128, NTT])
            kb.op("dve", lambda e: e.tensor_reduce(out=pa[:], in_=ms[:], axis=AX.X, op=ALU.max), reads=[ms_r], writes=[pa_r])
            kb.op("dve", lambda e: e.tensor_reduce(out=pb_[:], in_=ms[:], axis=AX.X, op=ALU.add), reads=[ms_r], writes=[pb_r])
            kb.op("dve", lambda e: e.tensor_tensor(out=pb_[:], in0=pb_[:], in1=pa[:], op=ALU.subtract),
                  reads=[pb_r, pa_r], writes=[pb_r])
            kb.op("dve", lambda e: e.tensor_tensor(out=eq[:], in0=ms[:], in1=bc_last(pa[:], 8), op=ALU.is_equal),
                  reads=[ms_r, pa_r], writes=[eq_r])
            kb.op("dve", lambda e: e.tensor_tensor(out=eq[:], in0=eq[:], in1=gts[:], op=ALU.mult),
                  reads=[eq_r, gts_r], writes=[eq_r])
            kb.op("dve", lambda e: e.tensor_reduce(out=g1[:], in_=eq[:], axis=AX.X, op=ALU.add), reads=[eq_r], writes=[g1_r])
            kb.op("dve", lambda e: e.tensor_scalar(out=g2[:], in0=g1[:], scalar1=-1.0, scalar2=1.0, op0=ALU.mult, op1=ALU.add),
                  reads=[g1_r], writes=[g2_r])
            kb.op("dve", lambda e: e.tensor_scalar(out=pa[:], in0=pa[:], scalar1=-1.0, scalar2=None, op0=ALU.add),
                  reads=[pa_r], writes=[pa_r])
            kb.op("dve", lambda e: e.tensor_scalar(out=pb_[:], in0=pb_[:], scalar1=-1.0, scalar2=None, op0=ALU.add),
                  reads=[pb_r], writes=[pb_r])
            kb.op("dve", lambda e: e.tensor_copy(out=p1i[:], in_=pa[:]), reads=[pa_r], writes=[p1i_r])
            kb.op("dve", lambda e: e.tensor_copy(out=p2i[:], in_=pb_[:]), reads=[pb_r], writes=[p2i_r])
            cmp, cmp_r = T("cmp", [128, NT_SLOT, 8])
            et, et_r = T("et", [128, NT_SLOT])
            for t in range(NT_SLOT):
                kb.op("dve", lambda e: e.tensor_scalar(out=cmp[:, t, :], in0=pend[:], scalar1=float(BS * t), scalar2=None,
                                                       op0=ALU.is_le), reads=[pend_r], writes=[cmp_r])
            kb.op("dve", lambda e: e.tensor_reduce(out=et[:], in_=cmp[:], axis=AX.X, op=ALU.add), reads=[cmp_r], writes=[et_r])
            kb.op("dve", lambda e: e.tensor_scalar(out=et[:], in0=et[:], scalar1=7.0, scalar2=None, op0=ALU.min),
                  reads=[et_r], writes=[et_r])
            sgi, sgi_r = T("sgi", [128, 32], I32)
            sdi, sdi_r = T("sdi", [128, 28], I32)
            sgf, sgf_r = T("sgf", [128, 32])
            sdf, sdf_r = T("sdf", [128, 28])
            kb.op("pool", lambda e: e.iota(sgi[:], pattern=[[512, 8], [1, 4]], base=0, channel_multiplier=4), writes=[sgi_r])
            kb.op("pool", lambda e: e.iota(sdi[:], pattern=[[896, 4], [128, 7]], base=0, channel_multiplier=1), writes=[sdi_r])
            kb.op("dve", lambda e: e.tensor_copy(out=sgf[:], in_=sgi[:]), reads=[sgi_r], writes=[sgf_r])
            kb.op("dve", lambda e: e.tensor_copy(out=sdf[:], in_=sdi[:]), reads=[sdi_r], writes=[sdf_r])
            igf, igf_r = T("igf", [128, NT_SLOT, 32])
            idf, idf_r = T("idf", [128, NT_SLOT, 28])
            etg, etg_r = T("etg", [128, NT_SLOT])
            etd, etd_r = T("etd", [128, NT_SLOT])
            kb.op("dve", lambda e: e.tensor_scalar(out=etg[:], in0=et[:], scalar1=4096.0, scalar2=None, op0=ALU.mult),
                  reads=[et_r], writes=[etg_r])
            kb.op("dve", lambda e: e.tensor_scalar(out=etd[:], in0=et[:], scalar1=3584.0, scalar2=None, op0=ALU.mult),
                  reads=[et_r], writes=[etd_r])
            for t in range(NT_SLOT):
                kb.op("dve", lambda e: e.tensor_scalar(out=igf[:, t, :], in0=sgf[:], scalar1=etg[:, t:t + 1], scalar2=None,
                                                       op0=ALU.add), reads=[etg_r, sgf_r], writes=[igf_r])
                kb.op("dve", lambda e: e.tensor_scalar(out=idf[:, t, :], in0=sdf[:], scalar1=etd[:, t:t + 1], scalar2=None,
                                                       op0=ALU.add), reads=[etd_r, sdf_r], writes=[idf_r])
            kb.op("dve", lambda e: e.tensor_copy(out=idxg[:], in_=igf[:]), reads=[igf_r], writes=[idxg_r])
            kb.op("dve", lambda e: e.tensor_copy(out=idxd[:], in_=idf[:]), reads=[idf_r], writes=[idxd_r])
            kb._wait("pool", kb._deps([xg_r], []))
            for tt in range(NTT):
                for (pi, pi_r) in ((p1i, p1i_r), (p2i, p2i_r)):
                    kb._wait("pool", kb._deps([hn16_r, pi_r], []))
                    if hn16_r.ssem is None:
                        hn16_r.ssem = kb.get_sem(fresh=True)
                    sem = hn16_r.ssem
                    ins = nc.gpsimd.indirect_dma_start(out=xg_ap, out_offset=bass.IndirectOffsetOnAxis(ap=pi[:, tt:tt + 1], axis=0),
                                                       in_=hn16[:, tt, :], in_offset=None)
                    ins.then_inc(sem, 16)
                    kb.cnt[sem] += 16
                    kb._register(sem, kb.cnt[sem], [hn16_r, pi_r], [xg_r])
            kb.barrier()
        with ExitStack() as es1:
            xgt = [es1.enter_context(nc.sbuf_tensor(tag + "_xgt%d" % i, [128, 4, D], BF16)) for i in range(2)]
            xgt_r = [Res("xgt%d" % i) for i in range(2)]
            xT = [es1.enter_context(nc.sbuf_tensor(tag + "_xT%d" % i, [128, KT, BS], BF16)) for i in range(2)]
            xT_r = [Res("xT%d" % i) for i in range(2)]
            wg = [es1.enter_context(nc.sbuf_tensor(tag + "_wg%d" % i, [128, KT, FC * 128], BF16)) for i in range(2)]
            wu = [es1.enter_context(nc.sbuf_tensor(tag + "_wu%d" % i, [128, KT, FC * 128], BF16)) for i in range(2)]
            wd = [es1.enter_context(nc.sbuf_tensor(tag + "_wd%d" % i, [128, FC, D], BF16)) for i in range(2)]
            wg_r = [Res("wg%d" % i) for i in range(2)]
            wu_r = [Res("wu%d" % i) for i in range(2)]
            wd_r = [Res("wd%d" % i) for i in range(2)]
            actT = [es1.enter_context(nc.sbuf_tensor(tag + "_actT%d" % i, [128, FC, BS], BF16)) for i in range(2)]
            actT_r = [Res("actT%d" % i) for i in range(2)]
            sg = [es1.enter_context(nc.sbuf_tensor(tag + "_sg%d" % i, [128, 512], F32)) for i in range(2)]
            sg_r = [Res("sg%d" % i) for i in range(2)]
            oacc = [es1.enter_context(nc.sbuf_tensor(tag + "_oacc%d" % i, [128, 4, D], F32)) for i in range(2)]
            oacc_r = [Res("oacc%d" % i) for i in range(2)]
            psT = [es1.enter_context(nc.psum_tensor(tag + "_psX%d" % i, [128, D], BF16)) for i in range(2)]
            psT_r = [Res("psX%d" % i) for i in range(2)]
            psG = [es1.enter_context(nc.psum_tensor(tag + "_psG%d" % i, [128, 512], F32)) for i in range(2)]
            psU = [es1.enter_context(nc.psum_tensor(tag + "_psU%d" % i, [128, 512], F32)) for i in range(2)]
            psO = [es1.enter_context(nc.psum_tensor(tag + "_psO%d" % i, [128, 512], F32)) for i in range(2)]
            psG_r = [Res("psG%d" % i) for i in range(2)]
            psU_r = [Res("psU%d" % i) for i in range(2)]
            psO_r = [Res("psO%d" % i) for i in range(2)]
            wg_flat = mwg.rearrange("e d (c f) -> (e d c) f", f=FC * 128)
            wu_flat = mwu.rearrange("e d (c f) -> (e d c) f", f=FC * 128)
            wd_flat = mwd.rearrange("e f d -> (e f) d")

            def gather(dst_ap_, dst_r_, src_flat, idx_ap, idx_r):
                kb._wait("pool", kb._deps([idx_r], []))
                if dst_r_.lsem is None:
                    dst_r_.lsem = kb.get_sem(fresh=True)
                sem = dst_r_.lsem
                ins = nc.gpsimd.indirect_dma_start(out=dst_ap_, out_offset=None, in_=src_flat,
                                                   in_offset=bass.IndirectOffsetOnAxis(ap=idx_ap, axis=0))
                ins.then_inc(sem, 16)
                kb.cnt[sem] += 16
                dst_r_.writes[sem] = kb.cnt[sem]
                idx_r.reads[sem] = kb.cnt[sem]

            wseq = [(t, c) for t in range(NT_SLOT) for c in range(NCH)]

            def load_w(i):
                t, c = wseq[i]
                b = i % 2
                for r_ in (wg_r[b], wu_r[b], wd_r[b]):
                    kb._wait("pool", kb._deps([], [r_]))
                    r_.writes = {}
                    r_.reads = {}
                for kt in range(KT):
                    gather(wg[b][:, kt, :], wg_r[b], wg_flat, idxg[:, t, kt * 4 + c:kt * 4 + c + 1], idxg_r)
                    gather(wu[b][:, kt, :], wu_r[b], wu_flat, idxg[:, t, kt * 4 + c:kt * 4 + c + 1], idxg_r)
                for fl in range(FC):
                    gather(wd[b][:, fl, :], wd_r[b], wd_flat, idxd[:, t, c * 7 + fl:c * 7 + fl + 1], idxd_r)

            def load_x(t):
                b = t % 2
                kb.dma("sp", xgt[b][:], xg_ap[t * BS:(t + 1) * BS, :].rearrange("(j p) d -> p j d", p=128),
                       [xg_r], [xgt_r[b]], xgt_r[b])

            load_x(0)
            load_w(0)
            wi = 0
            gcount = 0
            ocount = 0
            tcount = 0
            tcs = {"c": 0}

            def transp_x(t):
                xb_ = t % 2
                for j in range(4):
                    tb = tcs["c"] % 2
                    tcs["c"] += 1
                    for kt in range(KT):
                        kb.op("pe", lambda e: e.transpose(out=psT[tb][:, kt * 128:(kt + 1) * 128],
                                                          in_=xgt[xb_][:, j, kt * 128:(kt + 1) * 128], identity=ident[:]),
                              reads=[xgt_r[xb_], ident_r], writes=[psT_r[tb]], signal=(kt == KT - 1))
                    kb.op("act", lambda e: e.activation(out=xT[xb_][:, :, j * 128:(j + 1) * 128],
                                                        in_=psT[tb][:].rearrange("p (k t) -> p k t", k=KT), func=AF.Copy),
                          reads=[psT_r[tb]], writes=[xT_r[xb_]])

            transp_x(0)
            for t in range(NT_SLOT):
                xb = t % 2
                if t + 1 < NT_SLOT:
                    load_x(t + 1)
                ob = t % 2
                for c in range(NCH):
                    if c == 2 and t + 1 < NT_SLOT:
                        transp_x(t + 1)
                    b = wi % 2
                    if wi + 1 < len(wseq):
                        load_w(wi + 1)
                    ab = wi % 2
                    for fl in range(FC):
                        gb = gcount % 2
                        gcount += 1
                        for (wt, wt_r, pst, pst_r) in ((wg[b], wg_r[b], psG[gb], psG_r[gb]), (wu[b], wu_r[b], psU[gb], psU_r[gb])):
                            for kt in range(KT):
                                kb.op("pe", lambda e: e.matmul(out=pst[:], lhsT=wt[:, kt, fl * 128:(fl + 1) * 128],
                                                               rhs=xT[xb][:, kt, :], start=(kt == 0), stop=(kt == KT - 1)),
                                      reads=[wt_r, xT_r[xb]], writes=[pst_r], signal=(kt == KT - 1))
                        kb.op("act", lambda e: e.activation(out=sg[gb][:], in_=psG[gb][:], func=AF.Silu),
                              reads=[psG_r[gb]], writes=[sg_r[gb]])
                        kb.op("dve", lambda e: e.tensor_tensor(out=actT[ab][:, fl, :], in0=sg[gb][:], in1=psU[gb][:], op=ALU.mult),
                              reads=[sg_r[gb], psU_r[gb]], writes=[actT_r[ab]])
                    for j in range(4):
                        for dh in range(2):
                            pb2 = ocount % 2
                            ocount += 1
                            for fl in range(FC):
                                kb.op("pe", lambda e: e.matmul(out=psO[pb2][:], lhsT=actT[ab][:, fl, j * 128:(j + 1) * 128],
                                                               rhs=wd[b][:, fl, dh * 512:(dh + 1) * 512],
                                                               start=(fl == 0), stop=(fl == FC - 1)),
                                      reads=[actT_r[ab], wd_r[b]], writes=[psO_r[pb2]], signal=(fl == FC - 1))
                            if c == 0:
                                kb.op("act", lambda e: e.activation(out=oacc[ob][:, j, dh * 512:(dh + 1) * 512], in_=psO[pb2][:],
                                                                    func=AF.Copy), reads=[psO_r[pb2]], writes=[oacc_r[ob]])
                            else:
                                kb.op("dve", lambda e: e.tensor_tensor(out=oacc[ob][:, j, dh * 512:(dh + 1) * 512], in0=psO[pb2][:],
                                                                       in1=oacc[ob][:, j, dh * 512:(dh + 1) * 512], op=ALU.add),
                                      reads=[psO_r[pb2], oacc_r[ob]], writes=[oacc_r[ob]])
                    wi += 1
                kb.dma("sp", o_ap[t * BS:(t + 1) * BS, :].rearrange("(j p) d -> p j d", p=128), oacc[ob][:],
                       [oacc_r[ob]], [o_r], oacc_r[ob], store=True)
            kb.barrier()
        with ExitStack() as es2:
            fr2 = Front(kb, es2, ident, ident_r, final_g, tag + "g", with_T=False)
            x4 = [es2.enter_context(nc.sbuf_tensor(tag + "_y4%d" % i, [128, 4, D], F32)) for i in range(3)]
            x4_r = [Res("y4%d" % i) for i in range(3)]
            ga = [es2.enter_context(nc.sbuf_tensor(tag + "_ga%d" % i, [128, D], F32)) for i in range(8)]
            ga_r = [Res("ga%d" % i) for i in range(8)]
            n = 0

            def loadF(G):
                kb.dma("sp", x4[G % 3][:], src_ap[G * 512:(G + 1) * 512, :].rearrange("(j p) d -> p j d", p=128),
                       [src_r], [x4_r[G % 3]], x4_r[G % 3])

            loadF(0)
            loadF(1)
            for G in range(S // 512):
                b = G % 3
                if G + 2 < S // 512:
                    loadF(G + 2)
                for j in range(4):
                    tt = G * 4 + j
                    for (pi, pi_r, gg, gg_r) in ((p1i, p1i_r, g1, g1_r), (p2i, p2i_r, g2, g2_r)):
                        gb = n % 8
                        n += 1
                        kb._wait("pool", kb._deps([o_r, pi_r], [ga_r[gb]]))
                        if ga_r[gb].lsem is None:
                            ga_r[gb].lsem = kb.get_sem(fresh=True)
                        sem = ga_r[gb].lsem
                        ins = nc.gpsimd.indirect_dma_start(out=ga[gb][:], out_offset=None, in_=o_ap,
                                                           in_offset=bass.IndirectOffsetOnAxis(ap=pi[:, tt:tt + 1], axis=0))
                        ins.then_inc(sem, 16)
                        kb.cnt[sem] += 16
                        kb._register(sem, kb.cnt[sem], [o_r, pi_r], [ga_r[gb]])
                        kb.op("dve", lambda e: e.scalar_tensor_tensor(out=x4[b][:, j, :], in0=ga[gb][:], scalar=gg[:, tt:tt + 1],
                                                                      in1=x4[b][:, j, :], op0=ALU.mult, op1=ALU.add),
                              reads=[ga_r[gb], gg_r, x4_r[b]], writes=[x4_r[b]])
                fr2.rstd(x4[b][:], x4_r[b], 4)
                for j in range(4):
                    fr2.norm_to(x4[b][:, j, :], x4_r[b], x4[b][:, j, :], x4_r[b], j)
                kb.dma("sp", dst_ap[G * 512:(G + 1) * 512, :].rearrange("(j p) d -> p j d", p=128), x4[b][:],
                       [x4_r[b]], [dst_r], x4_r[b], store=True)
            kb.barrier()
```

```python
import numpy as np
import ml_dtypes
from contextlib import ExitStack
import concourse.bass as bass
import concourse.mybir as mybir
from concourse.bass_utils import run_bass_kernel_spmd

F32 = mybir.dt.float32
BF16 = mybir.dt.bfloat16
ALU = mybir.AluOpType
AF = mybir.ActivationFunctionType
AX = mybir.AxisListType

D = 1024
S = 4096
NCORES = 8
EPS = 1e-6
KT = D // 128


class Res:
    __slots__ = ("name", "writes", "reads", "accum", "lsem", "ssem")

    def __init__(self, name, accum=False):
        self.name = name
        self.writes = {}
        self.reads = {}
        self.accum = accum
        self.lsem = None
        self.ssem = None


class KB:
    def __init__(self, nc, es):
        self.nc = nc
        self.es = es
        self.engs = {"pe": nc.tensor, "act": nc.scalar, "dve": nc.vector, "pool": nc.gpsimd, "sp": nc.sync}
        self.esem = {}
        self.cnt = {}
        self.waited = {n: {} for n in self.engs}
        self.free_sems = []
        self.phase_sems = []
        self.nsem = 0
        for n in self.engs:
            s = es.enter_context(nc.semaphore("es_" + n))
            self.esem[n] = s
            self.cnt[s] = 0

    def get_sem(self, fresh=False):
        if self.free_sems and not fresh:
            s = self.free_sems.pop()
        else:
            s = self.es.enter_context(self.nc.semaphore("ds%d" % self.nsem))
            self.nsem += 1
            self.cnt[s] = 0
        if not fresh:
            self.phase_sems.append(s)
        return s

    def _wait(self, eng, evs):
        w = self.waited[eng]
        e = self.engs[eng]
        for sem, val in evs.items():
            if w.get(sem, 0) < val:
                e.wait_ge(sem, val)
                w[sem] = val

    @staticmethod
    def _deps(reads, writes):
        evs = {}
        for r in reads:
            for s, v in r.writes.items():
                if evs.get(s, 0) < v:
                    evs[s] = v
        for wr in writes:
            if not wr.accum:
                for s, v in wr.writes.items():
                    if evs.get(s, 0) < v:
                        evs[s] = v
            for s, v in wr.reads.items():
                if evs.get(s, 0) < v:
                    evs[s] = v
        return evs

    @staticmethod
    def _register(sem, v, reads, writes):
        for r in reads:
            r.reads[sem] = v
        for w in writes:
            if w.accum:
                w.writes[sem] = v
            else:
                w.writes = {sem: v}
                w.reads = {}

    def op(self, eng, fn, reads=(), writes=(), signal=True):
        self._wait(eng, self._deps(reads, writes))
        ins = fn(self.engs[eng])
        if signal:
            sem = self.esem[eng]
            self.cnt[sem] += 1
            ins.then_inc(sem, 1)
            self._register(sem, self.cnt[sem], reads, writes)
        return ins

    def dma(self, q, out, in_, reads, writes, own, store=False):
        self._wait(q, self._deps(reads, writes))
        if store:
            if own.ssem is None:
                own.ssem = self.get_sem(fresh=(q == "pool"))
            sem = own.ssem
        else:
            if own.lsem is None:
                own.lsem = self.get_sem(fresh=(q == "pool"))
            sem = own.lsem
        ins = self.engs[q].dma_start(out=out, in_=in_)
        ins.then_inc(sem, 16)
        self.cnt[sem] += 16
        self._register(sem, self.cnt[sem], reads, writes)
        return ins

    def barrier(self, recycle=True):
        allev = {s: c for s, c in self.cnt.items() if c > 0}
        for n in self.engs:
            self._wait(n, allev)
        if recycle:
            self.free_sems.extend(self.phase_sems)
            self.phase_sems = []

    def final_wait(self, eng="sp"):
        allev = {s: c for s, c in self.cnt.items() if c > 0}
        self._wait(eng, allev)


def bcast_row(ap1d, n):
    return ap1d.rearrange("(o n) -> o n", o=1).partition_broadcast(128)


class Front:
    def __init__(self, kb, es, ident, ident_r, gvec_ap, tag, with_T=True):
        nc = kb.nc
        self.kb = kb
        self.ident = ident
        self.ident_r = ident_r
        self.g = es.enter_context(nc.sbuf_tensor(tag + "_g", [128, D], F32))
        self.g_r = Res(tag + "_g")
        kb.dma("sp", self.g[:], bcast_row(gvec_ap, D), [], [self.g_r], self.g_r)
        self.junk = es.enter_context(nc.sbuf_tensor(tag + "_junk", [128, D], BF16))
        self.junk_r = Res(tag + "_junk")
        self.NR = 4
        self.ss_l = [es.enter_context(nc.sbuf_tensor(tag + "_ss%d" % i, [128, 8], F32)) for i in range(self.NR)]
        self.ss_rl = [Res(tag + "_ss%d" % i) for i in range(self.NR)]
        self.rs_l = [es.enter_context(nc.sbuf_tensor(tag + "_rs%d" % i, [128, 8], F32)) for i in range(self.NR)]
        self.rs_rl = [Res(tag + "_rs%d" % i) for i in range(self.NR)]
        self.cur = 0
        self.n = 0
        if not with_T:
            return
        self.hn = [es.enter_context(nc.sbuf_tensor(tag + "_hn%d" % i, [128, D], BF16)) for i in range(2)]
        self.hn_r = [Res(tag + "_hn%d" % i) for i in range(2)]
        self.ps = [es.enter_context(nc.psum_tensor(tag + "_pst%d" % i, [128, D], BF16)) for i in range(2)]
        self.ps_r = [Res(tag + "_pst%d" % i) for i in range(2)]

    @property
    def ss(self):
        return self.ss_l[self.cur]

    @property
    def ss_r(self):
        return self.ss_rl[self.cur]

    @property
    def rs(self):
        return self.rs_l[self.cur]

    @property
    def rs_r(self):
        return self.rs_rl[self.cur]

    def rstd(self, x3, x_r, nj):
        kb = self.kb
        self.cur = (self.cur + 1) % self.NR
        for j in range(nj):
            kb.op("act", lambda e, j=j: e.activation(out=self.junk[:], in_=x3[:, j, :], func=AF.Square,
                                                     accum_out=self.ss[:, j:j + 1]),
                  reads=[x_r], writes=[self.junk_r, self.ss_r])
        kb.op("dve", lambda e: e.tensor_scalar(out=self.rs[:, 0:nj], in0=self.ss[:, 0:nj], scalar1=1.0 / D,
                                                scalar2=EPS, op0=ALU.mult, op1=ALU.add),
              reads=[self.ss_r], writes=[self.rs_r])
        kb.op("act", lambda e: e.activation(out=self.rs[:, 0:nj], in_=self.rs[:, 0:nj], func=AF.Sqrt),
              reads=[self.rs_r], writes=[self.rs_r])
        kb.op("dve", lambda e: e.reciprocal(out=self.rs[:, 0:nj], in_=self.rs[:, 0:nj]),
              reads=[self.rs_r], writes=[self.rs_r])

    def norm_to(self, out_ap, out_r, x2, x_r, j):
        self.kb.op("dve", lambda e: e.scalar_tensor_tensor(out=out_ap, in0=x2, scalar=self.rs[:, j:j + 1],
                                                            in1=self.g[:], op0=ALU.mult, op1=ALU.mult),
                   reads=[x_r, self.rs_r, self.g_r], writes=[out_r])

    def norm_T(self, x3, x_r, nj, xT, xT_r, tok0):
        kb = self.kb
        self.rstd(x3, x_r, nj)
        for j in range(nj):
            b = self.n % 2
            self.n += 1
            hn, hn_r, ps, ps_r = self.hn[b], self.hn_r[b], self.ps[b], self.ps_r[b]
            self.norm_to(hn[:], hn_r, x3[:, j, :], x_r, j)
            for kt in range(KT):
                last = kt == KT - 1
                kb.op("pe", lambda e, kt=kt: e.transpose(out=ps[:, kt * 128:(kt + 1) * 128],
                                                         in_=hn[:, kt * 128:(kt + 1) * 128], identity=self.ident[:]),
                      reads=[hn_r, self.ident_r], writes=[ps_r], signal=last)
            t0 = tok0 + j * 128
            kb.op("act", lambda e, t0=t0: e.activation(out=xT[:, :, t0:t0 + 128],
                                                       in_=ps[:].rearrange("p (k t) -> p k t", k=KT),
                                                       func=AF.Copy),
                  reads=[ps_r], writes=[xT_r])


def ffn_phase(kb, src, dst, norm_g, experts, ident_d, router=None, final_g=None, FC=7, tag="ffn", chunk_sizes=None, nbuf=1):
    nc = kb.nc
    src_ap, src_r = src
    dst_ap, dst_r = dst
    F = experts[0][0].shape[1]
    NFT = F // 128
    if chunk_sizes is None:
        assert NFT % FC == 0
        chunk_sizes = [FC] * (NFT // FC)
    assert sum(chunk_sizes) == NFT
    FC = max(chunk_sizes)
    NCH = len(chunk_sizes)
    choff = [sum(chunk_sizes[:i]) for i in range(NCH)]
    TT = 1024
    NJ = TT // 128
    NE = len(experts)
    with ExitStack() as es:
        ident = es.enter_context(nc.sbuf_tensor(tag + "_ident", [128, 128], BF16))
        ident_r = Res("ident")
        kb.dma("sp", ident[:], ident_d, [], [ident_r], ident_r)
        fr = Front(kb, es, ident, ident_r, norm_g, tag + "f")
        if final_g is not None:
            gf = es.enter_context(nc.sbuf_tensor(tag + "_gf", [128, D], F32))
            gf_r = Res("gf")
            kb.dma("sp", gf[:], bcast_row(final_g, D), [], [gf_r], gf_r)
        assert nbuf == 1 or router is None
        accs = [es.enter_context(nc.sbuf_tensor(tag + "_acc%d" % i, [128, NJ, D], F32)) for i in range(nbuf)]
        accs_r = [Res("acc%d" % i) for i in range(nbuf)]
        xTs = [es.enter_context(nc.sbuf_tensor(tag + "_xT%d" % i, [128, KT, TT], BF16)) for i in range(nbuf)]
        xTs_r = [Res("xT%d" % i) for i in range(nbuf)]
        actT = [es.enter_context(nc.sbuf_tensor(tag + "_actT%d" % i, [128, FC, TT], BF16)) for i in range(2)]
        actT_r = [Res("actT%d" % i) for i in range(2)]
        wg = [es.enter_context(nc.sbuf_tensor(tag + "_wg%d" % i, [128, KT, FC * 128], BF16)) for i in range(2)]
        wu = [es.enter_context(nc.sbuf_tensor(tag + "_wu%d" % i, [128, KT, FC * 128], BF16)) for i in range(2)]
        wd = [es.enter_context(nc.sbuf_tensor(tag + "_wd%d" % i, [128, FC, D], BF16)) for i in range(2)]
        wg_r = [Res("wg%d" % i) for i in range(2)]
        wu_r = [Res("wu%d" % i) for i in range(2)]
        wd_r = [Res("wd%d" % i) for i in range(2)]
        sg = [es.enter_context(nc.sbuf_tensor(tag + "_sg%d" % i, [128, 512], F32)) for i in range(2)]
        sg_r = [Res("sg%d" % i) for i in range(2)]
        psG = [es.enter_context(nc.psum_tensor(tag + "_psG%d" % i, [128, 512], F32)) for i in range(2)]
        psU = [es.enter_context(nc.psum_tensor(tag + "_psU%d" % i, [128, 512], F32)) for i in range(2)]
        psG_r = [Res("psG%d" % i) for i in range(2)]
        psU_r = [Res("psU%d" % i) for i in range(2)]
        psO = [es.enter_context(nc.psum_tensor(tag + "_psO%d" % i, [128, 512], F32)) for i in range(2)]
        psO_r = [Res("psO%d" % i) for i in range(2)]
        if router is not None:
            wr = es.enter_context(nc.sbuf_tensor(tag + "_wr", [128, KT, 8], BF16))
            wr_r = Res("wr")
            kb.dma("pool", wr[:], router.rearrange("(kt p) e -> p kt e", p=128), [], [wr_r], wr_r)
            gates = es.enter_context(nc.sbuf_tensor(tag + "_gates", [128, NJ, 8], F32))
            gates_r = Res("gates")
            lg = es.enter_context(nc.sbuf_tensor(tag + "_lg", [128, 8], F32))
            lg_r = Res("lg")
            mx = es.enter_context(nc.sbuf_tensor(tag + "_mx", [128, 8], F32))
            mx_r = Res("mx")
            msk = es.enter_context(nc.sbuf_tensor(tag + "_msk", [128, 8], F32))
            msk_r = Res("msk")
            ex = es.enter_context(nc.sbuf_tensor(tag + "_ex", [128, 8], F32))
            ex_r = Res("ex")
            sm = es.enter_context(nc.sbuf_tensor(tag + "_sm", [128, 2], F32))
            sm_r = Res("sm")

        wseq = [(T, e, ch) for T in range(S // TT) for e in range(NE) for ch in range(NCH)]
        state = {"loaded": 0}

        def load_w(i):
            T, e, ch = wseq[i]
            b = i % 2
            wg_d, wu_d, wd_d = experts[e]
            f0 = choff[ch] * 128
            cw = chunk_sizes[ch] * 128
            kb.dma("pool", wg[b][:, :, 0:cw], wg_d[:, f0:f0 + cw].rearrange("(kt p) f -> p kt f", p=128),
                   [], [wg_r[b]], wg_r[b])
            kb.dma("pool", wu[b][:, :, 0:cw], wu_d[:, f0:f0 + cw].rearrange("(kt p) f -> p kt f", p=128),
                   [], [wu_r[b]], wu_r[b])
            kb.dma("pool", wd[b][:, 0:chunk_sizes[ch], :], wd_d[f0:f0 + cw, :].rearrange("(ft p) d -> p ft d", p=128),
                   [], [wd_r[b]], wd_r[b])

        load_w(0)
        wi = 0
        gcount = 0
        ocount = 0
        NT = S // TT

        def front(T):
            t0 = T * TT
            acc, acc_r = accs[T % nbuf], accs_r[T % nbuf]
            xT, xT_r = [xTs[T % nbuf]], [xTs_r[T % nbuf]]
            nonlocal ocount
            for hf in range(2):
                kb.dma("sp", acc[:, hf * 4:(hf + 1) * 4, :],
                       src_ap[t0 + hf * 512:t0 + (hf + 1) * 512, :].rearrange("(j p) d -> p j d", p=128),
                       [src_r], [acc_r], acc_r)
            for hf in range(2):
                fr.norm_T(acc[:, hf * 4:(hf + 1) * 4, :], acc_r, 4, xT[0], xT_r[0], hf * 512)
            if router is not None:
                for j in range(NJ):
                    pl = psO[ocount % 2]
                    pl_r = psO_r[ocount % 2]
                    ocount += 1
                    for kt in range(KT):
                        kb.op("pe", lambda e, kt=kt, j=j: e.matmul(out=pl[:, 0:8], lhsT=xT[0][:, kt, j * 128:(j + 1) * 128],
                                                                  rhs=wr[:, kt, :], start=(kt == 0), stop=(kt == KT - 1)),
                              reads=[xT_r[0], wr_r], writes=[pl_r], signal=(kt == KT - 1))
                    kb.op("act", lambda e: e.activation(out=lg[:], in_=pl[:, 0:8], func=AF.Copy),
                          reads=[pl_r], writes=[lg_r])
                    kb.op("dve", lambda e: e.max(out=mx[:], in_=lg[:]), reads=[lg_r], writes=[mx_r])
                    kb.op("dve", lambda e: e.tensor_scalar(out=msk[:], in0=lg[:], scalar1=mx[:, 1:2], scalar2=None,
                                                           op0=ALU.is_ge), reads=[lg_r, mx_r], writes=[msk_r])
                    kb.op("dve", lambda e: e.tensor_scalar(out=ex[:], in0=lg[:], scalar1=mx[:, 0:1], scalar2=None,
                                                           op0=ALU.subtract), reads=[lg_r, mx_r], writes=[ex_r])
                    kb.op("act", lambda e: e.activation(out=ex[:], in_=ex[:], func=AF.Exp), reads=[ex_r], writes=[ex_r])
                    kb.op("dve", lambda e: e.tensor_tensor(out=ex[:], in0=ex[:], in1=msk[:], op=ALU.mult),
                          reads=[ex_r, msk_r], writes=[ex_r])
                    kb.op("dve", lambda e: e.reduce_sum(out=sm[:, 0:1], in_=ex[:], axis=AX.X), reads=[ex_r], writes=[sm_r])
                    kb.op("dve", lambda e: e.reciprocal(out=sm[:, 1:2], in_=sm[:, 0:1]), reads=[sm_r], writes=[sm_r])
                    kb.op("dve", lambda e, j=j: e.tensor_scalar(out=gates[:, j, :], in0=ex[:], scalar1=sm[:, 1:2],
                                                                scalar2=None, op0=ALU.mult),
                          reads=[ex_r, sm_r], writes=[gates_r])
        front(0)
        for T in range(NT):
            t0 = T * TT
            acc, acc_r = accs[T % nbuf], accs_r[T % nbuf]
            xT, xT_r = [xTs[T % nbuf]], [xTs_r[T % nbuf]]
            for e_i in range(NE):
                for ch in range(NCH):
                    if nbuf == 2 and e_i == 0 and ch == min(1, NCH - 1) and T + 1 < NT:
                        front(T + 1)
                    b = wi % 2
                    if wi + 1 < len(wseq):
                        load_w(wi + 1)
                    ab = wi % 2
                    CS = chunk_sizes[ch]
                    for fl in range(CS):
                        for hf in range(2):
                            gb = gcount % 2
                            gcount += 1
                            for (wt, wt_r, pst, pst_r) in ((wg[b], wg_r[b], psG[gb], psG_r[gb]),
                                                           (wu[b], wu_r[b], psU[gb], psU_r[gb])):
                                for kt in range(KT):
                                    kb.op("pe", lambda e, kt=kt, wt=wt, pst=pst, fl=fl, hf=hf: e.matmul(
                                        out=pst[:], lhsT=wt[:, kt, fl * 128:(fl + 1) * 128],
                                        rhs=xT[0][:, kt, hf * 512:(hf + 1) * 512],
                                        start=(kt == 0), stop=(kt == KT - 1)),
                                        reads=[wt_r, xT_r[0]], writes=[pst_r], signal=(kt == KT - 1))
                            kb.op("act", lambda e, gb=gb: e.activation(out=sg[gb][:], in_=psG[gb][:], func=AF.Silu),
                                  reads=[psG_r[gb]], writes=[sg_r[gb]])
                            kb.op("dve", lambda e, gb=gb, fl=fl, hf=hf, ab=ab: e.tensor_tensor(
                                out=actT[ab][:, fl, hf * 512:(hf + 1) * 512], in0=sg[gb][:], in1=psU[gb][:], op=ALU.mult),
                                reads=[sg_r[gb], psU_r[gb]], writes=[actT_r[ab]])
                    for j in range(NJ):
                        for dh in range(2):
                            ob = ocount % 2
                            ocount += 1
                            for fl in range(CS):
                                kb.op("pe", lambda e, fl=fl, j=j, dh=dh, ob=ob, ab=ab, b=b: e.matmul(
                                    out=psO[ob][:], lhsT=actT[ab][:, fl, j * 128:(j + 1) * 128],
                                    rhs=wd[b][:, fl, dh * 512:(dh + 1) * 512],
                                    start=(fl == 0), stop=(fl == CS - 1)),
                                    reads=[actT_r[ab], wd_r[b]], writes=[psO_r[ob]], signal=(fl == CS - 1))
                            if router is not None:
                                kb.op("dve", lambda e, j=j, dh=dh, ob=ob, e_i=e_i: e.scalar_tensor_tensor(
                                    out=acc[:, j, dh * 512:(dh + 1) * 512], in0=psO[ob][:],
                                    scalar=gates[:, j, e_i:e_i + 1], in1=acc[:, j, dh * 512:(dh + 1) * 512],
                                    op0=ALU.mult, op1=ALU.add),
                                    reads=[psO_r[ob], gates_r, acc_r], writes=[acc_r])
                            else:
                                kb.op("dve", lambda e, j=j, dh=dh, ob=ob: e.tensor_tensor(
                                    out=acc[:, j, dh * 512:(dh + 1) * 512], in0=psO[ob][:],
                                    in1=acc[:, j, dh * 512:(dh + 1) * 512], op=ALU.add),
                                    reads=[psO_r[ob], acc_r], writes=[acc_r])
                    wi += 1
            if final_g is not None:
                fr.rstd(acc[:], acc_r, NJ)
                for j in range(NJ):
                    kb.op("dve", lambda e, j=j: e.scalar_tensor_tensor(out=acc[:, j, :], in0=acc[:, j, :],
                                                                       scalar=fr.rs[:, j:j + 1], in1=gf[:],
                                                                       op0=ALU.mult, op1=ALU.mult),
                          reads=[acc_r, fr.rs_r, gf_r], writes=[acc_r])
            for hf in range(2):
                kb.dma("sp", dst_ap[t0 + hf * 512:t0 + (hf + 1) * 512, :].rearrange("(j p) d -> p j d", p=128),
                       acc[:, hf * 4:(hf + 1) * 4, :], [acc_r], [dst_r], acc_r, store=True)
            if nbuf == 1 and T + 1 < NT:
                front(T + 1)
        kb.barrier()


def fourier_phase(kb, src, dst, norm_g, w_fnet, ident_d, dftc, dfts, cc_d, nsc_d, sc_d, rev_d, tag="fn"):
    nc = kb.nc
    src_ap, src_r = src
    dst_ap, dst_r = dst
    NST = S // 128
    with ExitStack() as es:
        ident = es.enter_context(nc.sbuf_tensor(tag + "_ident", [128, 128], BF16))
        ident_r = Res("ident")
        kb.dma("sp", ident[:], ident_d, [], [ident_r], ident_r)
        hnS = es.enter_context(nc.sbuf_tensor(tag + "_hnS", [128, NST, D], BF16))
        hnS_r = Res("hnS")
        wf = es.enter_context(nc.sbuf_tensor(tag + "_wf", [128, KT, D], BF16))
        wf_r = Res("wf")
        kb.dma("pool", wf[:], w_fnet.rearrange("(kt p) d -> p kt d", p=128), [], [wf_r], wf_r)
        cc = es.enter_context(nc.sbuf_tensor(tag + "_cc", [128, 2, 256], BF16))
        cc_r = Res("cc")
        kb.dma("sp", cc[:], cc_d, [], [cc_r], cc_r)
        nsc = es.enter_context(nc.sbuf_tensor(tag + "_nsc", [128, 2, 256], BF16))
        nsc_r = Res("nsc")
        kb.dma("sp", nsc[:], nsc_d, [], [nsc_r], nsc_r)
        with ExitStack() as es0:
            fr = Front(kb, es0, ident, ident_r, norm_g, tag + "f", with_T=False)
            x4 = [es0.enter_context(nc.sbuf_tensor(tag + "_x4%d" % i, [128, 4, D], F32)) for i in range(4)]
            x4_r = [Res("x4%d" % i) for i in range(4)]
            for T in range(S // 512):
                b = T % 4
                kb.dma("sp", x4[b][:], src_ap[T * 512:(T + 1) * 512, :].rearrange("(j p) d -> p j d", p=128),
                       [src_r], [x4_r[b]], x4_r[b])
                fr.rstd(x4[b][:], x4_r[b], 4)
                for j in range(4):
                    fr.norm_to(hnS[:, T * 4 + j, :], hnS_r, x4[b][:, j, :], x4_r[b], j)
            kb.barrier()
        with ExitStack() as es1:
            tc = [es1.enter_context(nc.sbuf_tensor(tag + "_tc%d" % i, [128, NST, 128], BF16)) for i in range(2)]
            ts = [es1.enter_context(nc.sbuf_tensor(tag + "_ts%d" % i, [128, NST, 128], BF16)) for i in range(2)]
            tc_r = [Res("tc%d" % i) for i in range(2)]
            ts_r = [Res("ts%d" % i) for i in range(2)]
            rev = es1.enter_context(nc.sbuf_tensor(tag + "_rev", [128, 128], BF16))
            rev_r = Res("rev")
            kb.dma("sp", rev[:], rev_d, [], [rev_r], rev_r)
            sc = es1.enter_context(nc.sbuf_tensor(tag + "_sc", [128, 2, 256], BF16))
            sc_r = Res("sc")
            kb.dma("sp", sc[:], sc_d, [], [sc_r], sc_r)
            Pb = es1.enter_context(nc.sbuf_tensor(tag + "_Pb", [128, D], BF16))
            Qb = es1.enter_context(nc.sbuf_tensor(tag + "_Qb", [128, D], BF16))
            Pb_r, Qb_r = Res("Pb"), Res("Qb")
            PT = [es1.enter_context(nc.sbuf_tensor(tag + "_PT%d" % i, [128, KT, 128], BF16)) for i in range(2)]
            QT = [es1.enter_context(nc.sbuf_tensor(tag + "_QT%d" % i, [128, KT, 128], BF16)) for i in range(2)]
            PT_r = [Res("PT%d" % i) for i in range(2)]
            QT_r = [Res("QT%d" % i) for i in range(2)]
            YT = [es1.enter_context(nc.sbuf_tensor(tag + "_YT%d" % i, [128, KT, 128], BF16)) for i in range(2)]
            YT_r = [Res("YT%d" % i) for i in range(2)]
            xt = [es1.enter_context(nc.sbuf_tensor(tag + "_xt%d" % i, [128, D], F32)) for i in range(4)]
            xt_r = [Res("xt%d" % i) for i in range(4)]
            psP = [es1.enter_context(nc.psum_tensor(tag + "_psP%d" % i, [128, 512], F32)) for i in range(2)]
            psQ = [es1.enter_context(nc.psum_tensor(tag + "_psQ%d" % i, [128, 512], F32)) for i in range(2)]
            psP_r = [Res("psP%d" % i) for i in range(2)]
            psQ_r = [Res("psQ%d" % i) for i in range(2)]
            psT = [es1.enter_context(nc.psum_tensor(tag + "_psT%d" % i, [128, D], BF16)) for i in range(2)]
            psT_r = [Res("psT%d" % i) for i in range(2)]
            psY = es1.enter_context(nc.psum_tensor(tag + "_psY", [128, D], F32))
            psY_r = Res("psY")
            NSRC = 17

            def load_tab(ai):
                b = ai % 2
                kb.dma("sp", tc[b][:], dftc[ai], [], [tc_r[b]], tc_r[b])
                kb.dma("sp", ts[b][:], dfts[ai], [], [ts_r[b]], ts_r[b])

            def load_x(ai):
                a_ = ai - 1
                xd, xd_r = xt[(ai % 2) * 2], xt_r[(ai % 2) * 2]
                xm, xm_r = xt[(ai % 2) * 2 + 1], xt_r[(ai % 2) * 2 + 1]
                if a_ < 0:
                    kb.op("dve", lambda e: e.memset(xd[:], 0.0), writes=[xd_r])
                    kb.dma("sp", xd[127:128, :], src_ap[0:1, :], [src_r], [xd_r], xd_r)
                else:
                    kb.dma("sp", xd[:], src_ap[128 * a_ + 1:128 * a_ + 129, :], [src_r], [xd_r], xd_r)
                    r0 = 128 * (31 - a_)
                    kb.dma("sp", xm[:], src_ap[r0:r0 + 128, :], [src_r], [xm_r], xm_r)

            Pb2 = [Pb, es1.enter_context(nc.sbuf_tensor(tag + "_Pb1", [128, D], BF16))]
            Qb2 = [Qb, es1.enter_context(nc.sbuf_tensor(tag + "_Qb1", [128, D], BF16))]
            Pb2_r = [Pb_r, Res("Pb1")]
            Qb2_r = [Qb_r, Res("Qb1")]

            def main_mm(ai):
                b = ai % 2
                for (tab, tab_r, psX, psX_r) in ((tc[b], tc_r[b], psP, psP_r), (ts[b], ts_r[b], psQ, psQ_r)):
                    for ch in range(2):
                        for st in range(NST):
                            kb.op("pe", lambda e: e.matmul(
                                out=psX[ch][:], lhsT=tab[:, st, :], rhs=hnS[:, st, ch * 512:(ch + 1) * 512],
                                start=(st == 0), stop=(st == NST - 1)),
                                reads=[tab_r, hnS_r], writes=[psX_r[ch]], signal=(st == NST - 1))
                for (psX, psX_r, Xb, Xb_r) in ((psP, psP_r, Pb2[b], Pb2_r[b]), (psQ, psQ_r, Qb2[b], Qb2_r[b])):
                    for ch in range(2):
                        kb.op("act", lambda e: e.activation(
                            out=Xb[:, ch * 512:(ch + 1) * 512], in_=psX[ch][:], func=AF.Copy),
                            reads=[psX_r[ch]], writes=[Xb_r])

            def tail(ai):
                a_ = ai - 1
                b = ai % 2
                variants = [(0, ident, ident_r, nsc, nsc_r)]
                if a_ >= 0:
                    variants.append((1, rev, rev_r, sc, sc_r))
                for (mi, perm, perm_r, stab, stab_r) in variants:
                    for i, (Xb, Xb_r, XT, XT_r) in enumerate(((Pb2[b], Pb2_r[b], PT[mi], PT_r[mi]),
                                                              (Qb2[b], Qb2_r[b], QT[mi], QT_r[mi]))):
                        for ct in range(KT):
                            kb.op("pe", lambda e: e.transpose(
                                out=psT[i][:, ct * 128:(ct + 1) * 128], in_=Xb[:, ct * 128:(ct + 1) * 128],
                                identity=perm[:]), reads=[Xb_r, perm_r], writes=[psT_r[i]], signal=(ct == KT - 1))
                        kb.op("dve", lambda e: e.tensor_copy(
                            out=XT[:], in_=psT[i][:].rearrange("p (k t) -> p k t", k=KT)),
                            reads=[psT_r[i]], writes=[XT_r])
                    for g in range(4):
                        for c2 in range(2):
                            o = (g * 2 + c2) * 128
                            n = 0
                            for (tabc, XT) in ((cc, PT[mi]), (stab, QT[mi])):
                                for ct in range(2):
                                    kb.op("pe", lambda e: e.matmul(
                                        out=psY[:, o:o + 128], lhsT=tabc[:, ct, c2 * 128:(c2 + 1) * 128],
                                        rhs=XT[:, g * 2 + ct, :], start=(n == 0), stop=(n == 3)),
                                        reads=[cc_r, stab_r, PT_r[mi], QT_r[mi]], writes=[psY_r],
                                        signal=(n == 3 and g == 3 and c2 == 1))
                                    n += 1
                    kb.op("act", lambda e: e.activation(out=YT[mi][:], in_=psY[:].rearrange("p (k t) -> p k t", k=KT),
                                                        func=AF.Identity, scale=1.0 / 1024.0),
                          reads=[psY_r], writes=[YT_r[mi]])
                    xo, xo_r = xt[(ai % 2) * 2 + mi], xt_r[(ai % 2) * 2 + mi]
                    for dh in range(2):
                        for ft in range(KT):
                            kb.op("pe", lambda e: e.matmul(
                                out=psP[dh][:], lhsT=YT[mi][:, ft, :], rhs=wf[:, ft, dh * 512:(dh + 1) * 512],
                                start=(ft == 0), stop=(ft == KT - 1)),
                                reads=[YT_r[mi], wf_r], writes=[psP_r[dh]], signal=(ft == KT - 1))
                        kb.op("dve", lambda e: e.tensor_tensor(
                            out=xo[:, dh * 512:(dh + 1) * 512], in0=psP[dh][:], in1=xo[:, dh * 512:(dh + 1) * 512],
                            op=ALU.add), reads=[psP_r[dh], xo_r], writes=[xo_r])
                    if mi == 0:
                        if a_ < 0:
                            kb.dma("sp", dst_ap[0:1, :], xo[127:128, :], [xo_r], [dst_r], xo_r, store=True)
                        elif a_ == 15:
                            kb.dma("sp", dst_ap[128 * a_ + 1:128 * a_ + 128, :], xo[0:127, :], [xo_r], [dst_r], xo_r, store=True)
                        else:
                            kb.dma("sp", dst_ap[128 * a_ + 1:128 * a_ + 129, :], xo[:], [xo_r], [dst_r], xo_r, store=True)
                    else:
                        r0 = 128 * (31 - a_)
                        kb.dma("sp", dst_ap[r0:r0 + 128, :], xo[:], [xo_r], [dst_r], xo_r, store=True)

            load_tab(0)
            load_tab(1)
            load_x(0)
            main_mm(0)
            for ai in range(NSRC):
                if ai + 1 < NSRC:
                    load_x(ai + 1)
                    main_mm(ai + 1)
                    if ai + 2 < NSRC:
                        load_tab(ai + 2)
                tail(ai)
            kb.barrier()


def ret_proj_phase(kb, src, norm_g, w_in, gn_gain, ident_d, rope_cos, rope_sin, qk_s, v_s, sg_s, tag="ra"):
    nc = kb.nc
    src_ap, src_r = src
    qk_ap, qk_r = qk_s
    v_ap, v_r = v_s
    sg_ap, sg_r = sg_s
    with ExitStack() as es:
        ident = es.enter_context(nc.sbuf_tensor(tag + "_ident", [128, 128], BF16))
        ident_r = Res("ident")
        kb.dma("sp", ident[:], ident_d, [], [ident_r], ident_r)
        xT = es.enter_context(nc.sbuf_tensor(tag + "_xT", [128, KT, S], BF16))
        xT_r = Res("xT")
        with ExitStack() as es0:
            fr = Front(kb, es0, ident, ident_r, norm_g, tag + "f")
            x4 = [es0.enter_context(nc.sbuf_tensor(tag + "_x4%d" % i, [128, 4, D], F32)) for i in range(4)]
            x4_r = [Res("x4%d" % i) for i in range(4)]
            for T in range(S // 512):
                b = T % 4
                kb.dma("sp", x4[b][:], src_ap[T * 512:(T + 1) * 512, :].rearrange("(j p) d -> p j d", p=128),
                       [src_r], [x4_r[b]], x4_r[b])
                fr.norm_T(x4[b][:], x4_r[b], 4, xT, xT_r, T * 512)
            kb.barrier()
        with ExitStack() as es1:
            wqk = es1.enter_context(nc.sbuf_tensor(tag + "_wqk", [128, KT, 2048], BF16))
            wqk_r = Res("wqk")
            for hh in range(2):
                kb.dma("pool", wqk[:, :, hh * 1024:(hh + 1) * 1024],
                       w_in[:, hh * 1024:(hh + 1) * 1024].rearrange("(kt p) f -> p kt f", p=128),
                       [], [wqk_r], wqk_r)
            cs = [es1.enter_context(nc.sbuf_tensor(tag + "_cs%d" % i, [128, 512], F32)) for i in range(2)]
            sn = [es1.enter_context(nc.sbuf_tensor(tag + "_sn%d" % i, [128, 512], F32)) for i in range(2)]
            cs_r = [Res("cs%d" % i) for i in range(2)]
            sn_r = [Res("sn%d" % i) for i in range(2)]
            As = [es1.enter_context(nc.sbuf_tensor(tag + "_As%d" % i, [128, 512], F32)) for i in range(2)]
            Bs = [es1.enter_context(nc.sbuf_tensor(tag + "_Bs%d" % i, [128, 512], F32)) for i in range(2)]
            As_r = [Res("As%d" % i) for i in range(2)]
            Bs_r = [Res("Bs%d" % i) for i in range(2)]
            tt = [es1.enter_context(nc.sbuf_tensor(tag + "_tt%d" % i, [128, 512], F32)) for i in range(8)]
            tt_r = [Res("tt%d" % i) for i in range(8)]
            stg = [es1.enter_context(nc.sbuf_tensor(tag + "_stg%d" % i, [128, 4, 16, 128], BF16)) for i in range(2)]
            stg_r = [Res("stg%d" % i) for i in range(2)]
            psA = [es1.enter_context(nc.psum_tensor(tag + "_psA%d" % i, [128, 512], F32)) for i in range(2)]
            psB = [es1.enter_context(nc.psum_tensor(tag + "_psB%d" % i, [128, 512], F32)) for i in range(2)]
            psA_r = [Res("psA%d" % i) for i in range(2)]
            psB_r = [Res("psB%d" % i) for i in range(2)]
            n = 0
            for T in range(S // 512):
                tb = T % 2
                kb.dma("sp", cs[tb][:], rope_cos[:, T * 512:(T + 1) * 512], [], [cs_r[tb]], cs_r[tb])
                kb.dma("sp", sn[tb][:], rope_sin[:, T * 512:(T + 1) * 512], [], [sn_r[tb]], sn_r[tb])
                for which in range(2):
                    scale = 1.0 if which == 0 else 1.0 / 16.0
                    for h in range(4):
                        b = n % 2
                        n += 1
                        f1 = which * 8 + 2 * h
                        for (ft, psX, psX_r) in ((f1, psA[b], psA_r[b]), (f1 + 1, psB[b], psB_r[b])):
                            for kt in range(KT):
                                kb.op("pe", lambda e, kt=kt, ft=ft, psX=psX: e.matmul(
                                    out=psX[:], lhsT=wqk[:, kt, ft * 128:(ft + 1) * 128],
                                    rhs=xT[:, kt, T * 512:(T + 1) * 512], start=(kt == 0), stop=(kt == KT - 1)),
                                    reads=[wqk_r, xT_r], writes=[psX_r], signal=(kt == KT - 1))
                        kb.op("act", lambda e: e.activation(out=As[b][:], in_=psA[b][:], func=AF.Identity, scale=scale),
                              reads=[psA_r[b]], writes=[As_r[b]])
                        kb.op("act", lambda e: e.activation(out=Bs[b][:], in_=psB[b][:], func=AF.Identity, scale=scale),
                              reads=[psB_r[b]], writes=[Bs_r[b]])
                        t = [tt[b * 4 + i] for i in range(4)]
                        t_r = [tt_r[b * 4 + i] for i in range(4)]
                        kb.op("dve", lambda e: e.tensor_tensor(out=t[0][:], in0=As[b][:], in1=cs[tb][:], op=ALU.mult),
                              reads=[As_r[b], cs_r[tb]], writes=[t_r[0]])
                        kb.op("dve", lambda e: e.tensor_tensor(out=t[1][:], in0=Bs[b][:], in1=sn[tb][:], op=ALU.mult),
                              reads=[Bs_r[b], sn_r[tb]], writes=[t_r[1]])
                        kb.op("dve", lambda e: e.tensor_tensor(out=t[2][:], in0=As[b][:], in1=sn[tb][:], op=ALU.mult),
                              reads=[As_r[b], sn_r[tb]], writes=[t_r[2]])
                        kb.op("dve", lambda e: e.tensor_tensor(out=t[3][:], in0=Bs[b][:], in1=cs[tb][:], op=ALU.mult),
                              reads=[Bs_r[b], cs_r[tb]], writes=[t_r[3]])
                        kb.op("pool", lambda e: e.tensor_tensor(
                            out=stg[tb][:, :, f1, :], in0=t[0][:].rearrange("p (c t) -> p c t", c=4),
                            in1=t[1][:].rearrange("p (c t) -> p c t", c=4), op=ALU.subtract),
                            reads=[t_r[0], t_r[1]], writes=[stg_r[tb]])
                        kb.op("pool", lambda e: e.tensor_tensor(
                            out=stg[tb][:, :, f1 + 1, :], in0=t[2][:].rearrange("p (c t) -> p c t", c=4),
                            in1=t[3][:].rearrange("p (c t) -> p c t", c=4), op=ALU.add),
                            reads=[t_r[2], t_r[3]], writes=[stg_r[tb]])
                kb.dma("sp", qk_ap[T], stg[tb][:], [stg_r[tb]], [qk_r], stg_r[tb], store=True)
            kb.barrier()
        with ExitStack() as es2:
            wv = [es2.enter_context(nc.sbuf_tensor(tag + "_wv%d" % i, [128, KT, 512], BF16)) for i in range(2)]
            wv_r = [Res("wv%d" % i) for i in range(2)]
            gg = es2.enter_context(nc.sbuf_tensor(tag + "_gg", [128, 2048], F32))
            gg_r = Res("gg")
            kb.dma("sp", gg[:], bcast_row(gn_gain, 2048), [], [gg_r], gg_r)
            vst = [es2.enter_context(nc.sbuf_tensor(tag + "_vst%d" % i, [128, 4, 512], BF16)) for i in range(2)]
            vst_r = [Res("vst%d" % i) for i in range(2)]
            sgt = [es2.enter_context(nc.sbuf_tensor(tag + "_sgt%d" % i, [128, 512], F32)) for i in range(2)]
            sgt_r = [Res("sgt%d" % i) for i in range(2)]
            psV = [es2.enter_context(nc.psum_tensor(tag + "_psV%d" % i, [128, 512], F32)) for i in range(4)]
            psV_r = [Res("psV%d" % i) for i in range(4)]

            def load_wv(cb):
                b = cb % 2
                c0 = 2048 + cb * 512
                kb.dma("pool", wv[b][:], w_in[:, c0:c0 + 512].rearrange("(kt p) f -> p kt f", p=128),
                       [], [wv_r[b]], wv_r[b])

            load_wv(0)
            n = 0
            m = 0
            for cb in range(8):
                b = cb % 2
                if cb + 1 < 8:
                    load_wv(cb + 1)
                is_g = cb >= 4
                cc0 = (cb % 4) * 512
                for T in range(S // 512):
                    sb_ = m % 2
                    m += 1
                    for j in range(4):
                        pb = n % 4
                        n += 1
                        t0 = T * 512 + j * 128
                        for kt in range(KT):
                            kb.op("pe", lambda e, kt=kt, t0=t0, pb=pb: e.matmul(
                                out=psV[pb][:], lhsT=xT[:, kt, t0:t0 + 128], rhs=wv[b][:, kt, :],
                                start=(kt == 0), stop=(kt == KT - 1)),
                                reads=[xT_r, wv_r[b]], writes=[psV_r[pb]], signal=(kt == KT - 1))
                        if not is_g:
                            kb.op("act", lambda e, j=j, pb=pb: e.activation(out=vst[sb_][:, j, :], in_=psV[pb][:],
                                                                            func=AF.Copy),
                                  reads=[psV_r[pb]], writes=[vst_r[sb_]])
                        else:
                            gb = n % 2
                            kb.op("act", lambda e, pb=pb, gb=gb: e.activation(out=sgt[gb][:], in_=psV[pb][:],
                                                                              func=AF.Silu),
                                  reads=[psV_r[pb]], writes=[sgt_r[gb]])
                            kb.op("dve", lambda e, j=j, gb=gb: e.tensor_tensor(
                                out=vst[sb_][:, j, :], in0=sgt[gb][:], in1=gg[:, cc0:cc0 + 512], op=ALU.mult),
                                reads=[sgt_r[gb], gg_r], writes=[vst_r[sb_]])
                    dst_ap, dst_r = (sg_ap, sg_r) if is_g else (v_ap, v_r)
                    kb.dma("sp", dst_ap[T * 512:(T + 1) * 512, cc0:cc0 + 512].rearrange("(j p) f -> p j f", p=128),
                           vst[sb_][:], [vst_r[sb_]], [dst_r], vst_r[sb_], store=True)
            kb.barrier()


class RetConsts:
    def __init__(self, kb, es, decay_logit, tag):
        nc = kb.nc
        I32 = mybir.dt.int32

        def T(name, shape, dtype=F32):
            return es.enter_context(nc.sbuf_tensor(tag + "_" + name, shape, dtype)), Res(name)

        dl, dl_r = T("dl", [128, 8])
        kb.dma("sp", dl[:], bcast_row(decay_logit.rearrange("a b -> (a b)"), 8), [], [dl_r], dl_r)
        self.lg, self.lg_r = T("lg", [128, 8])
        nlg, nlg_r = T("nlg", [128, 8])
        lg127, lg127_r = T("lg127", [128, 8])
        self.g128, self.g128_r = T("g128", [128, 8])
        lg, lg_r = self.lg, self.lg_r
        kb.op("act", lambda e: e.activation(out=nlg[:], in_=dl[:], func=AF.Exp, scale=-1.0), reads=[dl_r], writes=[nlg_r])
        kb.op("dve", lambda e: e.tensor_scalar(out=nlg[:], in0=nlg[:], scalar1=1.0, scalar2=None, op0=ALU.add),
              reads=[nlg_r], writes=[nlg_r])
        kb.op("act", lambda e: e.activation(out=nlg[:], in_=nlg[:], func=AF.Ln), reads=[nlg_r], writes=[nlg_r])
        kb.op("dve", lambda e: e.tensor_scalar(out=lg[:], in0=nlg[:], scalar1=-1.0, scalar2=None, op0=ALU.mult),
              reads=[nlg_r], writes=[lg_r])
        kb.op("dve", lambda e: e.tensor_scalar(out=lg127[:], in0=lg[:], scalar1=127.0, scalar2=None, op0=ALU.mult),
              reads=[lg_r], writes=[lg127_r])
        kb.op("act", lambda e: e.activation(out=self.g128[:], in_=lg[:], func=AF.Exp, scale=128.0),
              reads=[lg_r], writes=[self.g128_r])
        ii, ii_r = T("ii", [128, 128], I32)
        dmf, dmf_r = T("dmf", [128, 128])
        imat, imat_r = T("imat", [128, 128])
        jmat, jmat_r = T("jmat", [128, 128])
        pos, pos_r = T("pos", [128, 128])
        neg, neg_r = T("neg", [128, 128])
        tmp, tmp_r = T("tmp", [128, 128])
        kb.op("pool", lambda e: e.iota(ii[:], pattern=[[1, 128]], base=0, channel_multiplier=-1), writes=[ii_r])
        kb.op("dve", lambda e: e.tensor_copy(out=dmf[:], in_=ii[:]), reads=[ii_r], writes=[dmf_r])
        kb.op("pool", lambda e: e.iota(ii[:], pattern=[[1, 128]], base=0, channel_multiplier=0), reads=[], writes=[ii_r])
        kb.op("dve", lambda e: e.tensor_copy(out=imat[:], in_=ii[:]), reads=[ii_r], writes=[imat_r])
        kb.op("pool", lambda e: e.iota(ii[:], pattern=[[0, 128]], base=0, channel_multiplier=1), reads=[], writes=[ii_r])
        kb.op("dve", lambda e: e.tensor_copy(out=jmat[:], in_=ii[:]), reads=[ii_r], writes=[jmat_r])
        kb.op("dve", lambda e: e.tensor_scalar(out=pos[:], in0=dmf[:], scalar1=0.0, scalar2=None, op0=ALU.max),
              reads=[dmf_r], writes=[pos_r])
        kb.op("dve", lambda e: e.tensor_tensor(out=neg[:], in0=pos[:], in1=dmf[:], op=ALU.subtract),
              reads=[pos_r, dmf_r], writes=[neg_r])
        self.Dc, self.Dc_r = T("Dc", [128, 4, 128])
        self.XF, self.XF_r = T("XF", [128, 8, 128])
        self.XB, self.XB_r = T("XB", [128, 8, 128])
        self.ZF, self.ZF_r = T("ZF", [128, 8, 128])
        self.ZB, self.ZB_r = T("ZB", [128, 8, 128])
        for h in range(4):
            f, b_ = h, 4 + h
            kb.op("dve", lambda e: e.tensor_scalar(out=tmp[:], in0=pos[:], scalar1=lg[:, f:f + 1], scalar2=None,
                                                   op0=ALU.mult), reads=[pos_r, lg_r], writes=[tmp_r])
            kb.op("dve", lambda e: e.scalar_tensor_tensor(out=tmp[:], in0=neg[:], scalar=lg[:, b_:b_ + 1], in1=tmp[:],
                                                          op0=ALU.mult, op1=ALU.add),
                  reads=[neg_r, lg_r, tmp_r], writes=[tmp_r])
            kb.op("act", lambda e: e.activation(out=self.Dc[:, h, :], in_=tmp[:], func=AF.Exp),
                  reads=[tmp_r], writes=[self.Dc_r])
            for ft in (2 * h, 2 * h + 1):
                kb.op("act", lambda e: e.activation(out=self.XF[:, ft, :], in_=imat[:], func=AF.Exp,
                                                    scale=lg[:, f:f + 1], bias=lg[:, f:f + 1]),
                      reads=[imat_r, lg_r], writes=[self.XF_r])
                kb.op("act", lambda e: e.activation(out=self.XB[:, ft, :], in_=imat[:], func=AF.Exp,
                                                    scale=nlg[:, b_:b_ + 1], bias=lg127[:, b_:b_ + 1]),
                      reads=[imat_r, nlg_r, lg127_r], writes=[self.XB_r])
                kb.op("act", lambda e: e.activation(out=self.ZF[:, ft, :], in_=jmat[:], func=AF.Exp,
                                                    scale=nlg[:, f:f + 1], bias=lg127[:, f:f + 1]),
                      reads=[jmat_r, nlg_r, lg127_r], writes=[self.ZF_r])
                kb.op("act", lambda e: e.activation(out=self.ZB[:, ft, :], in_=jmat[:], func=AF.Exp,
                                                    scale=lg[:, b_:b_ + 1], bias=lg[:, b_:b_ + 1]),
                      reads=[jmat_r, lg_r], writes=[self.ZB_r])


def ret_core_phase(kb, decay_logit, ident_d, qk_s, v_s, sg_s, sb_s, z_s, tag="rb"):
    nc = kb.nc
    qk_ap, qk_r = qk_s
    v_ap, v_r = v_s
    sg_ap, sg_r = sg_s
    sb_ap, sb_r = sb_s
    z_ap, z_r = z_s
    NCK = S // 128
    with ExitStack() as es:
        ident = es.enter_context(nc.sbuf_tensor(tag + "_ident", [128, 128], BF16))
        ident_r = Res("ident")
        kb.dma("sp", ident[:], ident_d, [], [ident_r], ident_r)
        rc = RetConsts(kb, es, decay_logit, tag + "c")
        S32 = es.enter_context(nc.sbuf_tensor(tag + "_S32", [128, 8, 512], F32))
        S32_r = [Res("S32_%d" % i) for i in range(8)]
        S16 = [es.enter_context(nc.sbuf_tensor(tag + "_S16%d" % i, [128, 8, 512], BF16)) for i in range(2)]
        S16_r = [[Res("S16%d_%d" % (i, k)) for k in range(8)] for i in range(2)]
        S16_st = [Res("S16st%d" % i) for i in range(2)]
        qk = [es.enter_context(nc.sbuf_tensor(tag + "_qk%d" % i, [128, 16, 128], BF16)) for i in range(3)]
        qk_t = [Res("qk%d" % i) for i in range(3)]
        vt = [es.enter_context(nc.sbuf_tensor(tag + "_v%d" % i, [128, 2048], BF16)) for i in range(3)]
        vt_r = [Res("v%d" % i) for i in range(3)]
        kz = [es.enter_context(nc.sbuf_tensor(tag + "_kz%d" % i, [128, 8, 128], BF16)) for i in range(2)]
        kz_r = [Res("kz%d" % i) for i in range(2)]
        psK = es.enter_context(nc.psum_tensor(tag + "_psK", [128, 1024], BF16))
        psK_r = Res("psK")
        psS = [es.enter_context(nc.psum_tensor(tag + "_psS%d" % i, [128, 512], F32)) for i in range(2)]
        psS_r = [Res("psS%d" % i) for i in range(2)]
        cnt = {"s": 0}

        def load_chunk(n, want_q):
            b = n % 3
            T, c = n // 4, n % 4
            if want_q:
                kb.dma("sp", qk[b][:], qk_ap[T, :, c], [qk_r], [qk_t[b]], qk_t[b])
            else:
                kb.dma("sp", qk[b][:, 8:16, :], qk_ap[T, :, c, 8:16, :], [qk_r], [qk_t[b]], qk_t[b])
            kb.dma("sp", vt[b][:], v_ap[n * 128:(n + 1) * 128, :], [v_r], [vt_r[b]], vt_r[b])

        def k_tokmajor(n, Z, Z_r, use_act=False):
            b = n % 2
            b3 = n % 3
            for ft in range(8):
                kb.op("pe", lambda e, ft=ft: e.transpose(out=psK[:, ft * 128:(ft + 1) * 128], in_=qk[b3][:, 8 + ft, :],
                                                         identity=ident[:]),
                      reads=[qk_t[b3], ident_r], writes=[psK_r], signal=(ft == 7))
            if use_act:
                for h in range(4):
                    kb.op("act", lambda e: e.activation(out=kz[b][:, 2 * h:2 * h + 2, :],
                                                        in_=psK[:, h * 256:(h + 1) * 256].rearrange("p (f d) -> p f d", f=2),
                                                        func=AF.Identity, scale=Z[:, 2 * h, 0:1]),
                          reads=[psK_r, Z_r], writes=[kz_r[b]])
            else:
                kb.op("dve", lambda e: e.tensor_tensor(out=kz[b][:], in0=psK[:].rearrange("p (f d) -> p f d", f=8),
                                                       in1=Z[:], op=ALU.mult),
                      reads=[psK_r, Z_r], writes=[kz_r[b]])

        def state_update(n, gcol, s16_out, s16_out_r):
            b = n % 2
            b3 = n % 3
            for h in range(4):
                for dt_ in range(2):
                    ft = 2 * h + dt_
                    pb = cnt["s"] % len(psS)
                    cnt["s"] += 1
                    kb.op("pe", lambda e: e.matmul(out=psS[pb][:], lhsT=kz[b][:, ft, :],
                                                   rhs=vt[b3][:, h * 512:(h + 1) * 512], start=True, stop=True),
                          reads=[kz_r[b], vt_r[b3]], writes=[psS_r[pb]])
                    kb.op("dve", lambda e: e.scalar_tensor_tensor(
                        out=S32[:, ft, :], in0=S32[:, ft, :], scalar=rc.g128[:, gcol + h:gcol + h + 1], in1=psS[pb][:],
                        op0=ALU.mult, op1=ALU.add), reads=[S32_r[ft], rc.g128_r, psS_r[pb]], writes=[S32_r[ft]])
                    if gcol == 4 and ft % 4 == 3:
                        kb.op("pool", lambda e: e.tensor_copy(out=s16_out[:, ft, :], in_=S32[:, ft, :]),
                              reads=[S32_r[ft]], writes=[s16_out_r[ft]])
                    else:
                        kb.op("act", lambda e: e.activation(out=s16_out[:, ft, :], in_=S32[:, ft, :], func=AF.Copy),
                              reads=[S32_r[ft]], writes=[s16_out_r[ft]])

        esb1 = ExitStack()
        for i in range(2, 6):
            psS.append(esb1.enter_context(nc.psum_tensor(tag + "_psS%d" % i, [128, 512], F32)))
            psS_r.append(Res("psS%d" % i))
        kb.op("dve", lambda e: e.memset(S32[:], 0.0), writes=S32_r)
        kb.op("pool", lambda e: e.memset(S16[1][:], 0.0), writes=S16_r[1])
        load_chunk(NCK - 1, False)
        load_chunk(NCK - 2, False)
        k_tokmajor(NCK - 1, rc.ZB, rc.ZB_r, use_act=True)
        for n in range(NCK - 1, -1, -1):
            if n - 2 >= 0:
                load_chunk(n - 2, False)
            cur = S16[n % 2]
            cur_r = S16_r[n % 2]
            kb.dma("sp", sb_ap[n], cur[:], cur_r, [sb_r], S16_st[n % 2], store=True)
            if n > 1:
                k_tokmajor(n - 1, rc.ZB, rc.ZB_r, use_act=True)
            if n > 0:
                state_update(n, 4, S16[(n + 1) % 2], S16_r[(n + 1) % 2])
        kb.barrier(recycle=False)
        del psS[2:]
        del psS_r[2:]
        esb1.close()
        with ExitStack() as es2:
            sgt = [es2.enter_context(nc.sbuf_tensor(tag + "_sg%d" % i, [128, 2048], BF16)) for i in range(2)]
            sgt_r = [Res("sg%d" % i) for i in range(2)]
            sbn = [es2.enter_context(nc.sbuf_tensor(tag + "_sbn%d" % i, [128, 8, 512], BF16)) for i in range(3)]
            sbn_r = [Res("sbn%d" % i) for i in range(3)]
            qf2 = [es2.enter_context(nc.sbuf_tensor(tag + "_qf%d" % i, [128, 8, 128], BF16)) for i in range(2)]
            qb2 = [es2.enter_context(nc.sbuf_tensor(tag + "_qb%d" % i, [128, 8, 128], BF16)) for i in range(2)]
            qf2_r = [Res("qf%d" % i) for i in range(2)]
            qb2_r = [Res("qb%d" % i) for i in range(2)]
            PT = es2.enter_context(nc.sbuf_tensor(tag + "_PT", [128, 4, 128], BF16))
            PT_r = Res("PT")
            yn = [es2.enter_context(nc.sbuf_tensor(tag + "_yn%d" % i, [128, 512], F32)) for i in range(2)]
            yn_r = [Res("yn%d" % i) for i in range(2)]
            zt = [es2.enter_context(nc.sbuf_tensor(tag + "_z%d" % i, [128, 2048], BF16)) for i in range(2)]
            zt_r = [Res("z%d" % i) for i in range(2)]
            st6 = es2.enter_context(nc.sbuf_tensor(tag + "_st6", [128, 4, 6], F32))
            st6_r = Res("st6")
            mv = es2.enter_context(nc.sbuf_tensor(tag + "_mv", [128, 4, 2], F32))
            mv_r = Res("mv")
            rs = es2.enter_context(nc.sbuf_tensor(tag + "_rs", [128, 4], F32))
            rs_r = Res("rs")
            nmr = es2.enter_context(nc.sbuf_tensor(tag + "_nmr", [128, 4], F32))
            nmr_r = Res("nmr")
            psSc = es2.enter_context(nc.psum_tensor(tag + "_psSc", [128, 512], F32))
            psSc_r = Res("psSc")
            psY2 = [es2.enter_context(nc.psum_tensor(tag + "_psY%d" % i, [128, 512], F32)) for i in range(2)]
            psY2_r = [Res("psY%d" % i) for i in range(2)]
            psY = [psY2[h % 2] for h in range(4)]
            psY_r = [psY2_r[h % 2] for h in range(4)]
            for i in range(2, 4):
                psS.append(es2.enter_context(nc.psum_tensor(tag + "_psSf%d" % i, [128, 512], F32)))
                psS_r.append(Res("psSf%d" % i))

            ysb = [es2.enter_context(nc.sbuf_tensor(tag + "_ysb%d" % i, [128, 2048], F32)) for i in range(2)]
            ysb_r = [[Res("ysb%d_%d" % (i, h)) for h in range(4)] for i in range(2)]
            sg3 = [es2.enter_context(nc.sbuf_tensor(tag + "_sg3%d" % i, [128, 2048], BF16)) for i in range(4)]
            sg3_r = [Res("sg3%d" % i) for i in range(4)]

            def load_chunk2(n):
                b = n % 3
                load_chunk(n, True)
                kb.dma("sp", sg3[n % 4][:], sg_ap[n * 128:(n + 1) * 128, :], [sg_r], [sg3_r[n % 4]], sg3_r[n % 4])
                kb.dma("sp", sbn[b][:], sb_ap[n], [sb_r], [sbn_r[b]], sbn_r[b])

            def stage_q(n):
                b = n % 3
                kb.op("pool", lambda e: e.tensor_tensor(out=qf2[n % 2][:], in0=qk[b][:, 0:8, :], in1=rc.XF[:], op=ALU.mult),
                      reads=[qk_t[b], rc.XF_r], writes=[qf2_r[n % 2]])
                kb.op("pool", lambda e: e.tensor_tensor(out=qb2[n % 2][:], in0=qk[b][:, 0:8, :], in1=rc.XB[:], op=ALU.mult),
                      reads=[qk_t[b], rc.XB_r], writes=[qb2_r[n % 2]])

            def stage_x(n):
                b = n % 3
                yb2 = n % 2
                qf, qf_r, qb, qb_r = qf2[n % 2], qf2_r[n % 2], qb2[n % 2], qb2_r[n % 2]
                sf = S16[(n + 1) % 2]
                sf_r = S16_r[(n + 1) % 2]
                if n + 2 < NCK:
                    k_tokmajor(n + 1, rc.ZF, rc.ZF_r)
                if n + 1 < NCK:
                    state_update(n, 0, S16[n % 2], S16_r[n % 2])
                for h in range(4):
                    for dt_ in range(2):
                        kb.op("pe", lambda e: e.matmul(out=psSc[:, h * 128:(h + 1) * 128],
                                                       lhsT=qk[b][:, 8 + 2 * h + dt_, :], rhs=qk[b][:, 2 * h + dt_, :],
                                                       start=(dt_ == 0), stop=(dt_ == 1)),
                              reads=[qk_t[b]], writes=[psSc_r], signal=(h == 3 and dt_ == 1))
                kb.op("dve", lambda e: e.tensor_tensor(out=PT[:], in0=psSc[:].rearrange("p (h i) -> p h i", h=4),
                                                       in1=rc.Dc[:], op=ALU.mult),
                      reads=[psSc_r, rc.Dc_r], writes=[PT_r])
                for h in range(4):
                    kb.op("pe", lambda e: e.matmul(out=psY[h][:], lhsT=PT[:, h, :], rhs=vt[b][:, h * 512:(h + 1) * 512],
                                                   start=True, stop=False),
                          reads=[PT_r, vt_r[b]], writes=[psY_r[h]], signal=False)
                    for dt_ in range(2):
                        kb.op("pe", lambda e: e.matmul(out=psY[h][:], lhsT=qf[:, 2 * h + dt_, :], rhs=sf[:, 2 * h + dt_, :],
                                                       start=False, stop=False),
                              reads=[qf_r, sf_r[2 * h + dt_]], writes=[psY_r[h]], signal=False)
                    for dt_ in range(2):
                        kb.op("pe", lambda e: e.matmul(out=psY[h][:], lhsT=qb[:, 2 * h + dt_, :],
                                                       rhs=sbn[b][:, 2 * h + dt_, :], start=False, stop=(dt_ == 1)),
                              reads=[PT_r, vt_r[b], qf_r, sf_r[2 * h], sf_r[2 * h + 1], qb_r, sbn_r[b]],
                              writes=[psY_r[h]], signal=(dt_ == 1))
                    kb.op("act", lambda e: e.activation(out=ysb[yb2][:, h * 512:(h + 1) * 512], in_=psY[h][:], func=AF.Copy),
                          reads=[psY_r[h]], writes=[ysb_r[yb2][h]])

            ycnt = {"c": 0}

            def stage_z(n):
                b = n % 2
                for h in range(4):
                    kb.op("dve", lambda e: e.bn_stats(out=st6[:, h, :], in_=ysb[b][:, h * 512:(h + 1) * 512]),
                          reads=[ysb_r[b][h]], writes=[st6_r])
                    kb.op("dve", lambda e: e.bn_aggr(out=mv[:, h, :], in_=st6[:, h, :]), reads=[st6_r], writes=[mv_r])
                kb.op("dve", lambda e: e.tensor_scalar(out=rs[:], in0=mv[:, :, 1], scalar1=EPS, scalar2=None, op0=ALU.add),
                      reads=[mv_r], writes=[rs_r])
                kb.op("act", lambda e: e.activation(out=rs[:], in_=rs[:], func=AF.Sqrt), reads=[rs_r], writes=[rs_r])
                kb.op("dve", lambda e: e.reciprocal(out=rs[:], in_=rs[:]), reads=[rs_r], writes=[rs_r])
                kb.op("dve", lambda e: e.scalar_tensor_tensor(out=nmr[:], in0=mv[:, :, 0], scalar=-1.0, in1=rs[:],
                                                              op0=ALU.mult, op1=ALU.mult),
                      reads=[mv_r, rs_r], writes=[nmr_r])
                for h in range(4):
                    yb = ycnt["c"] % 2
                    ycnt["c"] += 1
                    kb.op("act", lambda e: e.activation(out=yn[yb][:], in_=ysb[b][:, h * 512:(h + 1) * 512], func=AF.Identity,
                                                        scale=rs[:, h:h + 1], bias=nmr[:, h:h + 1]),
                          reads=[ysb_r[b][h], rs_r, nmr_r], writes=[yn_r[yb]])
                    kb.op("pool", lambda e: e.tensor_tensor(out=zt[b][:, h * 512:(h + 1) * 512], in0=yn[yb][:],
                                                            in1=sg3[n % 4][:, h * 512:(h + 1) * 512], op=ALU.mult),
                          reads=[yn_r[yb], sg3_r[n % 4]], writes=[zt_r[b]])
                kb.dma("sp", z_ap[n * 128:(n + 1) * 128, :], zt[b][:], [zt_r[b]], [z_r], zt_r[b], store=True)

            kb.op("dve", lambda e: e.memset(S32[:], 0.0), reads=[], writes=S32_r)
            kb.op("pool", lambda e: e.memset(S16[1][:], 0.0), reads=[], writes=S16_r[1])
            load_chunk2(0)
            load_chunk2(1)
            stage_q(0)
            k_tokmajor(0, rc.ZF, rc.ZF_r)
            for n in range(NCK):
                if n + 2 < NCK:
                    load_chunk2(n + 2)
                if n + 1 < NCK:
                    stage_q(n + 1)
                stage_x(n)
                if n >= 1:
                    stage_z(n - 1)
            stage_z(NCK - 1)
            kb.barrier()


def ret_out_phase(kb, src, dst, z_s, w_out, ident_d, tag="rc"):
    nc = kb.nc
    src_ap, src_r = src
    dst_ap, dst_r = dst
    z_ap, z_r = z_s
    with ExitStack() as es:
        ident = es.enter_context(nc.sbuf_tensor(tag + "_ident", [128, 128], BF16))
        ident_r = Res("ident")
        kb.dma("sp", ident[:], ident_d, [], [ident_r], ident_r)
        wo = es.enter_context(nc.sbuf_tensor(tag + "_wo", [128, 16, D], BF16))
        wo_r = Res("wo")
        for hh in range(2):
            kb.dma("pool", wo[:, hh * 8:(hh + 1) * 8, :],
                   w_out[hh * 1024:(hh + 1) * 1024, :].rearrange("(et p) d -> p et d", p=128), [], [wo_r], wo_r)
        zt = [es.enter_context(nc.sbuf_tensor(tag + "_z%d" % i, [128, 2048], BF16)) for i in range(2)]
        zt_r = [Res("z%d" % i) for i in range(2)]
        xt = [es.enter_context(nc.sbuf_tensor(tag + "_x%d" % i, [128, D], F32)) for i in range(4)]
        xt_r = [Res("x%d" % i) for i in range(4)]
        zT = [es.enter_context(nc.sbuf_tensor(tag + "_zT%d" % i, [128, 16, 128], BF16)) for i in range(2)]
        zT_r = [Res("zT%d" % i) for i in range(2)]
        psT = [es.enter_context(nc.psum_tensor(tag + "_psT%d" % i, [128, 1024], BF16)) for i in range(2)]
        psT_r = [Res("psT%d" % i) for i in range(2)]
        psO = [es.enter_context(nc.psum_tensor(tag + "_psO%d" % i, [128, 512], F32)) for i in range(4)]
        psO_r = [Res("psO%d" % i) for i in range(4)]

        def load(t):
            b = t % 2
            kb.dma("sp", zt[b][:], z_ap[t * 128:(t + 1) * 128, :], [z_r], [zt_r[b]], zt_r[b])
            kb.dma("sp", xt[t % 4][:], src_ap[t * 128:(t + 1) * 128, :], [src_r], [xt_r[t % 4]], xt_r[t % 4])

        def transp(t):
            b = t % 2
            for hh in range(2):
                for e8 in range(8):
                    et = hh * 8 + e8
                    kb.op("pe", lambda e: e.transpose(out=psT[hh][:, e8 * 128:(e8 + 1) * 128],
                                                      in_=zt[b][:, et * 128:(et + 1) * 128], identity=ident[:]),
                          reads=[zt_r[b], ident_r], writes=[psT_r[hh]], signal=(e8 == 7))
                kb.op("act", lambda e: e.activation(out=zT[b][:, hh * 8:(hh + 1) * 8, :],
                                                    in_=psT[hh][:].rearrange("p (k t) -> p k t", k=8), func=AF.Copy),
                      reads=[psT_r[hh]], writes=[zT_r[b]])

        load(0)
        load(1)
        transp(0)
        load(2)
        for t in range(S // 128):
            b = t % 2
            xb = t % 4
            if t + 1 < S // 128:
                transp(t + 1)
                if t + 3 < S // 128:
                    load(t + 3)
            for dh in range(2):
                pb = (t % 2) * 2 + dh
                for et in range(16):
                    kb.op("pe", lambda e: e.matmul(out=psO[pb][:], lhsT=zT[b][:, et, :],
                                                   rhs=wo[:, et, dh * 512:(dh + 1) * 512],
                                                   start=(et == 0), stop=(et == 15)),
                          reads=[zT_r[b], wo_r], writes=[psO_r[pb]], signal=(et == 15))
                kb.op("dve", lambda e: e.tensor_tensor(out=xt[xb][:, dh * 512:(dh + 1) * 512], in0=psO[pb][:],
                                                       in1=xt[xb][:, dh * 512:(dh + 1) * 512], op=ALU.add),
                      reads=[psO_r[pb], xt_r[xb]], writes=[xt_r[xb]])
            kb.dma("sp", dst_ap[t * 128:(t + 1) * 128, :], xt[xb][:], [xt_r[xb]], [dst_r], xt_r[xb], store=True)
        kb.barrier()


def host_consts():
    c = {}
    c["ident"] = np.eye(128, dtype=np.float32).astype(ml_dtypes.bfloat16)
    c["identf"] = np.eye(128, dtype=np.float32)
    half = 128
    inv = (np.float32(10000.0) ** (-np.arange(half, dtype=np.float32) / np.float32(half))).astype(np.float32)
    ang = (np.arange(S, dtype=np.float32)[:, None] * inv[None, :]).astype(np.float32)
    c["rope_cos"] = np.ascontiguousarray(np.cos(ang).astype(np.float32).T)
    c["rope_sin"] = np.ascontiguousarray(np.sin(ang).astype(np.float32).T)
    s_idx = (np.arange(32)[None, :, None] * 128 + np.arange(128)[:, None, None]).astype(np.int64)
    k_idx = ((128 * (np.arange(17)[:, None] - 1) + 1 + np.arange(128)[None, :]) % S).astype(np.int64)
    m = (s_idx[None] * k_idx[:, None, None, :]) % S
    th = m.astype(np.float64) * (2.0 * np.pi / S)
    c["dftc"] = np.cos(th).astype(np.float32).astype(ml_dtypes.bfloat16)
    c["dfts"] = np.sin(th).astype(np.float32).astype(ml_dtypes.bfloat16)
    c["rev"] = np.ascontiguousarray(np.eye(128, dtype=np.float32)[:, ::-1]).astype(ml_dtypes.bfloat16)
    cidx = (np.arange(2)[None, :, None] * 128 + np.arange(128)[:, None, None]).astype(np.int64)
    m2 = (cidx * np.arange(256, dtype=np.int64)[None, None, :]) % 256
    th2 = m2.astype(np.float64) * (2.0 * np.pi / 256)
    c["cc"] = np.cos(th2).astype(np.float32).astype(ml_dtypes.bfloat16)
    c["nsc"] = (-np.sin(th2)).astype(np.float32).astype(ml_dtypes.bfloat16)
    c["sc"] = np.sin(th2).astype(np.float32).astype(ml_dtypes.bfloat16)
    return c


ALL_PHASES = ("ret", "ffn0", "fnet", "moe")
SPARSE_MOE = True


def build(phases=ALL_PHASES):
    nc = bass.Bass("TRN2", target_bir_lowering=False)

    def din(name, shape, dtype=F32):
        return nc.dram_tensor(name, list(shape), dtype, kind="ExternalInput").ap()

    def dscr(name, shape, dtype):
        return nc.dram_tensor(name, list(shape), dtype, kind="Internal").ap(), Res(name, accum=True)

    x = din("x", [S, D])
    mix_norm = din("mix_norm", [2, D])
    ffn_norm = din("ffn_norm", [2, D])
    ident = din("ident", [128, 128], BF16)
    identf = din("identf", [128, 128], F32)
    if "ret" in phases:
        w_in = din("ret_w_in", [D, 6144])
        decay = din("ret_decay_logit", [2, 4])
        gn_gain = din("ret_gn_gain", [2048])
        w_out = din("ret_w_out", [2048, D])
        rope_cos = din("rope_cos", [128, S])
        rope_sin = din("rope_sin", [128, S])
    if "ffn0" in phases:
        dwg = din("dense_w_gate", [D, 2816])
        dwu = din("dense_w_up", [D, 2816])
        dwd = din("dense_w_down", [2816, D])
    if "fnet" in phases:
        w_fnet = din("fnet_w_out", [D, D])
        dftc = din("dftc", [17, 128, 32, 128], BF16)
        dfts = din("dfts", [17, 128, 32, 128], BF16)
        cc = din("cc", [128, 2, 256], BF16)
        nsc = din("nsc", [128, 2, 256], BF16)
        scp = din("sc", [128, 2, 256], BF16)
        rev = din("rev", [128, 128], BF16)
    if "moe" in phases:
        router = din("moe_router", [D, 8])
        mwg = din("moe_w_gate", [8, D, 3584])
        mwu = din("moe_w_up", [8, D, 3584])
        mwd = din("moe_w_down", [8, 3584, D])
        final_norm = din("final_norm", [D])
    y = nc.dram_tensor("y", [S, D], F32, kind="ExternalOutput").ap()
    y_r = Res("y", accum=True)
    with ExitStack() as es:
        kb = KB(nc, es)
        cur = (x, Res("x", accum=True))
        order = [p for p in ALL_PHASES if p in phases]
        for p in order:
            last = p == order[-1]
            nxt = (y, y_r) if last else dscr("h_" + p, [S, D], F32)
            if p == "ret":
                qk_s = dscr("qk_s", [8, 128, 4, 16, 128], BF16)
                v_s = dscr("v_s", [S, 2048], BF16)
                sg_s = dscr("sg_s", [S, 2048], BF16)
                sb_s = dscr("sb_s", [32, 128, 8, 512], BF16)
                z_s = dscr("z_s", [S, 2048], BF16)
                ret_proj_phase(kb, cur, mix_norm[0], w_in, gn_gain, ident, rope_cos, rope_sin, qk_s, v_s, sg_s)
                ret_core_phase(kb, decay, ident, qk_s, v_s, sg_s, sb_s, z_s)
                ret_out_phase(kb, cur, nxt, z_s, w_out, ident)
            elif p == "ffn0":
                ffn_phase(kb, cur, nxt, ffn_norm[0], [(dwg, dwu, dwd)], ident, tag="f0", chunk_sizes=[6, 6, 5, 5], nbuf=2)
            elif p == "fnet":
                fourier_phase(kb, cur, nxt, mix_norm[1], w_fnet, ident, dftc, dfts, cc, nsc, scp, rev)
            elif p == "moe":
                if SPARSE_MOE:
                    xg_s = dscr("xg_s", [NT_SLOT * BS, D], BF16)
                    o_s = dscr("o_s", [NT_SLOT * BS, D], F32)
                    moe_sparse_phase(kb, cur, nxt, ffn_norm[1], router, mwg, mwu, mwd, final_norm, ident, identf,
                                     xg_s, o_s)
                else:
                    ffn_phase(kb, cur, nxt, ffn_norm[1], [(mwg[e], mwu[e], mwd[e]) for e in range(8)], ident,
                              router=router, final_g=final_norm, FC=7, tag="f1")
            cur = nxt
        kb.final_wait("sp")
    return nc


_CONSTS = None
PHASE_INPUTS = {
    "ret": ["ret_w_in", "ret_decay_logit", "ret_gn_gain", "ret_w_out"],
    "ffn0": ["dense_w_gate", "dense_w_up", "dense_w_down"],
    "fnet": ["fnet_w_out"],
    "moe": ["moe_router", "moe_w_gate", "moe_w_up", "moe_w_down", "final_norm"],
}
PHASE_CONSTS = {"ret": ["rope_cos", "rope_sin"], "ffn0": [], "fnet": ["dftc", "dfts", "cc", "nsc", "sc", "rev"], "moe": []}


def make_in_maps(inputs, phases=ALL_PHASES, cores=range(NCORES), x_override=None):
    global _CONSTS
    if _CONSTS is None:
        _CONSTS = host_consts()
    shared = {"mix_norm": np.ascontiguousarray(inputs["mix_norm"], dtype=np.float32),
              "ffn_norm": np.ascontiguousarray(inputs["ffn_norm"], dtype=np.float32),
              "ident": _CONSTS["ident"], "identf": _CONSTS["identf"]}
    for p in phases:
        for k in PHASE_INPUTS[p]:
            a = np.asarray(inputs[k], dtype=np.float32)
            if k != "final_norm":
                a = a[0]
            shared[k] = np.ascontiguousarray(a)
        for k in PHASE_CONSTS[p]:
            shared[k] = _CONSTS[k]
    maps = []
    for c in cores:
        m = dict(shared)
        m["x"] = np.ascontiguousarray(inputs["x"][c] if x_override is None else x_override[c], dtype=np.float32)
        maps.append(m)
    return maps


def kernel(**inputs):
    nc = build(ALL_PHASES)
    maps = make_in_maps(inputs)
    res = run_bass_kernel_spmd(nc, maps, core_ids=list(range(NCORES)))
    return np.stack([np.asarray(r["y"], dtype=np.float32) for r in res.results], axis=0)


NT_SLOT = 24
BS = 512


def bc_last(ap, n):
    return bass.AP(ap.tensor, ap.offset, [list(a) for a in ap.ap] + [[0, n]])


def bc_mid(ap2, n):
    a = [list(v) for v in ap2.ap]
    return bass.AP(ap2.tensor, ap2.offset, [a[0], [0, n]] + a[1:])


def moe_sparse_phase(kb, src, dst, norm_g, router, mwg, mwu, mwd, final_g, ident_d, identf_d, xg_s, o_s, tag="ms"):
    nc = kb.nc
    I32 = mybir.dt.int32
    src_ap, src_r = src
    dst_ap, dst_r = dst
    xg_ap, xg_r = xg_s
    o_ap, o_r = o_s
    NTT = S // 128
    FC = 7
    NCH = 4
    with ExitStack() as es:
        ident = es.enter_context(nc.sbuf_tensor(tag + "_ident", [128, 128], BF16))
        ident_r = Res("ident")
        kb.dma("sp", ident[:], ident_d, [], [ident_r], ident_r)
        p1i = es.enter_context(nc.sbuf_tensor(tag + "_p1i", [128, NTT], I32))
        p2i = es.enter_context(nc.sbuf_tensor(tag + "_p2i", [128, NTT], I32))
        g1 = es.enter_context(nc.sbuf_tensor(tag + "_g1", [128, NTT], F32))
        g2 = es.enter_context(nc.sbuf_tensor(tag + "_g2", [128, NTT], F32))
        p1i_r, p2i_r, g1_r, g2_r = Res("p1i"), Res("p2i"), Res("g1"), Res("g2")
        idxg = es.enter_context(nc.sbuf_tensor(tag + "_idxg", [128, NT_SLOT, 32], I32))
        idxd = es.enter_context(nc.sbuf_tensor(tag + "_idxd", [128, NT_SLOT, 28], I32))
        idxg_r, idxd_r = Res("idxg"), Res("idxd")

        with ExitStack() as es0:
            def T(name, shape, dtype=F32):
                return es0.enter_context(nc.sbuf_tensor(tag + "_" + name, shape, dtype)), Res(name)

            fr = Front(kb, es0, ident, ident_r, norm_g, tag + "f", with_T=False)
            identf, identf_r = T("identf", [128, 128])
            kb.dma("sp", identf[:], identf_d, [], [identf_r], identf_r)
            wr, wr_r = T("wr", [128, KT, 8])
            kb.dma("sp", wr[:], router.rearrange("(kt p) e -> p kt e", p=128), [], [wr_r], wr_r)
            hn16, hn16_r = T("hn16", [128, NTT, D], BF16)
            x4 = [es0.enter_context(nc.sbuf_tensor(tag + "_x4%d" % i, [128, 4, D], F32)) for i in range(2)]
            x4_r = [Res("x4%d" % i) for i in range(2)]
            hn32 = [es0.enter_context(nc.sbuf_tensor(tag + "_hn32%d" % i, [128, D], F32)) for i in range(2)]
            hn32_r = [Res("hn32%d" % i) for i in range(2)]
            hT32 = [es0.enter_context(nc.sbuf_tensor(tag + "_hT32%d" % i, [128, KT, 128], F32)) for i in range(2)]
            hT32_r = [Res("hT32%d" % i) for i in range(2)]
            L, L_r = T("L", [128, NTT, 8])
            psT = [es0.enter_context(nc.psum_tensor(tag + "_psT%d" % i, [128, D], F32)) for i in range(2)]
            psT_r = [Res("psT%d" % i) for i in range(2)]
            psL = [es0.enter_context(nc.psum_tensor(tag + "_psL%d" % i, [128, 512], F32)) for i in range(2)]
            psL_r = [Res("psL%d" % i) for i in range(2)]
            zero, zero_r = T("zero", [128, 4, D], BF16)
            kb.op("pool", lambda e: e.memset(zero[:], 0.0), writes=[zero_r])
            n = 0
            pend_router = []

            def router_mm(tt, hb):
                for kt in range(KT):
                    kb.op("pe", lambda e: e.matmul(out=psL[hb][:, 0:8], lhsT=hT32[hb][:, kt, :], rhs=wr[:, kt, :],
                                                   start=(kt == 0), stop=(kt == KT - 1)),
                          reads=[hT32_r[hb], wr_r], writes=[psL_r[hb]], signal=(kt == KT - 1))
                kb.op("dve", lambda e: e.tensor_copy(out=L[:, tt, :], in_=psL[hb][:, 0:8]),
                      reads=[psL_r[hb]], writes=[L_r])

            for G in range(S // 512):
                b = G % 2
                kb.dma("sp", x4[b][:], src_ap[G * 512:(G + 1) * 512, :].rearrange("(j p) d -> p j d", p=128),
                       [src_r], [x4_r[b]], x4_r[b])
                for t in range(G * 3, G * 3 + 3):
                    kb.dma("sp", xg_ap[t * BS:(t + 1) * BS, :].rearrange("(j p) d -> p j d", p=128), zero[:],
                           [zero_r], [xg_r], zero_r, store=True)
                fr.rstd(x4[b][:], x4_r[b], 4)
                for j in range(4):
                    tt = G * 4 + j
                    hb = n % 2
                    n += 1
                    fr.norm_to(hn32[hb][:], hn32_r[hb], x4[b][:, j, :], x4_r[b], j)
                    kb.op("act", lambda e: e.activation(out=hn16[:, tt, :], in_=hn32[hb][:], func=AF.Copy),
                          reads=[hn32_r[hb]], writes=[hn16_r])
                    for kt in range(KT):
                        kb.op("pe", lambda e: e.transpose(out=psT[hb][:, kt * 128:(kt + 1) * 128],
                                                          in_=hn32[hb][:, kt * 128:(kt + 1) * 128], identity=identf[:]),
                              reads=[hn32_r[hb], identf_r], writes=[psT_r[hb]], signal=(kt == KT - 1))
                    kb.op("act", lambda e: e.activation(out=hT32[hb][:], in_=psT[hb][:].rearrange("p (k t) -> p k t", k=KT),
                                                        func=AF.Copy), reads=[psT_r[hb]], writes=[hT32_r[hb]])
                    if pend_router:
                        router_mm(*pend_router.pop())
                    pend_router.append((tt, hb))
            router_mm(*pend_router.pop())
            m1, m1_r = T("m1", [128, NTT])
            m2, m2_r = T("m2", [128, NTT])
            eq, eq_r = T("eq", [128, NTT, 8])
            L2, L2_r = T("L2", [128, NTT, 8])
            mask, mask_r = T("mask", [128, NTT, 8])
            ex, ex_r = T("ex", [128, NTT, 8])
            den, den_r = T("den", [128, NTT])
            gts, gts_r = T("gts", [128, NTT, 8])
            kb.op("dve", lambda e: e.tensor_reduce(out=m1[:], in_=L[:], axis=AX.X, op=ALU.max), reads=[L_r], writes=[m1_r])
            kb.op("dve", lambda e: e.tensor_tensor(out=eq[:], in0=L[:], in1=bc_last(m1[:], 8), op=ALU.is_equal),
                  reads=[L_r, m1_r], writes=[eq_r])
            kb.op("dve", lambda e: e.scalar_tensor_tensor(out=L2[:], in0=eq[:], scalar=-1.0e30, in1=L[:],
                                                          op0=ALU.mult, op1=ALU.add), reads=[eq_r, L_r], writes=[L2_r])
            kb.op("dve", lambda e: e.tensor_reduce(out=m2[:], in_=L2[:], axis=AX.X, op=ALU.max), reads=[L2_r], writes=[m2_r])
            kb.op("dve", lambda e: e.tensor_tensor(out=mask[:], in0=L[:], in1=bc_last(m2[:], 8), op=ALU.is_ge),
                  reads=[L_r, m2_r], writes=[mask_r])
            kb.op("dve", lambda e: e.tensor_tensor(out=ex[:], in0=L[:], in1=bc_last(m1[:], 8), op=ALU.subtract),
                  reads=[L_r, m1_r], writes=[ex_r])
            kb.op("act", lambda e: e.activation(out=ex[:], in_=ex[:], func=AF.Exp), reads=[ex_r], writes=[ex_r])
            kb.op("dve", lambda e: e.tensor_tensor(out=ex[:], in0=ex[:], in1=mask[:], op=ALU.mult),
                  reads=[ex_r, mask_r], writes=[ex_r])
            kb.op("dve", lambda e: e.tensor_reduce(out=den[:], in_=ex[:], axis=AX.X, op=ALU.add), reads=[ex_r], writes=[den_r])
            kb.op("dve", lambda e: e.reciprocal(out=den[:], in_=den[:]), reads=[den_r], writes=[den_r])
            kb.op("dve", lambda e: e.tensor_tensor(out=gts[:], in0=ex[:], in1=bc_last(den[:], 8), op=ALU.mult),
                  reads=[ex_r, den_r], writes=[gts_r])
            maskb, maskb_r = T("maskb", [128, NTT * 8], BF16)
            kb.op("dve", lambda e: e.tensor_copy(out=maskb[:], in_=mask[:].rearrange("p t e -> p (t e)")),
                  reads=[mask_r], writes=[maskb_r])
            ii, ii_r = T("ii", [128, 128], I32)
            dmf, dmf_r = T("dmf", [128, 128])
            Lt, Lt_r = T("Lt", [128, 128], BF16)
            ones, ones_r = T("ones", [128, 128], BF16)
            kb.op("pool", lambda e: e.iota(ii[:], pattern=[[1, 128]], base=0, channel_multiplier=-1), writes=[ii_r])
            kb.op("dve", lambda e: e.tensor_copy(out=dmf[:], in_=ii[:]), reads=[ii_r], writes=[dmf_r])
            kb.op("dve", lambda e: e.tensor_scalar(out=Lt[:], in0=dmf[:], scalar1=0.0, scalar2=None, op0=ALU.is_gt),
                  reads=[dmf_r], writes=[Lt_r])
            kb.op("pool", lambda e: e.memset(ones[:], 1.0), writes=[ones_r])
            kb.op("pe", lambda e: e.matmul(out=psL[0][:, 0:256], lhsT=Lt[:], rhs=maskb[:], start=True, stop=True),
                  reads=[Lt_r, maskb_r], writes=[psL_r[0]])
            kb.op("pe", lambda e: e.matmul(out=psL[1][:, 0:256], lhsT=ones[:], rhs=maskb[:], start=True, stop=True),
                  reads=[ones_r, maskb_r], writes=[psL_r[1]])
            within, within_r = T("within", [128, NTT, 8])
            tot, tot_r = T("tot", [128, NTT, 8])
            off, off_r = T("off", [128, NTT, 8])
            kb.op("dve", lambda e: e.tensor_copy(out=within[:].rearrange("p t e -> p (t e)"), in_=psL[0][:, 0:256]),
                  reads=[psL_r[0]], writes=[within_r])
            kb.op("dve", lambda e: e.tensor_copy(out=tot[:].rearrange("p t e -> p (t e)"), in_=psL[1][:, 0:256]),
                  reads=[psL_r[1]], writes=[tot_r])
            kb.op("dve", lambda e: e.memset(off[:, 0, :], 0.0), writes=[off_r])
            for t in range(1, NTT):
                kb.op("dve", lambda e: e.tensor_tensor(out=off[:, t, :], in0=off[:, t - 1, :], in1=tot[:, t - 1, :], op=ALU.add),
                      reads=[off_r, tot_r], writes=[off_r])
            cntp, cntp_r = T("cntp", [128, 8])
            pc, pc_r = T("pc", [128, 8])
            poff, poff_r = T("poff", [128, 8])
            pend, pend_r = T("pend", [128, 8])
            kb.op("dve", lambda e: e.tensor_tensor(out=cntp[:], in0=off[:, NTT - 1, :], in1=tot[:, NTT - 1, :], op=ALU.add),
                  reads=[off_r, tot_r], writes=[cntp_r])
            kb.op("dve", lambda e: e.memset(pc[:], 0.0), writes=[pc_r])
            for k_ in range(8):
                kb.op("dve", lambda e: e.scalar_tensor_tensor(out=pc[:], in0=cntp[:], scalar=float(BS * k_), in1=pc[:],
                                                              op0=ALU.is_gt, op1=ALU.add),
                      reads=[cntp_r, pc_r], writes=[pc_r])
            kb.op("dve", lambda e: e.tensor_scalar(out=pc[:], in0=pc[:], scalar1=float(BS), scalar2=None, op0=ALU.mult),
                  reads=[pc_r], writes=[pc_r])
            kb.op("dve", lambda e: e.memset(poff[:, 0:1], 0.0), writes=[poff_r])
            for e_ in range(1, 8):
                kb.op("dve", lambda e: e.tensor_tensor(out=poff[:, e_:e_ + 1], in0=poff[:, e_ - 1:e_], in1=pc[:, e_ - 1:e_],
                                                       op=ALU.add), reads=[poff_r, pc_r], writes=[poff_r])
            kb.op("dve", lambda e: e.tensor_tensor(out=pend[:], in0=poff[:], in1=pc[:], op=ALU.add),
                  reads=[poff_r, pc_r], writes=[pend_r])
            ms, ms_r = T("ms", [128, NTT, 8])
            kb.op("dve", lambda e: e.tensor_tensor(out=ms[:], in0=within[:], in1=off[:], op=ALU.add),
                  reads=[within_r, off_r], writes=[ms_r])
            kb.op("dve", lambda e: e.scalar_tensor_tensor(out=ms[:], in0=ms[:], scalar=1.0, in1=bc_mid(poff[:], NTT),
                                                          op0=ALU.add, op1=ALU.add), reads=[ms_r, poff_r], writes=[ms_r])
            kb.op("dve", lambda e: e.tensor_tensor(out=ms[:], in0=ms[:], in1=mask[:], op=ALU.mult),
                  reads=[ms_r, mask_r], writes=[ms_r])
            pa, pa_r = T("pa", [128, NTT])
            pb_, pb_r = T("pb", [128, NTT])
            kb.op("dve", lambda e: e.tensor_reduce(out=pa[:], in_=ms[:], axis=AX.X, op=ALU.max), reads=[ms_r], writes=[pa_r])
            kb.op("dve", lambda e: e.tensor_reduce(out=pb_[:], in_=ms[:], axis=AX.X, op=ALU.add), reads=[ms_r], writes=[pb_r])
            kb.op("dve", lambda e: e.tensor_tensor(out=pb_[:], in0=pb_[:], in1=pa[:], op=ALU.subtract),
                  reads=[pb_r, pa_r], writes=[pb_r])
            kb.op("dve", lambda e: e.tensor_tensor(out=eq[:], in0=ms[:], in1=bc_last(pa[:], 8), op=ALU.is_equal),
                  reads=[ms_r, pa_r], writes=[eq_r])
            kb.op("dve", lambda e: e.tensor_tensor(out=eq[:], in0=eq[:], in1=gts[:], op=ALU.mult),
                  reads=[eq_r, gts_r], writes=[eq_r])
            kb.op("dve", lambda e: e.tensor_reduce(out=g1[:], in_=eq[:], axis=AX.X, op=ALU.add), reads=[eq_r], writes=[g1_r])
            kb.op("dve", lambda e: e.tensor_scalar(out=g2[:], in0=g1[:], scalar1=-1.0, scalar2=1.0, op0=ALU.mult, op1=ALU.add),
                  reads=[g1_r], writes=[g2_r])
            kb.op("dve", lambda e: e.tensor_scalar(out=pa[:], in0=pa[:], scalar1=-1.0, scalar2=None, op0=ALU.add),
                  reads=[pa_r], writes=[pa_r])
            kb.op("dve", lambda e: e.tensor_scalar(out=pb_[:], in0=pb_[:], scalar1=-1.0, scalar2=None, op0=ALU.add),
                  reads=[pb_r], writes=[pb_r])
            kb.op("dve", lambda e: e.tensor_copy(out=p1i[:], in_=pa[:]), reads=[pa_r], writes=[p1i_r])
            kb.op("dve", lambda e: e.tensor_copy(out=p2i[:], in_=pb_[:]), reads=[pb_r], writes=[p2i_r])
            cmp, cmp_r = T("cmp", [128, NT_SLOT, 8])
            et, et_r = T("et", [128, NT_SLOT])
            for t in range(NT_SLOT):
                kb.op("dve", lambda e: e.tensor_scalar(out=cmp[:, t, :], in0=pend[:], scalar1=float(BS * t), scalar2=None,
                                                       op0=ALU.is_le), reads=[pend_r], writes=[cmp_r])
            kb.op("dve", lambda e: e.tensor_reduce(out=et[:], in_=cmp[:], axis=AX.X, op=ALU.add), reads=[cmp_r], writes=[et_r])
            kb.op("dve", lambda e: e.tensor_scalar(out=et[:], in0=et[:], scalar1=7.0, scalar2=None, op0=ALU.min),
                  reads=[et_r], writes=[et_r])
            sgi, sgi_r = T("sgi", [128, 32], I32)
            sdi, sdi_r = T("sdi", [128, 28], I32)
            sgf, sgf_r = T("sgf", [128, 32])
            sdf, sdf_r = T("sdf", [128, 28])
            kb.op("pool", lambda e: e.iota(sgi[:], pattern=[[512, 8], [1, 4]], base=0, channel_multiplier=4), writes=[sgi_r])
            kb.op("pool", lambda e: e.iota(sdi[:], pattern=[[896, 4], [128, 7]], base=0, channel_multiplier=1), writes=[sdi_r])
            kb.op("dve", lambda e: e.tensor_copy(out=sgf[:], in_=sgi[:]), reads=[sgi_r], writes=[sgf_r])
            kb.op("dve", lambda e: e.tensor_copy(out=sdf[:], in_=sdi[:]), reads=[sdi_r], writes=[sdf_r])
            igf, igf_r = T("igf", [128, NT_SLOT, 32])
            idf, idf_r = T("idf", [128, NT_SLOT, 28])
            etg, etg_r = T("etg", [128, NT_SLOT])
            etd, etd_r = T("etd", [128, NT_SLOT])
            kb.op("dve", lambda e: e.tensor_scalar(out=etg[:], in0=et[:], scalar1=4096.0, scalar2=None, op0=ALU.mult),
                  reads=[et_r], writes=[etg_r])
            kb.op("dve", lambda e: e.tensor_scalar(out=etd[:], in0=et[:], scalar1=3584.0, scalar2=None, op0=ALU.mult),
                  reads=[et_r], writes=[etd_r])
            for t in range(NT_SLOT):
                kb.op("dve", lambda e: e.tensor_scalar(out=igf[:, t, :], in0=sgf[:], scalar1=etg[:, t:t + 1], scalar2=None,
                                                       op0=ALU.add), reads=[etg_r, sgf_r], writes=[igf_r])
                kb.op("dve", lambda e: e.tensor_scalar(out=idf[:, t, :], in0=sdf[:], scalar1=etd[:, t:t + 1], scalar2=None,
                                                       op0=ALU.add), reads=[etd_r, sdf_r], writes=[idf_r])
            kb.op("dve", lambda e: e.tensor_copy(out=idxg[:], in_=igf[:]), reads=[igf_r], writes=[idxg_r])
            kb.op("dve", lambda e: e.tensor_copy(out=idxd[:], in_=idf[:]), reads=[idf_r], writes=[idxd_r])
            kb._wait("pool", kb._deps([xg_r], []))
            for tt in range(NTT):
                for (pi, pi_r) in ((p1i, p1i_r), (p2i, p2i_r)):
                    kb._wait("pool", kb._deps([hn16_r, pi_r], []))
                    if hn16_r.ssem is None:
                        hn16_r.ssem = kb.get_sem(fresh=True)
                    sem = hn16_r.ssem
                    ins = nc.gpsimd.indirect_dma_start(out=xg_ap, out_offset=bass.IndirectOffsetOnAxis(ap=pi[:, tt:tt + 1], axis=0),
                                                       in_=hn16[:, tt, :], in_offset=None)
                    ins.then_inc(sem, 16)
                    kb.cnt[sem] += 16
                    kb._register(sem, kb.cnt[sem], [hn16_r, pi_r], [xg_r])
            kb.barrier()
        with ExitStack() as es1:
            xgt = [es1.enter_context(nc.sbuf_tensor(tag + "_xgt%d" % i, [128, 4, D], BF16)) for i in range(2)]
            xgt_r = [Res("xgt%d" % i) for i in range(2)]
            xT = [es1.enter_context(nc.sbuf_tensor(tag + "_xT%d" % i, [128, KT, BS], BF16)) for i in range(2)]
            xT_r = [Res("xT%d" % i) for i in range(2)]
            wg = [es1.enter_context(nc.sbuf_tensor(tag + "_wg%d" % i, [128, KT, FC * 128], BF16)) for i in range(2)]
            wu = [es1.enter_context(nc.sbuf_tensor(tag + "_wu%d" % i, [128, KT, FC * 128], BF16)) for i in range(2)]
            wd = [es1.enter_context(nc.sbuf_tensor(tag + "_wd%d" % i, [128, FC, D], BF16)) for i in range(2)]
            wg_r = [Res("wg%d" % i) for i in range(2)]
            wu_r = [Res("wu%d" % i) for i in range(2)]
            wd_r = [Res("wd%d" % i) for i in range(2)]
            actT = [es1.enter_context(nc.sbuf_tensor(tag + "_actT%d" % i, [128, FC, BS], BF16)) for i in range(2)]
            actT_r = [Res("actT%d" % i) for i in range(2)]
            sg = [es1.enter_context(nc.sbuf_tensor(tag + "_sg%d" % i, [128, 512], F32)) for i in range(2)]
            sg_r = [Res("sg%d" % i) for i in range(2)]
            oacc = [es1.enter_context(nc.sbuf_tensor(tag + "_oacc%d" % i, [128, 4, D], F32)) for i in range(2)]
            oacc_r = [Res("oacc%d" % i) for i in range(2)]
            psT = [es1.enter_context(nc.psum_tensor(tag + "_psX%d" % i, [128, D], BF16)) for i in range(2)]
            psT_r = [Res("psX%d" % i) for i in range(2)]
            psG = [es1.enter_context(nc.psum_tensor(tag + "_psG%d" % i, [128, 512], F32)) for i in range(2)]
            psU = [es1.enter_context(nc.psum_tensor(tag + "_psU%d" % i, [128, 512], F32)) for i in range(2)]
            psO = [es1.enter_context(nc.psum_tensor(tag + "_psO%d" % i, [128, 512], F32)) for i in range(2)]
            psG_r = [Res("psG%d" % i) for i in range(2)]
            psU_r = [Res("psU%d" % i) for i in range(2)]
            psO_r = [Res("psO%d" % i) for i in range(2)]
            wg_flat = mwg.rearrange("e d (c f) -> (e d c) f", f=FC * 128)
            wu_flat = mwu.rearrange("e d (c f) -> (e d c) f", f=FC * 128)
            wd_flat = mwd.rearrange("e f d -> (e f) d")

            def gather(dst_ap_, dst_r_, src_flat, idx_ap, idx_r):
                kb._wait("pool", kb._deps([idx_r], []))
                if dst_r_.lsem is None:
                    dst_r_.lsem = kb.get_sem(fresh=True)
                sem = dst_r_.lsem
                ins = nc.gpsimd.indirect_dma_start(out=dst_ap_, out_offset=None, in_=src_flat,
                                                   in_offset=bass.IndirectOffsetOnAxis(ap=idx_ap, axis=0))
                ins.then_inc(sem, 16)
                kb.cnt[sem] += 16
                dst_r_.writes[sem] = kb.cnt[sem]
                idx_r.reads[sem] = kb.cnt[sem]

            wseq = [(t, c) for t in range(NT_SLOT) for c in range(NCH)]

            def load_w(i):
                t, c = wseq[i]
                b = i % 2
                for r_ in (wg_r[b], wu_r[b], wd_r[b]):
                    kb._wait("pool", kb._deps([], [r_]))
                    r_.writes = {}
                    r_.reads = {}
                for kt in range(KT):
                    gather(wg[b][:, kt, :], wg_r[b], wg_flat, idxg[:, t, kt * 4 + c:kt * 4 + c + 1], idxg_r)
                    gather(wu[b][:, kt, :], wu_r[b], wu_flat, idxg[:, t, kt * 4 + c:kt * 4 + c + 1], idxg_r)
                for fl in range(FC):
                    gather(wd[b][:, fl, :], wd_r[b], wd_flat, idxd[:, t, c * 7 + fl:c * 7 + fl + 1], idxd_r)

            def load_x(t):
                b = t % 2
                kb.dma("sp", xgt[b][:], xg_ap[t * BS:(t + 1) * BS, :].rearrange("(j p) d -> p j d", p=128),
                       [xg_r], [xgt_r[b]], xgt_r[b])

            load_x(0)
            load_w(0)
            wi = 0
            gcount = 0
            ocount = 0
            tcount = 0
            tcs = {"c": 0}

            def transp_x(t):
                xb_ = t % 2
                for j in range(4):
                    tb = tcs["c"] % 2
                    tcs["c"] += 1
                    for kt in range(KT):
                        kb.op("pe", lambda e: e.transpose(out=psT[tb][:, kt * 128:(kt + 1) * 128],
                                                          in_=xgt[xb_][:, j, kt * 128:(kt + 1) * 128], identity=ident[:]),
                              reads=[xgt_r[xb_], ident_r], writes=[psT_r[tb]], signal=(kt == KT - 1))
                    kb.op("act", lambda e: e.activation(out=xT[xb_][:, :, j * 128:(j + 1) * 128],
                                                        in_=psT[tb][:].rearrange("p (k t) -> p k t", k=KT), func=AF.Copy),
                          reads=[psT_r[tb]], writes=[xT_r[xb_]])

            transp_x(0)
            for t in range(NT_SLOT):
                xb = t % 2
                if t + 1 < NT_SLOT:
                    load_x(t + 1)
                ob = t % 2
                for c in range(NCH):
                    if c == 2 and t + 1 < NT_SLOT:
                        transp_x(t + 1)
                    b = wi % 2
                    if wi + 1 < len(wseq):
                        load_w(wi + 1)
                    ab = wi % 2
                    for fl in range(FC):
                        gb = gcount % 2
                        gcount += 1
                        for (wt, wt_r, pst, pst_r) in ((wg[b], wg_r[b], psG[gb], psG_r[gb]), (wu[b], wu_r[b], psU[gb], psU_r[gb])):
                            for kt in range(KT):
                                kb.op("pe", lambda e: e.matmul(out=pst[:], lhsT=wt[:, kt, fl * 128:(fl + 1) * 128],
                                                               rhs=xT[xb][:, kt, :], start=(kt == 0), stop=(kt == KT - 1)),
                                      reads=[wt_r, xT_r[xb]], writes=[pst_r], signal=(kt == KT - 1))
                        kb.op("act", lambda e: e.activation(out=sg[gb][:], in_=psG[gb][:], func=AF.Silu),
                              reads=[psG_r[gb]], writes=[sg_r[gb]])
                        kb.op("dve", lambda e: e.tensor_tensor(out=actT[ab][:, fl, :], in0=sg[gb][:], in1=psU[gb][:], op=ALU.mult),
                              reads=[sg_r[gb], psU_r[gb]], writes=[actT_r[ab]])
                    for j in range(4):
                        for dh in range(2):
                            pb2 = ocount % 2
                            ocount += 1
                            for fl in range(FC):
                                kb.op("pe", lambda e: e.matmul(out=psO[pb2][:], lhsT=actT[ab][:, fl, j * 128:(j + 1) * 128],
                                                               rhs=wd[b][:, fl, dh * 512:(dh + 1) * 512],
                                                               start=(fl == 0), stop=(fl == FC - 1)),
                                      reads=[actT_r[ab], wd_r[b]], writes=[psO_r[pb2]], signal=(fl == FC - 1))
                            if c == 0:
                                kb.op("act", lambda e: e.activation(out=oacc[ob][:, j, dh * 512:(dh + 1) * 512], in_=psO[pb2][:],
                                                                    func=AF.Copy), reads=[psO_r[pb2]], writes=[oacc_r[ob]])
                            else:
                                kb.op("dve", lambda e: e.tensor_tensor(out=oacc[ob][:, j, dh * 512:(dh + 1) * 512], in0=psO[pb2][:],
                                                                       in1=oacc[ob][:, j, dh * 512:(dh + 1) * 512], op=ALU.add),
                                      reads=[psO_r[pb2], oacc_r[ob]], writes=[oacc_r[ob]])
                    wi += 1
                kb.dma("sp", o_ap[t * BS:(t + 1) * BS, :].rearrange("(j p) d -> p j d", p=128), oacc[ob][:],
                       [oacc_r[ob]], [o_r], oacc_r[ob], store=True)
            kb.barrier()
        with ExitStack() as es2:
            fr2 = Front(kb, es2, ident, ident_r, final_g, tag + "g", with_T=False)
            x4 = [es2.enter_context(nc.sbuf_tensor(tag + "_y4%d" % i, [128, 4, D], F32)) for i in range(3)]
            x4_r = [Res("y4%d" % i) for i in range(3)]
            ga = [es2.enter_context(nc.sbuf_tensor(tag + "_ga%d" % i, [128, D], F32)) for i in range(8)]
            ga_r = [Res("ga%d" % i) for i in range(8)]
            n = 0

            def loadF(G):
                kb.dma("sp", x4[G % 3][:], src_ap[G * 512:(G + 1) * 512, :].rearrange("(j p) d -> p j d", p=128),
                       [src_r], [x4_r[G % 3]], x4_r[G % 3])

            loadF(0)
            loadF(1)
            for G in range(S // 512):
                b = G % 3
                if G + 2 < S // 512:
                    loadF(G + 2)
                for j in range(4):
                    tt = G * 4 + j
                    for (pi, pi_r, gg, gg_r) in ((p1i, p1i_r, g1, g1_r), (p2i, p2i_r, g2, g2_r)):
                        gb = n % 8
                        n += 1
                        kb._wait("pool", kb._deps([o_r, pi_r], [ga_r[gb]]))
                        if ga_r[gb].lsem is None:
                            ga_r[gb].lsem = kb.get_sem(fresh=True)
                        sem = ga_r[gb].lsem
                        ins = nc.gpsimd.indirect_dma_start(out=ga[gb][:], out_offset=None, in_=o_ap,
                                                           in_offset=bass.IndirectOffsetOnAxis(ap=pi[:, tt:tt + 1], axis=0))
                        ins.then_inc(sem, 16)
                        kb.cnt[sem] += 16
                        kb._register(sem, kb.cnt[sem], [o_r, pi_r], [ga_r[gb]])
                        kb.op("dve", lambda e: e.scalar_tensor_tensor(out=x4[b][:, j, :], in0=ga[gb][:], scalar=gg[:, tt:tt + 1],
                                                                      in1=x4[b][:, j, :], op0=ALU.mult, op1=ALU.add),
                              reads=[ga_r[gb], gg_r, x4_r[b]], writes=[x4_r[b]])
                fr2.rstd(x4[b][:], x4_r[b], 4)
                for j in range(4):
                    fr2.norm_to(x4[b][:, j, :], x4_r[b], x4[b][:, j, :], x4_r[b], j)
                kb.dma("sp", dst_ap[G * 512:(G + 1) * 512, :].rearrange("(j p) d -> p j d", p=128), x4[b][:],
                       [x4_r[b]], [dst_r], x4_r[b], store=True)
            kb.barrier()
```
